# Optimizing a Trainium2 kernel written in Bass

```python
import math
import jax
import jax.numpy as jnp
from jax import lax
import numpy as np

D_MODEL = 1024
BATCH = 8
SEQ = 4096
DEPTH = 2

GRID_W = 64
CTX_LEN = 256
NORM_EPS = 1e-6
ROPE_BASE = 10000.0
NEG_INF = -1e30

NA_HEADS = 4
NA_DIM = 64
NA_WIN_ROWS = 8
NA_WIN_COLS = 16
NA_QCOLS = 16
NA_KCOLS = 32
DIFF_HEADS = 4
DIFF_DIM = 32
DIFF_VDIM = 2 * DIFF_DIM
DIFF_QBLOCK = 128
RET_HEADS = 4
RET_QK = 64
RET_V = 128
RET_CHUNK = 128

NA_W = NA_HEADS * NA_DIM
DIFF_QK_W = DIFF_HEADS * 2 * DIFF_DIM
DIFF_W = DIFF_HEADS * DIFF_VDIM
RET_QK_W = RET_HEADS * RET_QK
RET_W = RET_HEADS * RET_V
MIX_W = NA_W + DIFF_W + RET_W
IN_SPLITS = (NA_W, NA_W, NA_W, DIFF_QK_W, DIFF_QK_W, DIFF_W, RET_QK_W, RET_QK_W, RET_W, RET_W)
IN_COLS = 3 * NA_W + 2 * DIFF_QK_W + DIFF_W + 2 * RET_QK_W + 2 * RET_W

PEER_HEADS = 8
PEER_NKEYS = 128
PEER_EXPERTS = PEER_NKEYS * PEER_NKEYS
PEER_KDIM = 256
PEER_TOPK = 16
PEER_CHUNK = 128

kernel_name = "hybrid_natten_diffattn_retnet_peer_dit"


def _rms(x):
    xf = x.astype(jnp.float32)
    return xf * lax.rsqrt(jnp.mean(xf * xf, axis=-1, keepdims=True) + NORM_EPS)


def rmsnorm(x, g):
    return _rms(x) * g.astype(jnp.float32)


def split_cols(p):
    out = []
    o = 0
    for s in IN_SPLITS:
        out.append(p[..., o:o + s])
        o += s
    return out


def split_heads(t, h):
    b, l, _ = t.shape
    return t.reshape(b, l, h, -1).transpose(0, 2, 1, 3)


def merge_heads(t):
    b, h, l, d = t.shape
    return t.transpose(0, 2, 1, 3).reshape(b, l, h * d)


def diff_split(t):
    b, l, _ = t.shape
    return t.reshape(b, l, DIFF_HEADS, 2, DIFF_DIM).transpose(0, 2, 3, 1, 4)


def flip_seq(t):
    return jnp.flip(t, axis=2)


def axial_rope_tables(n_tokens, dim):
    t = jnp.arange(n_tokens)
    rows = (t // GRID_W).astype(jnp.float32)
    cols = (t % GRID_W).astype(jnp.float32)
    half = dim // 2
    inv = jnp.power(ROPE_BASE, -jnp.arange(0, half, 2, dtype=jnp.float32) / half)
    ang = jnp.concatenate([rows[:, None] * inv, cols[:, None] * inv], axis=-1)
    return jnp.cos(ang), jnp.sin(ang)


def apply_rope(x, cos, sin):
    xf = x.astype(jnp.float32).reshape(x.shape[:-1] + (x.shape[-1] // 2, 2))
    x1, x2 = xf[..., 0], xf[..., 1]
    out = jnp.stack([x1 * cos - x2 * sin, x1 * sin + x2 * cos], axis=-1)
    return out.reshape(x.shape)


def dense_attention(q, k, v):
    s = jnp.einsum('bhqd,bhkd->bhqk', q.astype(jnp.float32), k.astype(jnp.float32)) * q.shape[-1] ** -0.5
    p = jax.nn.softmax(s, axis=-1)
    return jnp.einsum('bhqk,bhkd->bhqd', p, v.astype(jnp.float32))


def neighbourhood_attention(q, k, v, kc, vc, rpb):
    b, h, l, d = q.shape
    f32 = jnp.float32
    rows = l // GRID_W
    wr = min(NA_WIN_ROWS, rows)
    r = np.arange(rows)
    row_idx = np.clip(r - wr // 2, 0, rows - wr)[:, None] + np.arange(wr)[None, :]
    dr = row_idx - r[:, None] + (NA_WIN_ROWS - 1)
    qg = q.astype(f32).reshape(b, h, rows, GRID_W, d) * d ** -0.5
    kband = k.reshape(b, h, rows, GRID_W, d)[:, :, row_idx]
    vband = v.reshape(b, h, rows, GRID_W, d)[:, :, row_idx]
    kc = kc.astype(f32)
    vc = vc.astype(f32)
    rpb = rpb.astype(f32)
    n_win = wr * NA_KCOLS
    outs = []
    for j in range(GRID_W // NA_QCOLS):
        q0 = j * NA_QCOLS
        k0 = min(max(q0 - (NA_KCOLS - NA_QCOLS) // 2, 0), GRID_W - NA_KCOLS)
        qcol = q0 + np.arange(NA_QCOLS)
        kcol = k0 + np.arange(NA_KCOLS)
        cs = np.clip(qcol - NA_WIN_COLS // 2, 0, GRID_W - NA_WIN_COLS)
        valid = (kcol[None, :] >= cs[:, None]) & (kcol[None, :] < cs[:, None] + NA_WIN_COLS)
        dc = np.clip(kcol[None, :] - qcol[:, None], 1 - NA_WIN_COLS, NA_WIN_COLS - 1) + NA_WIN_COLS - 1
        bias = rpb[:, dr[:, None, :, None], dc[None, :, None, :]]
        bias = jnp.where(valid[None, None, :, None, :], bias, NEG_INF).reshape(h, rows, NA_QCOLS, n_win)
        qb = qg[:, :, :, q0:q0 + NA_QCOLS]
        kb = kband[:, :, :, :, k0:k0 + NA_KCOLS].reshape(b, h, rows, n_win, d).astype(f32)
        vb = vband[:, :, :, :, k0:k0 + NA_KCOLS].reshape(b, h, rows, n_win, d).astype(f32)
        s = jnp.concatenate([jnp.einsum('bhrqd,bhrkd->bhrqk', qb, kb) + bias[None],
                             jnp.einsum('bhrqd,bhkd->bhrqk', qb, kc)], axis=-1)
        p = jax.nn.softmax(s, axis=-1)
        o = (jnp.einsum('bhrqk,bhrkd->bhrqd', p[..., :n_win], vb)
             + jnp.einsum('bhrqk,bhkd->bhrqd', p[..., n_win:], vc))
        outs.append(o)
    return jnp.concatenate(outs, axis=3).reshape(b, h, l, d)


def diff_attend(q, k, v, lam):
    s = jnp.einsum('bhcqd,bhckd->bhcqk', q.astype(jnp.float32), k.astype(jnp.float32)) * DIFF_DIM ** -0.5
    p = jax.nn.softmax(s, axis=-1)
    a = p[:, :, 0] - lam * p[:, :, 1]
    return jnp.einsum('bhqk,bhkd->bhqd', a, v.astype(jnp.float32))


def diff_attention_blocks(q, k_all, v_all, lam):
    b, h, _, l, d = q.shape
    nb = l // DIFF_QBLOCK
    qb = q.reshape(b, h, 2, nb, DIFF_QBLOCK, d).transpose(3, 0, 1, 2, 4, 5)
    o = lax.map(lambda qq: diff_attend(qq, k_all, v_all, lam), qb)
    return o.transpose(1, 2, 0, 3, 4).reshape(b, h, l, -1)


def diff_post(o, g, lam_init):
    return rmsnorm(o, g) * (1.0 - lam_init)


def retention_chunked(q, k, v, log_g, s0):
    f32 = jnp.float32
    b, h, l, dk = q.shape
    dv = v.shape[-1]
    c = RET_CHUNK
    n = l // c
    i = jnp.arange(c, dtype=f32)
    diff = i[:, None] - i[None, :]
    dmask = jnp.where(diff >= 0, jnp.exp(log_g[:, None, None] * jnp.maximum(diff, 0.0)), 0.0)
    q_dec = jnp.exp(log_g[:, None] * (i + 1.0)[None, :])
    k_dec = jnp.exp(log_g[:, None] * (c - 1.0 - i)[None, :])
    c_dec = jnp.exp(log_g * c)
    qc = q.astype(f32).reshape(b, h, n, c, dk)
    kc = k.astype(f32).reshape(b, h, n, c, dk)
    vc = v.astype(f32).reshape(b, h, n, c, dv)
    att = jnp.einsum('bhnid,bhnjd->bhnij', qc, kc) * dmask[None, :, None]
    o = jnp.einsum('bhnij,bhnje->bhnie', att, vc)
    kv = jnp.einsum('bhnjd,bhnje->bhnde', kc * k_dec[None, :, None, :, None], vc)

    def step(state, kv_c):
        return c_dec[None, :, None, None] * state + kv_c, state

    s_final, s_prev = lax.scan(step, s0.astype(f32), jnp.moveaxis(kv, 2, 0))
    s_prev = jnp.moveaxis(s_prev, 0, 2)
    o = o + jnp.einsum('bhnid,bhnde->bhnie', qc * q_dec[None, :, None, :, None], s_prev)
    return o.reshape(b, h, l, dv), s_final


def retention_final_state(k, v, log_g, reverse):
    l = k.shape[2]
    m = jnp.arange(l, dtype=jnp.float32)
    expo = m if reverse else (l - 1.0 - m)
    w = jnp.exp(log_g[:, None] * expo[None, :])
    return jnp.einsum('bhld,bhle->bhde', k.astype(jnp.float32) * w[None, :, :, None], v.astype(jnp.float32))


def retention_post(y, g):
    return merge_heads(_rms(y)) * jax.nn.silu(g.astype(jnp.float32))


def token_mixers(px, pc, rpb, lam_p, subln_g, decay_logit, layer_idx, rope_d, rope_r, need_ctx):
    f32 = jnp.float32
    na_q, na_k, na_v, df_q, df_k, df_v, rt_q, rt_k, rt_v, rt_g = px
    cna_q, cna_k, cna_v, cdf_q, cdf_k, cdf_v, crt_q, crt_k, crt_v, crt_g = pc
    b = na_q.shape[0]
    kc_na = split_heads(cna_k, NA_HEADS)
    vc_na = split_heads(cna_v, NA_HEADS)
    y_na = neighbourhood_attention(split_heads(na_q, NA_HEADS), split_heads(na_k, NA_HEADS),
                                   split_heads(na_v, NA_HEADS), kc_na, vc_na, rpb)
    lam_init = 0.8 - 0.6 * math.exp(-0.3 * layer_idx)
    lp = lam_p.astype(f32)
    lam = jnp.exp(jnp.sum(lp[0] * lp[1])) - jnp.exp(jnp.sum(lp[2] * lp[3])) + lam_init
    cos_d, sin_d = rope_d
    q_df = apply_rope(diff_split(df_q), cos_d, sin_d)
    k_df = apply_rope(diff_split(df_k), cos_d, sin_d)
    kc_df = diff_split(cdf_k).astype(f32)
    vc_df = split_heads(cdf_v, DIFF_HEADS).astype(f32)
    k_all = jnp.concatenate([kc_df, k_df], axis=3)
    v_all = jnp.concatenate([vc_df, split_heads(df_v, DIFF_HEADS).astype(f32)], axis=2)
    y_df = diff_post(diff_attention_blocks(q_df, k_all, v_all, lam), subln_g, lam_init)
    lg = jax.nn.log_sigmoid(decay_logit.astype(f32))
    cos_r, sin_r = rope_r
    q_rt = apply_rope(split_heads(rt_q, RET_HEADS), cos_r, sin_r)
    k_rt = apply_rope(split_heads(rt_k, RET_HEADS), cos_r, sin_r) * RET_QK ** -0.5
    v_rt = split_heads(rt_v, RET_HEADS).astype(f32)
    kc_rt = split_heads(crt_k, RET_HEADS).astype(f32) * RET_QK ** -0.5
    vc_rt = split_heads(crt_v, RET_HEADS).astype(f32)
    if need_ctx:
        z = jnp.zeros((b, RET_HEADS, RET_QK, RET_V), f32)
        qc_rt = split_heads(crt_q, RET_HEADS).astype(f32)
        oc_f, s_f = retention_chunked(qc_rt, kc_rt, vc_rt, lg[0], z)
        oc_b, s_b = retention_chunked(flip_seq(qc_rt), flip_seq(kc_rt), flip_seq(vc_rt), lg[1], z)
        yc_rt = retention_post(oc_f + flip_seq(oc_b), crt_g)
    else:
        s_f = retention_final_state(kc_rt, vc_rt, lg[0], False)
        s_b = retention_final_state(kc_rt, vc_rt, lg[1], True)
    o_f, _ = retention_chunked(q_rt, k_rt, v_rt, lg[0], s_f)
    o_b, _ = retention_chunked(flip_seq(q_rt), flip_seq(k_rt), flip_seq(v_rt), lg[1], s_b)
    y_rt = retention_post(o_f + flip_seq(o_b), rt_g)
    y_lat = jnp.concatenate([merge_heads(y_na), merge_heads(y_df), y_rt], axis=-1)
    if not need_ctx:
        return y_lat, None
    yc_na = dense_attention(split_heads(cna_q, NA_HEADS), kc_na, vc_na)
    yc_df = diff_post(diff_attend(diff_split(cdf_q), kc_df, vc_df, lam), subln_g, lam_init)
    y_ctx = jnp.concatenate([merge_heads(yc_na), merge_heads(yc_df), yc_rt], axis=-1)
    return y_lat, y_ctx


def peer(h, wq, subkeys, u, v):
    t, d = h.shape
    q = (h @ wq).astype(jnp.float32).reshape(t, PEER_HEADS, 2, PEER_KDIM // 2)
    s = jnp.einsum('thpd,hpnd->thpn', q, subkeys.astype(jnp.float32))
    s_top, i_top = lax.top_k(s, PEER_TOPK)
    cand = (s_top[:, :, 0, :, None] + s_top[:, :, 1, None, :]).reshape(t, PEER_HEADS, PEER_TOPK * PEER_TOPK)
    cand_idx = (i_top[:, :, 0, :, None] * PEER_NKEYS + i_top[:, :, 1, None, :]).reshape(t, PEER_HEADS, -1)
    best, pos = lax.top_k(cand, PEER_TOPK)
    expert = jnp.take_along_axis(cand_idx, pos, axis=-1)
    gate = jax.nn.softmax(best, axis=-1)
    nchunk = t // PEER_CHUNK

    def eval_chunk(args):
        hc, ec, gc = args
        a = jax.nn.gelu(jnp.einsum('cd,chkd->chk', hc, u[ec]).astype(jnp.float32), approximate=False)
        w = (gc * a).astype(v.dtype)
        return jnp.einsum('chk,chkd->cd', w, v[ec])

    out = lax.map(eval_chunk, (h.reshape(nchunk, PEER_CHUNK, d),
                               expert.reshape(nchunk, PEER_CHUNK, PEER_HEADS, PEER_TOPK),
                               gate.reshape(nchunk, PEER_CHUNK, PEER_HEADS, PEER_TOPK)))
    return out.reshape(t, d)


def setup_inputs(seed: int = 0) -> dict:
    key = jax.random.key(seed)
    ks = jax.random.split(key, 20)
    f32 = jnp.float32
    nrm = jax.random.normal
    d = D_MODEL
    x = nrm(ks[0], (BATCH, SEQ, d), f32)
    c = nrm(ks[1], (BATCH, d), f32)
    ctx = nrm(ks[2], (BATCH, CTX_LEN, d), f32)
    c_ctx = nrm(ks[3], (d,), f32)
    w_ada = nrm(ks[4], (DEPTH, d, 6 * d), f32) * (0.5 * d ** -0.5)
    b_ada = 0.02 * nrm(ks[5], (DEPTH, 6 * d), f32)
    norm1_g = 1.0 + 0.02 * nrm(ks[6], (DEPTH, d), f32)
    w_in = nrm(ks[7], (DEPTH, d, IN_COLS), f32) * d ** -0.5
    na_rpb = 0.1 * nrm(ks[8], (DEPTH, NA_HEADS, 2 * NA_WIN_ROWS - 1, 2 * NA_WIN_COLS - 1), f32)
    diff_lambda = 0.1 * nrm(ks[9], (DEPTH, 4, DIFF_DIM), f32)
    diff_subln_g = 1.0 + 0.02 * nrm(ks[10], (DEPTH, DIFF_VDIM), f32)
    base_logit = jnp.log(jnp.power(2.0, 5.0 + jnp.arange(RET_HEADS, dtype=f32)) - 1.0)
    ret_decay_logit = base_logit + 0.1 * nrm(ks[11], (DEPTH, 2, RET_HEADS), f32)
    w_out = nrm(ks[12], (DEPTH, MIX_W, d), f32) * MIX_W ** -0.5
    norm2_g = 1.0 + 0.02 * nrm(ks[13], (DEPTH, d), f32)
    peer_wq = nrm(ks[14], (DEPTH, d, PEER_HEADS * PEER_KDIM), f32) * d ** -0.5
    peer_subkeys = nrm(ks[15], (DEPTH, PEER_HEADS, 2, PEER_NKEYS, PEER_KDIM // 2), f32) * (PEER_KDIM // 2) ** -0.5
    peer_u = nrm(ks[16], (DEPTH, PEER_EXPERTS, d), f32) * d ** -0.5
    peer_v = nrm(ks[17], (DEPTH, PEER_EXPERTS, d), f32) * PEER_HEADS ** -0.5
    final_g = 1.0 + 0.02 * nrm(ks[18], (d,), f32)
    return {"x": x, "c": c, "ctx": ctx, "c_ctx": c_ctx, "w_ada": w_ada, "b_ada": b_ada,
            "norm1_g": norm1_g, "w_in": w_in, "na_rpb": na_rpb, "diff_lambda": diff_lambda,
            "diff_subln_g": diff_subln_g, "ret_decay_logit": ret_decay_logit, "w_out": w_out,
            "norm2_g": norm2_g, "peer_wq": peer_wq, "peer_subkeys": peer_subkeys,
            "peer_u": peer_u, "peer_v": peer_v, "final_g": final_g}


def reference(x, c, ctx, c_ctx, w_ada, b_ada, norm1_g, w_in, na_rpb, diff_lambda, diff_subln_g,
              ret_decay_logit, w_out, norm2_g, peer_wq, peer_subkeys, peer_u, peer_v, final_g):
    b, l, d = x.shape
    lc = ctx.shape[1]
    rope_d = axial_rope_tables(l, DIFF_DIM)
    rope_r = axial_rope_tables(l, RET_QK)
    silu_c = jax.nn.silu(c.astype(jnp.float32))
    silu_cc = jax.nn.silu(c_ctx.astype(jnp.float32))
    for layer in range(DEPTH):
        need_ctx = layer < DEPTH - 1
        mod = jnp.split((silu_c @ w_ada[layer] + b_ada[layer])[:, None, :], 6, axis=-1)
        mod_c = jnp.split(silu_cc @ w_ada[layer] + b_ada[layer], 6, axis=-1)
        hx = rmsnorm(x, norm1_g[layer]) * (1.0 + mod[1]) + mod[0]
        hc = rmsnorm(ctx, norm1_g[layer]) * (1.0 + mod_c[1]) + mod_c[0]
        y_lat, y_ctx = token_mixers(split_cols(hx @ w_in[layer]), split_cols(hc @ w_in[layer]),
                                    na_rpb[layer], diff_lambda[layer], diff_subln_g[layer],
                                    ret_decay_logit[layer], layer, rope_d, rope_r, need_ctx)
        x = x + mod[2] * (y_lat @ w_out[layer])
        h2 = rmsnorm(x, norm2_g[layer]) * (1.0 + mod[4]) + mod[3]
        x = x + mod[5] * peer(h2.reshape(b * l, d), peer_wq[layer], peer_subkeys[layer],
                              peer_u[layer], peer_v[layer]).reshape(b, l, d)
        if need_ctx:
            ctx = ctx + mod_c[2] * (y_ctx @ w_out[layer])
            h2c = rmsnorm(ctx, norm2_g[layer]) * (1.0 + mod_c[4]) + mod_c[3]
            ctx = ctx + mod_c[5] * peer(h2c.reshape(b * lc, d), peer_wq[layer], peer_subkeys[layer],
                                        peer_u[layer], peer_v[layer]).reshape(b, lc, d)
    return rmsnorm(x, final_g)
```

```python
import numpy as np
from contextlib import ExitStack, contextmanager
import concourse.bass as bass
import concourse.mybir as mybir
from concourse.bass_utils import run_bass_kernel_spmd

F32 = mybir.dt.float32
BF16 = mybir.dt.bfloat16
AF = mybir.ActivationFunctionType
ALU = mybir.AluOpType
AX = mybir.AxisListType


class Buf:
    def __init__(self, name):
        self.name = name
        self.w = None
        self.r = {}


class T(Buf):
    def __init__(self, name, h):
        super().__init__(name)
        self.h = h

    def __getitem__(self, idx):
        return self.h[idx]


class Prog:
    ENG = ('pe', 'act', 'dve', 'pool', 'sp')

    def __init__(self, nc, ndma=32):
        self.nc = nc
        self.ndma = ndma
        self.es = ExitStack()
        self.scope = None
        self.drams = {}

    def __enter__(self):
        nc = self.nc
        self.es.__enter__()
        self.engs = {'pe': nc.tensor, 'act': nc.scalar, 'dve': nc.vector, 'pool': nc.gpsimd, 'sp': nc.sync}
        self.sems = {k: self.es.enter_context(nc.semaphore("s_" + k)) for k in ('pe', 'act', 'dve', 'pool')}
        self.cnt = {k: 0 for k in self.sems}
        self.dsem = [self.es.enter_context(nc.semaphore("d%d" % i)) for i in range(self.ndma)]
        self.dval = [0] * self.ndma
        self.dnext = 0
        self.seen = {e: {} for e in self.ENG}
        self.scope = self.es
        self.ninst = 0
        return self

    def __exit__(self, *a):
        return self.es.__exit__(*a)

    def _uniq(self, name):
        self.nalloc = getattr(self, "nalloc", 0) + 1
        return "%s_%d" % (name, self.nalloc)

    def sb(self, name, shape, dtype):
        name = self._uniq(name)
        return T(name, self.scope.enter_context(self.nc.sbuf_tensor(name, list(shape), dtype)))

    def ps(self, name, shape, dtype):
        name = self._uniq(name)
        return T(name, self.scope.enter_context(self.nc.psum_tensor(name, list(shape), dtype)))

    def dram_buf(self, name):
        if name not in self.drams:
            self.drams[name] = Buf(name)
        return self.drams[name]

    @contextmanager
    def phase(self):
        old = self.scope
        with ExitStack() as st:
            self.scope = st
            yield
            self.barrier()
        self.scope = old

    def _semobj(self, key):
        return self.sems[key] if isinstance(key, str) else self.dsem[key[1]]

    def _wait(self, eng, tok, raw=False):
        key, val = tok
        if key == eng and not (raw and eng != 'pe'):
            return
        if self.seen[eng].get(key, 0) >= val:
            return
        self.engs[eng].wait_ge(self._semobj(key), val)
        self.seen[eng][key] = val
        self.ninst += 1

    def _deps(self, eng, r, w):
        for b in r:
            if b.w is not None:
                self._wait(eng, b.w, raw=True)
        for b in w:
            if b.w is not None:
                self._wait(eng, b.w)
            for k, v in b.r.items():
                self._wait(eng, (k, v))

    def _mark(self, tok, r, w):
        k, v = tok
        for b in r:
            if b.r.get(k, 0) < v:
                b.r[k] = v
        for b in w:
            b.w = tok
            b.r = {}

    def op(self, eng, fn, r=(), w=()):
        self._deps(eng, r, w)
        inst = fn(self.engs[eng])
        self.cnt[eng] += 1
        inst.then_inc(self.sems[eng], 1)
        self._mark((eng, self.cnt[eng]), r, w)
        self.ninst += 1

    def dma(self, wbuf, out_ap, in_ap, r=(), q='sp', **kw):
        i = self.dnext
        self.dnext = (i + 1) % self.ndma
        if self.dval[i] > 0:
            self._wait(q, (('d', i), self.dval[i]))
        w = [] if wbuf is None else ([wbuf] if isinstance(wbuf, Buf) else list(wbuf))
        self._deps(q, r, w)
        inst = self.engs[q].dma_start(out=out_ap, in_=in_ap, **kw)
        self.dval[i] += 16
        inst.then_inc(self.dsem[i], 16)
        self._mark((('d', i), self.dval[i]), r, w)
        self.ninst += 1

    def barrier(self):
        for e in self.ENG:
            for f in self.sems:
                if self.cnt[f] > 0:
                    self._wait(e, (f, self.cnt[f]))
            for i in range(self.ndma):
                if self.dval[i] > 0:
                    self._wait(e, (('d', i), self.dval[i]))

    def finish(self):
        self.barrier()


D = 1024
L = 4096
LC = 256
NT = L + LC
NTILE = NT // 128
DEPTH = 2
GW = 64
EPS = 1e-6
NEGB = -30000.0
O_NAQ, O_NAK, O_NAV, O_DFQ, O_DFK, O_DFV, O_RTQ, O_RTK, O_RTV, O_RTG = 0, 256, 512, 768, 1024, 1280, 1536, 1792, 2048, 2560
C_QNA, C_KNA, C_QDF, C_KDA, C_KDB, C_QRT, C_KRT = 0, 2, 4, 6, 8, 10, 12
NFMC = 14


def _rope_tables():
    t = np.arange(L)
    rows = (t // GW).astype(np.float32)
    cols = (t % GW).astype(np.float32)
    out = {}
    for name, dim in (("df", 32), ("rt", 64)):
        half = dim // 2
        inv = np.power(np.float32(10000.0), -np.arange(0, half, 2, dtype=np.float32) / np.float32(half)).astype(np.float32)
        ang = np.concatenate([rows[:, None] * inv, cols[:, None] * inv], axis=-1).astype(np.float32)
        cos = np.cos(ang).astype(np.float32)
        sin = np.sin(ang).astype(np.float32)
        C = np.ones((128, NT), np.float32)
        S = np.zeros((128, NT), np.float32)
        for r in range(128):
            d = r % dim
            pi = d // 2
            C[r, LC:] = cos[:, pi]
            S[r, LC:] = -sin[:, pi] if d % 2 == 0 else sin[:, pi]
        out[name] = (C, S)
    return out


def _na_patterns():
    pats = {}
    for pname, r in (("r0", 0), ("r2", 2), ("mid", 8), ("r60", 60), ("r62", 62)):
        kr0 = min(max(r - 4, 0), 54)
        chunks = []
        for m in range(5):
            dr = np.zeros((128, 128), np.int64)
            dc = np.zeros((128, 128), np.int64)
            va = np.zeros((128, 128), bool)
            for kk in range(128):
                krow = kr0 + 2 * m + kk // 64
                kc = kk % 64
                for qq in range(128):
                    qrow = r + qq // 64
                    qc = qq % 64
                    r0 = min(max(qrow - 4, 0), 56)
                    cs = min(max(qc - 8, 0), 48)
                    ok = (r0 <= krow < r0 + 8) and (cs <= kc < cs + 16) and krow < 64
                    va[kk, qq] = ok
                    if ok:
                        dr[kk, qq] = krow - qrow + 7
                        dc[kk, qq] = min(max(kc - qc, -15), 15) + 15
            chunks.append((dr, dc, va))
        pats[pname] = chunks
    return pats


_CONST_CACHE = {}


def _consts():
    if _CONST_CACHE:
        return _CONST_CACHE
    c = _CONST_CACHE
    rt = _rope_tables()
    s_df = np.float32(32 ** -0.5)
    c["T_DFQ_C"] = rt["df"][0] * s_df
    c["T_DFQ_S"] = rt["df"][1] * s_df
    c["T_DFK_C"] = rt["df"][0]
    c["T_DFK_S"] = rt["df"][1]
    c["T_RTQ_C"] = rt["rt"][0]
    c["T_RTQ_S"] = rt["rt"][1]
    c["T_RTK_C"] = rt["rt"][0] * np.float32(0.125)
    c["T_RTK_S"] = rt["rt"][1] * np.float32(0.125)
    c["IDENT"] = np.eye(128, dtype=np.float32)
    pats = _na_patterns()
    c["_pats"] = pats
    names = ["r0", "r2", "mid", "r60", "r62"]
    mask = np.zeros((128, 5, 5, 128), np.float32)
    for pi, pn in enumerate(names):
        for m in range(5):
            mask[:, pi, m, :] = np.where(pats[pn][m][2], 0.0, NEGB)
    c["NA_MASK"] = mask
    i = np.arange(128, dtype=np.float32)
    ij = i[None, :] - i[:, None]
    c["RT_RIJ"] = np.maximum(ij, 0).astype(np.float32)
    c["RT_MIJ"] = (ij >= 0).astype(np.float32)
    c["RT_RJI"] = np.maximum(-ij, 0).astype(np.float32)
    c["RT_MJI"] = (ij <= 0).astype(np.float32)
    c["RT_IROW"] = np.broadcast_to(i[None, :] + 1.0, (128, 128)).astype(np.float32).copy()
    c["RT_IROWB"] = np.broadcast_to(128.0 - i[None, :], (128, 128)).astype(np.float32).copy()
    c["RT_JCOL"] = np.stack([127.0 - i, i], axis=1).astype(np.float32)
    return c


def _layer_inputs(inp, l):
    c = _consts()
    w_in = np.asarray(inp["w_in"][l], np.float32)
    Z = np.zeros((D, 32), np.float32)

    def sw(cols):
        return cols ^ 1

    fm = []

    def add(cols):
        fm.append(w_in[:, cols])

    ar = np.arange
    for j in range(2):
        add(O_NAQ + j * 128 + ar(128))
    for j in range(2):
        add(O_NAK + j * 128 + ar(128))
    for j in range(2):
        cc = O_DFQ + j * 128 + ar(128)
        add(cc); add(sw(cc))
    for comp in range(2):
        for j in range(2):
            blocks_m, blocks_s = [], []
            for hh in range(2):
                h = 2 * j + hh
                cc = O_DFK + h * 64 + comp * 32 + ar(32)
                if comp == 0:
                    blocks_m += [w_in[:, cc], Z]; blocks_s += [w_in[:, sw(cc)], Z]
                else:
                    blocks_m += [Z, w_in[:, cc]]; blocks_s += [Z, w_in[:, sw(cc)]]
            fm.append(np.concatenate(blocks_m, axis=1)); fm.append(np.concatenate(blocks_s, axis=1))
    for j in range(2):
        cc = O_RTQ + j * 128 + ar(128)
        add(cc); add(sw(cc))
    for j in range(2):
        cc = O_RTK + j * 128 + ar(128)
        add(cc); add(sw(cc))
    WFM = np.ascontiguousarray(np.concatenate(fm, axis=1))
    WTM = np.ascontiguousarray(np.concatenate([w_in[:, O_NAV:O_NAV + 256], w_in[:, O_DFV:O_DFV + 256],
                                               w_in[:, O_RTV:O_RTV + 512], w_in[:, O_RTG:O_RTG + 512]], axis=1))
    rpb = np.asarray(inp["na_rpb"][l], np.float32)
    pats = c["_pats"]
    names = ["r0", "r2", "mid", "r60", "r62"]
    nab = np.zeros((128, 4, 5, 5, 128), np.float32)
    for pi, pn in enumerate(names):
        for m in range(5):
            dr, dc, va = pats[pn][m]
            for h in range(4):
                nab[:, h, pi, m, :] = rpb[h][dr, dc]
    sk = np.asarray(inp["peer_subkeys"][l], np.float32).reshape(16, 128, 128)
    skt = np.ascontiguousarray(sk.transpose(2, 0, 1))
    d = {
        "w_ada": np.asarray(inp["w_ada"][l], np.float32),
        "b_ada": np.asarray(inp["b_ada"][l], np.float32).reshape(1, 6 * D),
        "g1": np.asarray(inp["norm1_g"][l], np.float32).reshape(1, D),
        "g2": np.asarray(inp["norm2_g"][l], np.float32).reshape(1, D),
        "wfm": WFM, "wtm": WTM, "nab": nab,
        "lam": np.asarray(inp["diff_lambda"][l], np.float32).reshape(1, 128),
        "subg": np.asarray(inp["diff_subln_g"][l], np.float32).reshape(1, 64),
        "dec": np.asarray(inp["ret_decay_logit"][l], np.float32).reshape(1, 8),
        "wout": np.asarray(inp["w_out"][l], np.float32),
        "wq": np.asarray(inp["peer_wq"][l], np.float32),
        "skt": skt,
        "ut": np.ascontiguousarray(np.asarray(inp["peer_u"][l], np.float32).T),
        "v": np.asarray(inp["peer_v"][l], np.float32),
    }
    return d


LAYER_SHAPES = {
    "w_ada": [D, 6 * D], "b_ada": [1, 6 * D], "g1": [1, D], "g2": [1, D], "wfm": [D, 24 * 128], "wtm": [D, 1536],
    "nab": [128, 4, 5, 5, 128], "lam": [1, 128], "subg": [1, 64], "dec": [1, 8], "wout": [D, D], "wq": [D, 2048],
    "skt": [128, 16, 128], "ut": [D, 16384], "v": [16384, D],
}
CONST_NAMES = ["T_DFQ_C", "T_DFQ_S", "T_DFK_C", "T_DFK_S", "T_RTQ_C", "T_RTQ_S", "T_RTK_C", "T_RTK_S", "IDENT", "NA_MASK",
               "RT_RIJ", "RT_MIJ", "RT_RJI", "RT_MJI", "RT_IROW", "RT_IROWB", "RT_JCOL"]


class Ctx:
    pass


def build_program(nlayers=DEPTH, debug=None, stop_after=None):
    nc = bass.Bass("TRN2", target_bir_lowering=False)
    g = Ctx()
    g.nc = nc
    g.debug = debug or ()
    din = lambda name, shape, dt=F32: nc.dram_tensor(name, list(shape), dt, kind="ExternalInput").ap()
    dscr = lambda name, shape, dt: nc.dram_tensor(name, list(shape), dt, kind="Internal").ap()
    g.xin = din("xin", [NT, D])
    g.cvec = din("cvec", [128, 8, 2])
    g.final_g = din("final_g", [1, D])
    g.cst = {}
    cs = _consts()
    for n in CONST_NAMES:
        g.cst[n] = din(n, cs[n].shape)
    g.lay = []
    for l in range(nlayers):
        g.lay.append({k: din("L%d_%s" % (l, k), shp) for k, shp in LAYER_SHAPES.items()})
    g.out = nc.dram_tensor("out", [L, D], F32, kind="ExternalOutput").ap()
    g.MODS = dscr("MODS", [2, 6 * D], F32)
    g.X1 = dscr("X1", [NT, D], F32)
    g.X2 = dscr("X2", [NT, D], F32)
    g.FMS = dscr("FMS", [NFMC, 128, NT], BF16)
    g.VNA = dscr("VNA", [NT, 4, 65], BF16)
    g.VDF = dscr("VDF", [NT, 4, 65], BF16)
    g.VRT = dscr("VRT", [NT, 512], BF16)
    g.GRT = dscr("GRT", [NT, 512], BF16)
    g.KRTT = dscr("KRTT", [NT, 256], BF16)
    g.Y = dscr("Y", [NT, D], BF16)
    g.H2T = dscr("H2T", [128, 8, NT], BF16)
    g.UTB = dscr("UTB", [128, 8, 16384], BF16)
    g.VB = dscr("VB", [128, 128, D], BF16)
    g.dbg = {}
    for name, shape, dt in g.debug:
        g.dbg[name] = nc.dram_tensor("dbg_" + name, list(shape), dt, kind="ExternalOutput").ap()
    p = Prog(nc)
    g.p = p
    g.scr = Buf("scratch")
    with p:
        g.ident_f = p.sb("ident_f", [128, 128], F32)
        g.ident = p.sb("ident", [128, 128], BF16)
        p.dma(g.ident_f, g.ident_f[:], g.cst["IDENT"])
        p.op('dve', lambda e: e.tensor_copy(out=g.ident[:], in_=g.ident_f[:]), r=[g.ident_f], w=[g.ident])
        stages = []
        for l in range(nlayers):
            last = (l == DEPTH - 1)
            xa = g.xin if l == 0 else g.X2
            stages += [("mod%d" % l, lambda l=l: phase_mod(g, l)),
                       ("p1_%d" % l, lambda l=l, xa=xa: phase_p1(g, l, xa)),
                       ("na_%d" % l, lambda l=l, last=last: phase_na(g, l, not last)),
                       ("df_%d" % l, lambda l=l, last=last: phase_diff(g, l, not last)),
                       ("rt_%d" % l, lambda l=l, last=last: (phase_ret(g, l, not last), dbg_dump(g, "y%d" % l, g.Y))),
                       ("p3_%d" % l, lambda l=l, last=last, xa=xa: (phase_p3(g, l, xa, last), dbg_dump(g, "x1_%d" % l, g.X1))),
                       ("cast%d" % l, lambda l=l: phase_cast(g, l)),
                       ("peer%d" % l, lambda l=l, last=last: (phase_peer(g, l, last), dbg_dump(g, "x2_%d" % l, g.X2)))]
        for name, fn in stages:
            fn()
            if stop_after == name:
                break
        p.finish()
    g.ninst = p.ninst
    return nc, g


def dbg_dump(g, name, src):
    if name in g.dbg:
        g.p.dma(None, g.dbg[name], src, q='sp')
        g.p.barrier()


def load_bcast(p, t, dram_row, n, q='sp'):
    p.dma(t, t[:, 0:n], dram_row.broadcast_to([128, n]), q=q)


def phase_mod(g, l):
    p = g.p
    Lw = g.lay[l]
    with p.phase():
        cv = p.sb("cv", [128, 8, 2], F32)
        sil = p.sb("sil", [128, 8, 2], F32)
        bad = p.sb("bad", [2, 6 * D], F32)
        modt = p.sb("modt", [2, 6 * D], F32)
        wa = [p.sb("wa%d" % i, [128, 8, 512], F32) for i in range(2)]
        ps = [p.ps("modps%d" % i, [128, 512], F32) for i in range(2)]
        p.dma(cv, cv[:], g.cvec)
        p.dma(bad, bad[:], Lw["b_ada"].broadcast_to([2, 6 * D]))
        p.op('act', lambda e: e.activation(out=sil[:], in_=cv[:], func=AF.Silu), r=[cv], w=[sil])
        wsrc = Lw["w_ada"].rearrange("(k p) n -> p k n", p=128)
        for n in range(12):
            w = wa[n % 2]
            pp = ps[n % 2]
            p.dma(w, w[:], wsrc[:, :, n * 512:(n + 1) * 512])
            for k in range(8):
                p.op('pe', lambda e, k=k, w=w, pp=pp: e.matmul(pp[0:2, :], lhsT=sil[:, k, :], rhs=w[:, k, :],
                                                             start=(k == 0), stop=(k == 7)), r=[sil, w], w=[pp])
            p.op('dve', lambda e, n=n, pp=pp: e.tensor_tensor(out=modt[:, n * 512:(n + 1) * 512], in0=pp[0:2, :],
                                                             in1=bad[:, n * 512:(n + 1) * 512], op=ALU.add),
                 r=[pp, bad], w=[modt])
        p.dma(None, g.MODS, modt[:], r=[modt], q='pool')
        if "mods" in g.dbg:
            p.dma(None, g.dbg["mods"], modt[:], r=[modt], q='pool')


def mod_row(g, which, idx):
    return g.MODS[which:which + 1, idx * D:(idx + 1) * D]


def make_norm_consts(g, tag, grow, idx_shift, idx_scale):
    p = g.p
    gb = p.sb(tag + "_gb", [128, D], F32)
    load_bcast(p, gb, grow, D)
    res = []
    for which in range(2):
        sc = p.sb(tag + "_sc%d" % which, [128, D], F32)
        sh = p.sb(tag + "_sh%d" % which, [128, D], F32)
        load_bcast(p, sc, mod_row(g, which, idx_scale), D)
        load_bcast(p, sh, mod_row(g, which, idx_shift), D)
        p.op('dve', lambda e, sc=sc: e.scalar_tensor_tensor(out=sc[:], in0=sc[:], scalar=1.0, in1=gb[:],
                                                            op0=ALU.add, op1=ALU.mult), r=[sc, gb], w=[sc])
        res.append((sc, sh))
    return res


def emit_norm(g, xt, gs, sh, hb, tmp, st):
    p = g.p
    p.op('act', lambda e: e.activation(out=tmp[:], in_=xt[:], func=AF.Square, accum_out=st[:, 0:1]), r=[xt], w=[tmp, st])
    p.op('dve', lambda e: e.tensor_scalar(out=st[:, 1:2], in0=st[:, 0:1], scalar1=1.0 / D, scalar2=EPS,
                                          op0=ALU.mult, op1=ALU.add), r=[st], w=[st])
    p.op('act', lambda e: e.activation(out=st[:, 2:3], in_=st[:, 1:2], func=AF.Sqrt), r=[st], w=[st])
    p.op('dve', lambda e: e.reciprocal(out=st[:, 3:4], in_=st[:, 2:3]), r=[st], w=[st])
    p.op('dve', lambda e: e.scalar_tensor_tensor(out=tmp[:], in0=xt[:], scalar=st[:, 3:4], in1=gs[:],
                                                 op0=ALU.mult, op1=ALU.mult), r=[xt, st, gs], w=[tmp])
    p.op('dve', lambda e: e.tensor_tensor(out=hb[:], in0=tmp[:], in1=sh[:], op=ALU.add), r=[tmp, sh], w=[hb])


def emit_transposes(g, src, pst, dst_ap, dst_buf, n=8, eng='act'):
    p = g.p
    for k in range(n):
        p.op('pe', lambda e, k=k: e.transpose(pst[:, k, :], src[:, k * 128:(k + 1) * 128], g.ident[:]),
             r=[src, g.ident], w=[pst])
    if eng == 'act':
        p.op('act', lambda e: e.copy(out=dst_ap, in_=pst[:, 0:n, :]), r=[pst], w=[dst_buf])
    else:
        p.op('dve', lambda e: e.tensor_copy(out=dst_ap, in_=pst[:, 0:n, :]), r=[pst], w=[dst_buf])


def tok_groups():
    gs = [(0, 256)]
    for i in range(8):
        gs.append((256 + i * 512, 512))
    return gs


def phase_p1(g, l, xa):
    p = g.p
    Lw = g.lay[l]
    with p.phase():
        HT = p.sb("HT", [128, 8, NT], BF16)
        with p.phase():
            nrm = make_norm_consts(g, "n1", Lw["g1"], 0, 1)
            xt = [p.sb("xt%d" % i, [128, D], F32) for i in range(2)]
            tmp = p.sb("ntmp", [128, D], F32)
            hb = [p.sb("hb%d" % i, [128, D], BF16) for i in range(2)]
            st = [p.sb("nst%d" % i, [128, 4], F32) for i in range(2)]
            pst = [p.ps("pst%d" % i, [128, 8, 128], BF16) for i in range(2)]
            for tt in range(NTILE):
                which = 1 if tt < 2 else 0
                x_ = xt[tt % 2]
                p.dma(x_, x_[:], xa[tt * 128:(tt + 1) * 128, :])
                emit_norm(g, x_, nrm[which][0], nrm[which][1], hb[tt % 2], tmp, st[tt % 2])
                emit_transposes(g, hb[tt % 2], pst[tt % 2], HT[:, :, tt * 128:(tt + 1) * 128], HT)
        if "ht" in g.dbg:
            p.dma(None, g.dbg["ht"], HT[:], r=[HT], q='pool')
        with p.phase():
            wf = [p.sb("wf%d" % i, [128, 8, 128], F32) for i in range(2)]
            wb = [p.sb("wb%d" % i, [128, 8, 128], BF16) for i in range(4)]
            tc_ = [p.sb("tc%d" % i, [128, 512], F32) for i in range(2)]
            ts_ = [p.sb("ts%d" % i, [128, 512], F32) for i in range(2)]
            t1 = [p.sb("rt1_%d" % i, [128, 512], F32) for i in range(2)]
            stg = [p.sb("fstg%d" % i, [128, 512], BF16) for i in range(2)]
            ktm = [p.sb("ktm%d" % i, [128, 4, 128], BF16) for i in range(2)]
            psA = [p.ps("psA%d" % i, [128, 512], F32) for i in range(2)]
            psB = [p.ps("psB%d" % i, [128, 512], F32) for i in range(2)]
            psT = [p.ps("psT%d" % i, [128, 8, 128], BF16) for i in range(2)]
            wsrc = Lw["wfm"].rearrange("(k p) n -> p k n", p=128)
            cnt = {"w": 0, "it": 0}

            def load_w(ci):
                i = cnt["w"]
                cnt["w"] += 1
                f, b = wf[i % 2], wb[i % 4]
                p.dma(f, f[:], wsrc[:, :, ci * 128:(ci + 1) * 128])
                p.op('pool', lambda e: e.tensor_copy(out=b[:], in_=f[:]), r=[f], w=[b])
                return b

            def mm(ps_, w_, t0, n):
                for k in range(8):
                    p.op('pe', lambda e, k=k: e.matmul(ps_[:, 0:n], lhsT=w_[:, k, :], rhs=HT[:, k, t0:t0 + n],
                                                       start=(k == 0), stop=(k == 7)), r=[w_, HT], w=[ps_])

            jobs = []
            wi = 0
            for j in range(2):
                jobs.append((C_QNA + j, wi, None, 0.125, None)); wi += 1
            for j in range(2):
                jobs.append((C_KNA + j, wi, None, 1.0, None)); wi += 1
            for dest, tn in ((C_QDF, "T_DFQ"), (C_KDA, "T_DFK"), (C_KDB, "T_DFK"), (C_QRT, "T_RTQ"), (C_KRT, "T_RTK")):
                for j in range(2):
                    jobs.append((dest + j, wi, wi + 1, 1.0, tn)); wi += 2
            for (dest, wm, ws, scale, tn) in jobs:
                wmb = load_w(wm)
                wsb = load_w(ws) if ws is not None else None
                for (t0, n) in tok_groups():
                    it = cnt["it"]
                    cnt["it"] += 1
                    a, b_ = psA[it % 2], psB[it % 2]
                    sg = stg[it % 2]
                    mm(a, wmb, t0, n)
                    if ws is None:
                        p.op('act', lambda e: e.activation(out=sg[:, 0:n], in_=a[:, 0:n], func=AF.Copy, scale=scale),
                             r=[a], w=[sg])
                    else:
                        mm(b_, wsb, t0, n)
                        tcc, tss, tt1 = tc_[it % 2], ts_[it % 2], t1[it % 2]
                        p.dma(tcc, tcc[:, 0:n], g.cst[tn + "_C"][:, t0:t0 + n])
                        p.dma(tss, tss[:, 0:n], g.cst[tn + "_S"][:, t0:t0 + n])
                        p.op('dve', lambda e: e.tensor_tensor(out=tcc[:, 0:n], in0=a[:, 0:n], in1=tcc[:, 0:n], op=ALU.mult),
                             r=[a, tcc], w=[tcc])
                        p.op('dve', lambda e: e.tensor_tensor(out=tss[:, 0:n], in0=b_[:, 0:n], in1=tss[:, 0:n], op=ALU.mult),
                             r=[b_, tss], w=[tss])
                        p.op('pool', lambda e: e.tensor_tensor(out=sg[:, 0:n], in0=tcc[:, 0:n], in1=tss[:, 0:n], op=ALU.add),
                             r=[tcc, tss], w=[sg])
                    p.dma(None, g.FMS[dest, :, t0:t0 + n], sg[:, 0:n], r=[sg], q='pool')
                    if dest in (C_KRT, C_KRT + 1):
                        nt_ = n // 128
                        pt, kt = psT[it % 2], ktm[it % 2]
                        emit_transposes(g, sg, pt, kt[:, 0:nt_, :], kt, n=nt_)
                        cj = dest - C_KRT
                        p.dma(None, g.KRTT[t0:t0 + n, cj * 128:(cj + 1) * 128].rearrange("(a p) c -> p a c", p=128),
                              kt[:, 0:nt_, :], r=[kt], q='pool')
        with p.phase():
            wtf = p.sb("wtf", [128, 8, 512], F32)
            wtb = p.sb("wtb", [128, 8, 1536], BF16)
            wsrc = Lw["wtm"].rearrange("(k p) n -> p k n", p=128)
            for c3 in range(3):
                p.dma(wtf, wtf[:], wsrc[:, :, c3 * 512:(c3 + 1) * 512])
                p.op('dve', lambda e, c3=c3: e.tensor_copy(out=wtb[:, :, c3 * 512:(c3 + 1) * 512], in_=wtf[:]), r=[wtf], w=[wtb])
            pv = [p.ps("pv%d" % i, [128, 512], F32) for i in range(4)]
            vst = [p.sb("vst%d" % i, [128, 2, 4, 65], BF16) for i in range(2)]
            rst = [p.sb("rst%d" % i, [128, 2, 512], BF16) for i in range(2)]
            for i in range(2):
                p.op('pool', lambda e, i=i: e.memset(vst[i][:], 1.0), w=[vst[i]])
            it = 0
            for tt in range(NTILE):
                vs, rs = vst[tt % 2], rst[tt % 2]
                for c3 in range(3):
                    ps_ = pv[it % 4]
                    it += 1
                    for k in range(8):
                        p.op('pe', lambda e, k=k, ps_=ps_, c3=c3: e.matmul(ps_[:], lhsT=HT[:, k, tt * 128:(tt + 1) * 128],
                                                                          rhs=wtb[:, k, c3 * 512:(c3 + 1) * 512],
                                                                          start=(k == 0), stop=(k == 7)), r=[HT, wtb], w=[ps_])
                    if c3 == 0:
                        p.op('act', lambda e, ps_=ps_: e.copy(out=vs[:, :, :, 0:64],
                                                             in_=ps_[:].rearrange("p (a h d) -> p a h d", a=2, h=4)),
                             r=[ps_], w=[vs])
                    elif c3 == 1:
                        p.op('dve', lambda e, ps_=ps_: e.tensor_copy(out=rs[:, 0, :], in_=ps_[:]), r=[ps_], w=[rs])
                    else:
                        p.op('act', lambda e, ps_=ps_: e.activation(out=rs[:, 1, :], in_=ps_[:], func=AF.Silu), r=[ps_], w=[rs])
                sl = slice(tt * 128, (tt + 1) * 128)
                p.dma(None, g.VNA[sl], vs[:, 0], r=[vs], q='pool')
                p.dma(None, g.VDF[sl], vs[:, 1], r=[vs], q='pool')
                p.dma(None, g.VRT[sl], rs[:, 0, :], r=[rs], q='pool')
                p.dma(None, g.GRT[sl], rs[:, 1, :], r=[rs], q='pool')
        for nm, src in (("fms", g.FMS), ("vna", g.VNA), ("vdf", g.VDF), ("vrt", g.VRT), ("grt", g.GRT), ("krtt", g.KRTT)):
            if nm in g.dbg:
                p.dma(None, g.dbg[nm], src, q='sp')


def phase_na(g, l, do_ctx):
    p = g.p
    Lw = g.lay[l]
    with p.phase():
        bias = p.sb("na_bias", [128, 4, 25, 128], BF16)
        with p.phase():
            stg = p.sb("na_bstg", [128, 25, 128], F32)
            msk = p.sb("na_msk", [128, 25, 128], F32)
            p.dma(msk, msk[:], g.cst["NA_MASK"].rearrange("p a b q -> p (a b) q"))
            for h in range(4):
                p.dma(stg, stg[:], Lw["nab"][:, h].rearrange("p a b q -> p (a b) q"))
                p.op('dve', lambda e, h=h: e.tensor_tensor(out=bias[:, h], in0=stg[:], in1=msk[:], op=ALU.add),
                     r=[stg, msk], w=[bias])
        V = p.sb("na_v", [128, NTILE, 260], BF16)
        p.dma(V, V[:], g.VNA.rearrange("(a p) h d -> p a (h d)", p=128))
        qT = [p.sb("na_q%d" % i, [128, NT], BF16) for i in range(2)]
        kT = [p.sb("na_k%d" % i, [128, NT], BF16) for i in range(2)]
        for i in range(2):
            p.dma(qT[i], qT[i][:], g.FMS[C_QNA + i])
            p.dma(kT[i], kT[i][:], g.FMS[C_KNA + i])
        psS = [[p.ps("na_s%d%d" % (i, j), [128, 512], F32) for j in range(2)] for i in range(2)]
        pso = [p.ps("na_o%d" % i, [128, 512], F32) for i in range(2)]
        PT = [p.sb("na_pt%d" % i, [128, 8, 128], BF16) for i in range(2)]
        ys = [p.sb("na_ys%d" % i, [128, 256], BF16) for i in range(2)]
        rc = [p.sb("na_rc%d" % i, [128, 1], F32) for i in range(2)]
        tiles = []
        if do_ctx:
            for q0 in (0, 128):
                tiles.append((q0, [(0, None), (128, None)], 0))
        for rp in range(32):
            r = 2 * rp
            kr0 = min(max(r - 4, 0), 54)
            pat = {0: 0, 2: 1, 60: 3, 62: 4}.get(r, 2)
            ch = [(LC + (kr0 + 2 * m) * 64, m) for m in range(5)] + [(0, None), (128, None)]
            tiles.append((LC + rp * 128, ch, pat))
        it = 0
        for qi, (q0, ch, pat) in enumerate(tiles):
            y_ = ys[qi % 2]
            for h in range(4):
                hp, base = h // 2, (h % 2) * 64
                sA, sB = psS[it % 2]
                po, pt, rc_ = pso[it % 2], PT[it % 2], rc[it % 2]
                it += 1
                n = len(ch)
                for i, (tok0, m) in enumerate(ch):
                    bank = sA if i < 4 else sB
                    col = (i % 4) * 128
                    p.op('pe', lambda e, bank=bank, col=col, tok0=tok0, m=m: e.matmul(
                        bank[:, col:col + 128], lhsT=kT[hp][base:base + 64, tok0:tok0 + 128],
                        rhs=qT[hp][base:base + 64, q0:q0 + 128], start=True, stop=(m is None)),
                        r=[kT[hp], qT[hp]], w=[bank])
                    if m is not None:
                        p.op('pe', lambda e, bank=bank, col=col, m=m: e.matmul(
                            bank[:, col:col + 128], lhsT=g.ident[:], rhs=bias[:, h, pat * 5 + m, :],
                            start=False, stop=True), r=[g.ident, bias], w=[bank])
                na_ = min(n, 4)
                p.op('act', lambda e: e.activation(out=pt[:, 0:na_, :], in_=sA[:, 0:na_ * 128].rearrange("p (a q) -> p a q", q=128),
                                                   func=AF.Exp), r=[sA], w=[pt])
                if n > 4:
                    p.op('act', lambda e: e.activation(out=pt[:, 4:n, :], in_=sB[:, 0:(n - 4) * 128].rearrange("p (a q) -> p a q", q=128),
                                                       func=AF.Exp), r=[sB], w=[pt])
                for i, (tok0, m) in enumerate(ch):
                    vt = tok0 // 128
                    p.op('pe', lambda e, i=i, vt=vt: e.matmul(po[:, 0:65], lhsT=pt[:, i, :], rhs=V[:, vt, h * 65:(h + 1) * 65],
                                                              start=(i == 0), stop=(i == n - 1)), r=[pt, V], w=[po])
                p.op('dve', lambda e: e.reciprocal(out=rc_[:], in_=po[:, 64:65]), r=[po], w=[rc_])
                p.op('dve', lambda e: e.tensor_scalar(out=y_[:, h * 64:(h + 1) * 64], in0=po[:, 0:64], scalar1=rc_[:, 0:1],
                                                      scalar2=None, op0=ALU.mult), r=[po, rc_], w=[y_])
            p.dma(None, g.Y[q0:q0 + 128, 0:256], y_[:], r=[y_], q='pool')


def phase_diff(g, l, do_ctx):
    p = g.p
    Lw = g.lay[l]
    import math
    lam_init = 0.8 - 0.6 * math.exp(-0.3 * l)
    with p.phase():
        lp = p.sb("df_lp", [128, 4, 32], F32)
        pr = p.sb("df_pr", [128, 2, 32], F32)
        sm = p.sb("df_sm", [128, 4], F32)
        neglam = p.sb("df_nl", [128, 1], F32)
        gsub = p.sb("df_gs", [128, 64], F32)
        p.dma(lp, lp[:].rearrange("p a b -> p (a b)"), Lw["lam"].broadcast_to([128, 128]))
        load_bcast(p, gsub, Lw["subg"], 64)
        p.op('dve', lambda e: e.tensor_scalar(out=gsub[:], in0=gsub[:], scalar1=1.0 - lam_init, scalar2=None, op0=ALU.mult),
             r=[gsub], w=[gsub])
        p.op('dve', lambda e: e.tensor_tensor(out=pr[:], in0=lp[:, 0:4:2, :], in1=lp[:, 1:4:2, :], op=ALU.mult), r=[lp], w=[pr])
        p.op('dve', lambda e: e.tensor_reduce(out=sm[:, 0:2], in_=pr[:], axis=AX.X, op=ALU.add), r=[pr], w=[sm])
        p.op('act', lambda e: e.activation(out=sm[:, 2:4], in_=sm[:, 0:2], func=AF.Exp), r=[sm], w=[sm])
        p.op('dve', lambda e: e.tensor_tensor(out=neglam[:], in0=sm[:, 3:4], in1=sm[:, 2:3], op=ALU.subtract), r=[sm], w=[neglam])
        p.op('dve', lambda e: e.tensor_scalar(out=neglam[:], in0=neglam[:], scalar1=-lam_init, scalar2=None, op0=ALU.add),
             r=[neglam], w=[neglam])
        V = p.sb("df_v", [128, NTILE, 260], BF16)
        p.dma(V, V[:], g.VDF.rearrange("(a p) h d -> p a (h d)", p=128))
        qT = [p.sb("df_q%d" % i, [128, NT], BF16) for i in range(2)]
        kA = [p.sb("df_ka%d" % i, [128, NT], BF16) for i in range(2)]
        kB = [p.sb("df_kb%d" % i, [128, NT], BF16) for i in range(2)]
        for i in range(2):
            p.dma(qT[i], qT[i][:], g.FMS[C_QDF + i])
            p.dma(kA[i], kA[i][:], g.FMS[C_KDA + i])
            p.dma(kB[i], kB[i][:], g.FMS[C_KDB + i])
        acc = [p.ps("df_acc%d" % i, [128, 512], F32) for i in range(4)]
        psS = [p.ps("df_s%d" % i, [128, 512], F32) for i in range(2)]
        PT = [p.sb("df_pt%d" % i, [128, 512], BF16) for i in range(2)]
        oc = [p.sb("df_oc%d" % i, [128, 4, 64], F32) for i in range(2)]
        o = p.sb("df_o", [128, 4, 64], F32)
        sq = p.sb("df_sq", [128, 4, 64], F32)
        ss = p.sb("df_ss", [128, 8], F32)
        rc = p.sb("df_rc", [128, 4], F32)
        yd = [p.sb("df_y%d" % i, [128, 4, 64], BF16) for i in range(2)]
        groups = []
        if do_ctx:
            groups.append((0, 256, [0, 1]))
        for gi in range(8):
            groups.append((LC + gi * 512, 512, list(range(NTILE))))
        it = 0
        gi_ = 0
        for h in range(4):
            hp, base = h // 2, (h % 2) * 64
            for (t0, n, kcs) in groups:
                ns = n // 128
                for c in range(2):
                    kX = (kA if c == 0 else kB)[hp]
                    for ki, kc in enumerate(kcs):
                        ps_, pt = psS[it % 2], PT[it % 2]
                        it += 1
                        p.op('pe', lambda e, kc=kc, ps_=ps_, kX=kX: e.matmul(
                            ps_[:, 0:n], lhsT=kX[base:base + 64, kc * 128:(kc + 1) * 128], rhs=qT[hp][base:base + 64, t0:t0 + n],
                            start=True, stop=True), r=[kX, qT[hp]], w=[ps_])
                        p.op('act', lambda e, ps_=ps_, pt=pt: e.activation(out=pt[:, 0:n], in_=ps_[:, 0:n], func=AF.Exp),
                             r=[ps_], w=[pt])
                        for s in range(ns):
                            p.op('pe', lambda e, s=s, kc=kc, pt=pt, ki=ki: e.matmul(
                                acc[s][:, 0:65], lhsT=pt[:, s * 128:(s + 1) * 128], rhs=V[:, kc, h * 65:(h + 1) * 65],
                                start=(ki == 0), stop=(ki == len(kcs) - 1)), r=[pt, V], w=[acc[s]])
                    for s in range(ns):
                        p.op('dve', lambda e, s=s: e.reciprocal(out=rc[:, s:s + 1], in_=acc[s][:, 64:65]), r=[acc[s]], w=[rc])
                        p.op('dve', lambda e, s=s, c=c: e.tensor_scalar(out=oc[c][:, s, :], in0=acc[s][:, 0:64], scalar1=rc[:, s:s + 1],
                                                                       scalar2=None, op0=ALU.mult), r=[acc[s], rc], w=[oc[c]])
                y_ = yd[gi_ % 2]
                gi_ += 1
                p.op('dve', lambda e: e.scalar_tensor_tensor(out=o[:, 0:ns, :], in0=oc[1][:, 0:ns, :], scalar=neglam[:, 0:1],
                                                             in1=oc[0][:, 0:ns, :], op0=ALU.mult, op1=ALU.add),
                     r=[oc[0], oc[1], neglam], w=[o])
                p.op('dve', lambda e: e.tensor_tensor(out=sq[:, 0:ns, :], in0=o[:, 0:ns, :], in1=o[:, 0:ns, :], op=ALU.mult), r=[o], w=[sq])
                p.op('dve', lambda e: e.tensor_reduce(out=ss[:, 0:ns], in_=sq[:, 0:ns, :], axis=AX.X, op=ALU.add), r=[sq], w=[ss])
                p.op('dve', lambda e: e.tensor_scalar(out=ss[:, 0:ns], in0=ss[:, 0:ns], scalar1=1.0 / 64, scalar2=EPS,
                                                      op0=ALU.mult, op1=ALU.add), r=[ss], w=[ss])
                p.op('act', lambda e: e.activation(out=ss[:, 4:4 + ns], in_=ss[:, 0:ns], func=AF.Sqrt), r=[ss], w=[ss])
                p.op('dve', lambda e: e.reciprocal(out=ss[:, 0:ns], in_=ss[:, 4:4 + ns]), r=[ss], w=[ss])
                p.op('dve', lambda e: e.tensor_tensor(out=sq[:, 0:ns, :], in0=o[:, 0:ns, :],
                                                      in1=ss[:, 0:ns].unsqueeze(2).broadcast_to([128, ns, 64]), op=ALU.mult),
                     r=[o, ss], w=[sq])
                p.op('dve', lambda e: e.tensor_tensor(out=y_[:, 0:ns, :], in0=sq[:, 0:ns, :],
                                                      in1=gsub[:].unsqueeze(1).broadcast_to([128, ns, 64]), op=ALU.mult),
                     r=[sq, gsub], w=[y_])
                p.dma(None, g.Y[t0:t0 + n, 256 + h * 64:256 + (h + 1) * 64].rearrange("(s p) d -> p s d", p=128),
                      y_[:, 0:ns, :], r=[y_], q='pool')


def phase_ret(g, l, do_ctx):
    p = g.p
    Lw = g.lay[l]
    with p.phase():
        dec = p.sb("rt_dec", [128, 8], F32)
        lg = p.sb("rt_lg", [128, 8], F32)
        cdec = p.sb("rt_cdec", [128, 8], F32)
        kdec = p.sb("rt_kdec", [128, 8], F32)
        jcol = p.sb("rt_jcol", [128, 2], F32)
        cm = {}
        for nm in ("RT_RIJ", "RT_MIJ", "RT_RJI", "RT_MJI", "RT_IROW", "RT_IROWB"):
            cm[nm] = p.sb("c_" + nm, [128, 128], F32)
            p.dma(cm[nm], cm[nm][:], g.cst[nm])
        p.dma(jcol, jcol[:], g.cst["RT_JCOL"])
        load_bcast(p, dec, Lw["dec"], 8)
        p.op('act', lambda e: e.activation(out=lg[:], in_=dec[:], func=AF.Sigmoid), r=[dec], w=[lg])
        p.op('act', lambda e: e.activation(out=lg[:], in_=lg[:], func=AF.Ln), r=[lg], w=[lg])
        p.op('act', lambda e: e.activation(out=cdec[:], in_=lg[:], func=AF.Exp, scale=128.0), r=[lg], w=[cdec])
        QDF = p.sb("rt_qdf", [128, 4, 128], F32)
        QDB = p.sb("rt_qdb", [128, 4, 128], F32)
        DB = p.sb("rt_db", [128, 4, 128], F32)
        t1 = p.sb("rt_t1", [128, 128], F32)
        t2 = p.sb("rt_t2", [128, 128], F32)
        for d_ in range(2):
            for h in range(4):
                c = d_ * 4 + h
                p.op('act', lambda e, c=c, d_=d_: e.activation(out=kdec[:, c:c + 1], in_=jcol[:, d_:d_ + 1], func=AF.Exp,
                                                               scale=lg[:, c:c + 1]), r=[jcol, lg], w=[kdec])
        for h in range(4):
            p.op('act', lambda e, h=h: e.activation(out=QDF[:, h, :], in_=cm["RT_IROW"][:], func=AF.Exp, scale=lg[:, h:h + 1]),
                 r=[cm["RT_IROW"], lg], w=[QDF])
            p.op('act', lambda e, h=h: e.activation(out=QDB[:, h, :], in_=cm["RT_IROWB"][:], func=AF.Exp, scale=lg[:, 4 + h:5 + h]),
                 r=[cm["RT_IROWB"], lg], w=[QDB])
            p.op('act', lambda e, h=h: e.activation(out=t1[:], in_=cm["RT_RIJ"][:], func=AF.Exp, scale=lg[:, h:h + 1]),
                 r=[cm["RT_RIJ"], lg], w=[t1])
            p.op('act', lambda e, h=h: e.activation(out=t2[:], in_=cm["RT_RJI"][:], func=AF.Exp, scale=lg[:, 4 + h:5 + h]),
                 r=[cm["RT_RJI"], lg], w=[t2])
            p.op('dve', lambda e: e.tensor_tensor(out=t1[:], in0=t1[:], in1=cm["RT_MIJ"][:], op=ALU.mult), r=[t1, cm["RT_MIJ"]], w=[t1])
            p.op('dve', lambda e: e.tensor_tensor(out=t2[:], in0=t2[:], in1=cm["RT_MJI"][:], op=ALU.mult), r=[t2, cm["RT_MJI"]], w=[t2])
            p.op('dve', lambda e, h=h: e.tensor_tensor(out=DB[:, h, :], in0=t1[:], in1=t2[:], op=ALU.add), r=[t1, t2], w=[DB])
        qT = p.sb("rt_q", [64, NT], BF16)
        kT = p.sb("rt_k", [64, NT], BF16)
        Ktm = p.sb("rt_ktm", [128, NTILE, 64], BF16)
        V = p.sb("rt_v", [128, NTILE, 128], BF16)
        G = p.sb("rt_g", [128, NTILE, 128], BF16)
        KF = p.sb("rt_kf", [128, NTILE, 64], BF16)
        KB = p.sb("rt_kb", [128, NTILE, 64], BF16)
        SFa = p.sb("rt_sfa", [64, NTILE, 128], BF16)
        SBa = p.sb("rt_sba", [64, NTILE, 128], BF16)
        S = [p.sb("rt_S%d" % i, [64, 128], F32) for i in range(2)]
        psKV = [p.ps("rt_kv%d" % i, [128, 512], F32) for i in range(2)]
        psA = [p.ps("rt_att%d" % i, [128, 512], F32) for i in range(2)]
        psO = [p.ps("rt_o%d" % i, [128, 512], F32) for i in range(2)]
        attm = [p.sb("rt_attm%d" % i, [128, 128], BF16) for i in range(2)]
        qf = [p.sb("rt_qf%d" % i, [64, 128], BF16) for i in range(2)]
        qb = [p.sb("rt_qb%d" % i, [64, 128], BF16) for i in range(2)]
        junk = p.sb("rt_junk", [128, 128], F32)
        st = [p.sb("rt_st%d" % i, [128, 4], F32) for i in range(2)]
        ys = [p.sb("rt_ys%d" % i, [128, 128], BF16) for i in range(2)]
        it = 0
        for h in range(4):
            hp, base = h // 2, (h % 2) * 64
            p.dma(qT, qT[:], g.FMS[C_QRT + hp, base:base + 64, :])
            p.dma(kT, kT[:], g.FMS[C_KRT + hp, base:base + 64, :])
            p.dma(Ktm, Ktm[:], g.KRTT[:, h * 64:(h + 1) * 64].rearrange("(a p) d -> p a d", p=128))
            p.dma(V, V[:], g.VRT[:, h * 128:(h + 1) * 128].rearrange("(a p) d -> p a d", p=128))
            p.dma(G, G[:], g.GRT[:, h * 128:(h + 1) * 128].rearrange("(a p) d -> p a d", p=128))
            p.op('dve', lambda e: e.tensor_scalar(out=KF[:], in0=Ktm[:], scalar1=kdec[:, h:h + 1], scalar2=None, op0=ALU.mult),
                 r=[Ktm, kdec], w=[KF])
            p.op('dve', lambda e: e.tensor_scalar(out=KB[:], in0=Ktm[:], scalar1=kdec[:, 4 + h:5 + h], scalar2=None, op0=ALU.mult),
                 r=[Ktm, kdec], w=[KB])
            for d_, (Kd, Sa, order) in enumerate(((KF, SFa, list(range(NTILE))),
                                                  (KB, SBa, [1, 0] + list(range(NTILE - 1, 1, -1))))):
                S_ = S[d_]
                p.op('pool', lambda e: e.memset(S_[:], 0.0), w=[S_])
                cd = cdec[0:64, d_ * 4 + h:d_ * 4 + h + 1]
                for oi, c in enumerate(order):
                    p.op('act', lambda e, c=c: e.copy(out=Sa[:, c, :], in_=S_[:]), r=[S_], w=[Sa])
                    if oi == len(order) - 1:
                        break
                    kv = psKV[it % 2]
                    it += 1
                    p.op('pe', lambda e, c=c, kv=kv: e.matmul(kv[0:64, 0:128], lhsT=Kd[:, c, :], rhs=V[:, c, :], start=True, stop=True),
                         r=[Kd, V], w=[kv])
                    p.op('dve', lambda e, kv=kv: e.scalar_tensor_tensor(out=S_[:], in0=S_[:], scalar=cd, in1=kv[0:64, 0:128],
                                                                        op0=ALU.mult, op1=ALU.add), r=[S_, cdec, kv], w=[S_])
            for c in (range(NTILE) if do_ctx else range(2, NTILE)):
                tok = c * 128
                pa, po = psA[c % 2], psO[c % 2]
                am, qf_, qb_, st_, y_ = attm[c % 2], qf[c % 2], qb[c % 2], st[c % 2], ys[c % 2]
                p.op('pe', lambda e: e.matmul(pa[:, 0:128], lhsT=kT[:, tok:tok + 128], rhs=qT[:, tok:tok + 128], start=True, stop=True),
                     r=[kT, qT], w=[pa])
                p.op('dve', lambda e: e.tensor_tensor(out=am[:], in0=pa[:, 0:128], in1=DB[:, h, :], op=ALU.mult), r=[pa, DB], w=[am])
                p.op('pool', lambda e: e.tensor_tensor(out=qf_[:], in0=qT[:, tok:tok + 128], in1=QDF[0:64, h, :], op=ALU.mult),
                     r=[qT, QDF], w=[qf_])
                p.op('pool', lambda e: e.tensor_tensor(out=qb_[:], in0=qT[:, tok:tok + 128], in1=QDB[0:64, h, :], op=ALU.mult),
                     r=[qT, QDB], w=[qb_])
                p.op('pe', lambda e: e.matmul(po[:, 0:128], lhsT=am[:], rhs=V[:, c, :], start=True, stop=False), r=[am, V], w=[po])
                p.op('pe', lambda e: e.matmul(po[:, 0:128], lhsT=qf_[:], rhs=SFa[:, c, :], start=False, stop=False), r=[qf_, SFa], w=[po])
                p.op('pe', lambda e: e.matmul(po[:, 0:128], lhsT=qb_[:], rhs=SBa[:, c, :], start=False, stop=True), r=[qb_, SBa], w=[po])
                p.op('act', lambda e: e.activation(out=junk[:], in_=po[:, 0:128], func=AF.Square, accum_out=st_[:, 0:1]),
                     r=[po], w=[junk, st_])
                p.op('dve', lambda e: e.tensor_scalar(out=st_[:, 1:2], in0=st_[:, 0:1], scalar1=1.0 / 128, scalar2=EPS,
                                                      op0=ALU.mult, op1=ALU.add), r=[st_], w=[st_])
                p.op('act', lambda e: e.activation(out=st_[:, 2:3], in_=st_[:, 1:2], func=AF.Sqrt), r=[st_], w=[st_])
                p.op('dve', lambda e: e.reciprocal(out=st_[:, 3:4], in_=st_[:, 2:3]), r=[st_], w=[st_])
                p.op('dve', lambda e: e.scalar_tensor_tensor(out=y_[:], in0=po[:, 0:128], scalar=st_[:, 3:4], in1=G[:, c, :],
                                                             op0=ALU.mult, op1=ALU.mult), r=[po, st_, G], w=[y_])
                p.dma(None, g.Y[tok:tok + 128, 512 + h * 128:512 + (h + 1) * 128], y_[:], r=[y_], q='pool')


def phase_p3(g, l, xa, last):
    p = g.p
    Lw = g.lay[l]
    with p.phase():
        wob = p.sb("wob", [128, 8, D], BF16)
        with p.phase():
            wof = p.sb("wof", [128, 8, 512], F32)
            wsrc = Lw["wout"].rearrange("(k p) n -> p k n", p=128)
            for n in range(2):
                p.dma(wof, wof[:], wsrc[:, :, n * 512:(n + 1) * 512])
                p.op('dve', lambda e, n=n: e.tensor_copy(out=wob[:, :, n * 512:(n + 1) * 512], in_=wof[:]), r=[wof], w=[wob])
        gt = []
        for which in range(2):
            t = p.sb("p3_gt%d" % which, [128, D], F32)
            load_bcast(p, t, mod_row(g, which, 2), D)
            gt.append(t)
        nrm = make_norm_consts(g, "n2", Lw["g2"], 3, 4)
        yt = [p.sb("p3_y%d" % i, [128, D], BF16) for i in range(2)]
        xt = [p.sb("p3_x%d" % i, [128, D], F32) for i in range(2)]
        x1 = [p.sb("p3_x1%d" % i, [128, D], F32) for i in range(2)]
        yT = [p.sb("p3_yT%d" % i, [128, 8, 128], BF16) for i in range(2)]
        hT = [p.sb("p3_hT%d" % i, [128, 8, 128], BF16) for i in range(2)]
        hb = [p.sb("p3_hb%d" % i, [128, D], BF16) for i in range(2)]
        tmp = p.sb("p3_tmp", [128, D], F32)
        st = [p.sb("p3_st%d" % i, [128, 4], F32) for i in range(2)]
        pstY = [p.ps("p3_pty%d" % i, [128, 8, 128], BF16) for i in range(2)]
        pstH = [p.ps("p3_pth%d" % i, [128, 8, 128], BF16) for i in range(2)]
        psO = [[p.ps("p3_o%d%d" % (i, n), [128, 512], F32) for n in range(2)] for i in range(2)]
        for ti, tt in enumerate(range(2, NTILE) if last else range(NTILE)):
            which = 1 if tt < 2 else 0
            b = ti % 2
            sl = slice(tt * 128, (tt + 1) * 128)
            p.dma(yt[b], yt[b][:], g.Y[sl, :])
            p.dma(xt[b], xt[b][:], xa[sl, :])
            emit_transposes(g, yt[b], pstY[b], yT[b][:], yT[b])
            for n in range(2):
                po = psO[b][n]
                for k in range(8):
                    p.op('pe', lambda e, k=k, n=n, po=po: e.matmul(po[:], lhsT=yT[b][:, k, :], rhs=wob[:, k, n * 512:(n + 1) * 512],
                                                                  start=(k == 0), stop=(k == 7)), r=[yT[b], wob], w=[po])
                cs = slice(n * 512, (n + 1) * 512)
                p.op('dve', lambda e, po=po, cs=cs: e.tensor_tensor(out=tmp[:, cs], in0=po[:], in1=gt[which][:, cs], op=ALU.mult),
                     r=[po, gt[which]], w=[tmp])
                p.op('pool', lambda e, cs=cs: e.tensor_tensor(out=x1[b][:, cs], in0=tmp[:, cs], in1=xt[b][:, cs], op=ALU.add),
                     r=[tmp, xt[b]], w=[x1[b]])
            p.dma(None, g.X1[sl, :], x1[b][:], r=[x1[b]], q='pool')
            emit_norm(g, x1[b], nrm[which][0], nrm[which][1], hb[b], tmp, st[b])
            emit_transposes(g, hb[b], pstH[b], hT[b][:], hT[b])
            p.dma(None, g.H2T[:, :, sl], hT[b][:], r=[hT[b]], q='pool')


def phase_cast(g, l):
    p = g.p
    Lw = g.lay[l]
    with p.phase():
        f = [p.sb("cs_f%d" % i, [128, 2048], F32) for i in range(3)]
        b = [p.sb("cs_b%d" % i, [128, 2048], BF16) for i in range(3)]
        engs = ['act', 'dve', 'pool']
        it = 0
        usrc = Lw["ut"].rearrange("(k p) e -> p k e", p=128)
        vsrc = Lw["v"].rearrange("(i p) d -> p i d", p=128)
        for k in range(8):
            for ec in range(8):
                i = it % 3
                it += 1
                p.dma(f[i], f[i][:], usrc[:, k, ec * 2048:(ec + 1) * 2048])
                if engs[i] == 'act':
                    p.op('act', lambda e, i=i: e.copy(out=b[i][:], in_=f[i][:]), r=[f[i]], w=[b[i]])
                else:
                    p.op(engs[i], lambda e, i=i: e.tensor_copy(out=b[i][:], in_=f[i][:]), r=[f[i]], w=[b[i]])
                p.dma(None, g.UTB[:, k, ec * 2048:(ec + 1) * 2048], b[i][:], r=[b[i]], q='pool')
        for ic in range(64):
            i = it % 3
            it += 1
            p.dma(f[i], f[i][:].rearrange("p (a d) -> p a d", a=2), vsrc[:, ic * 2:(ic + 1) * 2, :])
            if engs[i] == 'act':
                p.op('act', lambda e, i=i: e.copy(out=b[i][:], in_=f[i][:]), r=[f[i]], w=[b[i]])
            else:
                p.op(engs[i], lambda e, i=i: e.tensor_copy(out=b[i][:], in_=f[i][:]), r=[f[i]], w=[b[i]])
            p.dma(None, g.VB[:, ic * 2:(ic + 1) * 2, :], b[i][:].rearrange("p (a d) -> p a d", a=2), r=[b[i]], q='pool')


NI = 8


def phase_peer(g, l, last):
    p = g.p
    Lw = g.lay[l]
    with p.phase():
        wq = p.sb("pe_wq", [128, 8, 2048], BF16)
        skt = p.sb("pe_skt", [128, 16, 128], BF16)
        with p.phase():
            wqf = p.sb("pe_wqf", [128, 8, 512], F32)
            sktf = p.sb("pe_sktf", [128, 16, 128], F32)
            wsrc = Lw["wq"].rearrange("(k p) n -> p k n", p=128)
            for n in range(4):
                p.dma(wqf, wqf[:], wsrc[:, :, n * 512:(n + 1) * 512])
                p.op('dve', lambda e, n=n: e.tensor_copy(out=wq[:, :, n * 512:(n + 1) * 512], in_=wqf[:]), r=[wqf], w=[wq])
            p.dma(sktf, sktf[:], Lw["skt"])
            p.op('dve', lambda e: e.tensor_copy(out=skt[:], in_=sktf[:]), r=[sktf], w=[skt])
        gt = []
        for which in range(2):
            t = p.sb("pe_gt%d" % which, [128, D], F32)
            load_bcast(p, t, mod_row(g, which, 5), D)
            gt.append(t)
        if last:
            fg = p.sb("pe_fg", [128, D], F32)
            load_bcast(p, fg, g.final_g, D)
        h2t = [p.sb("pe_h2t%d" % i, [128, 8, 256], BF16) for i in range(2)]
        e1 = [p.sb("pe_e1_%d" % i, [128, 8, 128], F32) for i in range(2)]
        e2 = [p.sb("pe_e2_%d" % i, [128, 8, 128], F32) for i in range(2)]
        Tt = [p.sb("pe_T%d" % i, [128, 8], F32) for i in range(2)]
        kap = [p.sb("pe_kap%d" % i, [128, 8], F32) for i in range(2)]
        Dk = [[p.sb("pe_dk%d_%d" % (i, h), [128, 128], BF16) for h in range(8)] for i in range(2)]
        acc = [[p.ps("pe_acc%d%d" % (i, n), [128, 512], F32) for n in range(2)] for i in range(2)]
        psA = p.ps("pe_A", [128, 512], F32)
        psW = p.ps("pe_W", [128, 512], F32)
        psT = p.ps("pe_T", [128, 8, 128], BF16)
        psM = p.ps("pe_M", [128, 512], F32)
        pairs = list(range(1, NTILE // 2)) if last else list(range(NTILE // 2))
        for pi, pr in enumerate(pairs):
            t0 = pr * 256
            which = 1 if pr == 0 else 0
            ht = h2t[pi % 2]
            p.dma(ht, ht[:], g.H2T[:, :, t0:t0 + 256])
            with p.phase():
                qTb = p.sb("pe_qT", [128, 16, 256], BF16)
                S_all = [p.sb("pe_S%d" % i, [128, 16, 128], F32) for i in range(2)]
                m8 = p.sb("pe_m8", [128, 2, 16], F32)
                sr = p.sb("pe_sr", [128, 128], F32)
                ngm = p.sb("pe_ngm", [128, 2], F32)
                et = p.sb("pe_et", [128, 2, 16], F32)
                cand = p.sb("pe_cand", [128, 16, 16], F32)
                cr = p.sb("pe_cr", [128, 256], F32)
                c16 = p.sb("pe_c16", [128, 16], F32)
                zz = p.sb("pe_zz", [128, 1], F32)
                for j in range(16):
                    for k in range(8):
                        p.op('pe', lambda e, j=j, k=k: e.matmul(psM[:, 0:256], lhsT=wq[:, k, j * 128:(j + 1) * 128], rhs=ht[:, k, :],
                                                                start=(k == 0), stop=(k == 7)), r=[wq, ht], w=[psM])
                    if j % 2 == 0:
                        p.op('act', lambda e, j=j: e.copy(out=qTb[:, j, :], in_=psM[:, 0:256]), r=[psM], w=[qTb])
                    else:
                        p.op('dve', lambda e, j=j: e.tensor_copy(out=qTb[:, j, :], in_=psM[:, 0:256]), r=[psM], w=[qTb])
                for sub in range(2):
                    for jq in range(4):
                        for jj in range(4):
                            j = jq * 4 + jj
                            p.op('pe', lambda e, j=j, jj=jj: e.matmul(psM[:, jj * 128:(jj + 1) * 128],
                                                                      lhsT=qTb[:, j, sub * 128:(sub + 1) * 128], rhs=skt[:, j, :],
                                                                      start=True, stop=True), r=[qTb, skt], w=[psM])
                        p.op('act', lambda e, jq=jq: e.copy(out=S_all[sub][:, jq * 4:(jq + 1) * 4, :],
                                                           in_=psM[:].rearrange("p (a n) -> p a n", a=4)), r=[psM], w=[S_all[sub]])
                    for h in range(8):
                        for pp in range(2):
                            s_ = S_all[sub][:, 2 * h + pp, :]
                            p.op('dve', lambda e, s_=s_, pp=pp: e.max(out=m8[:, pp, 0:8], in_=s_), r=[S_all[sub]], w=[m8])
                            p.op('dve', lambda e, s_=s_, pp=pp: e.match_replace(out=sr[:], in_to_replace=m8[:, pp, 0:8], in_values=s_,
                                                                               imm_value=-1e30), r=[S_all[sub], m8], w=[sr])
                            p.op('dve', lambda e, pp=pp: e.max(out=m8[:, pp, 8:16], in_=sr[:]), r=[sr], w=[m8])
                        p.op('dve', lambda e: e.tensor_scalar(out=ngm[:], in0=m8[:, :, 0], scalar1=-1.0, scalar2=None, op0=ALU.mult),
                             r=[m8], w=[ngm])
                        for pp, ee in ((0, e1[sub]), (1, e2[sub])):
                            p.op('act', lambda e, pp=pp, ee=ee: e.activation(out=ee[:, h, :], in_=S_all[sub][:, 2 * h + pp, :], func=AF.Exp,
                                                                            bias=ngm[:, pp:pp + 1], scale=1.0), r=[S_all[sub], ngm], w=[ee])
                            p.op('act', lambda e, pp=pp: e.activation(out=et[:, pp, :], in_=m8[:, pp, :], func=AF.Exp,
                                                                     bias=ngm[:, pp:pp + 1], scale=1.0), r=[m8, ngm], w=[et])
                        p.op('dve', lambda e: e.tensor_tensor(out=cand[:], in0=et[:, 0, :].unsqueeze(2).broadcast_to([128, 16, 16]),
                                                              in1=et[:, 1, :].unsqueeze(1).broadcast_to([128, 16, 16]), op=ALU.mult),
                             r=[et], w=[cand])
                        cf = cand[:].rearrange("p a b -> p (a b)")
                        p.op('dve', lambda e: e.max(out=c16[:, 0:8], in_=cf), r=[cand], w=[c16])
                        p.op('dve', lambda e: e.match_replace(out=cr[:], in_to_replace=c16[:, 0:8], in_values=cf, imm_value=-1e30),
                             r=[cand, c16], w=[cr])
                        p.op('dve', lambda e: e.max(out=c16[:, 8:16], in_=cr[:]), r=[cr], w=[c16])
                        p.op('dve', lambda e, h=h: e.tensor_scalar(out=Tt[sub][:, h:h + 1], in0=c16[:, 15:16], scalar1=1.0 - 1e-6,
                                                                   scalar2=None, op0=ALU.mult), r=[c16], w=[Tt[sub]])
                        p.op('dve', lambda e: e.tensor_reduce(out=zz[:], in_=c16[:], axis=AX.X, op=ALU.add), r=[c16], w=[zz])
                        p.op('dve', lambda e, h=h: e.reciprocal(out=kap[sub][:, h:h + 1], in_=zz[:]), r=[zz], w=[kap[sub]])
                        p.op('dve', lambda e, h=h: e.tensor_scalar(out=Dk[sub][h][:], in0=g.ident_f[:], scalar1=kap[sub][:, h:h + 1],
                                                                   scalar2=None, op0=ALU.mult), r=[g.ident_f, kap[sub]], w=[Dk[sub][h]])
            with p.phase():
                Pt = [p.sb("pe_P%d" % i, [128, NI, 128], F32) for i in range(2)]
                G = [p.sb("pe_G%d" % i, [128, 8, NI * 128], BF16) for i in range(2)]
                ut = [p.sb("pe_ut%d" % i, [128, 8, 512], BF16) for i in range(2)]
                vb = [p.sb("pe_vb%d" % i, [128, 4, D], BF16) for i in range(2)]
                ga = [p.sb("pe_ga%d" % i, [128, 512], F32) for i in range(2)]
                wg = [p.sb("pe_wg%d" % i, [128, 512], BF16) for i in range(2)]
                wgT = [p.sb("pe_wgT%d" % i, [128, 4, 128], BF16) for i in range(2)]
                xs = [p.sb("pe_x%d" % i, [128, D], F32) for i in range(2)]
                xo = [p.sb("pe_xo%d" % i, [128, D], F32) for i in range(2)]
                if last:
                    hb = p.sb("pe_hb", [128, D], F32)
                    st = p.sb("pe_st", [128, 4], F32)
                nblk = 128 // NI
                nesb = NI * 128 // 512
                it = 0
                pit = 0
                for ib in range(nblk):
                    for sub in range(2):
                        for h in range(8):
                            P_ = Pt[pit % 2]
                            pit += 1
                            p.op('pool', lambda e, P_=P_, h=h: e.tensor_tensor(
                                out=P_[:], in0=e1[sub][:, h, ib * NI:(ib + 1) * NI].unsqueeze(2).broadcast_to([128, NI, 128]),
                                in1=e2[sub][:, h, :].unsqueeze(1).broadcast_to([128, NI, 128]), op=ALU.mult),
                                r=[e1[sub], e2[sub]], w=[P_])
                            p.op('dve', lambda e, P_=P_, h=h: e.scalar_tensor_tensor(
                                out=G[sub][:, h, :], in0=P_[:].rearrange("p a b -> p (a b)"), scalar=Tt[sub][:, h:h + 1],
                                in1=P_[:].rearrange("p a b -> p (a b)"), op0=ALU.is_ge, op1=ALU.mult),
                                r=[P_, Tt[sub]], w=[G[sub]])
                    for esb in range(nesb):
                        e0 = ib * NI * 128 + esb * 512
                        i0 = e0 // 128
                        u_, v_ = ut[it % 2], vb[it % 2]
                        p.dma(u_, u_[:], g.UTB[:, :, e0:e0 + 512])
                        p.dma(v_, v_[:], g.VB[:, i0:i0 + 4, :])
                        for sub in range(2):
                            ga_, wg_, wgT_ = ga[it % 2], wg[it % 2], wgT[it % 2]
                            it += 1
                            for k in range(8):
                                p.op('pe', lambda e, k=k: e.matmul(psA[:], lhsT=ht[:, k, sub * 128:(sub + 1) * 128], rhs=u_[:, k, :],
                                                                   start=(k == 0), stop=(k == 7)), r=[ht, u_], w=[psA])
                            p.op('act', lambda e: e.activation(out=ga_[:], in_=psA[:], func=AF.Gelu), r=[psA], w=[ga_])
                            for h in range(8):
                                p.op('pe', lambda e, h=h: e.matmul(psW[:], lhsT=Dk[sub][h][:], rhs=G[sub][:, h, esb * 512:(esb + 1) * 512],
                                                                   start=(h == 0), stop=(h == 7)), r=[Dk[sub][h], G[sub]], w=[psW])
                            p.op('dve', lambda e: e.tensor_tensor(out=wg_[:], in0=psW[:], in1=ga_[:], op=ALU.mult), r=[psW, ga_], w=[wg_])
                            emit_transposes(g, wg_, psT, wgT_[:], wgT_, n=4)
                            first = (ib == 0 and esb == 0)
                            lastm = (ib == nblk - 1 and esb == nesb - 1)
                            for c4 in range(4):
                                for n in range(2):
                                    a_ = acc[sub][n]
                                    p.op('pe', lambda e, c4=c4, n=n, a_=a_: e.matmul(
                                        a_[:], lhsT=wgT_[:, c4, :], rhs=v_[:, c4, n * 512:(n + 1) * 512],
                                        start=(first and c4 == 0), stop=(lastm and c4 == 3)), r=[wgT_, v_], w=[a_])
                for sub in range(2):
                    sl = slice(t0 + sub * 128, t0 + (sub + 1) * 128)
                    x_, o_ = xs[sub], xo[sub]
                    p.dma(x_, x_[:], g.X1[sl, :])
                    for n in range(2):
                        cs = slice(n * 512, (n + 1) * 512)
                        p.op('dve', lambda e, n=n, cs=cs: e.tensor_tensor(out=o_[:, cs], in0=acc[sub][n][:], in1=gt[which][:, cs], op=ALU.mult),
                             r=[acc[sub][n], gt[which]], w=[o_])
                    p.op('pool', lambda e: e.tensor_tensor(out=o_[:], in0=o_[:], in1=x_[:], op=ALU.add), r=[o_, x_], w=[o_])
                    if not last:
                        p.dma(None, g.X2[sl, :], o_[:], r=[o_], q='pool')
                    else:
                        p.op('act', lambda e: e.activation(out=hb[:], in_=o_[:], func=AF.Square, accum_out=st[:, 0:1]), r=[o_], w=[hb, st])
                        p.op('dve', lambda e: e.tensor_scalar(out=st[:, 1:2], in0=st[:, 0:1], scalar1=1.0 / D, scalar2=EPS,
                                                              op0=ALU.mult, op1=ALU.add), r=[st], w=[st])
                        p.op('act', lambda e: e.activation(out=st[:, 2:3], in_=st[:, 1:2], func=AF.Sqrt), r=[st], w=[st])
                        p.op('dve', lambda e: e.reciprocal(out=st[:, 3:4], in_=st[:, 2:3]), r=[st], w=[st])
                        p.op('dve', lambda e: e.scalar_tensor_tensor(out=hb[:], in0=o_[:], scalar=st[:, 3:4], in1=fg[:],
                                                                     op0=ALU.mult, op1=ALU.mult), r=[o_, st, fg], w=[hb])
                        p.dma(None, g.out[t0 - LC + sub * 128:t0 - LC + (sub + 1) * 128, :], hb[:], r=[hb], q='pool')


_PROG_CACHE = {}


def kernel(x, c, ctx, c_ctx, w_ada, b_ada, norm1_g, w_in, na_rpb, diff_lambda, diff_subln_g, ret_decay_logit,
           w_out, norm2_g, peer_wq, peer_subkeys, peer_u, peer_v, final_g):
    inp = dict(x=x, c=c, ctx=ctx, c_ctx=c_ctx, w_ada=w_ada, b_ada=b_ada, norm1_g=norm1_g, w_in=w_in, na_rpb=na_rpb,
               diff_lambda=diff_lambda, diff_subln_g=diff_subln_g, ret_decay_logit=ret_decay_logit, w_out=w_out,
               norm2_g=norm2_g, peer_wq=peer_wq, peer_subkeys=peer_subkeys, peer_u=peer_u, peer_v=peer_v, final_g=final_g)
    inp = {k: np.asarray(v) for k, v in inp.items()}
    B = inp["x"].shape[0]
    cs = _consts()
    shared = {"final_g": np.asarray(inp["final_g"], np.float32).reshape(1, D)}
    for n in CONST_NAMES:
        shared[n] = cs[n]
    for l in range(DEPTH):
        for k, v in _layer_inputs(inp, l).items():
            shared["L%d_%s" % (l, k)] = v
    ccv = np.asarray(inp["c_ctx"], np.float32).reshape(8, 128).T
    in_maps = []
    for b in range(B):
        m = dict(shared)
        m["xin"] = np.ascontiguousarray(np.concatenate([inp["ctx"][b], inp["x"][b]], axis=0).astype(np.float32))
        m["cvec"] = np.ascontiguousarray(np.stack([np.asarray(inp["c"][b], np.float32).reshape(8, 128).T, ccv], axis=-1))
        in_maps.append(m)
    if "nc" not in _PROG_CACHE:
        _PROG_CACHE["nc"] = build_program()[0]
    nc = _PROG_CACHE["nc"]
    res = run_bass_kernel_spmd(nc, in_maps, core_ids=list(range(B)))
    return np.stack([np.asarray(r["out"], dtype=np.float32) for r in res.results], axis=0)
```

```python
import numpy as np
from contextlib import ExitStack, contextmanager
import concourse.bass as bass
import concourse.mybir as mybir
from concourse.bass_utils import run_bass_kernel_spmd

F32 = mybir.dt.float32
BF16 = mybir.dt.bfloat16
AF = mybir.ActivationFunctionType
ALU = mybir.AluOpType
AX = mybir.AxisListType


class Buf:
    def __init__(self, name):
        self.name = name
        self.w = None
        self.r = {}


class T(Buf):
    def __init__(self, name, h):
        super().__init__(name)
        self.h = h

    def __getitem__(self, idx):
        return self.h[idx]


class Prog:
    ENG = ('pe', 'act', 'dve', 'pool', 'sp')

    def __init__(self, nc, ndma=32):
        self.nc = nc
        self.ndma = ndma
        self.es = ExitStack()
        self.scope = None
        self.drams = {}

    def __enter__(self):
        nc = self.nc
        self.es.__enter__()
        self.engs = {'pe': nc.tensor, 'act': nc.scalar, 'dve': nc.vector, 'pool': nc.gpsimd, 'sp': nc.sync}
        self.sems = {k: self.es.enter_context(nc.semaphore("s_" + k)) for k in ('pe', 'act', 'dve', 'pool')}
        self.cnt = {k: 0 for k in self.sems}
        self.dsem = [self.es.enter_context(nc.semaphore("d%d" % i)) for i in range(self.ndma)]
        self.dval = [0] * self.ndma
        self.dnext = 0
        self.seen = {e: {} for e in self.ENG}
        self.scope = self.es
        self.ninst = 0
        return self

    def __exit__(self, *a):
        return self.es.__exit__(*a)

    def _uniq(self, name):
        self.nalloc = getattr(self, "nalloc", 0) + 1
        return "%s_%d" % (name, self.nalloc)

    def sb(self, name, shape, dtype):
        name = self._uniq(name)
        return T(name, self.scope.enter_context(self.nc.sbuf_tensor(name, list(shape), dtype)))

    def ps(self, name, shape, dtype):
        name = self._uniq(name)
        return T(name, self.scope.enter_context(self.nc.psum_tensor(name, list(shape), dtype)))

    def dram_buf(self, name):
        if name not in self.drams:
            self.drams[name] = Buf(name)
        return self.drams[name]

    @contextmanager
    def phase(self):
        old = self.scope
        with ExitStack() as st:
            self.scope = st
            yield
            self.barrier()
        self.scope = old

    def _semobj(self, key):
        return self.sems[key] if isinstance(key, str) else self.dsem[key[1]]

    def _wait(self, eng, tok, raw=False):
        key, val = tok
        if key == eng and not (raw and eng != 'pe'):
            return
        if self.seen[eng].get(key, 0) >= val:
            return
        self.engs[eng].wait_ge(self._semobj(key), val)
        self.seen[eng][key] = val
        self.ninst += 1

    def _deps(self, eng, r, w):
        for b in r:
            if b.w is not None:
                self._wait(eng, b.w, raw=True)
        for b in w:
            if b.w is not None:
                self._wait(eng, b.w)
            for k, v in b.r.items():
                self._wait(eng, (k, v))

    def _mark(self, tok, r, w):
        k, v = tok
        for b in r:
            if b.r.get(k, 0) < v:
                b.r[k] = v
        for b in w:
            b.w = tok
            b.r = {}

    def op(self, eng, fn, r=(), w=()):
        self._deps(eng, r, w)
        inst = fn(self.engs[eng])
        self.cnt[eng] += 1
        inst.then_inc(self.sems[eng], 1)
        self._mark((eng, self.cnt[eng]), r, w)
        self.ninst += 1

    def dma(self, wbuf, out_ap, in_ap, r=(), q='sp', **kw):
        i = self.dnext
        self.dnext = (i + 1) % self.ndma
        if self.dval[i] > 0:
            self._wait(q, (('d', i), self.dval[i]))
        w = [] if wbuf is None else ([wbuf] if isinstance(wbuf, Buf) else list(wbuf))
        self._deps(q, r, w)
        inst = self.engs[q].dma_start(out=out_ap, in_=in_ap, **kw)
        self.dval[i] += 16
        inst.then_inc(self.dsem[i], 16)
        self._mark((('d', i), self.dval[i]), r, w)
        self.ninst += 1

    def barrier(self):
        for e in self.ENG:
            for f in self.sems:
                if self.cnt[f] > 0:
                    self._wait(e, (f, self.cnt[f]))
            for i in range(self.ndma):
                if self.dval[i] > 0:
                    self._wait(e, (('d', i), self.dval[i]))

    def finish(self):
        self.barrier()


D = 1024
L = 4096
LC = 256
NT = L + LC
NTILE = NT // 128
DEPTH = 2
GW = 64
EPS = 1e-6
NEGB = -30000.0
O_NAQ, O_NAK, O_NAV, O_DFQ, O_DFK, O_DFV, O_RTQ, O_RTK, O_RTV, O_RTG = 0, 256, 512, 768, 1024, 1280, 1536, 1792, 2048, 2560
C_QNA, C_KNA, C_QDF, C_KDA, C_KDB, C_QRT, C_KRT = 0, 2, 4, 6, 8, 10, 12
NFMC = 14


def _rope_tables():
    t = np.arange(L)
    rows = (t // GW).astype(np.float32)
    cols = (t % GW).astype(np.float32)
    out = {}
    for name, dim in (("df", 32), ("rt", 64)):
        half = dim // 2
        inv = np.power(np.float32(10000.0), -np.arange(0, half, 2, dtype=np.float32) / np.float32(half)).astype(np.float32)
        ang = np.concatenate([rows[:, None] * inv, cols[:, None] * inv], axis=-1).astype(np.float32)
        cos = np.cos(ang).astype(np.float32)
        sin = np.sin(ang).astype(np.float32)
        C = np.ones((128, NT), np.float32)
        S = np.zeros((128, NT), np.float32)
        for r in range(128):
            d = r % dim
            pi = d // 2
            C[r, LC:] = cos[:, pi]
            S[r, LC:] = -sin[:, pi] if d % 2 == 0 else sin[:, pi]
        out[name] = (C, S)
    return out


def _na_patterns():
    pats = {}
    for pname, r in (("r0", 0), ("r2", 2), ("mid", 8), ("r60", 60), ("r62", 62)):
        kr0 = min(max(r - 4, 0), 54)
        chunks = []
        for m in range(5):
            dr = np.zeros((128, 128), np.int64)
            dc = np.zeros((128, 128), np.int64)
            va = np.zeros((128, 128), bool)
            for kk in range(128):
                krow = kr0 + 2 * m + kk // 64
                kc = kk % 64
                for qq in range(128):
                    qrow = r + qq // 64
                    qc = qq % 64
                    r0 = min(max(qrow - 4, 0), 56)
                    cs = min(max(qc - 8, 0), 48)
                    ok = (r0 <= krow < r0 + 8) and (cs <= kc < cs + 16) and krow < 64
                    va[kk, qq] = ok
                    if ok:
                        dr[kk, qq] = krow - qrow + 7
                        dc[kk, qq] = min(max(kc - qc, -15), 15) + 15
            chunks.append((dr, dc, va))
        pats[pname] = chunks
    return pats


_CONST_CACHE = {}


def _consts():
    if _CONST_CACHE:
        return _CONST_CACHE
    c = _CONST_CACHE
    rt = _rope_tables()
    s_df = np.float32(32 ** -0.5)
    c["T_DFQ_C"] = rt["df"][0] * s_df
    c["T_DFQ_S"] = rt["df"][1] * s_df
    c["T_DFK_C"] = rt["df"][0]
    c["T_DFK_S"] = rt["df"][1]
    c["T_RTQ_C"] = rt["rt"][0]
    c["T_RTQ_S"] = rt["rt"][1]
    c["T_RTK_C"] = rt["rt"][0] * np.float32(0.125)
    c["T_RTK_S"] = rt["rt"][1] * np.float32(0.125)
    c["IDENT"] = np.eye(128, dtype=np.float32)
    pats = _na_patterns()
    c["_pats"] = pats
    names = ["r0", "r2", "mid", "r60", "r62"]
    mask = np.zeros((128, 5, 5, 128), np.float32)
    for pi, pn in enumerate(names):
        for m in range(5):
            mask[:, pi, m, :] = np.where(pats[pn][m][2], 0.0, NEGB)
    c["NA_MASK"] = mask
    i = np.arange(128, dtype=np.float32)
    ij = i[None, :] - i[:, None]
    c["RT_RIJ"] = np.maximum(ij, 0).astype(np.float32)
    c["RT_MIJ"] = (ij >= 0).astype(np.float32)
    c["RT_RJI"] = np.maximum(-ij, 0).astype(np.float32)
    c["RT_MJI"] = (ij <= 0).astype(np.float32)
    c["RT_IROW"] = np.broadcast_to(i[None, :] + 1.0, (128, 128)).astype(np.float32).copy()
    c["RT_IROWB"] = np.broadcast_to(128.0 - i[None, :], (128, 128)).astype(np.float32).copy()
    c["RT_JCOL"] = np.stack([127.0 - i, i], axis=1).astype(np.float32)
    return c


def _layer_inputs(inp, l):
    c = _consts()
    w_in = np.asarray(inp["w_in"][l], np.float32)
    Z = np.zeros((D, 32), np.float32)

    def sw(cols):
        return cols ^ 1

    fm = []

    def add(cols):
        fm.append(w_in[:, cols])

    ar = np.arange
    for j in range(2):
        add(O_NAQ + j * 128 + ar(128))
    for j in range(2):
        add(O_NAK + j * 128 + ar(128))
    for j in range(2):
        cc = O_DFQ + j * 128 + ar(128)
        add(cc); add(sw(cc))
    for comp in range(2):
        for j in range(2):
            blocks_m, blocks_s = [], []
            for hh in range(2):
                h = 2 * j + hh
                cc = O_DFK + h * 64 + comp * 32 + ar(32)
                if comp == 0:
                    blocks_m += [w_in[:, cc], Z]; blocks_s += [w_in[:, sw(cc)], Z]
                else:
                    blocks_m += [Z, w_in[:, cc]]; blocks_s += [Z, w_in[:, sw(cc)]]
            fm.append(np.concatenate(blocks_m, axis=1)); fm.append(np.concatenate(blocks_s, axis=1))
    for j in range(2):
        cc = O_RTQ + j * 128 + ar(128)
        add(cc); add(sw(cc))
    for j in range(2):
        cc = O_RTK + j * 128 + ar(128)
        add(cc); add(sw(cc))
    WFM = np.ascontiguousarray(np.concatenate(fm, axis=1))
    WTM = np.ascontiguousarray(np.concatenate([w_in[:, O_NAV:O_NAV + 256], w_in[:, O_DFV:O_DFV + 256],
                                               w_in[:, O_RTV:O_RTV + 512], w_in[:, O_RTG:O_RTG + 512]], axis=1))
    rpb = np.asarray(inp["na_rpb"][l], np.float32)
    pats = c["_pats"]
    names = ["r0", "r2", "mid", "r60", "r62"]
    nab = np.zeros((128, 4, 5, 5, 128), np.float32)
    for pi, pn in enumerate(names):
        for m in range(5):
            dr, dc, va = pats[pn][m]
            for h in range(4):
                nab[:, h, pi, m, :] = rpb[h][dr, dc]
    sk = np.asarray(inp["peer_subkeys"][l], np.float32).reshape(16, 128, 128)
    skt = np.ascontiguousarray(sk.transpose(2, 0, 1))
    d = {
        "w_ada": np.asarray(inp["w_ada"][l], np.float32),
        "b_ada": np.asarray(inp["b_ada"][l], np.float32).reshape(1, 6 * D),
        "g1": np.asarray(inp["norm1_g"][l], np.float32).reshape(1, D),
        "g2": np.asarray(inp["norm2_g"][l], np.float32).reshape(1, D),
        "wfm": WFM, "wtm": WTM, "nab": nab,
        "lam": np.asarray(inp["diff_lambda"][l], np.float32).reshape(1, 128),
        "subg": np.asarray(inp["diff_subln_g"][l], np.float32).reshape(1, 64),
        "dec": np.asarray(inp["ret_decay_logit"][l], np.float32).reshape(1, 8),
        "wout": np.asarray(inp["w_out"][l], np.float32),
        "wq": np.asarray(inp["peer_wq"][l], np.float32),
        "skt": skt,
        "ut": np.ascontiguousarray(np.asarray(inp["peer_u"][l], np.float32).T),
        "v": np.asarray(inp["peer_v"][l], np.float32),
    }
    return d


LAYER_SHAPES = {
    "w_ada": [D, 6 * D], "b_ada": [1, 6 * D], "g1": [1, D], "g2": [1, D], "wfm": [D, 24 * 128], "wtm": [D, 1536],
    "nab": [128, 4, 5, 5, 128], "lam": [1, 128], "subg": [1, 64], "dec": [1, 8], "wout": [D, D], "wq": [D, 2048],
    "skt": [128, 16, 128], "ut": [D, 16384], "v": [16384, D],
}
CONST_NAMES = ["T_DFQ_C", "T_DFQ_S", "T_DFK_C", "T_DFK_S", "T_RTQ_C", "T_RTQ_S", "T_RTK_C", "T_RTK_S", "IDENT", "NA_MASK",
               "RT_RIJ", "RT_MIJ", "RT_RJI", "RT_MJI", "RT_IROW", "RT_IROWB", "RT_JCOL"]


class Ctx:
    pass


def build_program(nlayers=DEPTH, debug=None, stop_after=None):
    nc = bass.Bass("TRN2", target_bir_lowering=False)
    g = Ctx()
    g.nc = nc
    g.debug = debug or ()
    din = lambda name, shape, dt=F32: nc.dram_tensor(name, list(shape), dt, kind="ExternalInput").ap()
    dscr = lambda name, shape, dt: nc.dram_tensor(name, list(shape), dt, kind="Internal").ap()
    g.xin = din("xin", [NT, D])
    g.cvec = din("cvec", [128, 8, 2])
    g.final_g = din("final_g", [1, D])
    g.cst = {}
    cs = _consts()
    for n in CONST_NAMES:
        g.cst[n] = din(n, cs[n].shape)
    g.lay = []
    for l in range(nlayers):
        g.lay.append({k: din("L%d_%s" % (l, k), shp) for k, shp in LAYER_SHAPES.items()})
    g.out = nc.dram_tensor("out", [L, D], F32, kind="ExternalOutput").ap()
    g.MODS = dscr("MODS", [2, 6 * D], F32)
    g.X1 = dscr("X1", [NT, D], F32)
    g.X2 = dscr("X2", [NT, D], F32)
    g.FMS = dscr("FMS", [NFMC, 128, NT], BF16)
    g.VNA = dscr("VNA", [NT, 4, 65], BF16)
    g.VDF = dscr("VDF", [NT, 4, 65], BF16)
    g.VRT = dscr("VRT", [NT, 512], BF16)
    g.GRT = dscr("GRT", [NT, 512], BF16)
    g.KRTT = dscr("KRTT", [NT, 256], BF16)
    g.Y = dscr("Y", [NT, D], BF16)
    g.H2T = dscr("H2T", [128, 8, NT], BF16)
    g.QPT = dscr("QPT", [128, 16, NT], BF16)
    g.UTB = dscr("UTB", [128, 8, 16384], BF16)
    g.VB = dscr("VB", [128, 128, D], BF16)
    g.dbg = {}
    for name, shape, dt in g.debug:
        g.dbg[name] = nc.dram_tensor("dbg_" + name, list(shape), dt, kind="ExternalOutput").ap()
    p = Prog(nc)
    g.p = p
    g.scr = Buf("scratch")
    with p:
        g.ident_f = p.sb("ident_f", [128, 128], F32)
        g.ident = p.sb("ident", [128, 128], BF16)
        p.dma(g.ident_f, g.ident_f[:], g.cst["IDENT"])
        p.op('dve', lambda e: e.tensor_copy(out=g.ident[:], in_=g.ident_f[:]), r=[g.ident_f], w=[g.ident])
        stages = []
        for l in range(nlayers):
            last = (l == DEPTH - 1)
            xa = g.xin if l == 0 else g.X2
            stages += [("mod%d" % l, lambda l=l: phase_mod(g, l)),
                       ("p1_%d" % l, lambda l=l, xa=xa: phase_p1(g, l, xa)),
                       ("na_%d" % l, lambda l=l, last=last: phase_na(g, l, not last)),
                       ("df_%d" % l, lambda l=l, last=last: phase_diff(g, l, not last)),
                       ("rt_%d" % l, lambda l=l, last=last: (phase_ret(g, l, not last), dbg_dump(g, "y%d" % l, g.Y))),
                       ("p3_%d" % l, lambda l=l, last=last, xa=xa: (phase_p3(g, l, xa, last), dbg_dump(g, "x1_%d" % l, g.X1))),
                       ("cast%d" % l, lambda l=l: phase_cast(g, l)),
                       ("peerq%d" % l, lambda l=l, last=last: phase_peerq(g, l, last)),
                       ("peer%d" % l, lambda l=l, last=last: (phase_peer(g, l, last), dbg_dump(g, "x2_%d" % l, g.X2)))]
        for name, fn in stages:
            fn()
            if stop_after == name:
                break
        p.finish()
    g.ninst = p.ninst
    return nc, g


def dbg_dump(g, name, src):
    if name in g.dbg:
        g.p.dma(None, g.dbg[name], src, q='sp')
        g.p.barrier()


def load_bcast(p, t, dram_row, n, q='sp'):
    p.dma(t, t[:, 0:n], dram_row.broadcast_to([128, n]), q=q)


def phase_mod(g, l):
    p = g.p
    Lw = g.lay[l]
    with p.phase():
        cv = p.sb("cv", [128, 8, 2], F32)
        sil = p.sb("sil", [128, 8, 2], F32)
        bad = p.sb("bad", [2, 6 * D], F32)
        modt = p.sb("modt", [2, 6 * D], F32)
        wa = [p.sb("wa%d" % i, [128, 8, 512], F32) for i in range(2)]
        ps = [p.ps("modps%d" % i, [128, 512], F32) for i in range(2)]
        p.dma(cv, cv[:], g.cvec)
        p.dma(bad, bad[:], Lw["b_ada"].broadcast_to([2, 6 * D]))
        p.op('act', lambda e: e.activation(out=sil[:], in_=cv[:], func=AF.Silu), r=[cv], w=[sil])
        wsrc = Lw["w_ada"].rearrange("(k p) n -> p k n", p=128)
        for n in range(12):
            w = wa[n % 2]
            pp = ps[n % 2]
            p.dma(w, w[:], wsrc[:, :, n * 512:(n + 1) * 512])
            for k in range(8):
                p.op('pe', lambda e, k=k, w=w, pp=pp: e.matmul(pp[0:2, :], lhsT=sil[:, k, :], rhs=w[:, k, :],
                                                             start=(k == 0), stop=(k == 7)), r=[sil, w], w=[pp])
            p.op('dve', lambda e, n=n, pp=pp: e.tensor_tensor(out=modt[:, n * 512:(n + 1) * 512], in0=pp[0:2, :],
                                                             in1=bad[:, n * 512:(n + 1) * 512], op=ALU.add),
                 r=[pp, bad], w=[modt])
        p.dma(None, g.MODS, modt[:], r=[modt], q='pool')
        if "mods" in g.dbg:
            p.dma(None, g.dbg["mods"], modt[:], r=[modt], q='pool')


def mod_row(g, which, idx):
    return g.MODS[which:which + 1, idx * D:(idx + 1) * D]


def make_norm_consts(g, tag, grow, idx_shift, idx_scale):
    p = g.p
    gb = p.sb(tag + "_gb", [128, D], F32)
    load_bcast(p, gb, grow, D)
    res = []
    for which in range(2):
        sc = p.sb(tag + "_sc%d" % which, [128, D], F32)
        sh = p.sb(tag + "_sh%d" % which, [128, D], F32)
        load_bcast(p, sc, mod_row(g, which, idx_scale), D)
        load_bcast(p, sh, mod_row(g, which, idx_shift), D)
        p.op('dve', lambda e, sc=sc: e.scalar_tensor_tensor(out=sc[:], in0=sc[:], scalar=1.0, in1=gb[:],
                                                            op0=ALU.add, op1=ALU.mult), r=[sc, gb], w=[sc])
        res.append((sc, sh))
    return res


def emit_norm(g, xt, gs, sh, hb, tmp, st):
    p = g.p
    p.op('act', lambda e: e.activation(out=tmp[:], in_=xt[:], func=AF.Square, accum_out=st[:, 0:1]), r=[xt], w=[tmp, st])
    p.op('dve', lambda e: e.tensor_scalar(out=st[:, 1:2], in0=st[:, 0:1], scalar1=1.0 / D, scalar2=EPS,
                                          op0=ALU.mult, op1=ALU.add), r=[st], w=[st])
    p.op('act', lambda e: e.activation(out=st[:, 2:3], in_=st[:, 1:2], func=AF.Sqrt), r=[st], w=[st])
    p.op('dve', lambda e: e.reciprocal(out=st[:, 3:4], in_=st[:, 2:3]), r=[st], w=[st])
    p.op('dve', lambda e: e.scalar_tensor_tensor(out=tmp[:], in0=xt[:], scalar=st[:, 3:4], in1=gs[:],
                                                 op0=ALU.mult, op1=ALU.mult), r=[xt, st, gs], w=[tmp])
    p.op('dve', lambda e: e.tensor_tensor(out=hb[:], in0=tmp[:], in1=sh[:], op=ALU.add), r=[tmp, sh], w=[hb])


def emit_transposes(g, src, pst, dst_ap, dst_buf, n=8, eng='act'):
    p = g.p
    for k in range(n):
        p.op('pe', lambda e, k=k: e.transpose(pst[:, k, :], src[:, k * 128:(k + 1) * 128], g.ident[:]),
             r=[src, g.ident], w=[pst])
    if eng == 'act':
        p.op('act', lambda e: e.copy(out=dst_ap, in_=pst[:, 0:n, :]), r=[pst], w=[dst_buf])
    else:
        p.op('dve', lambda e: e.tensor_copy(out=dst_ap, in_=pst[:, 0:n, :]), r=[pst], w=[dst_buf])


def tok_groups():
    gs = [(0, 256)]
    for i in range(8):
        gs.append((256 + i * 512, 512))
    return gs


def phase_p1(g, l, xa):
    p = g.p
    Lw = g.lay[l]
    with p.phase():
        HT = p.sb("HT", [128, 8, NT], BF16)
        with p.phase():
            nrm = make_norm_consts(g, "n1", Lw["g1"], 0, 1)
            xt = [p.sb("xt%d" % i, [128, D], F32) for i in range(2)]
            tmp = p.sb("ntmp", [128, D], F32)
            hb = [p.sb("hb%d" % i, [128, D], BF16) for i in range(2)]
            st = [p.sb("nst%d" % i, [128, 4], F32) for i in range(2)]
            pst = [p.ps("pst%d" % i, [128, 8, 128], BF16) for i in range(2)]
            for tt in range(NTILE):
                which = 1 if tt < 2 else 0
                x_ = xt[tt % 2]
                p.dma(x_, x_[:], xa[tt * 128:(tt + 1) * 128, :])
                emit_norm(g, x_, nrm[which][0], nrm[which][1], hb[tt % 2], tmp, st[tt % 2])
                emit_transposes(g, hb[tt % 2], pst[tt % 2], HT[:, :, tt * 128:(tt + 1) * 128], HT)
        if "ht" in g.dbg:
            p.dma(None, g.dbg["ht"], HT[:], r=[HT], q='pool')
        with p.phase():
            wf = [p.sb("wf%d" % i, [128, 8, 128], F32) for i in range(2)]
            wb = [p.sb("wb%d" % i, [128, 8, 128], BF16) for i in range(4)]
            tc_ = [p.sb("tc%d" % i, [128, 512], F32) for i in range(2)]
            ts_ = [p.sb("ts%d" % i, [128, 512], F32) for i in range(2)]
            t1 = [p.sb("rt1_%d" % i, [128, 512], F32) for i in range(2)]
            stg = [p.sb("fstg%d" % i, [128, 512], BF16) for i in range(2)]
            ktm = [p.sb("ktm%d" % i, [128, 4, 128], BF16) for i in range(2)]
            psA = [p.ps("psA%d" % i, [128, 512], F32) for i in range(2)]
            psB = [p.ps("psB%d" % i, [128, 512], F32) for i in range(2)]
            psT = [p.ps("psT%d" % i, [128, 8, 128], BF16) for i in range(2)]
            wsrc = Lw["wfm"].rearrange("(k p) n -> p k n", p=128)
            cnt = {"w": 0, "it": 0}

            def load_w(ci):
                i = cnt["w"]
                cnt["w"] += 1
                f, b = wf[i % 2], wb[i % 4]
                p.dma(f, f[:], wsrc[:, :, ci * 128:(ci + 1) * 128])
                p.op('pool', lambda e: e.tensor_copy(out=b[:], in_=f[:]), r=[f], w=[b])
                return b

            def mm(ps_, w_, t0, n):
                for k in range(8):
                    p.op('pe', lambda e, k=k: e.matmul(ps_[:, 0:n], lhsT=w_[:, k, :], rhs=HT[:, k, t0:t0 + n],
                                                       start=(k == 0), stop=(k == 7)), r=[w_, HT], w=[ps_])

            jobs = []
            wi = 0
            for j in range(2):
                jobs.append((C_QNA + j, wi, None, 0.125, None)); wi += 1
            for j in range(2):
                jobs.append((C_KNA + j, wi, None, 1.0, None)); wi += 1
            for dest, tn in ((C_QDF, "T_DFQ"), (C_KDA, "T_DFK"), (C_KDB, "T_DFK"), (C_QRT, "T_RTQ"), (C_KRT, "T_RTK")):
                for j in range(2):
                    jobs.append((dest + j, wi, wi + 1, 1.0, tn)); wi += 2
            for (dest, wm, ws, scale, tn) in jobs:
                wmb = load_w(wm)
                wsb = load_w(ws) if ws is not None else None
                for (t0, n) in tok_groups():
                    it = cnt["it"]
                    cnt["it"] += 1
                    a, b_ = psA[it % 2], psB[it % 2]
                    sg = stg[it % 2]
                    mm(a, wmb, t0, n)
                    if ws is None:
                        p.op('act', lambda e: e.activation(out=sg[:, 0:n], in_=a[:, 0:n], func=AF.Copy, scale=scale),
                             r=[a], w=[sg])
                    else:
                        mm(b_, wsb, t0, n)
                        tcc, tss, tt1 = tc_[it % 2], ts_[it % 2], t1[it % 2]
                        p.dma(tcc, tcc[:, 0:n], g.cst[tn + "_C"][:, t0:t0 + n])
                        p.dma(tss, tss[:, 0:n], g.cst[tn + "_S"][:, t0:t0 + n])
                        p.op('dve', lambda e: e.tensor_tensor(out=tcc[:, 0:n], in0=a[:, 0:n], in1=tcc[:, 0:n], op=ALU.mult),
                             r=[a, tcc], w=[tcc])
                        p.op('dve', lambda e: e.tensor_tensor(out=tss[:, 0:n], in0=b_[:, 0:n], in1=tss[:, 0:n], op=ALU.mult),
                             r=[b_, tss], w=[tss])
                        p.op('pool', lambda e: e.tensor_tensor(out=sg[:, 0:n], in0=tcc[:, 0:n], in1=tss[:, 0:n], op=ALU.add),
                             r=[tcc, tss], w=[sg])
                    p.dma(None, g.FMS[dest, :, t0:t0 + n], sg[:, 0:n], r=[sg], q='pool')
                    if dest in (C_KRT, C_KRT + 1):
                        nt_ = n // 128
                        pt, kt = psT[it % 2], ktm[it % 2]
                        emit_transposes(g, sg, pt, kt[:, 0:nt_, :], kt, n=nt_)
                        cj = dest - C_KRT
                        p.dma(None, g.KRTT[t0:t0 + n, cj * 128:(cj + 1) * 128].rearrange("(a p) c -> p a c", p=128),
                              kt[:, 0:nt_, :], r=[kt], q='pool')
        with p.phase():
            wtf = p.sb("wtf", [128, 8, 512], F32)
            wtb = p.sb("wtb", [128, 8, 1536], BF16)
            wsrc = Lw["wtm"].rearrange("(k p) n -> p k n", p=128)
            for c3 in range(3):
                p.dma(wtf, wtf[:], wsrc[:, :, c3 * 512:(c3 + 1) * 512])
                p.op('dve', lambda e, c3=c3: e.tensor_copy(out=wtb[:, :, c3 * 512:(c3 + 1) * 512], in_=wtf[:]), r=[wtf], w=[wtb])
            pv = [p.ps("pv%d" % i, [128, 512], F32) for i in range(4)]
            vst = [p.sb("vst%d" % i, [128, 2, 4, 65], BF16) for i in range(2)]
            rst = [p.sb("rst%d" % i, [128, 2, 512], BF16) for i in range(2)]
            for i in range(2):
                p.op('pool', lambda e, i=i: e.memset(vst[i][:], 1.0), w=[vst[i]])
            it = 0
            for tt in range(NTILE):
                vs, rs = vst[tt % 2], rst[tt % 2]
                for c3 in range(3):
                    ps_ = pv[it % 4]
                    it += 1
                    for k in range(8):
                        p.op('pe', lambda e, k=k, ps_=ps_, c3=c3: e.matmul(ps_[:], lhsT=HT[:, k, tt * 128:(tt + 1) * 128],
                                                                          rhs=wtb[:, k, c3 * 512:(c3 + 1) * 512],
                                                                          start=(k == 0), stop=(k == 7)), r=[HT, wtb], w=[ps_])
                    if c3 == 0:
                        p.op('act', lambda e, ps_=ps_: e.copy(out=vs[:, :, :, 0:64],
                                                             in_=ps_[:].rearrange("p (a h d) -> p a h d", a=2, h=4)),
                             r=[ps_], w=[vs])
                    elif c3 == 1:
                        p.op('dve', lambda e, ps_=ps_: e.tensor_copy(out=rs[:, 0, :], in_=ps_[:]), r=[ps_], w=[rs])
                    else:
                        p.op('act', lambda e, ps_=ps_: e.activation(out=rs[:, 1, :], in_=ps_[:], func=AF.Silu), r=[ps_], w=[rs])
                sl = slice(tt * 128, (tt + 1) * 128)
                p.dma(None, g.VNA[sl], vs[:, 0], r=[vs], q='pool')
                p.dma(None, g.VDF[sl], vs[:, 1], r=[vs], q='pool')
                p.dma(None, g.VRT[sl], rs[:, 0, :], r=[rs], q='pool')
                p.dma(None, g.GRT[sl], rs[:, 1, :], r=[rs], q='pool')
        for nm, src in (("fms", g.FMS), ("vna", g.VNA), ("vdf", g.VDF), ("vrt", g.VRT), ("grt", g.GRT), ("krtt", g.KRTT)):
            if nm in g.dbg:
                p.dma(None, g.dbg[nm], src, q='sp')


def phase_na(g, l, do_ctx):
    p = g.p
    Lw = g.lay[l]
    with p.phase():
        bias = p.sb("na_bias", [128, 4, 25, 128], BF16)
        with p.phase():
            stg = p.sb("na_bstg", [128, 25, 128], F32)
            msk = p.sb("na_msk", [128, 25, 128], F32)
            p.dma(msk, msk[:], g.cst["NA_MASK"].rearrange("p a b q -> p (a b) q"))
            for h in range(4):
                p.dma(stg, stg[:], Lw["nab"][:, h].rearrange("p a b q -> p (a b) q"))
                p.op('dve', lambda e, h=h: e.tensor_tensor(out=bias[:, h], in0=stg[:], in1=msk[:], op=ALU.add),
                     r=[stg, msk], w=[bias])
        V = p.sb("na_v", [128, NTILE, 260], BF16)
        p.dma(V, V[:], g.VNA.rearrange("(a p) h d -> p a (h d)", p=128))
        qT = [p.sb("na_q%d" % i, [128, NT], BF16) for i in range(2)]
        kT = [p.sb("na_k%d" % i, [128, NT], BF16) for i in range(2)]
        for i in range(2):
            p.dma(qT[i], qT[i][:], g.FMS[C_QNA + i])
            p.dma(kT[i], kT[i][:], g.FMS[C_KNA + i])
        psS = [[p.ps("na_s%d%d" % (i, j), [128, 512], F32) for j in range(2)] for i in range(2)]
        pso = [p.ps("na_o%d" % i, [128, 512], F32) for i in range(2)]
        PT = [p.sb("na_pt%d" % i, [128, 8, 128], BF16) for i in range(2)]
        ys = [p.sb("na_ys%d" % i, [128, 256], BF16) for i in range(2)]
        rc = [p.sb("na_rc%d" % i, [128, 1], F32) for i in range(2)]
        tiles = []
        if do_ctx:
            for q0 in (0, 128):
                tiles.append((q0, [(0, None), (128, None)], 0))
        for rp in range(32):
            r = 2 * rp
            kr0 = min(max(r - 4, 0), 54)
            pat = {0: 0, 2: 1, 60: 3, 62: 4}.get(r, 2)
            ch = [(LC + (kr0 + 2 * m) * 64, m) for m in range(5)] + [(0, None), (128, None)]
            tiles.append((LC + rp * 128, ch, pat))
        it = 0
        for qi, (q0, ch, pat) in enumerate(tiles):
            y_ = ys[qi % 2]
            for h in range(4):
                hp, base = h // 2, (h % 2) * 64
                sA, sB = psS[it % 2]
                po, pt, rc_ = pso[it % 2], PT[it % 2], rc[it % 2]
                it += 1
                n = len(ch)
                for i, (tok0, m) in enumerate(ch):
                    bank = sA if i < 4 else sB
                    col = (i % 4) * 128
                    p.op('pe', lambda e, bank=bank, col=col, tok0=tok0, m=m: e.matmul(
                        bank[:, col:col + 128], lhsT=kT[hp][base:base + 64, tok0:tok0 + 128],
                        rhs=qT[hp][base:base + 64, q0:q0 + 128], start=True, stop=(m is None)),
                        r=[kT[hp], qT[hp]], w=[bank])
                    if m is not None:
                        p.op('pe', lambda e, bank=bank, col=col, m=m: e.matmul(
                            bank[:, col:col + 128], lhsT=g.ident[:], rhs=bias[:, h, pat * 5 + m, :],
                            start=False, stop=True), r=[g.ident, bias], w=[bank])
                na_ = min(n, 4)
                p.op('act', lambda e: e.activation(out=pt[:, 0:na_, :], in_=sA[:, 0:na_ * 128].rearrange("p (a q) -> p a q", q=128),
                                                   func=AF.Exp), r=[sA], w=[pt])
                if n > 4:
                    p.op('act', lambda e: e.activation(out=pt[:, 4:n, :], in_=sB[:, 0:(n - 4) * 128].rearrange("p (a q) -> p a q", q=128),
                                                       func=AF.Exp), r=[sB], w=[pt])
                for i, (tok0, m) in enumerate(ch):
                    vt = tok0 // 128
                    p.op('pe', lambda e, i=i, vt=vt: e.matmul(po[:, 0:65], lhsT=pt[:, i, :], rhs=V[:, vt, h * 65:(h + 1) * 65],
                                                              start=(i == 0), stop=(i == n - 1)), r=[pt, V], w=[po])
                p.op('dve', lambda e: e.reciprocal(out=rc_[:], in_=po[:, 64:65]), r=[po], w=[rc_])
                p.op('dve', lambda e: e.tensor_scalar(out=y_[:, h * 64:(h + 1) * 64], in0=po[:, 0:64], scalar1=rc_[:, 0:1],
                                                      scalar2=None, op0=ALU.mult), r=[po, rc_], w=[y_])
            p.dma(None, g.Y[q0:q0 + 128, 0:256], y_[:], r=[y_], q='pool')


def phase_diff(g, l, do_ctx):
    p = g.p
    Lw = g.lay[l]
    import math
    lam_init = 0.8 - 0.6 * math.exp(-0.3 * l)
    with p.phase():
        lp = p.sb("df_lp", [128, 4, 32], F32)
        pr = p.sb("df_pr", [128, 2, 32], F32)
        sm = p.sb("df_sm", [128, 4], F32)
        neglam = p.sb("df_nl", [128, 1], F32)
        gsub = p.sb("df_gs", [128, 64], F32)
        p.dma(lp, lp[:].rearrange("p a b -> p (a b)"), Lw["lam"].broadcast_to([128, 128]))
        load_bcast(p, gsub, Lw["subg"], 64)
        p.op('dve', lambda e: e.tensor_scalar(out=gsub[:], in0=gsub[:], scalar1=1.0 - lam_init, scalar2=None, op0=ALU.mult),
             r=[gsub], w=[gsub])
        p.op('dve', lambda e: e.tensor_tensor(out=pr[:], in0=lp[:, 0:4:2, :], in1=lp[:, 1:4:2, :], op=ALU.mult), r=[lp], w=[pr])
        p.op('dve', lambda e: e.tensor_reduce(out=sm[:, 0:2], in_=pr[:], axis=AX.X, op=ALU.add), r=[pr], w=[sm])
        p.op('act', lambda e: e.activation(out=sm[:, 2:4], in_=sm[:, 0:2], func=AF.Exp), r=[sm], w=[sm])
        p.op('dve', lambda e: e.tensor_tensor(out=neglam[:], in0=sm[:, 3:4], in1=sm[:, 2:3], op=ALU.subtract), r=[sm], w=[neglam])
        p.op('dve', lambda e: e.tensor_scalar(out=neglam[:], in0=neglam[:], scalar1=-lam_init, scalar2=None, op0=ALU.add),
             r=[neglam], w=[neglam])
        V = p.sb("df_v", [128, NTILE, 260], BF16)
        p.dma(V, V[:], g.VDF.rearrange("(a p) h d -> p a (h d)", p=128))
        qT = [p.sb("df_q%d" % i, [128, NT], BF16) for i in range(2)]
        kA = [p.sb("df_ka%d" % i, [128, NT], BF16) for i in range(2)]
        kB = [p.sb("df_kb%d" % i, [128, NT], BF16) for i in range(2)]
        for i in range(2):
            p.dma(qT[i], qT[i][:], g.FMS[C_QDF + i])
            p.dma(kA[i], kA[i][:], g.FMS[C_KDA + i])
            p.dma(kB[i], kB[i][:], g.FMS[C_KDB + i])
        acc = [p.ps("df_acc%d" % i, [128, 512], F32) for i in range(4)]
        psS = [p.ps("df_s%d" % i, [128, 512], F32) for i in range(2)]
        PT = [p.sb("df_pt%d" % i, [128, 512], BF16) for i in range(2)]
        oc = [p.sb("df_oc%d" % i, [128, 4, 64], F32) for i in range(2)]
        o = p.sb("df_o", [128, 4, 64], F32)
        sq = p.sb("df_sq", [128, 4, 64], F32)
        ss = p.sb("df_ss", [128, 8], F32)
        rc = p.sb("df_rc", [128, 4], F32)
        yd = [p.sb("df_y%d" % i, [128, 4, 64], BF16) for i in range(2)]
        groups = []
        if do_ctx:
            groups.append((0, 256, [0, 1]))
        for gi in range(8):
            groups.append((LC + gi * 512, 512, list(range(NTILE))))
        it = 0
        gi_ = 0
        for h in range(4):
            hp, base = h // 2, (h % 2) * 64
            for (t0, n, kcs) in groups:
                ns = n // 128
                for c in range(2):
                    kX = (kA if c == 0 else kB)[hp]

                    def qk(ki):
                        kc = kcs[ki]
                        ps_ = psS[ki % 2]
                        p.op('pe', lambda e: e.matmul(
                            ps_[:, 0:n], lhsT=kX[base:base + 64, kc * 128:(kc + 1) * 128], rhs=qT[hp][base:base + 64, t0:t0 + n],
                            start=True, stop=True), r=[kX, qT[hp]], w=[ps_])

                    def ex(ki):
                        ps_, pt = psS[ki % 2], PT[ki % 2]
                        p.op('act', lambda e: e.activation(out=pt[:, 0:n], in_=ps_[:, 0:n], func=AF.Exp), r=[ps_], w=[pt])

                    def pv(ki):
                        kc = kcs[ki]
                        pt = PT[ki % 2]
                        for s in range(ns):
                            p.op('pe', lambda e, s=s: e.matmul(
                                acc[s][:, 0:65], lhsT=pt[:, s * 128:(s + 1) * 128], rhs=V[:, kc, h * 65:(h + 1) * 65],
                                start=(ki == 0), stop=(ki == len(kcs) - 1)), r=[pt, V], w=[acc[s]])

                    qk(0)
                    ex(0)
                    for ki in range(len(kcs)):
                        if ki + 1 < len(kcs):
                            qk(ki + 1)
                            ex(ki + 1)
                        pv(ki)
                    for s in range(ns):
                        p.op('dve', lambda e, s=s: e.reciprocal(out=rc[:, s:s + 1], in_=acc[s][:, 64:65]), r=[acc[s]], w=[rc])
                        p.op('dve', lambda e, s=s, c=c: e.tensor_scalar(out=oc[c][:, s, :], in0=acc[s][:, 0:64], scalar1=rc[:, s:s + 1],
                                                                       scalar2=None, op0=ALU.mult), r=[acc[s], rc], w=[oc[c]])
                y_ = yd[gi_ % 2]
                gi_ += 1
                p.op('dve', lambda e: e.scalar_tensor_tensor(out=o[:, 0:ns, :], in0=oc[1][:, 0:ns, :], scalar=neglam[:, 0:1],
                                                             in1=oc[0][:, 0:ns, :], op0=ALU.mult, op1=ALU.add),
                     r=[oc[0], oc[1], neglam], w=[o])
                p.op('dve', lambda e: e.tensor_tensor(out=sq[:, 0:ns, :], in0=o[:, 0:ns, :], in1=o[:, 0:ns, :], op=ALU.mult), r=[o], w=[sq])
                p.op('dve', lambda e: e.tensor_reduce(out=ss[:, 0:ns], in_=sq[:, 0:ns, :], axis=AX.X, op=ALU.add), r=[sq], w=[ss])
                p.op('dve', lambda e: e.tensor_scalar(out=ss[:, 0:ns], in0=ss[:, 0:ns], scalar1=1.0 / 64, scalar2=EPS,
                                                      op0=ALU.mult, op1=ALU.add), r=[ss], w=[ss])
                p.op('act', lambda e: e.activation(out=ss[:, 4:4 + ns], in_=ss[:, 0:ns], func=AF.Sqrt), r=[ss], w=[ss])
                p.op('dve', lambda e: e.reciprocal(out=ss[:, 0:ns], in_=ss[:, 4:4 + ns]), r=[ss], w=[ss])
                p.op('dve', lambda e: e.tensor_tensor(out=sq[:, 0:ns, :], in0=o[:, 0:ns, :],
                                                      in1=ss[:, 0:ns].unsqueeze(2).broadcast_to([128, ns, 64]), op=ALU.mult),
                     r=[o, ss], w=[sq])
                p.op('dve', lambda e: e.tensor_tensor(out=y_[:, 0:ns, :], in0=sq[:, 0:ns, :],
                                                      in1=gsub[:].unsqueeze(1).broadcast_to([128, ns, 64]), op=ALU.mult),
                     r=[sq, gsub], w=[y_])
                p.dma(None, g.Y[t0:t0 + n, 256 + h * 64:256 + (h + 1) * 64].rearrange("(s p) d -> p s d", p=128),
                      y_[:, 0:ns, :], r=[y_], q='pool')


def phase_ret(g, l, do_ctx):
    p = g.p
    Lw = g.lay[l]
    with p.phase():
        dec = p.sb("rt_dec", [128, 8], F32)
        lg = p.sb("rt_lg", [128, 8], F32)
        cdec = p.sb("rt_cdec", [128, 8], F32)
        kdec = p.sb("rt_kdec", [128, 8], F32)
        jcol = p.sb("rt_jcol", [128, 2], F32)
        cm = {}
        for nm in ("RT_RIJ", "RT_MIJ", "RT_RJI", "RT_MJI", "RT_IROW", "RT_IROWB"):
            cm[nm] = p.sb("c_" + nm, [128, 128], F32)
            p.dma(cm[nm], cm[nm][:], g.cst[nm])
        p.dma(jcol, jcol[:], g.cst["RT_JCOL"])
        load_bcast(p, dec, Lw["dec"], 8)
        p.op('act', lambda e: e.activation(out=lg[:], in_=dec[:], func=AF.Sigmoid), r=[dec], w=[lg])
        p.op('act', lambda e: e.activation(out=lg[:], in_=lg[:], func=AF.Ln), r=[lg], w=[lg])
        p.op('act', lambda e: e.activation(out=cdec[:], in_=lg[:], func=AF.Exp, scale=128.0), r=[lg], w=[cdec])
        QDF = p.sb("rt_qdf", [128, 4, 128], F32)
        QDB = p.sb("rt_qdb", [128, 4, 128], F32)
        DB = p.sb("rt_db", [128, 4, 128], F32)
        t1 = p.sb("rt_t1", [128, 128], F32)
        t2 = p.sb("rt_t2", [128, 128], F32)
        for d_ in range(2):
            for h in range(4):
                c = d_ * 4 + h
                p.op('act', lambda e, c=c, d_=d_: e.activation(out=kdec[:, c:c + 1], in_=jcol[:, d_:d_ + 1], func=AF.Exp,
                                                               scale=lg[:, c:c + 1]), r=[jcol, lg], w=[kdec])
        for h in range(4):
            p.op('act', lambda e, h=h: e.activation(out=QDF[:, h, :], in_=cm["RT_IROW"][:], func=AF.Exp, scale=lg[:, h:h + 1]),
                 r=[cm["RT_IROW"], lg], w=[QDF])
            p.op('act', lambda e, h=h: e.activation(out=QDB[:, h, :], in_=cm["RT_IROWB"][:], func=AF.Exp, scale=lg[:, 4 + h:5 + h]),
                 r=[cm["RT_IROWB"], lg], w=[QDB])
            p.op('act', lambda e, h=h: e.activation(out=t1[:], in_=cm["RT_RIJ"][:], func=AF.Exp, scale=lg[:, h:h + 1]),
                 r=[cm["RT_RIJ"], lg], w=[t1])
            p.op('act', lambda e, h=h: e.activation(out=t2[:], in_=cm["RT_RJI"][:], func=AF.Exp, scale=lg[:, 4 + h:5 + h]),
                 r=[cm["RT_RJI"], lg], w=[t2])
            p.op('dve', lambda e: e.tensor_tensor(out=t1[:], in0=t1[:], in1=cm["RT_MIJ"][:], op=ALU.mult), r=[t1, cm["RT_MIJ"]], w=[t1])
            p.op('dve', lambda e: e.tensor_tensor(out=t2[:], in0=t2[:], in1=cm["RT_MJI"][:], op=ALU.mult), r=[t2, cm["RT_MJI"]], w=[t2])
            p.op('dve', lambda e, h=h: e.tensor_tensor(out=DB[:, h, :], in0=t1[:], in1=t2[:], op=ALU.add), r=[t1, t2], w=[DB])
        qT = p.sb("rt_q", [64, NT], BF16)
        kT = p.sb("rt_k", [64, NT], BF16)
        Ktm = p.sb("rt_ktm", [128, NTILE, 64], BF16)
        V = p.sb("rt_v", [128, NTILE, 128], BF16)
        G = p.sb("rt_g", [128, NTILE, 128], BF16)
        KF = p.sb("rt_kf", [128, NTILE, 64], BF16)
        KB = p.sb("rt_kb", [128, NTILE, 64], BF16)
        SFa = p.sb("rt_sfa", [64, NTILE, 128], BF16)
        SBa = p.sb("rt_sba", [64, NTILE, 128], BF16)
        S = [p.sb("rt_S%d" % i, [64, 128], F32) for i in range(2)]
        psKV = [p.ps("rt_kv%d" % i, [128, 512], F32) for i in range(2)]
        psA = [p.ps("rt_att%d" % i, [128, 512], F32) for i in range(2)]
        psO = [p.ps("rt_o%d" % i, [128, 512], F32) for i in range(2)]
        attm = [p.sb("rt_attm%d" % i, [128, 128], BF16) for i in range(2)]
        qf = [p.sb("rt_qf%d" % i, [64, 128], BF16) for i in range(2)]
        qb = [p.sb("rt_qb%d" % i, [64, 128], BF16) for i in range(2)]
        junk = p.sb("rt_junk", [128, 128], F32)
        st = [p.sb("rt_st%d" % i, [128, 4], F32) for i in range(2)]
        ys = [p.sb("rt_ys%d" % i, [128, 128], BF16) for i in range(2)]
        it = 0
        for h in range(4):
            hp, base = h // 2, (h % 2) * 64
            p.dma(qT, qT[:], g.FMS[C_QRT + hp, base:base + 64, :])
            p.dma(kT, kT[:], g.FMS[C_KRT + hp, base:base + 64, :])
            p.dma(Ktm, Ktm[:], g.KRTT[:, h * 64:(h + 1) * 64].rearrange("(a p) d -> p a d", p=128))
            p.dma(V, V[:], g.VRT[:, h * 128:(h + 1) * 128].rearrange("(a p) d -> p a d", p=128))
            p.dma(G, G[:], g.GRT[:, h * 128:(h + 1) * 128].rearrange("(a p) d -> p a d", p=128))
            p.op('dve', lambda e: e.tensor_scalar(out=KF[:], in0=Ktm[:], scalar1=kdec[:, h:h + 1], scalar2=None, op0=ALU.mult),
                 r=[Ktm, kdec], w=[KF])
            p.op('dve', lambda e: e.tensor_scalar(out=KB[:], in0=Ktm[:], scalar1=kdec[:, 4 + h:5 + h], scalar2=None, op0=ALU.mult),
                 r=[Ktm, kdec], w=[KB])
            for d_, (Kd, Sa, order) in enumerate(((KF, SFa, list(range(NTILE))),
                                                  (KB, SBa, [1, 0] + list(range(NTILE - 1, 1, -1))))):
                S_ = S[d_]
                p.op('pool', lambda e: e.memset(S_[:], 0.0), w=[S_])
                cd = cdec[0:64, d_ * 4 + h:d_ * 4 + h + 1]
                for oi, c in enumerate(order):
                    p.op('act', lambda e, c=c: e.copy(out=Sa[:, c, :], in_=S_[:]), r=[S_], w=[Sa])
                    if oi == len(order) - 1:
                        break
                    kv = psKV[it % 2]
                    it += 1
                    p.op('pe', lambda e, c=c, kv=kv: e.matmul(kv[0:64, 0:128], lhsT=Kd[:, c, :], rhs=V[:, c, :], start=True, stop=True),
                         r=[Kd, V], w=[kv])
                    p.op('dve', lambda e, kv=kv: e.scalar_tensor_tensor(out=S_[:], in0=S_[:], scalar=cd, in1=kv[0:64, 0:128],
                                                                        op0=ALU.mult, op1=ALU.add), r=[S_, cdec, kv], w=[S_])
            for c in (range(NTILE) if do_ctx else range(2, NTILE)):
                tok = c * 128
                pa, po = psA[c % 2], psO[c % 2]
                am, qf_, qb_, st_, y_ = attm[c % 2], qf[c % 2], qb[c % 2], st[c % 2], ys[c % 2]
                p.op('pe', lambda e: e.matmul(pa[:, 0:128], lhsT=kT[:, tok:tok + 128], rhs=qT[:, tok:tok + 128], start=True, stop=True),
                     r=[kT, qT], w=[pa])
                p.op('dve', lambda e: e.tensor_tensor(out=am[:], in0=pa[:, 0:128], in1=DB[:, h, :], op=ALU.mult), r=[pa, DB], w=[am])
                p.op('pool', lambda e: e.tensor_tensor(out=qf_[:], in0=qT[:, tok:tok + 128], in1=QDF[0:64, h, :], op=ALU.mult),
                     r=[qT, QDF], w=[qf_])
                p.op('pool', lambda e: e.tensor_tensor(out=qb_[:], in0=qT[:, tok:tok + 128], in1=QDB[0:64, h, :], op=ALU.mult),
                     r=[qT, QDB], w=[qb_])
                p.op('pe', lambda e: e.matmul(po[:, 0:128], lhsT=am[:], rhs=V[:, c, :], start=True, stop=False), r=[am, V], w=[po])
                p.op('pe', lambda e: e.matmul(po[:, 0:128], lhsT=qf_[:], rhs=SFa[:, c, :], start=False, stop=False), r=[qf_, SFa], w=[po])
                p.op('pe', lambda e: e.matmul(po[:, 0:128], lhsT=qb_[:], rhs=SBa[:, c, :], start=False, stop=True), r=[qb_, SBa], w=[po])
                p.op('act', lambda e: e.activation(out=junk[:], in_=po[:, 0:128], func=AF.Square, accum_out=st_[:, 0:1]),
                     r=[po], w=[junk, st_])
                p.op('dve', lambda e: e.tensor_scalar(out=st_[:, 1:2], in0=st_[:, 0:1], scalar1=1.0 / 128, scalar2=EPS,
                                                      op0=ALU.mult, op1=ALU.add), r=[st_], w=[st_])
                p.op('act', lambda e: e.activation(out=st_[:, 2:3], in_=st_[:, 1:2], func=AF.Sqrt), r=[st_], w=[st_])
                p.op('dve', lambda e: e.reciprocal(out=st_[:, 3:4], in_=st_[:, 2:3]), r=[st_], w=[st_])
                p.op('dve', lambda e: e.scalar_tensor_tensor(out=y_[:], in0=po[:, 0:128], scalar=st_[:, 3:4], in1=G[:, c, :],
                                                             op0=ALU.mult, op1=ALU.mult), r=[po, st_, G], w=[y_])
                p.dma(None, g.Y[tok:tok + 128, 512 + h * 128:512 + (h + 1) * 128], y_[:], r=[y_], q='pool')


def phase_p3(g, l, xa, last):
    p = g.p
    Lw = g.lay[l]
    with p.phase():
        wob = p.sb("wob", [128, 8, D], BF16)
        with p.phase():
            wof = p.sb("wof", [128, 8, 512], F32)
            wsrc = Lw["wout"].rearrange("(k p) n -> p k n", p=128)
            for n in range(2):
                p.dma(wof, wof[:], wsrc[:, :, n * 512:(n + 1) * 512])
                p.op('dve', lambda e, n=n: e.tensor_copy(out=wob[:, :, n * 512:(n + 1) * 512], in_=wof[:]), r=[wof], w=[wob])
        gt = []
        for which in range(2):
            t = p.sb("p3_gt%d" % which, [128, D], F32)
            load_bcast(p, t, mod_row(g, which, 2), D)
            gt.append(t)
        nrm = make_norm_consts(g, "n2", Lw["g2"], 3, 4)
        yt = [p.sb("p3_y%d" % i, [128, D], BF16) for i in range(2)]
        xt = [p.sb("p3_x%d" % i, [128, D], F32) for i in range(2)]
        x1 = [p.sb("p3_x1%d" % i, [128, D], F32) for i in range(2)]
        yT = [p.sb("p3_yT%d" % i, [128, 8, 128], BF16) for i in range(2)]
        hT = [p.sb("p3_hT%d" % i, [128, 8, 128], BF16) for i in range(2)]
        hb = [p.sb("p3_hb%d" % i, [128, D], BF16) for i in range(2)]
        tmp = p.sb("p3_tmp", [128, D], F32)
        st = [p.sb("p3_st%d" % i, [128, 4], F32) for i in range(2)]
        pstY = [p.ps("p3_pty%d" % i, [128, 8, 128], BF16) for i in range(2)]
        pstH = [p.ps("p3_pth%d" % i, [128, 8, 128], BF16) for i in range(2)]
        psO = [[p.ps("p3_o%d%d" % (i, n), [128, 512], F32) for n in range(2)] for i in range(2)]
        for ti, tt in enumerate(range(2, NTILE) if last else range(NTILE)):
            which = 1 if tt < 2 else 0
            b = ti % 2
            sl = slice(tt * 128, (tt + 1) * 128)
            p.dma(yt[b], yt[b][:], g.Y[sl, :])
            p.dma(xt[b], xt[b][:], xa[sl, :])
            emit_transposes(g, yt[b], pstY[b], yT[b][:], yT[b])
            for n in range(2):
                po = psO[b][n]
                for k in range(8):
                    p.op('pe', lambda e, k=k, n=n, po=po: e.matmul(po[:], lhsT=yT[b][:, k, :], rhs=wob[:, k, n * 512:(n + 1) * 512],
                                                                  start=(k == 0), stop=(k == 7)), r=[yT[b], wob], w=[po])
                cs = slice(n * 512, (n + 1) * 512)
                p.op('dve', lambda e, po=po, cs=cs: e.tensor_tensor(out=tmp[:, cs], in0=po[:], in1=gt[which][:, cs], op=ALU.mult),
                     r=[po, gt[which]], w=[tmp])
                p.op('pool', lambda e, cs=cs: e.tensor_tensor(out=x1[b][:, cs], in0=tmp[:, cs], in1=xt[b][:, cs], op=ALU.add),
                     r=[tmp, xt[b]], w=[x1[b]])
            p.dma(None, g.X1[sl, :], x1[b][:], r=[x1[b]], q='pool')
            emit_norm(g, x1[b], nrm[which][0], nrm[which][1], hb[b], tmp, st[b])
            emit_transposes(g, hb[b], pstH[b], hT[b][:], hT[b])
            p.dma(None, g.H2T[:, :, sl], hT[b][:], r=[hT[b]], q='pool')


def phase_cast(g, l):
    p = g.p
    Lw = g.lay[l]
    with p.phase():
        f = [p.sb("cs_f%d" % i, [128, 2048], F32) for i in range(3)]
        b = [p.sb("cs_b%d" % i, [128, 2048], BF16) for i in range(3)]
        engs = ['act', 'dve', 'pool']
        it = 0
        usrc = Lw["ut"].rearrange("(k p) e -> p k e", p=128)
        vsrc = Lw["v"].rearrange("(i p) d -> p i d", p=128)
        for k in range(8):
            for ec in range(8):
                i = it % 3
                it += 1
                p.dma(f[i], f[i][:], usrc[:, k, ec * 2048:(ec + 1) * 2048])
                if engs[i] == 'act':
                    p.op('act', lambda e, i=i: e.copy(out=b[i][:], in_=f[i][:]), r=[f[i]], w=[b[i]])
                else:
                    p.op(engs[i], lambda e, i=i: e.tensor_copy(out=b[i][:], in_=f[i][:]), r=[f[i]], w=[b[i]])
                p.dma(None, g.UTB[:, k, ec * 2048:(ec + 1) * 2048], b[i][:], r=[b[i]], q='pool')
        for ic in range(64):
            i = it % 3
            it += 1
            p.dma(f[i], f[i][:].rearrange("p (a d) -> p a d", a=2), vsrc[:, ic * 2:(ic + 1) * 2, :])
            if engs[i] == 'act':
                p.op('act', lambda e, i=i: e.copy(out=b[i][:], in_=f[i][:]), r=[f[i]], w=[b[i]])
            else:
                p.op(engs[i], lambda e, i=i: e.tensor_copy(out=b[i][:], in_=f[i][:]), r=[f[i]], w=[b[i]])
            p.dma(None, g.VB[:, ic * 2:(ic + 1) * 2, :], b[i][:].rearrange("p (a d) -> p a d", a=2), r=[b[i]], q='pool')


NI = 8


def phase_peerq(g, l, last):
    p = g.p
    Lw = g.lay[l]
    with p.phase():
        wq = p.sb("pq_wq", [128, 8, 2048], BF16)
        with p.phase():
            wqf = p.sb("pq_wqf", [128, 8, 512], F32)
            wsrc = Lw["wq"].rearrange("(k p) n -> p k n", p=128)
            for n in range(4):
                p.dma(wqf, wqf[:], wsrc[:, :, n * 512:(n + 1) * 512])
                p.op('dve', lambda e, n=n: e.tensor_copy(out=wq[:, :, n * 512:(n + 1) * 512], in_=wqf[:]), r=[wqf], w=[wq])
        ht = [p.sb("pq_ht%d" % i, [128, 8, 512], BF16) for i in range(2)]
        stg = [p.sb("pq_st%d" % i, [128, 4, 512], BF16) for i in range(2)]
        ps = [p.ps("pq_ps%d" % i, [128, 512], F32) for i in range(4)]
        it = 0
        for gi, (t0, n) in enumerate(tok_groups()):
            if last and t0 < LC:
                continue
            h_ = ht[gi % 2]
            p.dma(h_, h_[:, :, 0:n], g.H2T[:, :, t0:t0 + n])
            for jq in range(4):
                st_ = stg[(gi * 4 + jq) % 2]
                for jj in range(4):
                    j = jq * 4 + jj
                    ps_ = ps[it % 4]
                    it += 1
                    for k in range(8):
                        p.op('pe', lambda e, k=k, j=j, ps_=ps_: e.matmul(ps_[:, 0:n], lhsT=wq[:, k, j * 128:(j + 1) * 128], rhs=h_[:, k, 0:n],
                                                                        start=(k == 0), stop=(k == 7)), r=[wq, h_], w=[ps_])
                    if jj % 2 == 0:
                        p.op('act', lambda e, jj=jj, ps_=ps_: e.copy(out=st_[:, jj, 0:n], in_=ps_[:, 0:n]), r=[ps_], w=[st_])
                    else:
                        p.op('dve', lambda e, jj=jj, ps_=ps_: e.tensor_copy(out=st_[:, jj, 0:n], in_=ps_[:, 0:n]), r=[ps_], w=[st_])
                p.dma(None, g.QPT[:, jq * 4:(jq + 1) * 4, t0:t0 + n], st_[:, :, 0:n], r=[st_], q='pool')


def phase_peer(g, l, last):
    p = g.p
    Lw = g.lay[l]
    with p.phase():
        skt = p.sb("pe_skt", [128, 16, 128], BF16)
        with p.phase():
            sktf = p.sb("pe_sktf", [128, 16, 128], F32)
            p.dma(sktf, sktf[:], Lw["skt"])
            p.op('dve', lambda e: e.tensor_copy(out=skt[:], in_=sktf[:]), r=[sktf], w=[skt])
        gt = []
        for which in range(2):
            t = p.sb("pe_gt%d" % which, [128, D], F32)
            load_bcast(p, t, mod_row(g, which, 5), D)
            gt.append(t)
        if last:
            fg = p.sb("pe_fg", [128, D], F32)
            load_bcast(p, fg, g.final_g, D)
        h2t = [p.sb("pe_h2t%d" % i, [128, 8, 256], BF16) for i in range(2)]
        e1 = [p.sb("pe_e1_%d" % i, [128, 8, 128], F32) for i in range(2)]
        e2 = [p.sb("pe_e2_%d" % i, [128, 8, 128], F32) for i in range(2)]
        Tt = [p.sb("pe_T%d" % i, [128, 8], F32) for i in range(2)]
        kap = [p.sb("pe_kap%d" % i, [128, 8], F32) for i in range(2)]
        Dk = [[p.sb("pe_dk%d_%d" % (i, h), [128, 128], BF16) for h in range(8)] for i in range(2)]
        acc = [[p.ps("pe_acc%d%d" % (i, n), [128, 512], F32) for n in range(2)] for i in range(2)]
        psA = [p.ps("pe_A%d" % i, [128, 512], F32) for i in range(2)]
        psW = p.ps("pe_W", [128, 512], F32)
        psT_full = p.ps("pe_T", [128, 8, 128], BF16)
        psT = [T("pe_T%d" % i, psT_full[:, i * 4:(i + 1) * 4, :]) for i in range(2)]
        psM = psA[1]
        pairs = list(range(1, NTILE // 2)) if last else list(range(NTILE // 2))
        for pi, pr in enumerate(pairs):
            t0 = pr * 256
            which = 1 if pr == 0 else 0
            ht = h2t[pi % 2]
            p.dma(ht, ht[:], g.H2T[:, :, t0:t0 + 256])
            with p.phase():
                qTb = p.sb("pe_qT", [128, 16, 256], BF16)
                p.dma(qTb, qTb[:], g.QPT[:, :, t0:t0 + 256])
                S_all = [p.sb("pe_S%d" % i, [128, 16, 128], F32) for i in range(2)]
                m8 = p.sb("pe_m8", [128, 2, 16], F32)
                sr = p.sb("pe_sr", [128, 128], F32)
                ngm = p.sb("pe_ngm", [128, 2], F32)
                et = p.sb("pe_et", [128, 2, 16], F32)
                cand = p.sb("pe_cand", [128, 16, 16], F32)
                cr = p.sb("pe_cr", [128, 256], F32)
                c16 = p.sb("pe_c16", [128, 16], F32)
                zz = p.sb("pe_zz", [128, 1], F32)
                for sub in range(2):
                    for jq in range(4):
                        for jj in range(4):
                            j = jq * 4 + jj
                            p.op('pe', lambda e, j=j, jj=jj: e.matmul(psM[:, jj * 128:(jj + 1) * 128],
                                                                      lhsT=qTb[:, j, sub * 128:(sub + 1) * 128], rhs=skt[:, j, :],
                                                                      start=True, stop=True), r=[qTb, skt], w=[psM])
                        p.op('act', lambda e, jq=jq: e.copy(out=S_all[sub][:, jq * 4:(jq + 1) * 4, :],
                                                           in_=psM[:].rearrange("p (a n) -> p a n", a=4)), r=[psM], w=[S_all[sub]])
                    for h in range(8):
                        for pp in range(2):
                            s_ = S_all[sub][:, 2 * h + pp, :]
                            p.op('dve', lambda e, s_=s_, pp=pp: e.max(out=m8[:, pp, 0:8], in_=s_), r=[S_all[sub]], w=[m8])
                            p.op('dve', lambda e, s_=s_, pp=pp: e.match_replace(out=sr[:], in_to_replace=m8[:, pp, 0:8], in_values=s_,
                                                                               imm_value=-1e30), r=[S_all[sub], m8], w=[sr])
                            p.op('dve', lambda e, pp=pp: e.max(out=m8[:, pp, 8:16], in_=sr[:]), r=[sr], w=[m8])
                        p.op('dve', lambda e: e.tensor_scalar(out=ngm[:], in0=m8[:, :, 0], scalar1=-1.0, scalar2=None, op0=ALU.mult),
                             r=[m8], w=[ngm])
                        for pp, ee in ((0, e1[sub]), (1, e2[sub])):
                            p.op('act', lambda e, pp=pp, ee=ee: e.activation(out=ee[:, h, :], in_=S_all[sub][:, 2 * h + pp, :], func=AF.Exp,
                                                                            bias=ngm[:, pp:pp + 1], scale=1.0), r=[S_all[sub], ngm], w=[ee])
                            p.op('act', lambda e, pp=pp: e.activation(out=et[:, pp, :], in_=m8[:, pp, :], func=AF.Exp,
                                                                     bias=ngm[:, pp:pp + 1], scale=1.0), r=[m8, ngm], w=[et])
                        p.op('dve', lambda e: e.tensor_tensor(out=cand[:], in0=et[:, 0, :].unsqueeze(2).broadcast_to([128, 16, 16]),
                                                              in1=et[:, 1, :].unsqueeze(1).broadcast_to([128, 16, 16]), op=ALU.mult),
                             r=[et], w=[cand])
                        cf = cand[:].rearrange("p a b -> p (a b)")
                        p.op('dve', lambda e: e.max(out=c16[:, 0:8], in_=cf), r=[cand], w=[c16])
                        p.op('dve', lambda e: e.match_replace(out=cr[:], in_to_replace=c16[:, 0:8], in_values=cf, imm_value=-1e30),
                             r=[cand, c16], w=[cr])
                        p.op('dve', lambda e: e.max(out=c16[:, 8:16], in_=cr[:]), r=[cr], w=[c16])
                        p.op('dve', lambda e, h=h: e.tensor_scalar(out=Tt[sub][:, h:h + 1], in0=c16[:, 15:16], scalar1=1.0 - 1e-6,
                                                                   scalar2=None, op0=ALU.mult), r=[c16], w=[Tt[sub]])
                        p.op('dve', lambda e: e.tensor_reduce(out=zz[:], in_=c16[:], axis=AX.X, op=ALU.add), r=[c16], w=[zz])
                        p.op('dve', lambda e, h=h: e.reciprocal(out=kap[sub][:, h:h + 1], in_=zz[:]), r=[zz], w=[kap[sub]])
                        p.op('dve', lambda e, h=h: e.tensor_scalar(out=Dk[sub][h][:], in0=g.ident_f[:], scalar1=kap[sub][:, h:h + 1],
                                                                   scalar2=None, op0=ALU.mult), r=[g.ident_f, kap[sub]], w=[Dk[sub][h]])
            with p.phase():
                Pt = [p.sb("pe_P%d" % i, [128, NI, 128], F32) for i in range(3)]
                G = [[p.sb("pe_G%d_%d" % (b_, i), [128, 8, NI * 128], BF16) for i in range(2)] for b_ in range(2)]
                ut = [p.sb("pe_ut%d" % i, [128, 8, 512], BF16) for i in range(2)]
                vb = [p.sb("pe_vb%d" % i, [128, 4, D], BF16) for i in range(2)]
                ga = [p.sb("pe_ga%d" % i, [128, 512], F32) for i in range(2)]
                wg = [p.sb("pe_wg%d" % i, [128, 512], BF16) for i in range(2)]
                wgT = [p.sb("pe_wgT%d" % i, [128, 4, 128], BF16) for i in range(2)]
                xs = [p.sb("pe_x%d" % i, [128, D], F32) for i in range(2)]
                xo = [p.sb("pe_xo%d" % i, [128, D], F32) for i in range(2)]
                if last:
                    hb = p.sb("pe_hb", [128, D], F32)
                    st = p.sb("pe_st", [128, 4], F32)
                nblk = 128 // NI
                nesb = NI * 128 // 512
                cnt = {"p": 0}

                def unit(ib, sub, h):
                    P_ = Pt[cnt["p"] % 3]
                    cnt["p"] += 1
                    G_ = G[ib % 2][sub]
                    p.op('pool', lambda e: e.tensor_tensor(
                        out=P_[:], in0=e1[sub][:, h, ib * NI:(ib + 1) * NI].unsqueeze(2).broadcast_to([128, NI, 128]),
                        in1=e2[sub][:, h, :].unsqueeze(1).broadcast_to([128, NI, 128]), op=ALU.mult),
                        r=[e1[sub], e2[sub]], w=[P_])
                    p.op('dve', lambda e: e.scalar_tensor_tensor(
                        out=G_[:, h, :], in0=P_[:].rearrange("p a b -> p (a b)"), scalar=Tt[sub][:, h:h + 1],
                        in1=P_[:].rearrange("p a b -> p (a b)"), op0=ALU.is_ge, op1=ALU.mult),
                        r=[P_, Tt[sub]], w=[G_])

                iters = [(ib, esb, sub) for ib in range(nblk) for esb in range(nesb) for sub in range(2)]
                nit = len(iters)

                def s1(i):
                    ib, esb, sub = iters[i]
                    e0 = ib * NI * 128 + esb * 512
                    b_ = (i // 2) % 2
                    u_, v_ = ut[b_], vb[b_]
                    if sub == 0:
                        p.dma(u_, u_[:], g.UTB[:, :, e0:e0 + 512])
                        p.dma(v_, v_[:], g.VB[:, e0 // 128:e0 // 128 + 4, :])
                    pa, ga_ = psA[i % 2], ga[i % 2]
                    for k in range(8):
                        p.op('pe', lambda e, k=k: e.matmul(pa[:], lhsT=ht[:, k, sub * 128:(sub + 1) * 128], rhs=u_[:, k, :],
                                                           start=(k == 0), stop=(k == 7)), r=[ht, u_], w=[pa])
                    p.op('act', lambda e: e.activation(out=ga_[:], in_=pa[:], func=AF.Gelu), r=[pa], w=[ga_])

                def s2(i):
                    ib, esb, sub = iters[i]
                    G_ = G[ib % 2][sub]
                    ga_, wg_ = ga[i % 2], wg[i % 2]
                    for h in range(8):
                        p.op('pe', lambda e, h=h: e.matmul(psW[:], lhsT=Dk[sub][h][:], rhs=G_[:, h, esb * 512:(esb + 1) * 512],
                                                           start=(h == 0), stop=(h == 7)), r=[Dk[sub][h], G_], w=[psW])
                    p.op('dve', lambda e: e.tensor_tensor(out=wg_[:], in0=psW[:], in1=ga_[:], op=ALU.mult), r=[psW, ga_], w=[wg_])

                def s3(i):
                    emit_transposes(g, wg[i % 2], psT[i % 2], wgT[i % 2][:], wgT[i % 2], n=4)

                def s4(i):
                    ib, esb, sub = iters[i]
                    v_ = vb[(i // 2) % 2]
                    wgT_ = wgT[i % 2]
                    first = (ib == 0 and esb == 0)
                    lastm = (ib == nblk - 1 and esb == nesb - 1)
                    for c4 in range(4):
                        for n in range(2):
                            a_ = acc[sub][n]
                            p.op('pe', lambda e, c4=c4, n=n, a_=a_: e.matmul(
                                a_[:], lhsT=wgT_[:, c4, :], rhs=v_[:, c4, n * 512:(n + 1) * 512],
                                start=(first and c4 == 0), stop=(lastm and c4 == 3)), r=[wgT_, v_], w=[a_])

                pend = []
                for sub in range(2):
                    for h in range(8):
                        unit(0, sub, h)
                s1(0)
                s2(0)
                per_it = 16 // (nesb * 2)
                for i in range(nit):
                    ib = iters[i][0]
                    if i % (nesb * 2) == 0 and ib + 1 < nblk:
                        pend = [(ib + 1, sub, h) for sub in range(2) for h in range(8)]
                    for _ in range(per_it):
                        if pend:
                            unit(*pend.pop(0))
                    if i + 1 < nit:
                        s1(i + 1)
                    s3(i)
                    if i + 1 < nit:
                        s2(i + 1)
                    s4(i)
                for sub in range(2):
                    sl = slice(t0 + sub * 128, t0 + (sub + 1) * 128)
                    x_, o_ = xs[sub], xo[sub]
                    p.dma(x_, x_[:], g.X1[sl, :])
                    for n in range(2):
                        cs = slice(n * 512, (n + 1) * 512)
                        p.op('dve', lambda e, n=n, cs=cs: e.tensor_tensor(out=o_[:, cs], in0=acc[sub][n][:], in1=gt[which][:, cs], op=ALU.mult),
                             r=[acc[sub][n], gt[which]], w=[o_])
                    p.op('pool', lambda e: e.tensor_tensor(out=o_[:], in0=o_[:], in1=x_[:], op=ALU.add), r=[o_, x_], w=[o_])
                    if not last:
                        p.dma(None, g.X2[sl, :], o_[:], r=[o_], q='pool')
                    else:
                        p.op('act', lambda e: e.activation(out=hb[:], in_=o_[:], func=AF.Square, accum_out=st[:, 0:1]), r=[o_], w=[hb, st])
                        p.op('dve', lambda e: e.tensor_scalar(out=st[:, 1:2], in0=st[:, 0:1], scalar1=1.0 / D, scalar2=EPS,
                                                              op0=ALU.mult, op1=ALU.add), r=[st], w=[st])
                        p.op('act', lambda e: e.activation(out=st[:, 2:3], in_=st[:, 1:2], func=AF.Sqrt), r=[st], w=[st])
                        p.op('dve', lambda e: e.reciprocal(out=st[:, 3:4], in_=st[:, 2:3]), r=[st], w=[st])
                        p.op('dve', lambda e: e.scalar_tensor_tensor(out=hb[:], in0=o_[:], scalar=st[:, 3:4], in1=fg[:],
                                                                     op0=ALU.mult, op1=ALU.mult), r=[o_, st, fg], w=[hb])
                        p.dma(None, g.out[t0 - LC + sub * 128:t0 - LC + (sub + 1) * 128, :], hb[:], r=[hb], q='pool')


_PROG_CACHE = {}


def kernel(x, c, ctx, c_ctx, w_ada, b_ada, norm1_g, w_in, na_rpb, diff_lambda, diff_subln_g, ret_decay_logit,
           w_out, norm2_g, peer_wq, peer_subkeys, peer_u, peer_v, final_g):
    inp = dict(x=x, c=c, ctx=ctx, c_ctx=c_ctx, w_ada=w_ada, b_ada=b_ada, norm1_g=norm1_g, w_in=w_in, na_rpb=na_rpb,
               diff_lambda=diff_lambda, diff_subln_g=diff_subln_g, ret_decay_logit=ret_decay_logit, w_out=w_out,
               norm2_g=norm2_g, peer_wq=peer_wq, peer_subkeys=peer_subkeys, peer_u=peer_u, peer_v=peer_v, final_g=final_g)
    inp = {k: np.asarray(v) for k, v in inp.items()}
    B = inp["x"].shape[0]
    cs = _consts()
    shared = {"final_g": np.asarray(inp["final_g"], np.float32).reshape(1, D)}
    for n in CONST_NAMES:
        shared[n] = cs[n]
    for l in range(DEPTH):
        for k, v in _layer_inputs(inp, l).items():
            shared["L%d_%s" % (l, k)] = v
    ccv = np.asarray(inp["c_ctx"], np.float32).reshape(8, 128).T
    in_maps = []
    for b in range(B):
        m = dict(shared)
        m["xin"] = np.ascontiguousarray(np.concatenate([inp["ctx"][b], inp["x"][b]], axis=0).astype(np.float32))
        m["cvec"] = np.ascontiguousarray(np.stack([np.asarray(inp["c"][b], np.float32).reshape(8, 128).T, ccv], axis=-1))
        in_maps.append(m)
    if "nc" not in _PROG_CACHE:
        _PROG_CACHE["nc"] = build_program()[0]
    nc = _PROG_CACHE["nc"]
    res = run_bass_kernel_spmd(nc, in_maps, core_ids=list(range(B)))
    return np.stack([np.asarray(r["out"], dtype=np.float32) for r in res.results], axis=0)
```

```python
import numpy as np
from contextlib import ExitStack, contextmanager
import concourse.bass as bass
import concourse.mybir as mybir
from concourse.bass_utils import run_bass_kernel_spmd

F32 = mybir.dt.float32
BF16 = mybir.dt.bfloat16
AF = mybir.ActivationFunctionType
ALU = mybir.AluOpType
AX = mybir.AxisListType


class Buf:
    def __init__(self, name):
        self.name = name
        self.w = None
        self.r = {}


class T(Buf):
    def __init__(self, name, h):
        super().__init__(name)
        self.h = h

    def __getitem__(self, idx):
        return self.h[idx]


class Prog:
    ENG = ('pe', 'act', 'dve', 'pool', 'sp')

    def __init__(self, nc, ndma=32):
        self.nc = nc
        self.ndma = ndma
        self.es = ExitStack()
        self.scope = None
        self.drams = {}

    def __enter__(self):
        nc = self.nc
        self.es.__enter__()
        self.engs = {'pe': nc.tensor, 'act': nc.scalar, 'dve': nc.vector, 'pool': nc.gpsimd, 'sp': nc.sync}
        self.sems = {k: self.es.enter_context(nc.semaphore("s_" + k)) for k in ('pe', 'act', 'dve', 'pool')}
        self.cnt = {k: 0 for k in self.sems}
        self.dsem = [self.es.enter_context(nc.semaphore("d%d" % i)) for i in range(self.ndma)]
        self.dval = [0] * self.ndma
        self.dnext = 0
        self.seen = {e: {} for e in self.ENG}
        self.scope = self.es
        self.ninst = 0
        return self

    def __exit__(self, *a):
        return self.es.__exit__(*a)

    def _uniq(self, name):
        self.nalloc = getattr(self, "nalloc", 0) + 1
        return "%s_%d" % (name, self.nalloc)

    def sb(self, name, shape, dtype):
        name = self._uniq(name)
        return T(name, self.scope.enter_context(self.nc.sbuf_tensor(name, list(shape), dtype)))

    def ps(self, name, shape, dtype):
        name = self._uniq(name)
        return T(name, self.scope.enter_context(self.nc.psum_tensor(name, list(shape), dtype)))

    def dram_buf(self, name):
        if name not in self.drams:
            self.drams[name] = Buf(name)
        return self.drams[name]

    @contextmanager
    def phase(self):
        old = self.scope
        with ExitStack() as st:
            self.scope = st
            yield
            self.barrier()
        self.scope = old

    def _semobj(self, key):
        return self.sems[key] if isinstance(key, str) else self.dsem[key[1]]

    def _wait(self, eng, tok, raw=False):
        key, val = tok
        if key == eng and not (raw and eng != 'pe'):
            return
        if self.seen[eng].get(key, 0) >= val:
            return
        self.engs[eng].wait_ge(self._semobj(key), val)
        self.seen[eng][key] = val
        self.ninst += 1

    def _deps(self, eng, r, w):
        for b in r:
            if b.w is not None:
                self._wait(eng, b.w, raw=True)
        for b in w:
            if b.w is not None:
                self._wait(eng, b.w)
            for k, v in b.r.items():
                self._wait(eng, (k, v))

    def _mark(self, tok, r, w):
        k, v = tok
        for b in r:
            if b.r.get(k, 0) < v:
                b.r[k] = v
        for b in w:
            b.w = tok
            b.r = {}

    def op(self, eng, fn, r=(), w=()):
        self._deps(eng, r, w)
        inst = fn(self.engs[eng])
        self.cnt[eng] += 1
        inst.then_inc(self.sems[eng], 1)
        self._mark((eng, self.cnt[eng]), r, w)
        self.ninst += 1

    def dma(self, wbuf, out_ap, in_ap, r=(), q='sp', **kw):
        i = self.dnext
        self.dnext = (i + 1) % self.ndma
        if self.dval[i] > 0:
            self._wait(q, (('d', i), self.dval[i]))
        w = [] if wbuf is None else ([wbuf] if isinstance(wbuf, Buf) else list(wbuf))
        self._deps(q, r, w)
        inst = self.engs[q].dma_start(out=out_ap, in_=in_ap, **kw)
        self.dval[i] += 16
        inst.then_inc(self.dsem[i], 16)
        self._mark((('d', i), self.dval[i]), r, w)
        self.ninst += 1

    def barrier(self):
        for e in self.ENG:
            for f in self.sems:
                if self.cnt[f] > 0:
                    self._wait(e, (f, self.cnt[f]))
            for i in range(self.ndma):
                if self.dval[i] > 0:
                    self._wait(e, (('d', i), self.dval[i]))

    def finish(self):
        self.barrier()


D = 1024
L = 4096
LC = 256
NT = L + LC
NTILE = NT // 128
DEPTH = 2
GW = 64
EPS = 1e-6
NEGB = -30000.0
O_NAQ, O_NAK, O_NAV, O_DFQ, O_DFK, O_DFV, O_RTQ, O_RTK, O_RTV, O_RTG = 0, 256, 512, 768, 1024, 1280, 1536, 1792, 2048, 2560
C_QNA, C_KNA, C_QDF, C_KDA, C_KDB, C_QRT, C_KRT = 0, 2, 4, 6, 8, 10, 12
NFMC = 14


def _rope_tables():
    t = np.arange(L)
    rows = (t // GW).astype(np.float32)
    cols = (t % GW).astype(np.float32)
    out = {}
    for name, dim in (("df", 32), ("rt", 64)):
        half = dim // 2
        inv = np.power(np.float32(10000.0), -np.arange(0, half, 2, dtype=np.float32) / np.float32(half)).astype(np.float32)
        ang = np.concatenate([rows[:, None] * inv, cols[:, None] * inv], axis=-1).astype(np.float32)
        cos = np.cos(ang).astype(np.float32)
        sin = np.sin(ang).astype(np.float32)
        C = np.ones((128, NT), np.float32)
        S = np.zeros((128, NT), np.float32)
        for r in range(128):
            d = r % dim
            pi = d // 2
            C[r, LC:] = cos[:, pi]
            S[r, LC:] = -sin[:, pi] if d % 2 == 0 else sin[:, pi]
        out[name] = (C, S)
    return out


def _na_patterns():
    pats = {}
    for pname, r in (("r0", 0), ("r2", 2), ("mid", 8), ("r60", 60), ("r62", 62)):
        kr0 = min(max(r - 4, 0), 54)
        chunks = []
        for m in range(5):
            dr = np.zeros((128, 128), np.int64)
            dc = np.zeros((128, 128), np.int64)
            va = np.zeros((128, 128), bool)
            for kk in range(128):
                krow = kr0 + 2 * m + kk // 64
                kc = kk % 64
                for qq in range(128):
                    qrow = r + qq // 64
                    qc = qq % 64
                    r0 = min(max(qrow - 4, 0), 56)
                    cs = min(max(qc - 8, 0), 48)
                    ok = (r0 <= krow < r0 + 8) and (cs <= kc < cs + 16) and krow < 64
                    va[kk, qq] = ok
                    if ok:
                        dr[kk, qq] = krow - qrow + 7
                        dc[kk, qq] = min(max(kc - qc, -15), 15) + 15
            chunks.append((dr, dc, va))
        pats[pname] = chunks
    return pats


_CONST_CACHE = {}


def _consts():
    if _CONST_CACHE:
        return _CONST_CACHE
    c = _CONST_CACHE
    rt = _rope_tables()
    s_df = np.float32(32 ** -0.5)
    c["T_DFQ_C"] = rt["df"][0] * s_df
    c["T_DFQ_S"] = rt["df"][1] * s_df
    c["T_DFK_C"] = rt["df"][0]
    c["T_DFK_S"] = rt["df"][1]
    c["T_RTQ_C"] = rt["rt"][0]
    c["T_RTQ_S"] = rt["rt"][1]
    c["T_RTK_C"] = rt["rt"][0] * np.float32(0.125)
    c["T_RTK_S"] = rt["rt"][1] * np.float32(0.125)
    c["IDENT"] = np.eye(128, dtype=np.float32)
    pats = _na_patterns()
    c["_pats"] = pats
    names = ["r0", "r2", "mid", "r60", "r62"]
    mask = np.zeros((128, 5, 5, 128), np.float32)
    for pi, pn in enumerate(names):
        for m in range(5):
            mask[:, pi, m, :] = np.where(pats[pn][m][2], 0.0, NEGB)
    c["NA_MASK"] = mask
    i = np.arange(128, dtype=np.float32)
    ij = i[None, :] - i[:, None]
    c["RT_RIJ"] = np.maximum(ij, 0).astype(np.float32)
    c["RT_MIJ"] = (ij >= 0).astype(np.float32)
    c["RT_RJI"] = np.maximum(-ij, 0).astype(np.float32)
    c["RT_MJI"] = (ij <= 0).astype(np.float32)
    c["RT_IROW"] = np.broadcast_to(i[None, :] + 1.0, (128, 128)).astype(np.float32).copy()
    c["RT_IROWB"] = np.broadcast_to(128.0 - i[None, :], (128, 128)).astype(np.float32).copy()
    c["RT_JCOL"] = np.stack([127.0 - i, i], axis=1).astype(np.float32)
    return c


def _layer_inputs(inp, l):
    c = _consts()
    w_in = np.asarray(inp["w_in"][l], np.float32)
    Z = np.zeros((D, 32), np.float32)

    def sw(cols):
        return cols ^ 1

    fm = []

    def add(cols):
        fm.append(w_in[:, cols])

    ar = np.arange
    for j in range(2):
        add(O_NAQ + j * 128 + ar(128))
    for j in range(2):
        add(O_NAK + j * 128 + ar(128))
    for j in range(2):
        cc = O_DFQ + j * 128 + ar(128)
        add(cc); add(sw(cc))
    for comp in range(2):
        for j in range(2):
            blocks_m, blocks_s = [], []
            for hh in range(2):
                h = 2 * j + hh
                cc = O_DFK + h * 64 + comp * 32 + ar(32)
                if comp == 0:
                    blocks_m += [w_in[:, cc], Z]; blocks_s += [w_in[:, sw(cc)], Z]
                else:
                    blocks_m += [Z, w_in[:, cc]]; blocks_s += [Z, w_in[:, sw(cc)]]
            fm.append(np.concatenate(blocks_m, axis=1)); fm.append(np.concatenate(blocks_s, axis=1))
    for j in range(2):
        cc = O_RTQ + j * 128 + ar(128)
        add(cc); add(sw(cc))
    for j in range(2):
        cc = O_RTK + j * 128 + ar(128)
        add(cc); add(sw(cc))
    WFM = np.ascontiguousarray(np.concatenate(fm, axis=1))
    WTM = np.ascontiguousarray(np.concatenate([w_in[:, O_NAV:O_NAV + 256], w_in[:, O_DFV:O_DFV + 256],
                                               w_in[:, O_RTV:O_RTV + 512], w_in[:, O_RTG:O_RTG + 512]], axis=1))
    rpb = np.asarray(inp["na_rpb"][l], np.float32)
    pats = c["_pats"]
    names = ["r0", "r2", "mid", "r60", "r62"]
    nab = np.zeros((128, 4, 5, 5, 128), np.float32)
    for pi, pn in enumerate(names):
        for m in range(5):
            dr, dc, va = pats[pn][m]
            for h in range(4):
                nab[:, h, pi, m, :] = rpb[h][dr, dc]
    sk = np.asarray(inp["peer_subkeys"][l], np.float32).reshape(16, 128, 128)
    skt = np.ascontiguousarray(sk.transpose(2, 0, 1))
    d = {
        "w_ada": np.asarray(inp["w_ada"][l], np.float32),
        "b_ada": np.asarray(inp["b_ada"][l], np.float32).reshape(1, 6 * D),
        "g1": np.asarray(inp["norm1_g"][l], np.float32).reshape(1, D),
        "g2": np.asarray(inp["norm2_g"][l], np.float32).reshape(1, D),
        "wfm": WFM, "wtm": WTM, "nab": nab,
        "lam": np.asarray(inp["diff_lambda"][l], np.float32).reshape(1, 128),
        "subg": np.asarray(inp["diff_subln_g"][l], np.float32).reshape(1, 64),
        "dec": np.asarray(inp["ret_decay_logit"][l], np.float32).reshape(1, 8),
        "wout": np.asarray(inp["w_out"][l], np.float32),
        "wq": np.asarray(inp["peer_wq"][l], np.float32),
        "skt": skt,
        "ut": np.ascontiguousarray(np.asarray(inp["peer_u"][l], np.float32).T),
        "v": np.asarray(inp["peer_v"][l], np.float32),
    }
    return d


LAYER_SHAPES = {
    "w_ada": [D, 6 * D], "b_ada": [1, 6 * D], "g1": [1, D], "g2": [1, D], "wfm": [D, 24 * 128], "wtm": [D, 1536],
    "nab": [128, 4, 5, 5, 128], "lam": [1, 128], "subg": [1, 64], "dec": [1, 8], "wout": [D, D], "wq": [D, 2048],
    "skt": [128, 16, 128], "ut": [D, 16384], "v": [16384, D],
}
CONST_NAMES = ["T_DFQ_C", "T_DFQ_S", "T_DFK_C", "T_DFK_S", "T_RTQ_C", "T_RTQ_S", "T_RTK_C", "T_RTK_S", "IDENT", "NA_MASK",
               "RT_RIJ", "RT_MIJ", "RT_RJI", "RT_MJI", "RT_IROW", "RT_IROWB", "RT_JCOL"]


class Ctx:
    pass


def build_program(nlayers=DEPTH, debug=None, stop_after=None):
    nc = bass.Bass("TRN2", target_bir_lowering=False)
    g = Ctx()
    g.nc = nc
    g.debug = debug or ()
    din = lambda name, shape, dt=F32: nc.dram_tensor(name, list(shape), dt, kind="ExternalInput").ap()
    dscr = lambda name, shape, dt: nc.dram_tensor(name, list(shape), dt, kind="Internal").ap()
    g.xin = din("xin", [NT, D])
    g.cvec = din("cvec", [128, 8, 2])
    g.final_g = din("final_g", [1, D])
    g.cst = {}
    cs = _consts()
    for n in CONST_NAMES:
        g.cst[n] = din(n, cs[n].shape)
    g.lay = []
    for l in range(nlayers):
        g.lay.append({k: din("L%d_%s" % (l, k), shp) for k, shp in LAYER_SHAPES.items()})
    g.out = nc.dram_tensor("out", [L, D], F32, kind="ExternalOutput").ap()
    g.MODS = dscr("MODS", [2, 6 * D], F32)
    g.X1 = dscr("X1", [NT, D], F32)
    g.X2 = dscr("X2", [NT, D], F32)
    g.FMS = dscr("FMS", [NFMC, 128, NT], BF16)
    g.VNA = dscr("VNA", [NT, 4, 65], BF16)
    g.VDF = dscr("VDF", [NT, 4, 65], BF16)
    g.VRT = dscr("VRT", [NT, 512], BF16)
    g.GRT = dscr("GRT", [NT, 512], BF16)
    g.KRTT = dscr("KRTT", [NT, 256], BF16)
    g.Y = dscr("Y", [NT, D], BF16)
    g.H2T = dscr("H2T", [128, 8, NT], BF16)
    g.QPT = dscr("QPT", [128, 16, NT], BF16)
    g.UTB = dscr("UTB", [128, 8, 16384], BF16)
    g.VB = dscr("VB", [128, 128, D], BF16)
    g.dbg = {}
    for name, shape, dt in g.debug:
        g.dbg[name] = nc.dram_tensor("dbg_" + name, list(shape), dt, kind="ExternalOutput").ap()
    p = Prog(nc)
    g.p = p
    g.scr = Buf("scratch")
    with p:
        g.ident_f = p.sb("ident_f", [128, 128], F32)
        g.ident = p.sb("ident", [128, 128], BF16)
        p.dma(g.ident_f, g.ident_f[:], g.cst["IDENT"])
        p.op('dve', lambda e: e.tensor_copy(out=g.ident[:], in_=g.ident_f[:]), r=[g.ident_f], w=[g.ident])
        stages = []
        for l in range(nlayers):
            last = (l == DEPTH - 1)
            xa = g.xin if l == 0 else g.X2
            stages += [("mod%d" % l, lambda l=l: phase_mod(g, l)),
                       ("p1_%d" % l, lambda l=l, xa=xa: phase_p1(g, l, xa)),
                       ("na_%d" % l, lambda l=l, last=last: phase_na(g, l, not last)),
                       ("df_%d" % l, lambda l=l, last=last: phase_diff(g, l, not last)),
                       ("rt_%d" % l, lambda l=l, last=last: (phase_ret(g, l, not last), dbg_dump(g, "y%d" % l, g.Y))),
                       ("p3_%d" % l, lambda l=l, last=last, xa=xa: (phase_p3(g, l, xa, last), dbg_dump(g, "x1_%d" % l, g.X1))),
                       ("cast%d" % l, lambda l=l: phase_cast(g, l)),
                       ("peerq%d" % l, lambda l=l, last=last: phase_peerq(g, l, last)),
                       ("peer%d" % l, lambda l=l, last=last: (phase_peer(g, l, last), dbg_dump(g, "x2_%d" % l, g.X2)))]
        for name, fn in stages:
            fn()
            if stop_after == name:
                break
        p.finish()
    g.ninst = p.ninst
    return nc, g


def dbg_dump(g, name, src):
    if name in g.dbg:
        g.p.dma(None, g.dbg[name], src, q='sp')
        g.p.barrier()


def load_bcast(p, t, dram_row, n, q='sp'):
    p.dma(t, t[:, 0:n], dram_row.broadcast_to([128, n]), q=q)


def phase_mod(g, l):
    p = g.p
    Lw = g.lay[l]
    with p.phase():
        cv = p.sb("cv", [128, 8, 2], F32)
        sil = p.sb("sil", [128, 8, 2], F32)
        bad = p.sb("bad", [2, 6 * D], F32)
        modt = p.sb("modt", [2, 6 * D], F32)
        wa = [p.sb("wa%d" % i, [128, 8, 512], F32) for i in range(2)]
        ps = [p.ps("modps%d" % i, [128, 512], F32) for i in range(2)]
        p.dma(cv, cv[:], g.cvec)
        p.dma(bad, bad[:], Lw["b_ada"].broadcast_to([2, 6 * D]))
        p.op('act', lambda e: e.activation(out=sil[:], in_=cv[:], func=AF.Silu), r=[cv], w=[sil])
        wsrc = Lw["w_ada"].rearrange("(k p) n -> p k n", p=128)
        for n in range(12):
            w = wa[n % 2]
            pp = ps[n % 2]
            p.dma(w, w[:], wsrc[:, :, n * 512:(n + 1) * 512])
            for k in range(8):
                p.op('pe', lambda e, k=k, w=w, pp=pp: e.matmul(pp[0:2, :], lhsT=sil[:, k, :], rhs=w[:, k, :],
                                                             start=(k == 0), stop=(k == 7)), r=[sil, w], w=[pp])
            p.op('dve', lambda e, n=n, pp=pp: e.tensor_tensor(out=modt[:, n * 512:(n + 1) * 512], in0=pp[0:2, :],
                                                             in1=bad[:, n * 512:(n + 1) * 512], op=ALU.add),
                 r=[pp, bad], w=[modt])
        p.dma(None, g.MODS, modt[:], r=[modt], q='pool')
        if "mods" in g.dbg:
            p.dma(None, g.dbg["mods"], modt[:], r=[modt], q='pool')


def mod_row(g, which, idx):
    return g.MODS[which:which + 1, idx * D:(idx + 1) * D]


def make_norm_consts(g, tag, grow, idx_shift, idx_scale):
    p = g.p
    gb = p.sb(tag + "_gb", [128, D], F32)
    load_bcast(p, gb, grow, D)
    res = []
    for which in range(2):
        sc = p.sb(tag + "_sc%d" % which, [128, D], F32)
        sh = p.sb(tag + "_sh%d" % which, [128, D], F32)
        load_bcast(p, sc, mod_row(g, which, idx_scale), D)
        load_bcast(p, sh, mod_row(g, which, idx_shift), D)
        p.op('dve', lambda e, sc=sc: e.scalar_tensor_tensor(out=sc[:], in0=sc[:], scalar=1.0, in1=gb[:],
                                                            op0=ALU.add, op1=ALU.mult), r=[sc, gb], w=[sc])
        res.append((sc, sh))
    return res


def emit_norm(g, xt, gs, sh, hb, tmp, st):
    p = g.p
    p.op('act', lambda e: e.activation(out=tmp[:], in_=xt[:], func=AF.Square, accum_out=st[:, 0:1]), r=[xt], w=[tmp, st])
    p.op('dve', lambda e: e.tensor_scalar(out=st[:, 1:2], in0=st[:, 0:1], scalar1=1.0 / D, scalar2=EPS,
                                          op0=ALU.mult, op1=ALU.add), r=[st], w=[st])
    p.op('act', lambda e: e.activation(out=st[:, 2:3], in_=st[:, 1:2], func=AF.Sqrt), r=[st], w=[st])
    p.op('dve', lambda e: e.reciprocal(out=st[:, 3:4], in_=st[:, 2:3]), r=[st], w=[st])
    p.op('dve', lambda e: e.scalar_tensor_tensor(out=tmp[:], in0=xt[:], scalar=st[:, 3:4], in1=gs[:],
                                                 op0=ALU.mult, op1=ALU.mult), r=[xt, st, gs], w=[tmp])
    p.op('dve', lambda e: e.tensor_tensor(out=hb[:], in0=tmp[:], in1=sh[:], op=ALU.add), r=[tmp, sh], w=[hb])


def emit_transposes(g, src, pst, dst_ap, dst_buf, n=8, eng='act'):
    p = g.p
    for k in range(n):
        p.op('pe', lambda e, k=k: e.transpose(pst[:, k, :], src[:, k * 128:(k + 1) * 128], g.ident[:]),
             r=[src, g.ident], w=[pst])
    if eng == 'act':
        p.op('act', lambda e: e.copy(out=dst_ap, in_=pst[:, 0:n, :]), r=[pst], w=[dst_buf])
    else:
        p.op('dve', lambda e: e.tensor_copy(out=dst_ap, in_=pst[:, 0:n, :]), r=[pst], w=[dst_buf])


def tok_groups():
    gs = [(0, 256)]
    for i in range(8):
        gs.append((256 + i * 512, 512))
    return gs


def phase_p1(g, l, xa):
    p = g.p
    Lw = g.lay[l]
    with p.phase():
        HT = p.sb("HT", [128, 8, NT], BF16)
        with p.phase():
            nrm = make_norm_consts(g, "n1", Lw["g1"], 0, 1)
            xt = [p.sb("xt%d" % i, [128, D], F32) for i in range(2)]
            tmp = p.sb("ntmp", [128, D], F32)
            hb = [p.sb("hb%d" % i, [128, D], BF16) for i in range(2)]
            st = [p.sb("nst%d" % i, [128, 4], F32) for i in range(2)]
            pst = [p.ps("pst%d" % i, [128, 8, 128], BF16) for i in range(2)]
            for tt in range(NTILE):
                which = 1 if tt < 2 else 0
                x_ = xt[tt % 2]
                p.dma(x_, x_[:], xa[tt * 128:(tt + 1) * 128, :])
                emit_norm(g, x_, nrm[which][0], nrm[which][1], hb[tt % 2], tmp, st[tt % 2])
                emit_transposes(g, hb[tt % 2], pst[tt % 2], HT[:, :, tt * 128:(tt + 1) * 128], HT)
        if "ht" in g.dbg:
            p.dma(None, g.dbg["ht"], HT[:], r=[HT], q='pool')
        with p.phase():
            wf = [p.sb("wf%d" % i, [128, 8, 128], F32) for i in range(2)]
            wb = [p.sb("wb%d" % i, [128, 8, 128], BF16) for i in range(4)]
            tc_ = [p.sb("tc%d" % i, [128, 512], F32) for i in range(2)]
            ts_ = [p.sb("ts%d" % i, [128, 512], F32) for i in range(2)]
            t1 = [p.sb("rt1_%d" % i, [128, 512], F32) for i in range(2)]
            stg = [p.sb("fstg%d" % i, [128, 512], BF16) for i in range(2)]
            ktm = [p.sb("ktm%d" % i, [128, 4, 128], BF16) for i in range(2)]
            psA = [p.ps("psA%d" % i, [128, 512], F32) for i in range(2)]
            psB = [p.ps("psB%d" % i, [128, 512], F32) for i in range(2)]
            psT = [p.ps("psT%d" % i, [128, 8, 128], BF16) for i in range(2)]
            wsrc = Lw["wfm"].rearrange("(k p) n -> p k n", p=128)
            cnt = {"w": 0, "it": 0}

            def load_w(ci):
                i = cnt["w"]
                cnt["w"] += 1
                f, b = wf[i % 2], wb[i % 4]
                p.dma(f, f[:], wsrc[:, :, ci * 128:(ci + 1) * 128])
                p.op('pool', lambda e: e.tensor_copy(out=b[:], in_=f[:]), r=[f], w=[b])
                return b

            def mm(ps_, w_, t0, n):
                for k in range(8):
                    p.op('pe', lambda e, k=k: e.matmul(ps_[:, 0:n], lhsT=w_[:, k, :], rhs=HT[:, k, t0:t0 + n],
                                                       start=(k == 0), stop=(k == 7)), r=[w_, HT], w=[ps_])

            jobs = []
            wi = 0
            for j in range(2):
                jobs.append((C_QNA + j, wi, None, 0.125, None)); wi += 1
            for j in range(2):
                jobs.append((C_KNA + j, wi, None, 1.0, None)); wi += 1
            for dest, tn in ((C_QDF, "T_DFQ"), (C_KDA, "T_DFK"), (C_KDB, "T_DFK"), (C_QRT, "T_RTQ"), (C_KRT, "T_RTK")):
                for j in range(2):
                    jobs.append((dest + j, wi, wi + 1, 1.0, tn)); wi += 2
            for (dest, wm, ws, scale, tn) in jobs:
                wmb = load_w(wm)
                wsb = load_w(ws) if ws is not None else None
                for (t0, n) in tok_groups():
                    it = cnt["it"]
                    cnt["it"] += 1
                    a, b_ = psA[it % 2], psB[it % 2]
                    sg = stg[it % 2]
                    mm(a, wmb, t0, n)
                    if ws is None:
                        p.op('act', lambda e: e.activation(out=sg[:, 0:n], in_=a[:, 0:n], func=AF.Copy, scale=scale),
                             r=[a], w=[sg])
                    else:
                        mm(b_, wsb, t0, n)
                        tcc, tss, tt1 = tc_[it % 2], ts_[it % 2], t1[it % 2]
                        p.dma(tcc, tcc[:, 0:n], g.cst[tn + "_C"][:, t0:t0 + n])
                        p.dma(tss, tss[:, 0:n], g.cst[tn + "_S"][:, t0:t0 + n])
                        p.op('dve', lambda e: e.tensor_tensor(out=tcc[:, 0:n], in0=a[:, 0:n], in1=tcc[:, 0:n], op=ALU.mult),
                             r=[a, tcc], w=[tcc])
                        p.op('dve', lambda e: e.tensor_tensor(out=tss[:, 0:n], in0=b_[:, 0:n], in1=tss[:, 0:n], op=ALU.mult),
                             r=[b_, tss], w=[tss])
                        p.op('pool', lambda e: e.tensor_tensor(out=sg[:, 0:n], in0=tcc[:, 0:n], in1=tss[:, 0:n], op=ALU.add),
                             r=[tcc, tss], w=[sg])
                    p.dma(None, g.FMS[dest, :, t0:t0 + n], sg[:, 0:n], r=[sg], q='pool')
                    if dest in (C_KRT, C_KRT + 1):
                        nt_ = n // 128
                        pt, kt = psT[it % 2], ktm[it % 2]
                        emit_transposes(g, sg, pt, kt[:, 0:nt_, :], kt, n=nt_)
                        cj = dest - C_KRT
                        p.dma(None, g.KRTT[t0:t0 + n, cj * 128:(cj + 1) * 128].rearrange("(a p) c -> p a c", p=128),
                              kt[:, 0:nt_, :], r=[kt], q='pool')
        with p.phase():
            wtf = p.sb("wtf", [128, 8, 512], F32)
            wtb = p.sb("wtb", [128, 8, 1536], BF16)
            wsrc = Lw["wtm"].rearrange("(k p) n -> p k n", p=128)
            for c3 in range(3):
                p.dma(wtf, wtf[:], wsrc[:, :, c3 * 512:(c3 + 1) * 512])
                p.op('dve', lambda e, c3=c3: e.tensor_copy(out=wtb[:, :, c3 * 512:(c3 + 1) * 512], in_=wtf[:]), r=[wtf], w=[wtb])
            pv = [p.ps("pv%d" % i, [128, 512], F32) for i in range(4)]
            vst = [p.sb("vst%d" % i, [128, 2, 4, 65], BF16) for i in range(2)]
            rst = [p.sb("rst%d" % i, [128, 2, 512], BF16) for i in range(2)]
            for i in range(2):
                p.op('pool', lambda e, i=i: e.memset(vst[i][:], 1.0), w=[vst[i]])
            it = 0
            for tt in range(NTILE):
                vs, rs = vst[tt % 2], rst[tt % 2]
                for c3 in range(3):
                    ps_ = pv[it % 4]
                    it += 1
                    for k in range(8):
                        p.op('pe', lambda e, k=k, ps_=ps_, c3=c3: e.matmul(ps_[:], lhsT=HT[:, k, tt * 128:(tt + 1) * 128],
                                                                          rhs=wtb[:, k, c3 * 512:(c3 + 1) * 512],
                                                                          start=(k == 0), stop=(k == 7)), r=[HT, wtb], w=[ps_])
                    if c3 == 0:
                        p.op('act', lambda e, ps_=ps_: e.copy(out=vs[:, :, :, 0:64],
                                                             in_=ps_[:].rearrange("p (a h d) -> p a h d", a=2, h=4)),
                             r=[ps_], w=[vs])
                    elif c3 == 1:
                        p.op('dve', lambda e, ps_=ps_: e.tensor_copy(out=rs[:, 0, :], in_=ps_[:]), r=[ps_], w=[rs])
                    else:
                        p.op('act', lambda e, ps_=ps_: e.activation(out=rs[:, 1, :], in_=ps_[:], func=AF.Silu), r=[ps_], w=[rs])
                sl = slice(tt * 128, (tt + 1) * 128)
                p.dma(None, g.VNA[sl], vs[:, 0], r=[vs], q='pool')
                p.dma(None, g.VDF[sl], vs[:, 1], r=[vs], q='pool')
                p.dma(None, g.VRT[sl], rs[:, 0, :], r=[rs], q='pool')
                p.dma(None, g.GRT[sl], rs[:, 1, :], r=[rs], q='pool')
        for nm, src in (("fms", g.FMS), ("vna", g.VNA), ("vdf", g.VDF), ("vrt", g.VRT), ("grt", g.GRT), ("krtt", g.KRTT)):
            if nm in g.dbg:
                p.dma(None, g.dbg[nm], src, q='sp')


def phase_na(g, l, do_ctx):
    p = g.p
    Lw = g.lay[l]
    with p.phase():
        bias = p.sb("na_bias", [128, 4, 25, 128], BF16)
        with p.phase():
            stg = p.sb("na_bstg", [128, 25, 128], F32)
            msk = p.sb("na_msk", [128, 25, 128], F32)
            p.dma(msk, msk[:], g.cst["NA_MASK"].rearrange("p a b q -> p (a b) q"))
            for h in range(4):
                p.dma(stg, stg[:], Lw["nab"][:, h].rearrange("p a b q -> p (a b) q"))
                p.op('dve', lambda e, h=h: e.tensor_tensor(out=bias[:, h], in0=stg[:], in1=msk[:], op=ALU.add),
                     r=[stg, msk], w=[bias])
        V = p.sb("na_v", [128, NTILE, 260], BF16)
        p.dma(V, V[:], g.VNA.rearrange("(a p) h d -> p a (h d)", p=128))
        qT = [p.sb("na_q%d" % i, [128, NT], BF16) for i in range(2)]
        kT = [p.sb("na_k%d" % i, [128, NT], BF16) for i in range(2)]
        for i in range(2):
            p.dma(qT[i], qT[i][:], g.FMS[C_QNA + i])
            p.dma(kT[i], kT[i][:], g.FMS[C_KNA + i])
        psS = [[p.ps("na_s%d%d" % (i, j), [128, 512], F32) for j in range(2)] for i in range(2)]
        pso = [p.ps("na_o%d" % i, [128, 512], F32) for i in range(2)]
        PT = [p.sb("na_pt%d" % i, [128, 8, 128], BF16) for i in range(2)]
        ys = [p.sb("na_ys%d" % i, [128, 256], BF16) for i in range(2)]
        rc = [p.sb("na_rc%d" % i, [128, 1], F32) for i in range(2)]
        tiles = []
        if do_ctx:
            for q0 in (0, 128):
                tiles.append((q0, [(0, None), (128, None)], 0))
        for rp in range(32):
            r = 2 * rp
            kr0 = min(max(r - 4, 0), 54)
            pat = {0: 0, 2: 1, 60: 3, 62: 4}.get(r, 2)
            ch = [(LC + (kr0 + 2 * m) * 64, m) for m in range(5)] + [(0, None), (128, None)]
            tiles.append((LC + rp * 128, ch, pat))
        it = 0
        for qi, (q0, ch, pat) in enumerate(tiles):
            y_ = ys[qi % 2]
            for h in range(4):
                hp, base = h // 2, (h % 2) * 64
                sA, sB = psS[it % 2]
                po, pt, rc_ = pso[it % 2], PT[it % 2], rc[it % 2]
                it += 1
                n = len(ch)
                for i, (tok0, m) in enumerate(ch):
                    bank = sA if i < 4 else sB
                    col = (i % 4) * 128
                    p.op('pe', lambda e, bank=bank, col=col, tok0=tok0, m=m: e.matmul(
                        bank[:, col:col + 128], lhsT=kT[hp][base:base + 64, tok0:tok0 + 128],
                        rhs=qT[hp][base:base + 64, q0:q0 + 128], start=True, stop=(m is None)),
                        r=[kT[hp], qT[hp]], w=[bank])
                    if m is not None:
                        p.op('pe', lambda e, bank=bank, col=col, m=m: e.matmul(
                            bank[:, col:col + 128], lhsT=g.ident[:], rhs=bias[:, h, pat * 5 + m, :],
                            start=False, stop=True), r=[g.ident, bias], w=[bank])
                na_ = min(n, 4)
                p.op('act', lambda e: e.activation(out=pt[:, 0:na_, :], in_=sA[:, 0:na_ * 128].rearrange("p (a q) -> p a q", q=128),
                                                   func=AF.Exp), r=[sA], w=[pt])
                if n > 4:
                    p.op('act', lambda e: e.activation(out=pt[:, 4:n, :], in_=sB[:, 0:(n - 4) * 128].rearrange("p (a q) -> p a q", q=128),
                                                       func=AF.Exp), r=[sB], w=[pt])
                for i, (tok0, m) in enumerate(ch):
                    vt = tok0 // 128
                    p.op('pe', lambda e, i=i, vt=vt: e.matmul(po[:, 0:65], lhsT=pt[:, i, :], rhs=V[:, vt, h * 65:(h + 1) * 65],
                                                              start=(i == 0), stop=(i == n - 1)), r=[pt, V], w=[po])
                p.op('dve', lambda e: e.reciprocal(out=rc_[:], in_=po[:, 64:65]), r=[po], w=[rc_])
                p.op('dve', lambda e: e.tensor_scalar(out=y_[:, h * 64:(h + 1) * 64], in0=po[:, 0:64], scalar1=rc_[:, 0:1],
                                                      scalar2=None, op0=ALU.mult), r=[po, rc_], w=[y_])
            p.dma(None, g.Y[q0:q0 + 128, 0:256], y_[:], r=[y_], q='pool')


def phase_diff(g, l, do_ctx):
    p = g.p
    Lw = g.lay[l]
    import math
    lam_init = 0.8 - 0.6 * math.exp(-0.3 * l)
    with p.phase():
        lp = p.sb("df_lp", [128, 4, 32], F32)
        pr = p.sb("df_pr", [128, 2, 32], F32)
        sm = p.sb("df_sm", [128, 4], F32)
        neglam = p.sb("df_nl", [128, 1], F32)
        gsub = p.sb("df_gs", [128, 64], F32)
        p.dma(lp, lp[:].rearrange("p a b -> p (a b)"), Lw["lam"].broadcast_to([128, 128]))
        load_bcast(p, gsub, Lw["subg"], 64)
        p.op('dve', lambda e: e.tensor_scalar(out=gsub[:], in0=gsub[:], scalar1=1.0 - lam_init, scalar2=None, op0=ALU.mult),
             r=[gsub], w=[gsub])
        p.op('dve', lambda e: e.tensor_tensor(out=pr[:], in0=lp[:, 0:4:2, :], in1=lp[:, 1:4:2, :], op=ALU.mult), r=[lp], w=[pr])
        p.op('dve', lambda e: e.tensor_reduce(out=sm[:, 0:2], in_=pr[:], axis=AX.X, op=ALU.add), r=[pr], w=[sm])
        p.op('act', lambda e: e.activation(out=sm[:, 2:4], in_=sm[:, 0:2], func=AF.Exp), r=[sm], w=[sm])
        p.op('dve', lambda e: e.tensor_tensor(out=neglam[:], in0=sm[:, 3:4], in1=sm[:, 2:3], op=ALU.subtract), r=[sm], w=[neglam])
        p.op('dve', lambda e: e.tensor_scalar(out=neglam[:], in0=neglam[:], scalar1=-lam_init, scalar2=None, op0=ALU.add),
             r=[neglam], w=[neglam])
        V = p.sb("df_v", [128, NTILE, 260], BF16)
        p.dma(V, V[:], g.VDF.rearrange("(a p) h d -> p a (h d)", p=128))
        qT = [p.sb("df_q%d" % i, [128, NT], BF16) for i in range(2)]
        kA = [p.sb("df_ka%d" % i, [128, NT], BF16) for i in range(2)]
        kB = [p.sb("df_kb%d" % i, [128, NT], BF16) for i in range(2)]
        for i in range(2):
            p.dma(qT[i], qT[i][:], g.FMS[C_QDF + i])
            p.dma(kA[i], kA[i][:], g.FMS[C_KDA + i])
            p.dma(kB[i], kB[i][:], g.FMS[C_KDB + i])
        acc = [p.ps("df_acc%d" % i, [128, 512], F32) for i in range(4)]
        psS = [p.ps("df_s%d" % i, [128, 512], F32) for i in range(2)]
        PT = [p.sb("df_pt%d" % i, [128, 512], BF16) for i in range(2)]
        oc = [p.sb("df_oc%d" % i, [128, 4, 64], F32) for i in range(2)]
        o = p.sb("df_o", [128, 4, 64], F32)
        sq = p.sb("df_sq", [128, 4, 64], F32)
        ss = p.sb("df_ss", [128, 8], F32)
        rc = p.sb("df_rc", [128, 4], F32)
        yd = [p.sb("df_y%d" % i, [128, 4, 64], BF16) for i in range(2)]
        groups = []
        if do_ctx:
            groups.append((0, 256, [0, 1]))
        for gi in range(8):
            groups.append((LC + gi * 512, 512, list(range(NTILE))))
        it = 0
        gi_ = 0
        for h in range(4):
            hp, base = h // 2, (h % 2) * 64
            for (t0, n, kcs) in groups:
                ns = n // 128
                for c in range(2):
                    kX = (kA if c == 0 else kB)[hp]

                    def qk(ki):
                        kc = kcs[ki]
                        ps_ = psS[ki % 2]
                        p.op('pe', lambda e: e.matmul(
                            ps_[:, 0:n], lhsT=kX[base:base + 64, kc * 128:(kc + 1) * 128], rhs=qT[hp][base:base + 64, t0:t0 + n],
                            start=True, stop=True), r=[kX, qT[hp]], w=[ps_])

                    def ex(ki):
                        ps_, pt = psS[ki % 2], PT[ki % 2]
                        p.op('act', lambda e: e.activation(out=pt[:, 0:n], in_=ps_[:, 0:n], func=AF.Exp), r=[ps_], w=[pt])

                    def pv(ki):
                        kc = kcs[ki]
                        pt = PT[ki % 2]
                        for s in range(ns):
                            p.op('pe', lambda e, s=s: e.matmul(
                                acc[s][:, 0:65], lhsT=pt[:, s * 128:(s + 1) * 128], rhs=V[:, kc, h * 65:(h + 1) * 65],
                                start=(ki == 0), stop=(ki == len(kcs) - 1)), r=[pt, V], w=[acc[s]])

                    qk(0)
                    ex(0)
                    for ki in range(len(kcs)):
                        if ki + 1 < len(kcs):
                            qk(ki + 1)
                            ex(ki + 1)
                        pv(ki)
                    for s in range(ns):
                        p.op('dve', lambda e, s=s: e.reciprocal(out=rc[:, s:s + 1], in_=acc[s][:, 64:65]), r=[acc[s]], w=[rc])
                        p.op('dve', lambda e, s=s, c=c: e.tensor_scalar(out=oc[c][:, s, :], in0=acc[s][:, 0:64], scalar1=rc[:, s:s + 1],
                                                                       scalar2=None, op0=ALU.mult), r=[acc[s], rc], w=[oc[c]])
                y_ = yd[gi_ % 2]
                gi_ += 1
                p.op('dve', lambda e: e.scalar_tensor_tensor(out=o[:, 0:ns, :], in0=oc[1][:, 0:ns, :], scalar=neglam[:, 0:1],
                                                             in1=oc[0][:, 0:ns, :], op0=ALU.mult, op1=ALU.add),
                     r=[oc[0], oc[1], neglam], w=[o])
                p.op('dve', lambda e: e.tensor_tensor(out=sq[:, 0:ns, :], in0=o[:, 0:ns, :], in1=o[:, 0:ns, :], op=ALU.mult), r=[o], w=[sq])
                p.op('dve', lambda e: e.tensor_reduce(out=ss[:, 0:ns], in_=sq[:, 0:ns, :], axis=AX.X, op=ALU.add), r=[sq], w=[ss])
                p.op('dve', lambda e: e.tensor_scalar(out=ss[:, 0:ns], in0=ss[:, 0:ns], scalar1=1.0 / 64, scalar2=EPS,
                                                      op0=ALU.mult, op1=ALU.add), r=[ss], w=[ss])
                p.op('act', lambda e: e.activation(out=ss[:, 4:4 + ns], in_=ss[:, 0:ns], func=AF.Sqrt), r=[ss], w=[ss])
                p.op('dve', lambda e: e.reciprocal(out=ss[:, 0:ns], in_=ss[:, 4:4 + ns]), r=[ss], w=[ss])
                p.op('dve', lambda e: e.tensor_tensor(out=sq[:, 0:ns, :], in0=o[:, 0:ns, :],
                                                      in1=ss[:, 0:ns].unsqueeze(2).broadcast_to([128, ns, 64]), op=ALU.mult),
                     r=[o, ss], w=[sq])
                p.op('dve', lambda e: e.tensor_tensor(out=y_[:, 0:ns, :], in0=sq[:, 0:ns, :],
                                                      in1=gsub[:].unsqueeze(1).broadcast_to([128, ns, 64]), op=ALU.mult),
                     r=[sq, gsub], w=[y_])
                p.dma(None, g.Y[t0:t0 + n, 256 + h * 64:256 + (h + 1) * 64].rearrange("(s p) d -> p s d", p=128),
                      y_[:, 0:ns, :], r=[y_], q='pool')


def phase_ret(g, l, do_ctx):
    p = g.p
    Lw = g.lay[l]
    with p.phase():
        dec = p.sb("rt_dec", [128, 8], F32)
        lg = p.sb("rt_lg", [128, 8], F32)
        cdec = p.sb("rt_cdec", [128, 8], F32)
        kdec = p.sb("rt_kdec", [128, 8], F32)
        jcol = p.sb("rt_jcol", [128, 2], F32)
        cm = {}
        for nm in ("RT_RIJ", "RT_MIJ", "RT_RJI", "RT_MJI", "RT_IROW", "RT_IROWB"):
            cm[nm] = p.sb("c_" + nm, [128, 128], F32)
            p.dma(cm[nm], cm[nm][:], g.cst[nm])
        p.dma(jcol, jcol[:], g.cst["RT_JCOL"])
        load_bcast(p, dec, Lw["dec"], 8)
        p.op('act', lambda e: e.activation(out=lg[:], in_=dec[:], func=AF.Sigmoid), r=[dec], w=[lg])
        p.op('act', lambda e: e.activation(out=lg[:], in_=lg[:], func=AF.Ln), r=[lg], w=[lg])
        p.op('act', lambda e: e.activation(out=cdec[:], in_=lg[:], func=AF.Exp, scale=128.0), r=[lg], w=[cdec])
        QDF = p.sb("rt_qdf", [128, 4, 128], F32)
        QDB = p.sb("rt_qdb", [128, 4, 128], F32)
        DB = p.sb("rt_db", [128, 4, 128], F32)
        t1 = p.sb("rt_t1", [128, 128], F32)
        t2 = p.sb("rt_t2", [128, 128], F32)
        for d_ in range(2):
            for h in range(4):
                c = d_ * 4 + h
                p.op('act', lambda e, c=c, d_=d_: e.activation(out=kdec[:, c:c + 1], in_=jcol[:, d_:d_ + 1], func=AF.Exp,
                                                               scale=lg[:, c:c + 1]), r=[jcol, lg], w=[kdec])
        for h in range(4):
            p.op('act', lambda e, h=h: e.activation(out=QDF[:, h, :], in_=cm["RT_IROW"][:], func=AF.Exp, scale=lg[:, h:h + 1]),
                 r=[cm["RT_IROW"], lg], w=[QDF])
            p.op('act', lambda e, h=h: e.activation(out=QDB[:, h, :], in_=cm["RT_IROWB"][:], func=AF.Exp, scale=lg[:, 4 + h:5 + h]),
                 r=[cm["RT_IROWB"], lg], w=[QDB])
            p.op('act', lambda e, h=h: e.activation(out=t1[:], in_=cm["RT_RIJ"][:], func=AF.Exp, scale=lg[:, h:h + 1]),
                 r=[cm["RT_RIJ"], lg], w=[t1])
            p.op('act', lambda e, h=h: e.activation(out=t2[:], in_=cm["RT_RJI"][:], func=AF.Exp, scale=lg[:, 4 + h:5 + h]),
                 r=[cm["RT_RJI"], lg], w=[t2])
            p.op('dve', lambda e: e.tensor_tensor(out=t1[:], in0=t1[:], in1=cm["RT_MIJ"][:], op=ALU.mult), r=[t1, cm["RT_MIJ"]], w=[t1])
            p.op('dve', lambda e: e.tensor_tensor(out=t2[:], in0=t2[:], in1=cm["RT_MJI"][:], op=ALU.mult), r=[t2, cm["RT_MJI"]], w=[t2])
            p.op('dve', lambda e, h=h: e.tensor_tensor(out=DB[:, h, :], in0=t1[:], in1=t2[:], op=ALU.add), r=[t1, t2], w=[DB])
        qT = p.sb("rt_q", [64, NT], BF16)
        kT = p.sb("rt_k", [64, NT], BF16)
        Ktm = p.sb("rt_ktm", [128, NTILE, 64], BF16)
        V = p.sb("rt_v", [128, NTILE, 128], BF16)
        G = p.sb("rt_g", [128, NTILE, 128], BF16)
        KF = p.sb("rt_kf", [128, NTILE, 64], BF16)
        KB = p.sb("rt_kb", [128, NTILE, 64], BF16)
        SFa = p.sb("rt_sfa", [64, NTILE, 128], BF16)
        SBa = p.sb("rt_sba", [64, NTILE, 128], BF16)
        S = [p.sb("rt_S%d" % i, [64, 128], F32) for i in range(2)]
        psKV = [p.ps("rt_kv%d" % i, [128, 512], F32) for i in range(2)]
        psA = [p.ps("rt_att%d" % i, [128, 512], F32) for i in range(2)]
        psO = [p.ps("rt_o%d" % i, [128, 512], F32) for i in range(2)]
        attm = [p.sb("rt_attm%d" % i, [128, 128], BF16) for i in range(2)]
        qf = [p.sb("rt_qf%d" % i, [64, 128], BF16) for i in range(2)]
        qb = [p.sb("rt_qb%d" % i, [64, 128], BF16) for i in range(2)]
        junk = p.sb("rt_junk", [128, 128], F32)
        st = [p.sb("rt_st%d" % i, [128, 4], F32) for i in range(2)]
        ys = [p.sb("rt_ys%d" % i, [128, 128], BF16) for i in range(2)]
        it = 0
        for h in range(4):
            hp, base = h // 2, (h % 2) * 64
            p.dma(qT, qT[:], g.FMS[C_QRT + hp, base:base + 64, :])
            p.dma(kT, kT[:], g.FMS[C_KRT + hp, base:base + 64, :])
            p.dma(Ktm, Ktm[:], g.KRTT[:, h * 64:(h + 1) * 64].rearrange("(a p) d -> p a d", p=128))
            p.dma(V, V[:], g.VRT[:, h * 128:(h + 1) * 128].rearrange("(a p) d -> p a d", p=128))
            p.dma(G, G[:], g.GRT[:, h * 128:(h + 1) * 128].rearrange("(a p) d -> p a d", p=128))
            p.op('dve', lambda e: e.tensor_scalar(out=KF[:], in0=Ktm[:], scalar1=kdec[:, h:h + 1], scalar2=None, op0=ALU.mult),
                 r=[Ktm, kdec], w=[KF])
            p.op('dve', lambda e: e.tensor_scalar(out=KB[:], in0=Ktm[:], scalar1=kdec[:, 4 + h:5 + h], scalar2=None, op0=ALU.mult),
                 r=[Ktm, kdec], w=[KB])
            for d_, (Kd, Sa, order) in enumerate(((KF, SFa, list(range(NTILE))),
                                                  (KB, SBa, [1, 0] + list(range(NTILE - 1, 1, -1))))):
                S_ = S[d_]
                p.op('pool', lambda e: e.memset(S_[:], 0.0), w=[S_])
                cd = cdec[0:64, d_ * 4 + h:d_ * 4 + h + 1]
                for oi, c in enumerate(order):
                    p.op('act', lambda e, c=c: e.copy(out=Sa[:, c, :], in_=S_[:]), r=[S_], w=[Sa])
                    if oi == len(order) - 1:
                        break
                    kv = psKV[it % 2]
                    it += 1
                    p.op('pe', lambda e, c=c, kv=kv: e.matmul(kv[0:64, 0:128], lhsT=Kd[:, c, :], rhs=V[:, c, :], start=True, stop=True),
                         r=[Kd, V], w=[kv])
                    p.op('dve', lambda e, kv=kv: e.scalar_tensor_tensor(out=S_[:], in0=S_[:], scalar=cd, in1=kv[0:64, 0:128],
                                                                        op0=ALU.mult, op1=ALU.add), r=[S_, cdec, kv], w=[S_])
            for c in (range(NTILE) if do_ctx else range(2, NTILE)):
                tok = c * 128
                pa, po = psA[c % 2], psO[c % 2]
                am, qf_, qb_, st_, y_ = attm[c % 2], qf[c % 2], qb[c % 2], st[c % 2], ys[c % 2]
                p.op('pe', lambda e: e.matmul(pa[:, 0:128], lhsT=kT[:, tok:tok + 128], rhs=qT[:, tok:tok + 128], start=True, stop=True),
                     r=[kT, qT], w=[pa])
                p.op('dve', lambda e: e.tensor_tensor(out=am[:], in0=pa[:, 0:128], in1=DB[:, h, :], op=ALU.mult), r=[pa, DB], w=[am])
                p.op('pool', lambda e: e.tensor_tensor(out=qf_[:], in0=qT[:, tok:tok + 128], in1=QDF[0:64, h, :], op=ALU.mult),
                     r=[qT, QDF], w=[qf_])
                p.op('pool', lambda e: e.tensor_tensor(out=qb_[:], in0=qT[:, tok:tok + 128], in1=QDB[0:64, h, :], op=ALU.mult),
                     r=[qT, QDB], w=[qb_])
                p.op('pe', lambda e: e.matmul(po[:, 0:128], lhsT=am[:], rhs=V[:, c, :], start=True, stop=False), r=[am, V], w=[po])
                p.op('pe', lambda e: e.matmul(po[:, 0:128], lhsT=qf_[:], rhs=SFa[:, c, :], start=False, stop=False), r=[qf_, SFa], w=[po])
                p.op('pe', lambda e: e.matmul(po[:, 0:128], lhsT=qb_[:], rhs=SBa[:, c, :], start=False, stop=True), r=[qb_, SBa], w=[po])
                p.op('act', lambda e: e.activation(out=junk[:], in_=po[:, 0:128], func=AF.Square, accum_out=st_[:, 0:1]),
                     r=[po], w=[junk, st_])
                p.op('dve', lambda e: e.tensor_scalar(out=st_[:, 1:2], in0=st_[:, 0:1], scalar1=1.0 / 128, scalar2=EPS,
                                                      op0=ALU.mult, op1=ALU.add), r=[st_], w=[st_])
                p.op('act', lambda e: e.activation(out=st_[:, 2:3], in_=st_[:, 1:2], func=AF.Sqrt), r=[st_], w=[st_])
                p.op('dve', lambda e: e.reciprocal(out=st_[:, 3:4], in_=st_[:, 2:3]), r=[st_], w=[st_])
                p.op('dve', lambda e: e.scalar_tensor_tensor(out=y_[:], in0=po[:, 0:128], scalar=st_[:, 3:4], in1=G[:, c, :],
                                                             op0=ALU.mult, op1=ALU.mult), r=[po, st_, G], w=[y_])
                p.dma(None, g.Y[tok:tok + 128, 512 + h * 128:512 + (h + 1) * 128], y_[:], r=[y_], q='pool')


def phase_p3(g, l, xa, last):
    p = g.p
    Lw = g.lay[l]
    with p.phase():
        wob = p.sb("wob", [128, 8, D], BF16)
        with p.phase():
            wof = p.sb("wof", [128, 8, 512], F32)
            wsrc = Lw["wout"].rearrange("(k p) n -> p k n", p=128)
            for n in range(2):
                p.dma(wof, wof[:], wsrc[:, :, n * 512:(n + 1) * 512])
                p.op('dve', lambda e, n=n: e.tensor_copy(out=wob[:, :, n * 512:(n + 1) * 512], in_=wof[:]), r=[wof], w=[wob])
        gt = []
        for which in range(2):
            t = p.sb("p3_gt%d" % which, [128, D], F32)
            load_bcast(p, t, mod_row(g, which, 2), D)
            gt.append(t)
        nrm = make_norm_consts(g, "n2", Lw["g2"], 3, 4)
        yt = [p.sb("p3_y%d" % i, [128, D], BF16) for i in range(2)]
        xt = [p.sb("p3_x%d" % i, [128, D], F32) for i in range(2)]
        x1 = [p.sb("p3_x1%d" % i, [128, D], F32) for i in range(2)]
        yT = [p.sb("p3_yT%d" % i, [128, 8, 128], BF16) for i in range(2)]
        hT = [p.sb("p3_hT%d" % i, [128, 8, 128], BF16) for i in range(2)]
        hb = [p.sb("p3_hb%d" % i, [128, D], BF16) for i in range(2)]
        tmp = p.sb("p3_tmp", [128, D], F32)
        st = [p.sb("p3_st%d" % i, [128, 4], F32) for i in range(2)]
        pstY = [p.ps("p3_pty%d" % i, [128, 8, 128], BF16) for i in range(2)]
        pstH = [p.ps("p3_pth%d" % i, [128, 8, 128], BF16) for i in range(2)]
        psO = [[p.ps("p3_o%d%d" % (i, n), [128, 512], F32) for n in range(2)] for i in range(2)]
        for ti, tt in enumerate(range(2, NTILE) if last else range(NTILE)):
            which = 1 if tt < 2 else 0
            b = ti % 2
            sl = slice(tt * 128, (tt + 1) * 128)
            p.dma(yt[b], yt[b][:], g.Y[sl, :])
            p.dma(xt[b], xt[b][:], xa[sl, :])
            emit_transposes(g, yt[b], pstY[b], yT[b][:], yT[b])
            for n in range(2):
                po = psO[b][n]
                for k in range(8):
                    p.op('pe', lambda e, k=k, n=n, po=po: e.matmul(po[:], lhsT=yT[b][:, k, :], rhs=wob[:, k, n * 512:(n + 1) * 512],
                                                                  start=(k == 0), stop=(k == 7)), r=[yT[b], wob], w=[po])
                cs = slice(n * 512, (n + 1) * 512)
                p.op('dve', lambda e, po=po, cs=cs: e.tensor_tensor(out=tmp[:, cs], in0=po[:], in1=gt[which][:, cs], op=ALU.mult),
                     r=[po, gt[which]], w=[tmp])
                p.op('pool', lambda e, cs=cs: e.tensor_tensor(out=x1[b][:, cs], in0=tmp[:, cs], in1=xt[b][:, cs], op=ALU.add),
                     r=[tmp, xt[b]], w=[x1[b]])
            p.dma(None, g.X1[sl, :], x1[b][:], r=[x1[b]], q='pool')
            emit_norm(g, x1[b], nrm[which][0], nrm[which][1], hb[b], tmp, st[b])
            emit_transposes(g, hb[b], pstH[b], hT[b][:], hT[b])
            p.dma(None, g.H2T[:, :, sl], hT[b][:], r=[hT[b]], q='pool')


def phase_cast(g, l):
    p = g.p
    Lw = g.lay[l]
    with p.phase():
        f = [p.sb("cs_f%d" % i, [128, 2048], F32) for i in range(3)]
        b = [p.sb("cs_b%d" % i, [128, 2048], BF16) for i in range(3)]
        engs = ['act', 'dve', 'pool']
        it = 0
        usrc = Lw["ut"].rearrange("(k p) e -> p k e", p=128)
        vsrc = Lw["v"].rearrange("(i p) d -> p i d", p=128)
        for k in range(8):
            for ec in range(8):
                i = it % 3
                it += 1
                p.dma(f[i], f[i][:], usrc[:, k, ec * 2048:(ec + 1) * 2048])
                if engs[i] == 'act':
                    p.op('act', lambda e, i=i: e.copy(out=b[i][:], in_=f[i][:]), r=[f[i]], w=[b[i]])
                else:
                    p.op(engs[i], lambda e, i=i: e.tensor_copy(out=b[i][:], in_=f[i][:]), r=[f[i]], w=[b[i]])
                p.dma(None, g.UTB[:, k, ec * 2048:(ec + 1) * 2048], b[i][:], r=[b[i]], q='pool')
        for ic in range(64):
            i = it % 3
            it += 1
            p.dma(f[i], f[i][:].rearrange("p (a d) -> p a d", a=2), vsrc[:, ic * 2:(ic + 1) * 2, :])
            if engs[i] == 'act':
                p.op('act', lambda e, i=i: e.copy(out=b[i][:], in_=f[i][:]), r=[f[i]], w=[b[i]])
            else:
                p.op(engs[i], lambda e, i=i: e.tensor_copy(out=b[i][:], in_=f[i][:]), r=[f[i]], w=[b[i]])
            p.dma(None, g.VB[:, ic * 2:(ic + 1) * 2, :], b[i][:].rearrange("p (a d) -> p a d", a=2), r=[b[i]], q='pool')


NI = 8


def phase_peerq(g, l, last):
    p = g.p
    Lw = g.lay[l]
    with p.phase():
        wq = p.sb("pq_wq", [128, 8, 2048], BF16)
        with p.phase():
            wqf = p.sb("pq_wqf", [128, 8, 512], F32)
            wsrc = Lw["wq"].rearrange("(k p) n -> p k n", p=128)
            for n in range(4):
                p.dma(wqf, wqf[:], wsrc[:, :, n * 512:(n + 1) * 512])
                p.op('dve', lambda e, n=n: e.tensor_copy(out=wq[:, :, n * 512:(n + 1) * 512], in_=wqf[:]), r=[wqf], w=[wq])
        ht = [p.sb("pq_ht%d" % i, [128, 8, 512], BF16) for i in range(2)]
        stg = [p.sb("pq_st%d" % i, [128, 4, 512], BF16) for i in range(2)]
        ps = [p.ps("pq_ps%d" % i, [128, 512], F32) for i in range(4)]
        it = 0
        for gi, (t0, n) in enumerate(tok_groups()):
            if last and t0 < LC:
                continue
            h_ = ht[gi % 2]
            p.dma(h_, h_[:, :, 0:n], g.H2T[:, :, t0:t0 + n])
            for jq in range(4):
                st_ = stg[(gi * 4 + jq) % 2]
                for jj in range(4):
                    j = jq * 4 + jj
                    ps_ = ps[it % 4]
                    it += 1
                    for k in range(8):
                        p.op('pe', lambda e, k=k, j=j, ps_=ps_: e.matmul(ps_[:, 0:n], lhsT=wq[:, k, j * 128:(j + 1) * 128], rhs=h_[:, k, 0:n],
                                                                        start=(k == 0), stop=(k == 7)), r=[wq, h_], w=[ps_])
                    if jj % 2 == 0:
                        p.op('act', lambda e, jj=jj, ps_=ps_: e.copy(out=st_[:, jj, 0:n], in_=ps_[:, 0:n]), r=[ps_], w=[st_])
                    else:
                        p.op('dve', lambda e, jj=jj, ps_=ps_: e.tensor_copy(out=st_[:, jj, 0:n], in_=ps_[:, 0:n]), r=[ps_], w=[st_])
                p.dma(None, g.QPT[:, jq * 4:(jq + 1) * 4, t0:t0 + n], st_[:, :, 0:n], r=[st_], q='pool')


def phase_peer(g, l, last):
    p = g.p
    Lw = g.lay[l]
    with p.phase():
        skt = p.sb("pe_skt", [128, 16, 128], BF16)
        with p.phase():
            sktf = p.sb("pe_sktf", [128, 16, 128], F32)
            p.dma(sktf, sktf[:], Lw["skt"])
            p.op('dve', lambda e: e.tensor_copy(out=skt[:], in_=sktf[:]), r=[sktf], w=[skt])
        gt = []
        for which in range(2):
            t = p.sb("pe_gt%d" % which, [128, D], F32)
            load_bcast(p, t, mod_row(g, which, 5), D)
            gt.append(t)
        if last:
            fg = p.sb("pe_fg", [128, D], F32)
            load_bcast(p, fg, g.final_g, D)
        h2t = [p.sb("pe_h2t%d" % i, [128, 8, 256], BF16) for i in range(2)]
        E = [p.sb("pe_E%d" % i, [128, 16, 128], F32) for i in range(2)]
        Tt = [p.sb("pe_T%d" % i, [128, 8], F32) for i in range(2)]
        kap = [p.sb("pe_kap%d" % i, [128, 8], F32) for i in range(2)]
        Dk = [[p.sb("pe_dk%d_%d" % (i, h), [128, 128], BF16) for h in range(8)] for i in range(2)]
        acc = [[p.ps("pe_acc%d%d" % (i, n), [128, 512], F32) for n in range(2)] for i in range(2)]
        psA = [p.ps("pe_A%d" % i, [128, 512], F32) for i in range(2)]
        psW = p.ps("pe_W", [128, 512], F32)
        psT_full = p.ps("pe_T", [128, 8, 128], BF16)
        psT = [T("pe_T%d" % i, psT_full[:, i * 4:(i + 1) * 4, :]) for i in range(2)]
        psM = psA[1]
        pairs = list(range(1, NTILE // 2)) if last else list(range(NTILE // 2))
        for pi, pr in enumerate(pairs):
            t0 = pr * 256
            which = 1 if pr == 0 else 0
            ht = h2t[pi % 2]
            p.dma(ht, ht[:], g.H2T[:, :, t0:t0 + 256])
            with p.phase():
                qTb = p.sb("pe_qT", [128, 16, 256], BF16)
                p.dma(qTb, qTb[:], g.QPT[:, :, t0:t0 + 256])
                S_all = [p.sb("pe_S%d" % i, [128, 16, 128], F32) for i in range(2)]
                m8 = p.sb("pe_m8", [128, 16, 16], F32)
                sr = p.sb("pe_sr", [128, 16, 128], F32)
                sd = p.sb("pe_sd", [128, 16, 128], F32)
                md = p.sb("pe_md", [128, 16, 16], F32)
                et = p.sb("pe_et", [128, 16, 16], F32)
                cand = p.sb("pe_cand", [128, 8, 256], F32)
                cr = p.sb("pe_cr", [128, 8, 256], F32)
                c16 = p.sb("pe_c16", [128, 8, 16], F32)
                zz = p.sb("pe_zz", [128, 8], F32)
                for sub in range(2):
                    S_ = S_all[sub]
                    for jq in range(4):
                        for jj in range(4):
                            j = jq * 4 + jj
                            p.op('pe', lambda e, j=j, jj=jj: e.matmul(psM[:, jj * 128:(jj + 1) * 128],
                                                                      lhsT=qTb[:, j, sub * 128:(sub + 1) * 128], rhs=skt[:, j, :],
                                                                      start=True, stop=True), r=[qTb, skt], w=[psM])
                        p.op('act', lambda e, jq=jq: e.copy(out=S_[:, jq * 4:(jq + 1) * 4, :],
                                                           in_=psM[:].rearrange("p (a n) -> p a n", a=4)), r=[psM], w=[S_])
                    for j in range(16):
                        p.op('dve', lambda e, j=j: e.max(out=m8[:, j, 0:8], in_=S_[:, j, :]), r=[S_], w=[m8])
                    for j in range(16):
                        p.op('dve', lambda e, j=j: e.match_replace(out=sr[:, j, :], in_to_replace=m8[:, j, 0:8], in_values=S_[:, j, :],
                                                                   imm_value=-1e30), r=[S_, m8], w=[sr])
                    for j in range(16):
                        p.op('dve', lambda e, j=j: e.max(out=m8[:, j, 8:16], in_=sr[:, j, :]), r=[sr], w=[m8])
                    p.op('dve', lambda e: e.tensor_tensor(out=sd[:], in0=S_[:], in1=m8[:, :, 0:1].broadcast_to([128, 16, 128]),
                                                          op=ALU.subtract), r=[S_, m8], w=[sd])
                    p.op('dve', lambda e: e.tensor_tensor(out=md[:], in0=m8[:], in1=m8[:, :, 0:1].broadcast_to([128, 16, 16]),
                                                          op=ALU.subtract), r=[m8], w=[md])
                    p.op('act', lambda e: e.activation(out=E[sub][:], in_=sd[:], func=AF.Exp), r=[sd], w=[E[sub]])
                    p.op('act', lambda e: e.activation(out=et[:], in_=md[:], func=AF.Exp), r=[md], w=[et])
                    et4 = et[:].rearrange("p (h c) a -> p h c a", c=2)
                    p.op('dve', lambda e: e.tensor_tensor(out=cand[:].rearrange("p h (a b) -> p h a b", a=16),
                                                          in0=et4[:, :, 0, :].unsqueeze(3).broadcast_to([128, 8, 16, 16]),
                                                          in1=et4[:, :, 1, :].unsqueeze(2).broadcast_to([128, 8, 16, 16]), op=ALU.mult),
                         r=[et], w=[cand])
                    for h in range(8):
                        p.op('dve', lambda e, h=h: e.max(out=c16[:, h, 0:8], in_=cand[:, h, :]), r=[cand], w=[c16])
                    for h in range(8):
                        p.op('dve', lambda e, h=h: e.match_replace(out=cr[:, h, :], in_to_replace=c16[:, h, 0:8], in_values=cand[:, h, :],
                                                                   imm_value=-1e30), r=[cand, c16], w=[cr])
                    for h in range(8):
                        p.op('dve', lambda e, h=h: e.max(out=c16[:, h, 8:16], in_=cr[:, h, :]), r=[cr], w=[c16])
                    p.op('dve', lambda e: e.tensor_scalar(out=Tt[sub][:], in0=c16[:, :, 15], scalar1=1.0 - 1e-6, scalar2=None, op0=ALU.mult),
                         r=[c16], w=[Tt[sub]])
                    p.op('dve', lambda e: e.tensor_reduce(out=zz[:], in_=c16[:], axis=AX.X, op=ALU.add), r=[c16], w=[zz])
                    p.op('dve', lambda e: e.reciprocal(out=kap[sub][:], in_=zz[:]), r=[zz], w=[kap[sub]])
                    for h in range(8):
                        p.op('dve', lambda e, h=h: e.tensor_scalar(out=Dk[sub][h][:], in0=g.ident_f[:], scalar1=kap[sub][:, h:h + 1],
                                                                   scalar2=None, op0=ALU.mult), r=[g.ident_f, kap[sub]], w=[Dk[sub][h]])
            with p.phase():
                Pt = [p.sb("pe_P%d" % i, [128, NI, 128], F32) for i in range(3)]
                G = [[p.sb("pe_G%d_%d" % (b_, i), [128, 8, NI * 128], BF16) for i in range(2)] for b_ in range(2)]
                ut = [p.sb("pe_ut%d" % i, [128, 8, 512], BF16) for i in range(2)]
                vb = [p.sb("pe_vb%d" % i, [128, 4, D], BF16) for i in range(2)]
                ga = [p.sb("pe_ga%d" % i, [128, 512], F32) for i in range(2)]
                wg = [p.sb("pe_wg%d" % i, [128, 512], BF16) for i in range(2)]
                wgT = [p.sb("pe_wgT%d" % i, [128, 4, 128], BF16) for i in range(2)]
                xs = [p.sb("pe_x%d" % i, [128, D], F32) for i in range(2)]
                xo = [p.sb("pe_xo%d" % i, [128, D], F32) for i in range(2)]
                if last:
                    hb = p.sb("pe_hb", [128, D], F32)
                    st = p.sb("pe_st", [128, 4], F32)
                nblk = 128 // NI
                nesb = NI * 128 // 512
                cnt = {"p": 0}

                def unit(ib, sub, h):
                    P_ = Pt[cnt["p"] % 3]
                    cnt["p"] += 1
                    G_ = G[ib % 2][sub]
                    if h % 2 == 1:
                        for ii in range(NI):
                            i_ = ib * NI + ii
                            p.op('act', lambda e, ii=ii, i_=i_: e.activation(out=P_[:, ii, :], in_=E[sub][:, 2 * h + 1, :], func=AF.Copy,
                                                                            scale=E[sub][:, 2 * h, i_:i_ + 1]), r=[E[sub]], w=[P_])
                    else:
                        p.op('pool', lambda e: e.tensor_tensor(
                            out=P_[:], in0=E[sub][:, 2 * h, ib * NI:(ib + 1) * NI].unsqueeze(2).broadcast_to([128, NI, 128]),
                            in1=E[sub][:, 2 * h + 1, :].unsqueeze(1).broadcast_to([128, NI, 128]), op=ALU.mult),
                            r=[E[sub]], w=[P_])
                    p.op('dve', lambda e: e.scalar_tensor_tensor(
                        out=G_[:, h, :], in0=P_[:].rearrange("p a b -> p (a b)"), scalar=Tt[sub][:, h:h + 1],
                        in1=P_[:].rearrange("p a b -> p (a b)"), op0=ALU.is_ge, op1=ALU.mult),
                        r=[P_, Tt[sub]], w=[G_])

                iters = [(ib, esb, sub) for ib in range(nblk) for esb in range(nesb) for sub in range(2)]
                import os
                if os.environ.get("PEER_SKIP_MAIN"):
                    iters = iters[:4]
                    nblk = 1
                nit = len(iters)

                def s1(i):
                    ib, esb, sub = iters[i]
                    e0 = ib * NI * 128 + esb * 512
                    b_ = (i // 2) % 2
                    u_, v_ = ut[b_], vb[b_]
                    if sub == 0:
                        p.dma(u_, u_[:], g.UTB[:, :, e0:e0 + 512])
                        p.dma(v_, v_[:], g.VB[:, e0 // 128:e0 // 128 + 4, :])
                    pa, ga_ = psA[i % 2], ga[i % 2]
                    for k in range(8):
                        p.op('pe', lambda e, k=k: e.matmul(pa[:], lhsT=ht[:, k, sub * 128:(sub + 1) * 128], rhs=u_[:, k, :],
                                                           start=(k == 0), stop=(k == 7)), r=[ht, u_], w=[pa])
                    p.op('act', lambda e: e.activation(out=ga_[:], in_=pa[:], func=AF.Gelu), r=[pa], w=[ga_])

                def s2(i):
                    ib, esb, sub = iters[i]
                    G_ = G[ib % 2][sub]
                    ga_, wg_ = ga[i % 2], wg[i % 2]
                    for h in range(8):
                        p.op('pe', lambda e, h=h: e.matmul(psW[:], lhsT=Dk[sub][h][:], rhs=G_[:, h, esb * 512:(esb + 1) * 512],
                                                           start=(h == 0), stop=(h == 7)), r=[Dk[sub][h], G_], w=[psW])
                    p.op('dve', lambda e: e.tensor_tensor(out=wg_[:], in0=psW[:], in1=ga_[:], op=ALU.mult), r=[psW, ga_], w=[wg_])

                def s3(i):
                    emit_transposes(g, wg[i % 2], psT[i % 2], wgT[i % 2][:], wgT[i % 2], n=4)

                def s4(i):
                    ib, esb, sub = iters[i]
                    v_ = vb[(i // 2) % 2]
                    wgT_ = wgT[i % 2]
                    first = (ib == 0 and esb == 0)
                    lastm = (ib == nblk - 1 and esb == nesb - 1)
                    for c4 in range(4):
                        for n in range(2):
                            a_ = acc[sub][n]
                            p.op('pe', lambda e, c4=c4, n=n, a_=a_: e.matmul(
                                a_[:], lhsT=wgT_[:, c4, :], rhs=v_[:, c4, n * 512:(n + 1) * 512],
                                start=(first and c4 == 0), stop=(lastm and c4 == 3)), r=[wgT_, v_], w=[a_])

                pend = []
                for sub in range(2):
                    for h in range(8):
                        unit(0, sub, h)
                s1(0)
                s2(0)
                per_it = 16 // (nesb * 2)
                for i in range(nit):
                    ib = iters[i][0]
                    if i % (nesb * 2) == 0 and ib + 1 < nblk:
                        pend = [(ib + 1, sub, h) for sub in range(2) for h in range(8)]
                    if i + 1 < nit:
                        s1(i + 1)
                    s3(i)
                    if i + 1 < nit:
                        s2(i + 1)
                    for _ in range(per_it):
                        if pend:
                            unit(*pend.pop(0))
                    s4(i)
                for sub in range(2):
                    sl = slice(t0 + sub * 128, t0 + (sub + 1) * 128)
                    x_, o_ = xs[sub], xo[sub]
                    p.dma(x_, x_[:], g.X1[sl, :])
                    for n in range(2):
                        cs = slice(n * 512, (n + 1) * 512)
                        p.op('dve', lambda e, n=n, cs=cs: e.tensor_tensor(out=o_[:, cs], in0=acc[sub][n][:], in1=gt[which][:, cs], op=ALU.mult),
                             r=[acc[sub][n], gt[which]], w=[o_])
                    p.op('pool', lambda e: e.tensor_tensor(out=o_[:], in0=o_[:], in1=x_[:], op=ALU.add), r=[o_, x_], w=[o_])
                    if not last:
                        p.dma(None, g.X2[sl, :], o_[:], r=[o_], q='pool')
                    else:
                        p.op('act', lambda e: e.activation(out=hb[:], in_=o_[:], func=AF.Square, accum_out=st[:, 0:1]), r=[o_], w=[hb, st])
                        p.op('dve', lambda e: e.tensor_scalar(out=st[:, 1:2], in0=st[:, 0:1], scalar1=1.0 / D, scalar2=EPS,
                                                              op0=ALU.mult, op1=ALU.add), r=[st], w=[st])
                        p.op('act', lambda e: e.activation(out=st[:, 2:3], in_=st[:, 1:2], func=AF.Sqrt), r=[st], w=[st])
                        p.op('dve', lambda e: e.reciprocal(out=st[:, 3:4], in_=st[:, 2:3]), r=[st], w=[st])
                        p.op('dve', lambda e: e.scalar_tensor_tensor(out=hb[:], in0=o_[:], scalar=st[:, 3:4], in1=fg[:],
                                                                     op0=ALU.mult, op1=ALU.mult), r=[o_, st, fg], w=[hb])
                        p.dma(None, g.out[t0 - LC + sub * 128:t0 - LC + (sub + 1) * 128, :], hb[:], r=[hb], q='pool')


_PROG_CACHE = {}


def kernel(x, c, ctx, c_ctx, w_ada, b_ada, norm1_g, w_in, na_rpb, diff_lambda, diff_subln_g, ret_decay_logit,
           w_out, norm2_g, peer_wq, peer_subkeys, peer_u, peer_v, final_g):
    inp = dict(x=x, c=c, ctx=ctx, c_ctx=c_ctx, w_ada=w_ada, b_ada=b_ada, norm1_g=norm1_g, w_in=w_in, na_rpb=na_rpb,
               diff_lambda=diff_lambda, diff_subln_g=diff_subln_g, ret_decay_logit=ret_decay_logit, w_out=w_out,
               norm2_g=norm2_g, peer_wq=peer_wq, peer_subkeys=peer_subkeys, peer_u=peer_u, peer_v=peer_v, final_g=final_g)
    inp = {k: np.asarray(v) for k, v in inp.items()}
    B = inp["x"].shape[0]
    cs = _consts()
    shared = {"final_g": np.asarray(inp["final_g"], np.float32).reshape(1, D)}
    for n in CONST_NAMES:
        shared[n] = cs[n]
    for l in range(DEPTH):
        for k, v in _layer_inputs(inp, l).items():
            shared["L%d_%s" % (l, k)] = v
    ccv = np.asarray(inp["c_ctx"], np.float32).reshape(8, 128).T
    in_maps = []
    for b in range(B):
        m = dict(shared)
        m["xin"] = np.ascontiguousarray(np.concatenate([inp["ctx"][b], inp["x"][b]], axis=0).astype(np.float32))
        m["cvec"] = np.ascontiguousarray(np.stack([np.asarray(inp["c"][b], np.float32).reshape(8, 128).T, ccv], axis=-1))
        in_maps.append(m)
    if "nc" not in _PROG_CACHE:
        _PROG_CACHE["nc"] = build_program()[0]
    nc = _PROG_CACHE["nc"]
    res = run_bass_kernel_spmd(nc, in_maps, core_ids=list(range(B)))
    return np.stack([np.asarray(r["out"], dtype=np.float32) for r in res.results], axis=0)
```

```python
import numpy as np
from contextlib import ExitStack, contextmanager
import concourse.bass as bass
import concourse.mybir as mybir
from concourse.bass_utils import run_bass_kernel_spmd

F32 = mybir.dt.float32
BF16 = mybir.dt.bfloat16
AF = mybir.ActivationFunctionType
ALU = mybir.AluOpType
AX = mybir.AxisListType


class Buf:
    def __init__(self, name):
        self.name = name
        self.w = None
        self.r = {}


class T(Buf):
    def __init__(self, name, h):
        super().__init__(name)
        self.h = h

    def __getitem__(self, idx):
        return self.h[idx]


class Prog:
    ENG = ('pe', 'act', 'dve', 'pool', 'sp')

    def __init__(self, nc, ndma=32):
        self.nc = nc
        self.ndma = ndma
        self.es = ExitStack()
        self.scope = None
        self.drams = {}

    def __enter__(self):
        nc = self.nc
        self.es.__enter__()
        self.engs = {'pe': nc.tensor, 'act': nc.scalar, 'dve': nc.vector, 'pool': nc.gpsimd, 'sp': nc.sync}
        self.sems = {k: self.es.enter_context(nc.semaphore("s_" + k)) for k in ('pe', 'act', 'dve', 'pool')}
        self.cnt = {k: 0 for k in self.sems}
        self.dsem = [self.es.enter_context(nc.semaphore("d%d" % i)) for i in range(self.ndma)]
        self.dval = [0] * self.ndma
        self.dnext = 0
        self.seen = {e: {} for e in self.ENG}
        self.scope = self.es
        self.ninst = 0
        return self

    def __exit__(self, *a):
        return self.es.__exit__(*a)

    def _uniq(self, name):
        self.nalloc = getattr(self, "nalloc", 0) + 1
        return "%s_%d" % (name, self.nalloc)

    def sb(self, name, shape, dtype):
        name = self._uniq(name)
        return T(name, self.scope.enter_context(self.nc.sbuf_tensor(name, list(shape), dtype)))

    def ps(self, name, shape, dtype):
        name = self._uniq(name)
        return T(name, self.scope.enter_context(self.nc.psum_tensor(name, list(shape), dtype)))

    def dram_buf(self, name):
        if name not in self.drams:
            self.drams[name] = Buf(name)
        return self.drams[name]

    @contextmanager
    def phase(self):
        old = self.scope
        with ExitStack() as st:
            self.scope = st
            yield
            self.barrier()
        self.scope = old

    def _semobj(self, key):
        return self.sems[key] if isinstance(key, str) else self.dsem[key[1]]

    def _wait(self, eng, tok, raw=False):
        key, val = tok
        if key == eng and not (raw and eng != 'pe'):
            return
        if self.seen[eng].get(key, 0) >= val:
            return
        self.engs[eng].wait_ge(self._semobj(key), val)
        self.seen[eng][key] = val
        self.ninst += 1

    def _deps(self, eng, r, w):
        for b in r:
            if b.w is not None:
                self._wait(eng, b.w, raw=True)
        for b in w:
            if b.w is not None:
                self._wait(eng, b.w)
            for k, v in b.r.items():
                self._wait(eng, (k, v))

    def _mark(self, tok, r, w):
        k, v = tok
        for b in r:
            if b.r.get(k, 0) < v:
                b.r[k] = v
        for b in w:
            b.w = tok
            b.r = {}

    def op(self, eng, fn, r=(), w=()):
        self._deps(eng, r, w)
        inst = fn(self.engs[eng])
        self.cnt[eng] += 1
        inst.then_inc(self.sems[eng], 1)
        self._mark((eng, self.cnt[eng]), r, w)
        self.ninst += 1

    def dma(self, wbuf, out_ap, in_ap, r=(), q='sp', **kw):
        i = self.dnext
        self.dnext = (i + 1) % self.ndma
        if self.dval[i] > 0:
            self._wait(q, (('d', i), self.dval[i]))
        w = [] if wbuf is None else ([wbuf] if isinstance(wbuf, Buf) else list(wbuf))
        self._deps(q, r, w)
        inst = self.engs[q].dma_start(out=out_ap, in_=in_ap, **kw)
        self.dval[i] += 16
        inst.then_inc(self.dsem[i], 16)
        self._mark((('d', i), self.dval[i]), r, w)
        self.ninst += 1

    def barrier(self):
        for e in self.ENG:
            for f in self.sems:
                if self.cnt[f] > 0:
                    self._wait(e, (f, self.cnt[f]))
            for i in range(self.ndma):
                if self.dval[i] > 0:
                    self._wait(e, (('d', i), self.dval[i]))

    def finish(self):
        self.barrier()


D = 1024
L = 4096
LC = 256
NT = L + LC
NTILE = NT // 128
DEPTH = 2
GW = 64
EPS = 1e-6
NEGB = -30000.0
O_NAQ, O_NAK, O_NAV, O_DFQ, O_DFK, O_DFV, O_RTQ, O_RTK, O_RTV, O_RTG = 0, 256, 512, 768, 1024, 1280, 1536, 1792, 2048, 2560
C_QNA, C_KNA, C_QDF, C_KDA, C_KDB, C_QRT, C_KRT = 0, 2, 4, 6, 8, 10, 12
NFMC = 14


def _rope_tables():
    t = np.arange(L)
    rows = (t // GW).astype(np.float32)
    cols = (t % GW).astype(np.float32)
    out = {}
    for name, dim in (("df", 32), ("rt", 64)):
        half = dim // 2
        inv = np.power(np.float32(10000.0), -np.arange(0, half, 2, dtype=np.float32) / np.float32(half)).astype(np.float32)
        ang = np.concatenate([rows[:, None] * inv, cols[:, None] * inv], axis=-1).astype(np.float32)
        cos = np.cos(ang).astype(np.float32)
        sin = np.sin(ang).astype(np.float32)
        C = np.ones((128, NT), np.float32)
        S = np.zeros((128, NT), np.float32)
        for r in range(128):
            d = r % dim
            pi = d // 2
            C[r, LC:] = cos[:, pi]
            S[r, LC:] = -sin[:, pi] if d % 2 == 0 else sin[:, pi]
        out[name] = (C, S)
    return out


def _na_patterns():
    pats = {}
    for pname, r in (("r0", 0), ("r2", 2), ("mid", 8), ("r60", 60), ("r62", 62)):
        kr0 = min(max(r - 4, 0), 54)
        chunks = []
        for m in range(5):
            dr = np.zeros((128, 128), np.int64)
            dc = np.zeros((128, 128), np.int64)
            va = np.zeros((128, 128), bool)
            for kk in range(128):
                krow = kr0 + 2 * m + kk // 64
                kc = kk % 64
                for qq in range(128):
                    qrow = r + qq // 64
                    qc = qq % 64
                    r0 = min(max(qrow - 4, 0), 56)
                    cs = min(max(qc - 8, 0), 48)
                    ok = (r0 <= krow < r0 + 8) and (cs <= kc < cs + 16) and krow < 64
                    va[kk, qq] = ok
                    if ok:
                        dr[kk, qq] = krow - qrow + 7
                        dc[kk, qq] = min(max(kc - qc, -15), 15) + 15
            chunks.append((dr, dc, va))
        pats[pname] = chunks
    return pats


_CONST_CACHE = {}


def _consts():
    if _CONST_CACHE:
        return _CONST_CACHE
    c = _CONST_CACHE
    rt = _rope_tables()
    s_df = np.float32(32 ** -0.5)
    c["T_DFQ_C"] = rt["df"][0] * s_df
    c["T_DFQ_S"] = rt["df"][1] * s_df
    c["T_DFK_C"] = rt["df"][0]
    c["T_DFK_S"] = rt["df"][1]
    c["T_RTQ_C"] = rt["rt"][0]
    c["T_RTQ_S"] = rt["rt"][1]
    c["T_RTK_C"] = rt["rt"][0] * np.float32(0.125)
    c["T_RTK_S"] = rt["rt"][1] * np.float32(0.125)
    c["IDENT"] = np.eye(128, dtype=np.float32)
    pats = _na_patterns()
    c["_pats"] = pats
    names = ["r0", "r2", "mid", "r60", "r62"]
    mask = np.zeros((128, 5, 5, 128), np.float32)
    for pi, pn in enumerate(names):
        for m in range(5):
            mask[:, pi, m, :] = np.where(pats[pn][m][2], 0.0, NEGB)
    c["NA_MASK"] = mask
    i = np.arange(128, dtype=np.float32)
    ij = i[None, :] - i[:, None]
    c["RT_RIJ"] = np.maximum(ij, 0).astype(np.float32)
    c["RT_MIJ"] = (ij >= 0).astype(np.float32)
    c["RT_RJI"] = np.maximum(-ij, 0).astype(np.float32)
    c["RT_MJI"] = (ij <= 0).astype(np.float32)
    c["RT_IROW"] = np.broadcast_to(i[None, :] + 1.0, (128, 128)).astype(np.float32).copy()
    c["RT_IROWB"] = np.broadcast_to(128.0 - i[None, :], (128, 128)).astype(np.float32).copy()
    c["RT_JCOL"] = np.stack([127.0 - i, i], axis=1).astype(np.float32)
    return c


def _layer_inputs(inp, l):
    c = _consts()
    w_in = np.asarray(inp["w_in"][l], np.float32)
    Z = np.zeros((D, 32), np.float32)

    def sw(cols):
        return cols ^ 1

    fm = []

    def add(cols):
        fm.append(w_in[:, cols])

    ar = np.arange
    for j in range(2):
        add(O_NAQ + j * 128 + ar(128))
    for j in range(2):
        add(O_NAK + j * 128 + ar(128))
    for j in range(2):
        cc = O_DFQ + j * 128 + ar(128)
        add(cc); add(sw(cc))
    for comp in range(2):
        for j in range(2):
            blocks_m, blocks_s = [], []
            for hh in range(2):
                h = 2 * j + hh
                cc = O_DFK + h * 64 + comp * 32 + ar(32)
                if comp == 0:
                    blocks_m += [w_in[:, cc], Z]; blocks_s += [w_in[:, sw(cc)], Z]
                else:
                    blocks_m += [Z, w_in[:, cc]]; blocks_s += [Z, w_in[:, sw(cc)]]
            fm.append(np.concatenate(blocks_m, axis=1)); fm.append(np.concatenate(blocks_s, axis=1))
    for j in range(2):
        cc = O_RTQ + j * 128 + ar(128)
        add(cc); add(sw(cc))
    for j in range(2):
        cc = O_RTK + j * 128 + ar(128)
        add(cc); add(sw(cc))
    WFM = np.ascontiguousarray(np.concatenate(fm, axis=1))
    WTM = np.ascontiguousarray(np.concatenate([w_in[:, O_NAV:O_NAV + 256], w_in[:, O_DFV:O_DFV + 256],
                                               w_in[:, O_RTV:O_RTV + 512], w_in[:, O_RTG:O_RTG + 512]], axis=1))
    rpb = np.asarray(inp["na_rpb"][l], np.float32)
    pats = c["_pats"]
    names = ["r0", "r2", "mid", "r60", "r62"]
    nab = np.zeros((128, 4, 5, 5, 128), np.float32)
    for pi, pn in enumerate(names):
        for m in range(5):
            dr, dc, va = pats[pn][m]
            for h in range(4):
                nab[:, h, pi, m, :] = rpb[h][dr, dc]
    sk = np.asarray(inp["peer_subkeys"][l], np.float32).reshape(16, 128, 128)
    skt = np.ascontiguousarray(sk.transpose(2, 0, 1))
    d = {
        "w_ada": np.asarray(inp["w_ada"][l], np.float32),
        "b_ada": np.asarray(inp["b_ada"][l], np.float32).reshape(1, 6 * D),
        "g1": np.asarray(inp["norm1_g"][l], np.float32).reshape(1, D),
        "g2": np.asarray(inp["norm2_g"][l], np.float32).reshape(1, D),
        "wfm": WFM, "wtm": WTM, "nab": nab,
        "lam": np.asarray(inp["diff_lambda"][l], np.float32).reshape(1, 128),
        "subg": np.asarray(inp["diff_subln_g"][l], np.float32).reshape(1, 64),
        "dec": np.asarray(inp["ret_decay_logit"][l], np.float32).reshape(1, 8),
        "wout": np.asarray(inp["w_out"][l], np.float32),
        "wq": np.asarray(inp["peer_wq"][l], np.float32),
        "skt": skt,
        "ut": np.ascontiguousarray(np.asarray(inp["peer_u"][l], np.float32).T),
        "v": np.asarray(inp["peer_v"][l], np.float32),
    }
    return d


LAYER_SHAPES = {
    "w_ada": [D, 6 * D], "b_ada": [1, 6 * D], "g1": [1, D], "g2": [1, D], "wfm": [D, 24 * 128], "wtm": [D, 1536],
    "nab": [128, 4, 5, 5, 128], "lam": [1, 128], "subg": [1, 64], "dec": [1, 8], "wout": [D, D], "wq": [D, 2048],
    "skt": [128, 16, 128], "ut": [D, 16384], "v": [16384, D],
}
CONST_NAMES = ["T_DFQ_C", "T_DFQ_S", "T_DFK_C", "T_DFK_S", "T_RTQ_C", "T_RTQ_S", "T_RTK_C", "T_RTK_S", "IDENT", "NA_MASK",
               "RT_RIJ", "RT_MIJ", "RT_RJI", "RT_MJI", "RT_IROW", "RT_IROWB", "RT_JCOL"]


class Ctx:
    pass


def build_program(nlayers=DEPTH, debug=None, stop_after=None):
    nc = bass.Bass("TRN2", target_bir_lowering=False)
    g = Ctx()
    g.nc = nc
    g.debug = debug or ()
    din = lambda name, shape, dt=F32: nc.dram_tensor(name, list(shape), dt, kind="ExternalInput").ap()
    dscr = lambda name, shape, dt: nc.dram_tensor(name, list(shape), dt, kind="Internal").ap()
    g.xin = din("xin", [NT, D])
    g.cvec = din("cvec", [128, 8, 2])
    g.final_g = din("final_g", [1, D])
    g.cst = {}
    cs = _consts()
    for n in CONST_NAMES:
        g.cst[n] = din(n, cs[n].shape)
    g.lay = []
    for l in range(nlayers):
        g.lay.append({k: din("L%d_%s" % (l, k), shp) for k, shp in LAYER_SHAPES.items()})
    g.out = nc.dram_tensor("out", [L, D], F32, kind="ExternalOutput").ap()
    g.MODS = dscr("MODS", [2, 6 * D], F32)
    g.X1 = dscr("X1", [NT, D], F32)
    g.X2 = dscr("X2", [NT, D], F32)
    g.FMS = dscr("FMS", [NFMC, 128, NT], BF16)
    g.VNA = dscr("VNA", [NT, 4, 65], BF16)
    g.VDF = dscr("VDF", [NT, 4, 65], BF16)
    g.VRT = dscr("VRT", [NT, 512], BF16)
    g.GRT = dscr("GRT", [NT, 512], BF16)
    g.KRTT = dscr("KRTT", [NT, 256], BF16)
    g.Y = dscr("Y", [NT, D], BF16)
    g.H2T = dscr("H2T", [128, 8, NT], BF16)
    g.QPT = dscr("QPT", [128, 16, NT], BF16)
    g.UTB = dscr("UTB", [128, 8, 16384], BF16)
    g.VB = dscr("VB", [128, 128, D], BF16)
    g.dbg = {}
    for name, shape, dt in g.debug:
        g.dbg[name] = nc.dram_tensor("dbg_" + name, list(shape), dt, kind="ExternalOutput").ap()
    p = Prog(nc)
    g.p = p
    g.scr = Buf("scratch")
    with p:
        g.ident_f = p.sb("ident_f", [128, 128], F32)
        g.ident = p.sb("ident", [128, 128], BF16)
        p.dma(g.ident_f, g.ident_f[:], g.cst["IDENT"])
        p.op('dve', lambda e: e.tensor_copy(out=g.ident[:], in_=g.ident_f[:]), r=[g.ident_f], w=[g.ident])
        stages = []
        for l in range(nlayers):
            last = (l == DEPTH - 1)
            xa = g.xin if l == 0 else g.X2
            stages += [("mod%d" % l, lambda l=l: phase_mod(g, l)),
                       ("p1_%d" % l, lambda l=l, xa=xa: phase_p1(g, l, xa)),
                       ("na_%d" % l, lambda l=l, last=last: phase_na(g, l, not last)),
                       ("df_%d" % l, lambda l=l, last=last: phase_diff(g, l, not last)),
                       ("rt_%d" % l, lambda l=l, last=last: (phase_ret(g, l, not last), dbg_dump(g, "y%d" % l, g.Y))),
                       ("p3_%d" % l, lambda l=l, last=last, xa=xa: (phase_p3(g, l, xa, last), dbg_dump(g, "x1_%d" % l, g.X1))),
                       ("peerq%d" % l, lambda l=l, last=last: phase_peerq(g, l, last)),
                       ("peer%d" % l, lambda l=l, last=last: (phase_peer(g, l, last), dbg_dump(g, "x2_%d" % l, g.X2)))]
        for name, fn in stages:
            fn()
            if stop_after == name:
                break
        p.finish()
    g.ninst = p.ninst
    return nc, g


def dbg_dump(g, name, src):
    if name in g.dbg:
        g.p.dma(None, g.dbg[name], src, q='sp')
        g.p.barrier()


def load_bcast(p, t, dram_row, n, q='sp'):
    p.dma(t, t[:, 0:n], dram_row.broadcast_to([128, n]), q=q)


def phase_mod(g, l):
    p = g.p
    Lw = g.lay[l]
    with p.phase():
        cv = p.sb("cv", [128, 8, 2], F32)
        sil = p.sb("sil", [128, 8, 2], F32)
        bad = p.sb("bad", [2, 6 * D], F32)
        modt = p.sb("modt", [2, 6 * D], F32)
        wa = [p.sb("wa%d" % i, [128, 8, 512], F32) for i in range(2)]
        ps = [p.ps("modps%d" % i, [128, 512], F32) for i in range(2)]
        p.dma(cv, cv[:], g.cvec)
        p.dma(bad, bad[:], Lw["b_ada"].broadcast_to([2, 6 * D]))
        p.op('act', lambda e: e.activation(out=sil[:], in_=cv[:], func=AF.Silu), r=[cv], w=[sil])
        wsrc = Lw["w_ada"].rearrange("(k p) n -> p k n", p=128)
        for n in range(12):
            w = wa[n % 2]
            pp = ps[n % 2]
            p.dma(w, w[:], wsrc[:, :, n * 512:(n + 1) * 512])
            for k in range(8):
                p.op('pe', lambda e, k=k, w=w, pp=pp: e.matmul(pp[0:2, :], lhsT=sil[:, k, :], rhs=w[:, k, :],
                                                             start=(k == 0), stop=(k == 7)), r=[sil, w], w=[pp])
            p.op('dve', lambda e, n=n, pp=pp: e.tensor_tensor(out=modt[:, n * 512:(n + 1) * 512], in0=pp[0:2, :],
                                                             in1=bad[:, n * 512:(n + 1) * 512], op=ALU.add),
                 r=[pp, bad], w=[modt])
        p.dma(None, g.MODS, modt[:], r=[modt], q='pool')
        if "mods" in g.dbg:
            p.dma(None, g.dbg["mods"], modt[:], r=[modt], q='pool')


def mod_row(g, which, idx):
    return g.MODS[which:which + 1, idx * D:(idx + 1) * D]


def make_norm_consts(g, tag, grow, idx_shift, idx_scale):
    p = g.p
    gb = p.sb(tag + "_gb", [128, D], F32)
    load_bcast(p, gb, grow, D)
    res = []
    for which in range(2):
        sc = p.sb(tag + "_sc%d" % which, [128, D], F32)
        sh = p.sb(tag + "_sh%d" % which, [128, D], F32)
        load_bcast(p, sc, mod_row(g, which, idx_scale), D)
        load_bcast(p, sh, mod_row(g, which, idx_shift), D)
        p.op('dve', lambda e, sc=sc: e.scalar_tensor_tensor(out=sc[:], in0=sc[:], scalar=1.0, in1=gb[:],
                                                            op0=ALU.add, op1=ALU.mult), r=[sc, gb], w=[sc])
        res.append((sc, sh))
    return res


def emit_norm(g, xt, gs, sh, hb, tmp, st):
    p = g.p
    p.op('act', lambda e: e.activation(out=tmp[:], in_=xt[:], func=AF.Square, accum_out=st[:, 0:1]), r=[xt], w=[tmp, st])
    p.op('dve', lambda e: e.tensor_scalar(out=st[:, 1:2], in0=st[:, 0:1], scalar1=1.0 / D, scalar2=EPS,
                                          op0=ALU.mult, op1=ALU.add), r=[st], w=[st])
    p.op('act', lambda e: e.activation(out=st[:, 2:3], in_=st[:, 1:2], func=AF.Sqrt), r=[st], w=[st])
    p.op('dve', lambda e: e.reciprocal(out=st[:, 3:4], in_=st[:, 2:3]), r=[st], w=[st])
    p.op('dve', lambda e: e.scalar_tensor_tensor(out=tmp[:], in0=xt[:], scalar=st[:, 3:4], in1=gs[:],
                                                 op0=ALU.mult, op1=ALU.mult), r=[xt, st, gs], w=[tmp])
    p.op('dve', lambda e: e.tensor_tensor(out=hb[:], in0=tmp[:], in1=sh[:], op=ALU.add), r=[tmp, sh], w=[hb])


def emit_transposes(g, src, pst, dst_ap, dst_buf, n=8, eng='act'):
    p = g.p
    for k in range(n):
        p.op('pe', lambda e, k=k: e.transpose(pst[:, k, :], src[:, k * 128:(k + 1) * 128], g.ident[:]),
             r=[src, g.ident], w=[pst])
    if eng == 'act':
        p.op('act', lambda e: e.copy(out=dst_ap, in_=pst[:, 0:n, :]), r=[pst], w=[dst_buf])
    else:
        p.op('dve', lambda e: e.tensor_copy(out=dst_ap, in_=pst[:, 0:n, :]), r=[pst], w=[dst_buf])


def tok_groups():
    gs = [(0, 256)]
    for i in range(8):
        gs.append((256 + i * 512, 512))
    return gs


def phase_p1(g, l, xa):
    p = g.p
    Lw = g.lay[l]
    with p.phase():
        HT = p.sb("HT", [128, 8, NT], BF16)
        with p.phase():
            nrm = make_norm_consts(g, "n1", Lw["g1"], 0, 1)
            xt = [p.sb("xt%d" % i, [128, D], F32) for i in range(2)]
            tmp = p.sb("ntmp", [128, D], F32)
            hb = [p.sb("hb%d" % i, [128, D], BF16) for i in range(2)]
            st = [p.sb("nst%d" % i, [128, 4], F32) for i in range(2)]
            pst = [p.ps("pst%d" % i, [128, 8, 128], BF16) for i in range(2)]
            for tt in range(NTILE):
                which = 1 if tt < 2 else 0
                x_ = xt[tt % 2]
                p.dma(x_, x_[:], xa[tt * 128:(tt + 1) * 128, :])
                emit_norm(g, x_, nrm[which][0], nrm[which][1], hb[tt % 2], tmp, st[tt % 2])
                emit_transposes(g, hb[tt % 2], pst[tt % 2], HT[:, :, tt * 128:(tt + 1) * 128], HT)
        if "ht" in g.dbg:
            p.dma(None, g.dbg["ht"], HT[:], r=[HT], q='pool')
        with p.phase():
            wf = [p.sb("wf%d" % i, [128, 8, 128], F32) for i in range(2)]
            wb = [p.sb("wb%d" % i, [128, 8, 128], BF16) for i in range(4)]
            tc_ = [p.sb("tc%d" % i, [128, 512], F32) for i in range(2)]
            ts_ = [p.sb("ts%d" % i, [128, 512], F32) for i in range(2)]
            t1 = [p.sb("rt1_%d" % i, [128, 512], F32) for i in range(2)]
            stg = [p.sb("fstg%d" % i, [128, 512], BF16) for i in range(2)]
            ktm = [p.sb("ktm%d" % i, [128, 4, 128], BF16) for i in range(2)]
            psA = [p.ps("psA%d" % i, [128, 512], F32) for i in range(2)]
            psB = [p.ps("psB%d" % i, [128, 512], F32) for i in range(2)]
            psT = [p.ps("psT%d" % i, [128, 8, 128], BF16) for i in range(2)]
            wsrc = Lw["wfm"].rearrange("(k p) n -> p k n", p=128)
            cnt = {"w": 0, "it": 0}

            def load_w(ci):
                i = cnt["w"]
                cnt["w"] += 1
                f, b = wf[i % 2], wb[i % 4]
                p.dma(f, f[:], wsrc[:, :, ci * 128:(ci + 1) * 128])
                p.op('pool', lambda e: e.tensor_copy(out=b[:], in_=f[:]), r=[f], w=[b])
                return b

            def mm(ps_, w_, t0, n):
                for k in range(8):
                    p.op('pe', lambda e, k=k: e.matmul(ps_[:, 0:n], lhsT=w_[:, k, :], rhs=HT[:, k, t0:t0 + n],
                                                       start=(k == 0), stop=(k == 7)), r=[w_, HT], w=[ps_])

            jobs = []
            wi = 0
            for j in range(2):
                jobs.append((C_QNA + j, wi, None, 0.125, None)); wi += 1
            for j in range(2):
                jobs.append((C_KNA + j, wi, None, 1.0, None)); wi += 1
            for dest, tn in ((C_QDF, "T_DFQ"), (C_KDA, "T_DFK"), (C_KDB, "T_DFK"), (C_QRT, "T_RTQ"), (C_KRT, "T_RTK")):
                for j in range(2):
                    jobs.append((dest + j, wi, wi + 1, 1.0, tn)); wi += 2
            for (dest, wm, ws, scale, tn) in jobs:
                wmb = load_w(wm)
                wsb = load_w(ws) if ws is not None else None
                for (t0, n) in tok_groups():
                    it = cnt["it"]
                    cnt["it"] += 1
                    a, b_ = psA[it % 2], psB[it % 2]
                    sg = stg[it % 2]
                    mm(a, wmb, t0, n)
                    if ws is None:
                        p.op('act', lambda e: e.activation(out=sg[:, 0:n], in_=a[:, 0:n], func=AF.Copy, scale=scale),
                             r=[a], w=[sg])
                    else:
                        mm(b_, wsb, t0, n)
                        tcc, tss, tt1 = tc_[it % 2], ts_[it % 2], t1[it % 2]
                        p.dma(tcc, tcc[:, 0:n], g.cst[tn + "_C"][:, t0:t0 + n])
                        p.dma(tss, tss[:, 0:n], g.cst[tn + "_S"][:, t0:t0 + n])
                        p.op('dve', lambda e: e.tensor_tensor(out=tcc[:, 0:n], in0=a[:, 0:n], in1=tcc[:, 0:n], op=ALU.mult),
                             r=[a, tcc], w=[tcc])
                        p.op('dve', lambda e: e.tensor_tensor(out=tss[:, 0:n], in0=b_[:, 0:n], in1=tss[:, 0:n], op=ALU.mult),
                             r=[b_, tss], w=[tss])
                        p.op('pool', lambda e: e.tensor_tensor(out=sg[:, 0:n], in0=tcc[:, 0:n], in1=tss[:, 0:n], op=ALU.add),
                             r=[tcc, tss], w=[sg])
                    p.dma(None, g.FMS[dest, :, t0:t0 + n], sg[:, 0:n], r=[sg], q='pool')
                    if dest in (C_KRT, C_KRT + 1):
                        nt_ = n // 128
                        pt, kt = psT[it % 2], ktm[it % 2]
                        emit_transposes(g, sg, pt, kt[:, 0:nt_, :], kt, n=nt_)
                        cj = dest - C_KRT
                        p.dma(None, g.KRTT[t0:t0 + n, cj * 128:(cj + 1) * 128].rearrange("(a p) c -> p a c", p=128),
                              kt[:, 0:nt_, :], r=[kt], q='pool')
        with p.phase():
            wtf = p.sb("wtf", [128, 8, 512], F32)
            wtb = p.sb("wtb", [128, 8, 1536], BF16)
            wsrc = Lw["wtm"].rearrange("(k p) n -> p k n", p=128)
            for c3 in range(3):
                p.dma(wtf, wtf[:], wsrc[:, :, c3 * 512:(c3 + 1) * 512])
                p.op('dve', lambda e, c3=c3: e.tensor_copy(out=wtb[:, :, c3 * 512:(c3 + 1) * 512], in_=wtf[:]), r=[wtf], w=[wtb])
            pv = [p.ps("pv%d" % i, [128, 512], F32) for i in range(4)]
            vst = [p.sb("vst%d" % i, [128, 2, 4, 65], BF16) for i in range(2)]
            rst = [p.sb("rst%d" % i, [128, 2, 512], BF16) for i in range(2)]
            for i in range(2):
                p.op('pool', lambda e, i=i: e.memset(vst[i][:], 1.0), w=[vst[i]])
            it = 0
            for tt in range(NTILE):
                vs, rs = vst[tt % 2], rst[tt % 2]
                for c3 in range(3):
                    ps_ = pv[it % 4]
                    it += 1
                    for k in range(8):
                        p.op('pe', lambda e, k=k, ps_=ps_, c3=c3: e.matmul(ps_[:], lhsT=HT[:, k, tt * 128:(tt + 1) * 128],
                                                                          rhs=wtb[:, k, c3 * 512:(c3 + 1) * 512],
                                                                          start=(k == 0), stop=(k == 7)), r=[HT, wtb], w=[ps_])
                    if c3 == 0:
                        p.op('act', lambda e, ps_=ps_: e.copy(out=vs[:, :, :, 0:64],
                                                             in_=ps_[:].rearrange("p (a h d) -> p a h d", a=2, h=4)),
                             r=[ps_], w=[vs])
                    elif c3 == 1:
                        p.op('dve', lambda e, ps_=ps_: e.tensor_copy(out=rs[:, 0, :], in_=ps_[:]), r=[ps_], w=[rs])
                    else:
                        p.op('act', lambda e, ps_=ps_: e.activation(out=rs[:, 1, :], in_=ps_[:], func=AF.Silu), r=[ps_], w=[rs])
                sl = slice(tt * 128, (tt + 1) * 128)
                p.dma(None, g.VNA[sl], vs[:, 0], r=[vs], q='pool')
                p.dma(None, g.VDF[sl], vs[:, 1], r=[vs], q='pool')
                p.dma(None, g.VRT[sl], rs[:, 0, :], r=[rs], q='pool')
                p.dma(None, g.GRT[sl], rs[:, 1, :], r=[rs], q='pool')
        for nm, src in (("fms", g.FMS), ("vna", g.VNA), ("vdf", g.VDF), ("vrt", g.VRT), ("grt", g.GRT), ("krtt", g.KRTT)):
            if nm in g.dbg:
                p.dma(None, g.dbg[nm], src, q='sp')


def phase_na(g, l, do_ctx):
    p = g.p
    Lw = g.lay[l]
    with p.phase():
        bias = p.sb("na_bias", [128, 4, 25, 128], BF16)
        with p.phase():
            stg = p.sb("na_bstg", [128, 25, 128], F32)
            msk = p.sb("na_msk", [128, 25, 128], F32)
            p.dma(msk, msk[:], g.cst["NA_MASK"].rearrange("p a b q -> p (a b) q"))
            for h in range(4):
                p.dma(stg, stg[:], Lw["nab"][:, h].rearrange("p a b q -> p (a b) q"))
                p.op('dve', lambda e, h=h: e.tensor_tensor(out=bias[:, h], in0=stg[:], in1=msk[:], op=ALU.add),
                     r=[stg, msk], w=[bias])
        V = p.sb("na_v", [128, NTILE, 260], BF16)
        p.dma(V, V[:], g.VNA.rearrange("(a p) h d -> p a (h d)", p=128))
        qT = [p.sb("na_q%d" % i, [128, NT], BF16) for i in range(2)]
        kT = [p.sb("na_k%d" % i, [128, NT], BF16) for i in range(2)]
        for i in range(2):
            p.dma(qT[i], qT[i][:], g.FMS[C_QNA + i])
            p.dma(kT[i], kT[i][:], g.FMS[C_KNA + i])
        psS = [[p.ps("na_s%d%d" % (i, j), [128, 512], F32) for j in range(2)] for i in range(2)]
        pso = [p.ps("na_o%d" % i, [128, 512], F32) for i in range(2)]
        PT = [p.sb("na_pt%d" % i, [128, 8, 128], BF16) for i in range(2)]
        ys = [p.sb("na_ys%d" % i, [128, 256], BF16) for i in range(2)]
        rc = [p.sb("na_rc%d" % i, [128, 1], F32) for i in range(2)]
        tiles = []
        if do_ctx:
            for q0 in (0, 128):
                tiles.append((q0, [(0, None), (128, None)], 0))
        for rp in range(32):
            r = 2 * rp
            kr0 = min(max(r - 4, 0), 54)
            pat = {0: 0, 2: 1, 60: 3, 62: 4}.get(r, 2)
            ch = [(LC + (kr0 + 2 * m) * 64, m) for m in range(5)] + [(0, None), (128, None)]
            tiles.append((LC + rp * 128, ch, pat))
        it = 0
        for qi, (q0, ch, pat) in enumerate(tiles):
            y_ = ys[qi % 2]
            for h in range(4):
                hp, base = h // 2, (h % 2) * 64
                sA, sB = psS[it % 2]
                po, pt, rc_ = pso[it % 2], PT[it % 2], rc[it % 2]
                it += 1
                n = len(ch)
                for i, (tok0, m) in enumerate(ch):
                    bank = sA if i < 4 else sB
                    col = (i % 4) * 128
                    p.op('pe', lambda e, bank=bank, col=col, tok0=tok0, m=m: e.matmul(
                        bank[:, col:col + 128], lhsT=kT[hp][base:base + 64, tok0:tok0 + 128],
                        rhs=qT[hp][base:base + 64, q0:q0 + 128], start=True, stop=(m is None)),
                        r=[kT[hp], qT[hp]], w=[bank])
                    if m is not None:
                        p.op('pe', lambda e, bank=bank, col=col, m=m: e.matmul(
                            bank[:, col:col + 128], lhsT=g.ident[:], rhs=bias[:, h, pat * 5 + m, :],
                            start=False, stop=True), r=[g.ident, bias], w=[bank])
                na_ = min(n, 4)
                p.op('act', lambda e: e.activation(out=pt[:, 0:na_, :], in_=sA[:, 0:na_ * 128].rearrange("p (a q) -> p a q", q=128),
                                                   func=AF.Exp), r=[sA], w=[pt])
                if n > 4:
                    p.op('act', lambda e: e.activation(out=pt[:, 4:n, :], in_=sB[:, 0:(n - 4) * 128].rearrange("p (a q) -> p a q", q=128),
                                                       func=AF.Exp), r=[sB], w=[pt])
                for i, (tok0, m) in enumerate(ch):
                    vt = tok0 // 128
                    p.op('pe', lambda e, i=i, vt=vt: e.matmul(po[:, 0:65], lhsT=pt[:, i, :], rhs=V[:, vt, h * 65:(h + 1) * 65],
                                                              start=(i == 0), stop=(i == n - 1)), r=[pt, V], w=[po])
                p.op('dve', lambda e: e.reciprocal(out=rc_[:], in_=po[:, 64:65]), r=[po], w=[rc_])
                p.op('dve', lambda e: e.tensor_scalar(out=y_[:, h * 64:(h + 1) * 64], in0=po[:, 0:64], scalar1=rc_[:, 0:1],
                                                      scalar2=None, op0=ALU.mult), r=[po, rc_], w=[y_])
            p.dma(None, g.Y[q0:q0 + 128, 0:256], y_[:], r=[y_], q='pool')


def phase_diff(g, l, do_ctx):
    p = g.p
    Lw = g.lay[l]
    import math
    lam_init = 0.8 - 0.6 * math.exp(-0.3 * l)
    with p.phase():
        lp = p.sb("df_lp", [128, 4, 32], F32)
        pr = p.sb("df_pr", [128, 2, 32], F32)
        sm = p.sb("df_sm", [128, 4], F32)
        neglam = p.sb("df_nl", [128, 1], F32)
        gsub = p.sb("df_gs", [128, 64], F32)
        p.dma(lp, lp[:].rearrange("p a b -> p (a b)"), Lw["lam"].broadcast_to([128, 128]))
        load_bcast(p, gsub, Lw["subg"], 64)
        p.op('dve', lambda e: e.tensor_scalar(out=gsub[:], in0=gsub[:], scalar1=1.0 - lam_init, scalar2=None, op0=ALU.mult),
             r=[gsub], w=[gsub])
        p.op('dve', lambda e: e.tensor_tensor(out=pr[:], in0=lp[:, 0:4:2, :], in1=lp[:, 1:4:2, :], op=ALU.mult), r=[lp], w=[pr])
        p.op('dve', lambda e: e.tensor_reduce(out=sm[:, 0:2], in_=pr[:], axis=AX.X, op=ALU.add), r=[pr], w=[sm])
        p.op('act', lambda e: e.activation(out=sm[:, 2:4], in_=sm[:, 0:2], func=AF.Exp), r=[sm], w=[sm])
        p.op('dve', lambda e: e.tensor_tensor(out=neglam[:], in0=sm[:, 3:4], in1=sm[:, 2:3], op=ALU.subtract), r=[sm], w=[neglam])
        p.op('dve', lambda e: e.tensor_scalar(out=neglam[:], in0=neglam[:], scalar1=-lam_init, scalar2=None, op0=ALU.add),
             r=[neglam], w=[neglam])
        V = p.sb("df_v", [128, NTILE, 260], BF16)
        p.dma(V, V[:], g.VDF.rearrange("(a p) h d -> p a (h d)", p=128))
        qT = [p.sb("df_q%d" % i, [128, NT], BF16) for i in range(2)]
        kA = [p.sb("df_ka%d" % i, [128, NT], BF16) for i in range(2)]
        kB = [p.sb("df_kb%d" % i, [128, NT], BF16) for i in range(2)]
        for i in range(2):
            p.dma(qT[i], qT[i][:], g.FMS[C_QDF + i])
            p.dma(kA[i], kA[i][:], g.FMS[C_KDA + i])
            p.dma(kB[i], kB[i][:], g.FMS[C_KDB + i])
        acc = [p.ps("df_acc%d" % i, [128, 512], F32) for i in range(4)]
        psS = [p.ps("df_s%d" % i, [128, 512], F32) for i in range(2)]
        PT = [p.sb("df_pt%d" % i, [128, 512], BF16) for i in range(2)]
        oc = [p.sb("df_oc%d" % i, [128, 4, 64], F32) for i in range(2)]
        o = p.sb("df_o", [128, 4, 64], F32)
        sq = p.sb("df_sq", [128, 4, 64], F32)
        ss = p.sb("df_ss", [128, 8], F32)
        rc = p.sb("df_rc", [128, 4], F32)
        yd = [p.sb("df_y%d" % i, [128, 4, 64], BF16) for i in range(2)]
        groups = []
        if do_ctx:
            groups.append((0, 256, [0, 1]))
        for gi in range(8):
            groups.append((LC + gi * 512, 512, list(range(NTILE))))
        it = 0
        gi_ = 0
        cgen = cast_gen(g, l)
        cstate = {"n": 0}

        def cast_tick():
            cstate["n"] += 1
            if cstate["n"] % 16 == 0:
                next(cgen, None)

        for h in range(4):
            hp, base = h // 2, (h % 2) * 64
            for (t0, n, kcs) in groups:
                ns = n // 128
                for c in range(2):
                    kX = (kA if c == 0 else kB)[hp]

                    def qk(ki):
                        kc = kcs[ki]
                        ps_ = psS[ki % 2]
                        p.op('pe', lambda e: e.matmul(
                            ps_[:, 0:n], lhsT=kX[base:base + 64, kc * 128:(kc + 1) * 128], rhs=qT[hp][base:base + 64, t0:t0 + n],
                            start=True, stop=True), r=[kX, qT[hp]], w=[ps_])

                    def ex(ki):
                        ps_, pt = psS[ki % 2], PT[ki % 2]
                        p.op('act', lambda e: e.activation(out=pt[:, 0:n], in_=ps_[:, 0:n], func=AF.Exp), r=[ps_], w=[pt])

                    def pv(ki):
                        kc = kcs[ki]
                        pt = PT[ki % 2]
                        for s in range(ns):
                            p.op('pe', lambda e, s=s: e.matmul(
                                acc[s][:, 0:65], lhsT=pt[:, s * 128:(s + 1) * 128], rhs=V[:, kc, h * 65:(h + 1) * 65],
                                start=(ki == 0), stop=(ki == len(kcs) - 1)), r=[pt, V], w=[acc[s]])

                    qk(0)
                    ex(0)
                    for ki in range(len(kcs)):
                        if ki + 1 < len(kcs):
                            qk(ki + 1)
                            ex(ki + 1)
                        pv(ki)
                        cast_tick()
                    for s in range(ns):
                        p.op('dve', lambda e, s=s: e.reciprocal(out=rc[:, s:s + 1], in_=acc[s][:, 64:65]), r=[acc[s]], w=[rc])
                        p.op('dve', lambda e, s=s, c=c: e.tensor_scalar(out=oc[c][:, s, :], in0=acc[s][:, 0:64], scalar1=rc[:, s:s + 1],
                                                                       scalar2=None, op0=ALU.mult), r=[acc[s], rc], w=[oc[c]])
                y_ = yd[gi_ % 2]
                gi_ += 1
                p.op('dve', lambda e: e.scalar_tensor_tensor(out=o[:, 0:ns, :], in0=oc[1][:, 0:ns, :], scalar=neglam[:, 0:1],
                                                             in1=oc[0][:, 0:ns, :], op0=ALU.mult, op1=ALU.add),
                     r=[oc[0], oc[1], neglam], w=[o])
                p.op('dve', lambda e: e.tensor_tensor(out=sq[:, 0:ns, :], in0=o[:, 0:ns, :], in1=o[:, 0:ns, :], op=ALU.mult), r=[o], w=[sq])
                p.op('dve', lambda e: e.tensor_reduce(out=ss[:, 0:ns], in_=sq[:, 0:ns, :], axis=AX.X, op=ALU.add), r=[sq], w=[ss])
                p.op('dve', lambda e: e.tensor_scalar(out=ss[:, 0:ns], in0=ss[:, 0:ns], scalar1=1.0 / 64, scalar2=EPS,
                                                      op0=ALU.mult, op1=ALU.add), r=[ss], w=[ss])
                p.op('act', lambda e: e.activation(out=ss[:, 4:4 + ns], in_=ss[:, 0:ns], func=AF.Sqrt), r=[ss], w=[ss])
                p.op('dve', lambda e: e.reciprocal(out=ss[:, 0:ns], in_=ss[:, 4:4 + ns]), r=[ss], w=[ss])
                p.op('dve', lambda e: e.tensor_tensor(out=sq[:, 0:ns, :], in0=o[:, 0:ns, :],
                                                      in1=ss[:, 0:ns].unsqueeze(2).broadcast_to([128, ns, 64]), op=ALU.mult),
                     r=[o, ss], w=[sq])
                p.op('dve', lambda e: e.tensor_tensor(out=y_[:, 0:ns, :], in0=sq[:, 0:ns, :],
                                                      in1=gsub[:].unsqueeze(1).broadcast_to([128, ns, 64]), op=ALU.mult),
                     r=[sq, gsub], w=[y_])
                p.dma(None, g.Y[t0:t0 + n, 256 + h * 64:256 + (h + 1) * 64].rearrange("(s p) d -> p s d", p=128),
                      y_[:, 0:ns, :], r=[y_], q='pool')
        for _ in cgen:
            pass


def phase_ret(g, l, do_ctx):
    p = g.p
    Lw = g.lay[l]
    with p.phase():
        dec = p.sb("rt_dec", [128, 8], F32)
        lg = p.sb("rt_lg", [128, 8], F32)
        cdec = p.sb("rt_cdec", [128, 8], F32)
        kdec = p.sb("rt_kdec", [128, 8], F32)
        jcol = p.sb("rt_jcol", [128, 2], F32)
        cm = {}
        for nm in ("RT_RIJ", "RT_MIJ", "RT_RJI", "RT_MJI", "RT_IROW", "RT_IROWB"):
            cm[nm] = p.sb("c_" + nm, [128, 128], F32)
            p.dma(cm[nm], cm[nm][:], g.cst[nm])
        p.dma(jcol, jcol[:], g.cst["RT_JCOL"])
        load_bcast(p, dec, Lw["dec"], 8)
        p.op('act', lambda e: e.activation(out=lg[:], in_=dec[:], func=AF.Sigmoid), r=[dec], w=[lg])
        p.op('act', lambda e: e.activation(out=lg[:], in_=lg[:], func=AF.Ln), r=[lg], w=[lg])
        p.op('act', lambda e: e.activation(out=cdec[:], in_=lg[:], func=AF.Exp, scale=128.0), r=[lg], w=[cdec])
        QDF = p.sb("rt_qdf", [128, 4, 128], F32)
        QDB = p.sb("rt_qdb", [128, 4, 128], F32)
        DB = p.sb("rt_db", [128, 4, 128], F32)
        t1 = p.sb("rt_t1", [128, 128], F32)
        t2 = p.sb("rt_t2", [128, 128], F32)
        for d_ in range(2):
            for h in range(4):
                c = d_ * 4 + h
                p.op('act', lambda e, c=c, d_=d_: e.activation(out=kdec[:, c:c + 1], in_=jcol[:, d_:d_ + 1], func=AF.Exp,
                                                               scale=lg[:, c:c + 1]), r=[jcol, lg], w=[kdec])
        for h in range(4):
            p.op('act', lambda e, h=h: e.activation(out=QDF[:, h, :], in_=cm["RT_IROW"][:], func=AF.Exp, scale=lg[:, h:h + 1]),
                 r=[cm["RT_IROW"], lg], w=[QDF])
            p.op('act', lambda e, h=h: e.activation(out=QDB[:, h, :], in_=cm["RT_IROWB"][:], func=AF.Exp, scale=lg[:, 4 + h:5 + h]),
                 r=[cm["RT_IROWB"], lg], w=[QDB])
            p.op('act', lambda e, h=h: e.activation(out=t1[:], in_=cm["RT_RIJ"][:], func=AF.Exp, scale=lg[:, h:h + 1]),
                 r=[cm["RT_RIJ"], lg], w=[t1])
            p.op('act', lambda e, h=h: e.activation(out=t2[:], in_=cm["RT_RJI"][:], func=AF.Exp, scale=lg[:, 4 + h:5 + h]),
                 r=[cm["RT_RJI"], lg], w=[t2])
            p.op('dve', lambda e: e.tensor_tensor(out=t1[:], in0=t1[:], in1=cm["RT_MIJ"][:], op=ALU.mult), r=[t1, cm["RT_MIJ"]], w=[t1])
            p.op('dve', lambda e: e.tensor_tensor(out=t2[:], in0=t2[:], in1=cm["RT_MJI"][:], op=ALU.mult), r=[t2, cm["RT_MJI"]], w=[t2])
            p.op('dve', lambda e, h=h: e.tensor_tensor(out=DB[:, h, :], in0=t1[:], in1=t2[:], op=ALU.add), r=[t1, t2], w=[DB])
        qT = p.sb("rt_q", [64, NT], BF16)
        kT = p.sb("rt_k", [64, NT], BF16)
        Ktm = p.sb("rt_ktm", [128, NTILE, 64], BF16)
        V = p.sb("rt_v", [128, NTILE, 128], BF16)
        G = p.sb("rt_g", [128, NTILE, 128], BF16)
        KF = p.sb("rt_kf", [128, NTILE, 64], BF16)
        KB = p.sb("rt_kb", [128, NTILE, 64], BF16)
        SFa = p.sb("rt_sfa", [64, NTILE, 128], BF16)
        SBa = p.sb("rt_sba", [64, NTILE, 128], BF16)
        S32 = [p.sb("rt_S32_%d" % i, [64, NTILE, 128], F32) for i in range(2)]
        psKV = [p.ps("rt_kv%d" % i, [128, 512], F32) for i in range(2)]
        psA = [p.ps("rt_att%d" % i, [128, 512], F32) for i in range(2)]
        psO = [p.ps("rt_o%d" % i, [128, 512], F32) for i in range(2)]
        attm = [p.sb("rt_attm%d" % i, [128, 128], BF16) for i in range(2)]
        qf = [p.sb("rt_qf%d" % i, [64, 128], BF16) for i in range(2)]
        qb = [p.sb("rt_qb%d" % i, [64, 128], BF16) for i in range(2)]
        junk = p.sb("rt_junk", [128, 128], F32)
        st = [p.sb("rt_st%d" % i, [128, 4], F32) for i in range(2)]
        ys = [p.sb("rt_ys%d" % i, [128, 128], BF16) for i in range(2)]
        it = 0
        for h in range(4):
            hp, base = h // 2, (h % 2) * 64
            p.dma(qT, qT[:], g.FMS[C_QRT + hp, base:base + 64, :])
            p.dma(kT, kT[:], g.FMS[C_KRT + hp, base:base + 64, :])
            p.dma(Ktm, Ktm[:], g.KRTT[:, h * 64:(h + 1) * 64].rearrange("(a p) d -> p a d", p=128))
            p.dma(V, V[:], g.VRT[:, h * 128:(h + 1) * 128].rearrange("(a p) d -> p a d", p=128))
            p.dma(G, G[:], g.GRT[:, h * 128:(h + 1) * 128].rearrange("(a p) d -> p a d", p=128))
            p.op('dve', lambda e: e.tensor_scalar(out=KF[:], in0=Ktm[:], scalar1=kdec[:, h:h + 1], scalar2=None, op0=ALU.mult),
                 r=[Ktm, kdec], w=[KF])
            p.op('dve', lambda e: e.tensor_scalar(out=KB[:], in0=Ktm[:], scalar1=kdec[:, 4 + h:5 + h], scalar2=None, op0=ALU.mult),
                 r=[Ktm, kdec], w=[KB])
            for d_, (Kd, Sa, order) in enumerate(((KF, SFa, list(range(NTILE))),
                                                  (KB, SBa, [1, 0] + list(range(NTILE - 1, 1, -1))))):
                S_ = [T("rt_Ss%d_%d_%d" % (h, d_, c), S32[d_][:, c, :]) for c in range(NTILE)]
                p.op('pool', lambda e: e.memset(S_[order[0]][:], 0.0), r=[S32[d_]], w=[S_[order[0]], S32[d_]])
                cd = cdec[0:64, d_ * 4 + h:d_ * 4 + h + 1]
                for oi, c in enumerate(order):
                    p.op('act', lambda e, c=c: e.copy(out=Sa[:, c, :], in_=S_[c][:]), r=[S_[c]], w=[Sa])
                    if oi == len(order) - 1:
                        p.op('act', lambda e: e.copy(out=Sa[:, c, 0:1], in_=S_[c][:, 0:1]), r=[S_[c]], w=[S32[d_]])
                        break
                    cn = order[oi + 1]
                    kv = psKV[it % 2]
                    it += 1
                    p.op('pe', lambda e, c=c, kv=kv: e.matmul(kv[0:64, 0:128], lhsT=Kd[:, c, :], rhs=V[:, c, :], start=True, stop=True),
                         r=[Kd, V], w=[kv])
                    p.op('dve', lambda e, kv=kv, c=c, cn=cn: e.scalar_tensor_tensor(out=S_[cn][:], in0=S_[c][:], scalar=cd,
                                                                                  in1=kv[0:64, 0:128], op0=ALU.mult, op1=ALU.add),
                         r=[S_[c], cdec, kv], w=[S_[cn]])
            for c in (range(NTILE) if do_ctx else range(2, NTILE)):
                tok = c * 128
                pa, po = psA[c % 2], psO[c % 2]
                am, qf_, qb_, st_, y_ = attm[c % 2], qf[c % 2], qb[c % 2], st[c % 2], ys[c % 2]
                p.op('pe', lambda e: e.matmul(pa[:, 0:128], lhsT=kT[:, tok:tok + 128], rhs=qT[:, tok:tok + 128], start=True, stop=True),
                     r=[kT, qT], w=[pa])
                p.op('dve', lambda e: e.tensor_tensor(out=am[:], in0=pa[:, 0:128], in1=DB[:, h, :], op=ALU.mult), r=[pa, DB], w=[am])
                p.op('pool', lambda e: e.tensor_tensor(out=qf_[:], in0=qT[:, tok:tok + 128], in1=QDF[0:64, h, :], op=ALU.mult),
                     r=[qT, QDF], w=[qf_])
                p.op('pool', lambda e: e.tensor_tensor(out=qb_[:], in0=qT[:, tok:tok + 128], in1=QDB[0:64, h, :], op=ALU.mult),
                     r=[qT, QDB], w=[qb_])
                p.op('pe', lambda e: e.matmul(po[:, 0:128], lhsT=am[:], rhs=V[:, c, :], start=True, stop=False), r=[am, V], w=[po])
                p.op('pe', lambda e: e.matmul(po[:, 0:128], lhsT=qf_[:], rhs=SFa[:, c, :], start=False, stop=False), r=[qf_, SFa], w=[po])
                p.op('pe', lambda e: e.matmul(po[:, 0:128], lhsT=qb_[:], rhs=SBa[:, c, :], start=False, stop=True), r=[qb_, SBa], w=[po])
                p.op('act', lambda e: e.activation(out=junk[:], in_=po[:, 0:128], func=AF.Square, accum_out=st_[:, 0:1]),
                     r=[po], w=[junk, st_])
                p.op('dve', lambda e: e.tensor_scalar(out=st_[:, 1:2], in0=st_[:, 0:1], scalar1=1.0 / 128, scalar2=EPS,
                                                      op0=ALU.mult, op1=ALU.add), r=[st_], w=[st_])
                p.op('act', lambda e: e.activation(out=st_[:, 2:3], in_=st_[:, 1:2], func=AF.Sqrt), r=[st_], w=[st_])
                p.op('dve', lambda e: e.reciprocal(out=st_[:, 3:4], in_=st_[:, 2:3]), r=[st_], w=[st_])
                p.op('dve', lambda e: e.scalar_tensor_tensor(out=y_[:], in0=po[:, 0:128], scalar=st_[:, 3:4], in1=G[:, c, :],
                                                             op0=ALU.mult, op1=ALU.mult), r=[po, st_, G], w=[y_])
                p.dma(None, g.Y[tok:tok + 128, 512 + h * 128:512 + (h + 1) * 128], y_[:], r=[y_], q='pool')


def phase_p3(g, l, xa, last):
    p = g.p
    Lw = g.lay[l]
    with p.phase():
        wob = p.sb("wob", [128, 8, D], BF16)
        with p.phase():
            wof = p.sb("wof", [128, 8, 512], F32)
            wsrc = Lw["wout"].rearrange("(k p) n -> p k n", p=128)
            for n in range(2):
                p.dma(wof, wof[:], wsrc[:, :, n * 512:(n + 1) * 512])
                p.op('dve', lambda e, n=n: e.tensor_copy(out=wob[:, :, n * 512:(n + 1) * 512], in_=wof[:]), r=[wof], w=[wob])
        gt = []
        for which in range(2):
            t = p.sb("p3_gt%d" % which, [128, D], F32)
            load_bcast(p, t, mod_row(g, which, 2), D)
            gt.append(t)
        nrm = make_norm_consts(g, "n2", Lw["g2"], 3, 4)
        yt = [p.sb("p3_y%d" % i, [128, D], BF16) for i in range(2)]
        xt = [p.sb("p3_x%d" % i, [128, D], F32) for i in range(2)]
        x1 = [p.sb("p3_x1%d" % i, [128, D], F32) for i in range(2)]
        yT = [p.sb("p3_yT%d" % i, [128, 8, 128], BF16) for i in range(2)]
        hT = [p.sb("p3_hT%d" % i, [128, 8, 128], BF16) for i in range(2)]
        hb = [p.sb("p3_hb%d" % i, [128, D], BF16) for i in range(2)]
        tmp = p.sb("p3_tmp", [128, D], F32)
        st = [p.sb("p3_st%d" % i, [128, 4], F32) for i in range(2)]
        pstY = [p.ps("p3_pty%d" % i, [128, 8, 128], BF16) for i in range(2)]
        pstH = [p.ps("p3_pth%d" % i, [128, 8, 128], BF16) for i in range(2)]
        psO = [[p.ps("p3_o%d%d" % (i, n), [128, 512], F32) for n in range(2)] for i in range(2)]
        for ti, tt in enumerate(range(2, NTILE) if last else range(NTILE)):
            which = 1 if tt < 2 else 0
            b = ti % 2
            sl = slice(tt * 128, (tt + 1) * 128)
            p.dma(yt[b], yt[b][:], g.Y[sl, :])
            p.dma(xt[b], xt[b][:], xa[sl, :])
            emit_transposes(g, yt[b], pstY[b], yT[b][:], yT[b])
            for n in range(2):
                po = psO[b][n]
                for k in range(8):
                    p.op('pe', lambda e, k=k, n=n, po=po: e.matmul(po[:], lhsT=yT[b][:, k, :], rhs=wob[:, k, n * 512:(n + 1) * 512],
                                                                  start=(k == 0), stop=(k == 7)), r=[yT[b], wob], w=[po])
                cs = slice(n * 512, (n + 1) * 512)
                p.op('dve', lambda e, po=po, cs=cs: e.tensor_tensor(out=tmp[:, cs], in0=po[:], in1=gt[which][:, cs], op=ALU.mult),
                     r=[po, gt[which]], w=[tmp])
                p.op('pool', lambda e, cs=cs: e.tensor_tensor(out=x1[b][:, cs], in0=tmp[:, cs], in1=xt[b][:, cs], op=ALU.add),
                     r=[tmp, xt[b]], w=[x1[b]])
            p.dma(None, g.X1[sl, :], x1[b][:], r=[x1[b]], q='pool')
            emit_norm(g, x1[b], nrm[which][0], nrm[which][1], hb[b], tmp, st[b])
            emit_transposes(g, hb[b], pstH[b], hT[b][:], hT[b])
            p.dma(None, g.H2T[:, :, sl], hT[b][:], r=[hT[b]], q='pool')


def cast_gen(g, l):
    p = g.p
    Lw = g.lay[l]
    f = [p.sb("cs_f%d" % i, [128, 2048], F32) for i in range(3)]
    b = [p.sb("cs_b%d" % i, [128, 2048], BF16) for i in range(3)]
    it = 0
    usrc = Lw["ut"].rearrange("(k p) e -> p k e", p=128)
    vsrc = Lw["v"].rearrange("(i p) d -> p i d", p=128)
    for k in range(8):
        for ec in range(8):
            i = it % 3
            it += 1
            p.dma(f[i], f[i][:], usrc[:, k, ec * 2048:(ec + 1) * 2048])
            p.op('dve', lambda e, i=i: e.tensor_copy(out=b[i][:], in_=f[i][:]), r=[f[i]], w=[b[i]])
            p.dma(None, g.UTB[:, k, ec * 2048:(ec + 1) * 2048], b[i][:], r=[b[i]], q='pool')
            yield
    for ic in range(64):
        i = it % 3
        it += 1
        p.dma(f[i], f[i][:].rearrange("p (a d) -> p a d", a=2), vsrc[:, ic * 2:(ic + 1) * 2, :])
        p.op('dve', lambda e, i=i: e.tensor_copy(out=b[i][:], in_=f[i][:]), r=[f[i]], w=[b[i]])
        p.dma(None, g.VB[:, ic * 2:(ic + 1) * 2, :], b[i][:].rearrange("p (a d) -> p a d", a=2), r=[b[i]], q='pool')
        yield


NI = 8


def phase_peerq(g, l, last):
    p = g.p
    Lw = g.lay[l]
    with p.phase():
        wq = p.sb("pq_wq", [128, 8, 2048], BF16)
        with p.phase():
            wqf = p.sb("pq_wqf", [128, 8, 512], F32)
            wsrc = Lw["wq"].rearrange("(k p) n -> p k n", p=128)
            for n in range(4):
                p.dma(wqf, wqf[:], wsrc[:, :, n * 512:(n + 1) * 512])
                p.op('dve', lambda e, n=n: e.tensor_copy(out=wq[:, :, n * 512:(n + 1) * 512], in_=wqf[:]), r=[wqf], w=[wq])
        ht = [p.sb("pq_ht%d" % i, [128, 8, 512], BF16) for i in range(2)]
        stg = [p.sb("pq_st%d" % i, [128, 4, 512], BF16) for i in range(2)]
        ps = [p.ps("pq_ps%d" % i, [128, 512], F32) for i in range(4)]
        it = 0
        for gi, (t0, n) in enumerate(tok_groups()):
            if last and t0 < LC:
                continue
            h_ = ht[gi % 2]
            p.dma(h_, h_[:, :, 0:n], g.H2T[:, :, t0:t0 + n])
            for jq in range(4):
                st_ = stg[(gi * 4 + jq) % 2]
                for jj in range(4):
                    j = jq * 4 + jj
                    ps_ = ps[it % 4]
                    it += 1
                    for k in range(8):
                        p.op('pe', lambda e, k=k, j=j, ps_=ps_: e.matmul(ps_[:, 0:n], lhsT=wq[:, k, j * 128:(j + 1) * 128], rhs=h_[:, k, 0:n],
                                                                        start=(k == 0), stop=(k == 7)), r=[wq, h_], w=[ps_])
                    if jj % 2 == 0:
                        p.op('act', lambda e, jj=jj, ps_=ps_: e.copy(out=st_[:, jj, 0:n], in_=ps_[:, 0:n]), r=[ps_], w=[st_])
                    else:
                        p.op('dve', lambda e, jj=jj, ps_=ps_: e.tensor_copy(out=st_[:, jj, 0:n], in_=ps_[:, 0:n]), r=[ps_], w=[st_])
                p.dma(None, g.QPT[:, jq * 4:(jq + 1) * 4, t0:t0 + n], st_[:, :, 0:n], r=[st_], q='pool')


def phase_peer(g, l, last):
    p = g.p
    Lw = g.lay[l]
    with p.phase():
        skt = p.sb("pe_skt", [128, 16, 128], BF16)
        with p.phase():
            sktf = p.sb("pe_sktf", [128, 16, 128], F32)
            p.dma(sktf, sktf[:], Lw["skt"])
            p.op('dve', lambda e: e.tensor_copy(out=skt[:], in_=sktf[:]), r=[sktf], w=[skt])
        gt = []
        for which in range(2):
            t = p.sb("pe_gt%d" % which, [128, D], F32)
            load_bcast(p, t, mod_row(g, which, 5), D)
            gt.append(t)
        if last:
            fg = p.sb("pe_fg", [128, D], F32)
            load_bcast(p, fg, g.final_g, D)
        h2t = [p.sb("pe_h2t%d" % i, [128, 8, 256], BF16) for i in range(2)]
        E = [p.sb("pe_E%d" % i, [128, 16, 128], F32) for i in range(2)]
        Tt = [p.sb("pe_T%d" % i, [128, 8], F32) for i in range(2)]
        kap = [p.sb("pe_kap%d" % i, [128, 8], F32) for i in range(2)]
        Dk = [[p.sb("pe_dk%d_%d" % (i, h), [128, 128], BF16) for h in range(8)] for i in range(2)]
        acc = [[p.ps("pe_acc%d%d" % (i, n), [128, 512], F32) for n in range(2)] for i in range(2)]
        psA = [p.ps("pe_A%d" % i, [128, 512], F32) for i in range(2)]
        psW = p.ps("pe_W", [128, 512], F32)
        psT_full = p.ps("pe_T", [128, 8, 128], BF16)
        psT = [T("pe_T%d" % i, psT_full[:, i * 4:(i + 1) * 4, :]) for i in range(2)]
        psM = psA[1]
        pairs = list(range(1, NTILE // 2)) if last else list(range(NTILE // 2))
        for pi, pr in enumerate(pairs):
            t0 = pr * 256
            which = 1 if pr == 0 else 0
            ht = h2t[pi % 2]
            p.dma(ht, ht[:], g.H2T[:, :, t0:t0 + 256])
            with p.phase():
                qTb = p.sb("pe_qT", [128, 16, 256], BF16)
                p.dma(qTb, qTb[:], g.QPT[:, :, t0:t0 + 256])
                S_all = [p.sb("pe_S%d" % i, [128, 16, 128], F32) for i in range(2)]
                m8 = p.sb("pe_m8", [128, 16, 16], F32)
                sr = p.sb("pe_sr", [128, 16, 128], F32)
                sd = p.sb("pe_sd", [128, 16, 128], F32)
                md = p.sb("pe_md", [128, 16, 16], F32)
                et = p.sb("pe_et", [128, 16, 16], F32)
                cand = p.sb("pe_cand", [128, 8, 256], F32)
                cr = p.sb("pe_cr", [128, 8, 256], F32)
                c16 = p.sb("pe_c16", [128, 8, 16], F32)
                zz = p.sb("pe_zz", [128, 8], F32)
                for sub in range(2):
                    S_ = S_all[sub]
                    for jq in range(4):
                        for jj in range(4):
                            j = jq * 4 + jj
                            p.op('pe', lambda e, j=j, jj=jj: e.matmul(psM[:, jj * 128:(jj + 1) * 128],
                                                                      lhsT=qTb[:, j, sub * 128:(sub + 1) * 128], rhs=skt[:, j, :],
                                                                      start=True, stop=True), r=[qTb, skt], w=[psM])
                        p.op('act', lambda e, jq=jq: e.copy(out=S_[:, jq * 4:(jq + 1) * 4, :],
                                                           in_=psM[:].rearrange("p (a n) -> p a n", a=4)), r=[psM], w=[S_])
                    for j in range(16):
                        p.op('dve', lambda e, j=j: e.max(out=m8[:, j, 0:8], in_=S_[:, j, :]), r=[S_], w=[m8])
                    for j in range(16):
                        p.op('dve', lambda e, j=j: e.match_replace(out=sr[:, j, :], in_to_replace=m8[:, j, 0:8], in_values=S_[:, j, :],
                                                                   imm_value=-1e30), r=[S_, m8], w=[sr])
                    for j in range(16):
                        p.op('dve', lambda e, j=j: e.max(out=m8[:, j, 8:16], in_=sr[:, j, :]), r=[sr], w=[m8])
                    p.op('dve', lambda e: e.tensor_tensor(out=sd[:], in0=S_[:], in1=m8[:, :, 0:1].broadcast_to([128, 16, 128]),
                                                          op=ALU.subtract), r=[S_, m8], w=[sd])
                    p.op('dve', lambda e: e.tensor_tensor(out=md[:], in0=m8[:], in1=m8[:, :, 0:1].broadcast_to([128, 16, 16]),
                                                          op=ALU.subtract), r=[m8], w=[md])
                    p.op('act', lambda e: e.activation(out=E[sub][:], in_=sd[:], func=AF.Exp), r=[sd], w=[E[sub]])
                    p.op('act', lambda e: e.activation(out=et[:], in_=md[:], func=AF.Exp), r=[md], w=[et])
                    et4 = et[:].rearrange("p (h c) a -> p h c a", c=2)
                    p.op('dve', lambda e: e.tensor_tensor(out=cand[:].rearrange("p h (a b) -> p h a b", a=16),
                                                          in0=et4[:, :, 0, :].unsqueeze(3).broadcast_to([128, 8, 16, 16]),
                                                          in1=et4[:, :, 1, :].unsqueeze(2).broadcast_to([128, 8, 16, 16]), op=ALU.mult),
                         r=[et], w=[cand])
                    for h in range(8):
                        p.op('dve', lambda e, h=h: e.max(out=c16[:, h, 0:8], in_=cand[:, h, :]), r=[cand], w=[c16])
                    for h in range(8):
                        p.op('dve', lambda e, h=h: e.match_replace(out=cr[:, h, :], in_to_replace=c16[:, h, 0:8], in_values=cand[:, h, :],
                                                                   imm_value=-1e30), r=[cand, c16], w=[cr])
                    for h in range(8):
                        p.op('dve', lambda e, h=h: e.max(out=c16[:, h, 8:16], in_=cr[:, h, :]), r=[cr], w=[c16])
                    p.op('dve', lambda e: e.tensor_scalar(out=Tt[sub][:], in0=c16[:, :, 15], scalar1=1.0 - 1e-6, scalar2=None, op0=ALU.mult),
                         r=[c16], w=[Tt[sub]])
                    p.op('dve', lambda e: e.tensor_reduce(out=zz[:], in_=c16[:], axis=AX.X, op=ALU.add), r=[c16], w=[zz])
                    p.op('dve', lambda e: e.reciprocal(out=kap[sub][:], in_=zz[:]), r=[zz], w=[kap[sub]])
                    for h in range(8):
                        p.op('dve', lambda e, h=h: e.tensor_scalar(out=Dk[sub][h][:], in0=g.ident_f[:], scalar1=kap[sub][:, h:h + 1],
                                                                   scalar2=None, op0=ALU.mult), r=[g.ident_f, kap[sub]], w=[Dk[sub][h]])
            with p.phase():
                NPT = 5
                Pt = [p.sb("pe_P%d" % i, [128, NI, 128], F32) for i in range(NPT)]
                G = [[p.sb("pe_G%d_%d" % (b_, i), [128, 8, NI * 128], BF16) for i in range(2)] for b_ in range(2)]
                ut = [p.sb("pe_ut%d" % i, [128, 8, 512], BF16) for i in range(2)]
                vb = [p.sb("pe_vb%d" % i, [128, 4, D], BF16) for i in range(2)]
                ga = [p.sb("pe_ga%d" % i, [128, 512], F32) for i in range(2)]
                wg = [p.sb("pe_wg%d" % i, [128, 512], BF16) for i in range(2)]
                wgT = [p.sb("pe_wgT%d" % i, [128, 4, 128], BF16) for i in range(2)]
                xs = [p.sb("pe_x%d" % i, [128, D], F32) for i in range(2)]
                xo = [p.sb("pe_xo%d" % i, [128, D], F32) for i in range(2)]
                if last:
                    hb = p.sb("pe_hb", [128, D], F32)
                    st = p.sb("pe_st", [128, 4], F32)
                nblk = 128 // NI
                nesb = NI * 128 // 512
                cnt = {"p": 0}

                def unit(ib, sub, h):
                    P_ = Pt[cnt["p"] % NPT]
                    cnt["p"] += 1
                    G_ = G[ib % 2][sub]
                    if h % 2 == 1:
                        for ii in range(NI):
                            i_ = ib * NI + ii
                            p.op('act', lambda e, ii=ii, i_=i_: e.activation(out=P_[:, ii, :], in_=E[sub][:, 2 * h + 1, :], func=AF.Copy,
                                                                            scale=E[sub][:, 2 * h, i_:i_ + 1]), r=[E[sub]], w=[P_])
                    else:
                        p.op('pool', lambda e: e.tensor_tensor(
                            out=P_[:], in0=E[sub][:, 2 * h, ib * NI:(ib + 1) * NI].unsqueeze(2).broadcast_to([128, NI, 128]),
                            in1=E[sub][:, 2 * h + 1, :].unsqueeze(1).broadcast_to([128, NI, 128]), op=ALU.mult),
                            r=[E[sub]], w=[P_])
                    p.op('dve', lambda e: e.scalar_tensor_tensor(
                        out=G_[:, h, :], in0=P_[:].rearrange("p a b -> p (a b)"), scalar=Tt[sub][:, h:h + 1],
                        in1=P_[:].rearrange("p a b -> p (a b)"), op0=ALU.is_ge, op1=ALU.mult),
                        r=[P_, Tt[sub]], w=[G_])

                iters = [(ib, esb, sub) for ib in range(nblk) for esb in range(nesb) for sub in range(2)]
                import os
                if os.environ.get("PEER_SKIP_MAIN"):
                    iters = iters[:4]
                    nblk = 1
                nit = len(iters)

                def s1(i):
                    ib, esb, sub = iters[i]
                    e0 = ib * NI * 128 + esb * 512
                    b_ = (i // 2) % 2
                    u_, v_ = ut[b_], vb[b_]
                    if sub == 0:
                        p.dma(u_, u_[:], g.UTB[:, :, e0:e0 + 512])
                        p.dma(v_, v_[:], g.VB[:, e0 // 128:e0 // 128 + 4, :])
                    pa, ga_ = psA[i % 2], ga[i % 2]
                    for k in range(8):
                        p.op('pe', lambda e, k=k: e.matmul(pa[:], lhsT=ht[:, k, sub * 128:(sub + 1) * 128], rhs=u_[:, k, :],
                                                           start=(k == 0), stop=(k == 7)), r=[ht, u_], w=[pa])
                    p.op('act', lambda e: e.activation(out=ga_[:], in_=pa[:], func=AF.Gelu), r=[pa], w=[ga_])

                def s2(i):
                    ib, esb, sub = iters[i]
                    G_ = G[ib % 2][sub]
                    ga_, wg_ = ga[i % 2], wg[i % 2]
                    for h in range(8):
                        p.op('pe', lambda e, h=h: e.matmul(psW[:], lhsT=Dk[sub][h][:], rhs=G_[:, h, esb * 512:(esb + 1) * 512],
                                                           start=(h == 0), stop=(h == 7)), r=[Dk[sub][h], G_], w=[psW])
                    p.op('dve', lambda e: e.tensor_tensor(out=wg_[:], in0=psW[:], in1=ga_[:], op=ALU.mult), r=[psW, ga_], w=[wg_])

                def s3(i):
                    emit_transposes(g, wg[i % 2], psT[i % 2], wgT[i % 2][:], wgT[i % 2], n=4)

                def s4(i):
                    ib, esb, sub = iters[i]
                    v_ = vb[(i // 2) % 2]
                    wgT_ = wgT[i % 2]
                    first = (ib == 0 and esb == 0)
                    lastm = (ib == nblk - 1 and esb == nesb - 1)
                    for c4 in range(4):
                        for n in range(2):
                            a_ = acc[sub][n]
                            p.op('pe', lambda e, c4=c4, n=n, a_=a_: e.matmul(
                                a_[:], lhsT=wgT_[:, c4, :], rhs=v_[:, c4, n * 512:(n + 1) * 512],
                                start=(first and c4 == 0), stop=(lastm and c4 == 3)), r=[wgT_, v_], w=[a_])

                pend = []
                for sub in range(2):
                    for h in range(8):
                        unit(0, sub, h)
                s1(0)
                s2(0)
                per_it = 16 // (nesb * 2)
                for i in range(nit):
                    ib = iters[i][0]
                    if i % (nesb * 2) == 0 and ib + 1 < nblk:
                        pend = [(ib + 1, sub, h) for sub in range(2) for h in range(8)]
                    if i + 1 < nit:
                        s1(i + 1)
                    s3(i)
                    if i + 1 < nit:
                        s2(i + 1)
                    for _ in range(per_it):
                        if pend:
                            unit(*pend.pop(0))
                    s4(i)
                for sub in range(2):
                    sl = slice(t0 + sub * 128, t0 + (sub + 1) * 128)
                    x_, o_ = xs[sub], xo[sub]
                    p.dma(x_, x_[:], g.X1[sl, :])
                    for n in range(2):
                        cs = slice(n * 512, (n + 1) * 512)
                        p.op('dve', lambda e, n=n, cs=cs: e.tensor_tensor(out=o_[:, cs], in0=acc[sub][n][:], in1=gt[which][:, cs], op=ALU.mult),
                             r=[acc[sub][n], gt[which]], w=[o_])
                    p.op('pool', lambda e: e.tensor_tensor(out=o_[:], in0=o_[:], in1=x_[:], op=ALU.add), r=[o_, x_], w=[o_])
                    if not last:
                        p.dma(None, g.X2[sl, :], o_[:], r=[o_], q='pool')
                    else:
                        p.op('act', lambda e: e.activation(out=hb[:], in_=o_[:], func=AF.Square, accum_out=st[:, 0:1]), r=[o_], w=[hb, st])
                        p.op('dve', lambda e: e.tensor_scalar(out=st[:, 1:2], in0=st[:, 0:1], scalar1=1.0 / D, scalar2=EPS,
                                                              op0=ALU.mult, op1=ALU.add), r=[st], w=[st])
                        p.op('act', lambda e: e.activation(out=st[:, 2:3], in_=st[:, 1:2], func=AF.Sqrt), r=[st], w=[st])
                        p.op('dve', lambda e: e.reciprocal(out=st[:, 3:4], in_=st[:, 2:3]), r=[st], w=[st])
                        p.op('dve', lambda e: e.scalar_tensor_tensor(out=hb[:], in0=o_[:], scalar=st[:, 3:4], in1=fg[:],
                                                                     op0=ALU.mult, op1=ALU.mult), r=[o_, st, fg], w=[hb])
                        p.dma(None, g.out[t0 - LC + sub * 128:t0 - LC + (sub + 1) * 128, :], hb[:], r=[hb], q='pool')


_PROG_CACHE = {}


def kernel(x, c, ctx, c_ctx, w_ada, b_ada, norm1_g, w_in, na_rpb, diff_lambda, diff_subln_g, ret_decay_logit,
           w_out, norm2_g, peer_wq, peer_subkeys, peer_u, peer_v, final_g):
    inp = dict(x=x, c=c, ctx=ctx, c_ctx=c_ctx, w_ada=w_ada, b_ada=b_ada, norm1_g=norm1_g, w_in=w_in, na_rpb=na_rpb,
               diff_lambda=diff_lambda, diff_subln_g=diff_subln_g, ret_decay_logit=ret_decay_logit, w_out=w_out,
               norm2_g=norm2_g, peer_wq=peer_wq, peer_subkeys=peer_subkeys, peer_u=peer_u, peer_v=peer_v, final_g=final_g)
    inp = {k: np.asarray(v) for k, v in inp.items()}
    B = inp["x"].shape[0]
    cs = _consts()
    shared = {"final_g": np.asarray(inp["final_g"], np.float32).reshape(1, D)}
    for n in CONST_NAMES:
        shared[n] = cs[n]
    for l in range(DEPTH):
        for k, v in _layer_inputs(inp, l).items():
            shared["L%d_%s" % (l, k)] = v
    ccv = np.asarray(inp["c_ctx"], np.float32).reshape(8, 128).T
    in_maps = []
    for b in range(B):
        m = dict(shared)
        m["xin"] = np.ascontiguousarray(np.concatenate([inp["ctx"][b], inp["x"][b]], axis=0).astype(np.float32))
        m["cvec"] = np.ascontiguousarray(np.stack([np.asarray(inp["c"][b], np.float32).reshape(8, 128).T, ccv], axis=-1))
        in_maps.append(m)
    if "nc" not in _PROG_CACHE:
        _PROG_CACHE["nc"] = build_program()[0]
    nc = _PROG_CACHE["nc"]
    res = run_bass_kernel_spmd(nc, in_maps, core_ids=list(range(B)))
    return np.stack([np.asarray(r["out"], dtype=np.float32) for r in res.results], axis=0)
```

```python
import numpy as np
from contextlib import ExitStack, contextmanager
import concourse.bass as bass
import concourse.mybir as mybir
from concourse.bass_utils import run_bass_kernel_spmd

F32 = mybir.dt.float32
BF16 = mybir.dt.bfloat16
AF = mybir.ActivationFunctionType
ALU = mybir.AluOpType
AX = mybir.AxisListType


class Buf:
    def __init__(self, name):
        self.name = name
        self.w = None
        self.r = {}


class T(Buf):
    def __init__(self, name, h):
        super().__init__(name)
        self.h = h

    def __getitem__(self, idx):
        return self.h[idx]


class Prog:
    ENG = ('pe', 'act', 'dve', 'pool', 'sp')

    def __init__(self, nc, ndma=48):
        self.nc = nc
        self.ndma = ndma
        self.es = ExitStack()
        self.scope = None
        self.drams = {}

    def __enter__(self):
        nc = self.nc
        self.es.__enter__()
        self.engs = {'pe': nc.tensor, 'act': nc.scalar, 'dve': nc.vector, 'pool': nc.gpsimd, 'sp': nc.sync}
        self.sems = {k: self.es.enter_context(nc.semaphore("s_" + k)) for k in ('pe', 'act', 'dve', 'pool')}
        self.cnt = {k: 0 for k in self.sems}
        self.dsem = [self.es.enter_context(nc.semaphore("d%d" % i)) for i in range(self.ndma)]
        self.dval = [0] * self.ndma
        self.dnext = 0
        self.dnext_sw = 0
        self.seen = {e: {} for e in self.ENG}
        self.scope = self.es
        self.ninst = 0
        return self

    def __exit__(self, *a):
        return self.es.__exit__(*a)

    def _uniq(self, name):
        self.nalloc = getattr(self, "nalloc", 0) + 1
        return "%s_%d" % (name, self.nalloc)

    def sb(self, name, shape, dtype):
        name = self._uniq(name)
        return T(name, self.scope.enter_context(self.nc.sbuf_tensor(name, list(shape), dtype)))

    def ps(self, name, shape, dtype):
        name = self._uniq(name)
        return T(name, self.scope.enter_context(self.nc.psum_tensor(name, list(shape), dtype)))

    def dram_buf(self, name):
        if name not in self.drams:
            self.drams[name] = Buf(name)
        return self.drams[name]

    @contextmanager
    def phase(self):
        old = self.scope
        with ExitStack() as st:
            self.scope = st
            yield
            self.barrier()
        self.scope = old

    def _semobj(self, key):
        return self.sems[key] if isinstance(key, str) else self.dsem[key[1]]

    def _wait(self, eng, tok, raw=False):
        key, val = tok
        if key == eng and not (raw and eng != 'pe'):
            return
        if self.seen[eng].get(key, 0) >= val:
            return
        self.engs[eng].wait_ge(self._semobj(key), val)
        self.seen[eng][key] = val
        self.ninst += 1

    def _deps(self, eng, r, w):
        for b in r:
            if b.w is not None:
                self._wait(eng, b.w, raw=True)
        for b in w:
            if b.w is not None:
                self._wait(eng, b.w)
            for k, v in b.r.items():
                self._wait(eng, (k, v))

    def _mark(self, tok, r, w):
        k, v = tok
        for b in r:
            if b.r.get(k, 0) < v:
                b.r[k] = v
        for b in w:
            b.w = tok
            b.r = {}

    def op(self, eng, fn, r=(), w=()):
        self._deps(eng, r, w)
        inst = fn(self.engs[eng])
        self.cnt[eng] += 1
        inst.then_inc(self.sems[eng], 1)
        self._mark((eng, self.cnt[eng]), r, w)
        self.ninst += 1

    def dma(self, wbuf, out_ap, in_ap, r=(), q='sp', **kw):
        half = self.ndma // 2
        if q == 'pool':
            i = half + self.dnext_sw
            self.dnext_sw = (self.dnext_sw + 1) % (self.ndma - half)
        else:
            i = self.dnext
            self.dnext = (self.dnext + 1) % half
        if self.dval[i] > 0:
            self._wait(q, (('d', i), self.dval[i]))
        w = [] if wbuf is None else ([wbuf] if isinstance(wbuf, Buf) else list(wbuf))
        self._deps(q, r, w)
        inst = self.engs[q].dma_start(out=out_ap, in_=in_ap, **kw)
        self.dval[i] += 16
        inst.then_inc(self.dsem[i], 16)
        self._mark((('d', i), self.dval[i]), r, w)
        self.ninst += 1

    def barrier(self):
        for e in self.ENG:
            for f in self.sems:
                if self.cnt[f] > 0:
                    self._wait(e, (f, self.cnt[f]))
            for i in range(self.ndma):
                if self.dval[i] > 0:
                    self._wait(e, (('d', i), self.dval[i]))

    def finish(self):
        self.barrier()


D = 1024
L = 4096
LC = 256
NT = L + LC
NTILE = NT // 128
DEPTH = 2
GW = 64
EPS = 1e-6
NEGB = -30000.0
O_NAQ, O_NAK, O_NAV, O_DFQ, O_DFK, O_DFV, O_RTQ, O_RTK, O_RTV, O_RTG = 0, 256, 512, 768, 1024, 1280, 1536, 1792, 2048, 2560
C_QNA, C_KNA, C_QDF, C_KDA, C_KDB, C_QRT, C_KRT = 0, 2, 4, 6, 8, 10, 12
NFMC = 14


def _rope_tables():
    t = np.arange(L)
    rows = (t // GW).astype(np.float32)
    cols = (t % GW).astype(np.float32)
    out = {}
    for name, dim in (("df", 32), ("rt", 64)):
        half = dim // 2
        inv = np.power(np.float32(10000.0), -np.arange(0, half, 2, dtype=np.float32) / np.float32(half)).astype(np.float32)
        ang = np.concatenate([rows[:, None] * inv, cols[:, None] * inv], axis=-1).astype(np.float32)
        cos = np.cos(ang).astype(np.float32)
        sin = np.sin(ang).astype(np.float32)
        C = np.ones((128, NT), np.float32)
        S = np.zeros((128, NT), np.float32)
        for r in range(128):
            d = r % dim
            pi = d // 2
            C[r, LC:] = cos[:, pi]
            S[r, LC:] = -sin[:, pi] if d % 2 == 0 else sin[:, pi]
        out[name] = (C, S)
    return out


def _na_patterns():
    pats = {}
    for pname, r in (("r0", 0), ("r2", 2), ("mid", 8), ("r60", 60), ("r62", 62)):
        kr0 = min(max(r - 4, 0), 54)
        chunks = []
        for m in range(5):
            dr = np.zeros((128, 128), np.int64)
            dc = np.zeros((128, 128), np.int64)
            va = np.zeros((128, 128), bool)
            for kk in range(128):
                krow = kr0 + 2 * m + kk // 64
                kc = kk % 64
                for qq in range(128):
                    qrow = r + qq // 64
                    qc = qq % 64
                    r0 = min(max(qrow - 4, 0), 56)
                    cs = min(max(qc - 8, 0), 48)
                    ok = (r0 <= krow < r0 + 8) and (cs <= kc < cs + 16) and krow < 64
                    va[kk, qq] = ok
                    if ok:
                        dr[kk, qq] = krow - qrow + 7
                        dc[kk, qq] = min(max(kc - qc, -15), 15) + 15
            chunks.append((dr, dc, va))
        pats[pname] = chunks
    return pats


_CONST_CACHE = {}


def _consts():
    if _CONST_CACHE:
        return _CONST_CACHE
    c = _CONST_CACHE
    rt = _rope_tables()
    s_df = np.float32(32 ** -0.5)
    c["T_DFQ_C"] = rt["df"][0] * s_df
    c["T_DFQ_S"] = rt["df"][1] * s_df
    c["T_DFK_C"] = rt["df"][0]
    c["T_DFK_S"] = rt["df"][1]
    c["T_RTQ_C"] = rt["rt"][0]
    c["T_RTQ_S"] = rt["rt"][1]
    c["T_RTK_C"] = rt["rt"][0] * np.float32(0.125)
    c["T_RTK_S"] = rt["rt"][1] * np.float32(0.125)
    c["IDENT"] = np.eye(128, dtype=np.float32)
    pats = _na_patterns()
    c["_pats"] = pats
    names = ["r0", "r2", "mid", "r60", "r62"]
    mask = np.zeros((128, 5, 5, 128), np.float32)
    for pi, pn in enumerate(names):
        for m in range(5):
            mask[:, pi, m, :] = np.where(pats[pn][m][2], 0.0, NEGB)
    c["NA_MASK"] = mask
    i = np.arange(128, dtype=np.float32)
    ij = i[None, :] - i[:, None]
    c["RT_RIJ"] = np.maximum(ij, 0).astype(np.float32)
    c["RT_MIJ"] = (ij >= 0).astype(np.float32)
    c["RT_RJI"] = np.maximum(-ij, 0).astype(np.float32)
    c["RT_MJI"] = (ij <= 0).astype(np.float32)
    c["RT_IROW"] = np.broadcast_to(i[None, :] + 1.0, (128, 128)).astype(np.float32).copy()
    c["RT_IROWB"] = np.broadcast_to(128.0 - i[None, :], (128, 128)).astype(np.float32).copy()
    c["RT_JCOL"] = np.stack([127.0 - i, i], axis=1).astype(np.float32)
    return c


def _layer_inputs(inp, l):
    c = _consts()
    w_in = np.asarray(inp["w_in"][l], np.float32)
    Z = np.zeros((D, 32), np.float32)

    def sw(cols):
        return cols ^ 1

    fm = []

    def add(cols):
        fm.append(w_in[:, cols])

    ar = np.arange
    for j in range(2):
        add(O_NAQ + j * 128 + ar(128))
    for j in range(2):
        add(O_NAK + j * 128 + ar(128))
    for j in range(2):
        cc = O_DFQ + j * 128 + ar(128)
        add(cc); add(sw(cc))
    for comp in range(2):
        for j in range(2):
            blocks_m, blocks_s = [], []
            for hh in range(2):
                h = 2 * j + hh
                cc = O_DFK + h * 64 + comp * 32 + ar(32)
                if comp == 0:
                    blocks_m += [w_in[:, cc], Z]; blocks_s += [w_in[:, sw(cc)], Z]
                else:
                    blocks_m += [Z, w_in[:, cc]]; blocks_s += [Z, w_in[:, sw(cc)]]
            fm.append(np.concatenate(blocks_m, axis=1)); fm.append(np.concatenate(blocks_s, axis=1))
    for j in range(2):
        cc = O_RTQ + j * 128 + ar(128)
        add(cc); add(sw(cc))
    for j in range(2):
        cc = O_RTK + j * 128 + ar(128)
        add(cc); add(sw(cc))
    WFM = np.ascontiguousarray(np.concatenate(fm, axis=1))
    WTM = np.ascontiguousarray(np.concatenate([w_in[:, O_NAV:O_NAV + 256], w_in[:, O_DFV:O_DFV + 256],
                                               w_in[:, O_RTV:O_RTV + 512], w_in[:, O_RTG:O_RTG + 512]], axis=1))
    rpb = np.asarray(inp["na_rpb"][l], np.float32)
    pats = c["_pats"]
    names = ["r0", "r2", "mid", "r60", "r62"]
    nab = np.zeros((128, 4, 5, 5, 128), np.float32)
    for pi, pn in enumerate(names):
        for m in range(5):
            dr, dc, va = pats[pn][m]
            for h in range(4):
                nab[:, h, pi, m, :] = rpb[h][dr, dc]
    sk = np.asarray(inp["peer_subkeys"][l], np.float32).reshape(16, 128, 128)
    skt = np.ascontiguousarray(sk.transpose(2, 0, 1))
    d = {
        "w_ada": np.asarray(inp["w_ada"][l], np.float32),
        "b_ada": np.asarray(inp["b_ada"][l], np.float32).reshape(1, 6 * D),
        "g1": np.asarray(inp["norm1_g"][l], np.float32).reshape(1, D),
        "g2": np.asarray(inp["norm2_g"][l], np.float32).reshape(1, D),
        "wfm": WFM, "wtm": WTM, "nab": nab,
        "lam": np.asarray(inp["diff_lambda"][l], np.float32).reshape(1, 128),
        "subg": np.asarray(inp["diff_subln_g"][l], np.float32).reshape(1, 64),
        "dec": np.asarray(inp["ret_decay_logit"][l], np.float32).reshape(1, 8),
        "wout": np.asarray(inp["w_out"][l], np.float32),
        "wq": np.asarray(inp["peer_wq"][l], np.float32),
        "skt": skt,
        "ut": np.ascontiguousarray(np.asarray(inp["peer_u"][l], np.float32).T),
        "v": np.asarray(inp["peer_v"][l], np.float32),
    }
    return d


LAYER_SHAPES = {
    "w_ada": [D, 6 * D], "b_ada": [1, 6 * D], "g1": [1, D], "g2": [1, D], "wfm": [D, 24 * 128], "wtm": [D, 1536],
    "nab": [128, 4, 5, 5, 128], "lam": [1, 128], "subg": [1, 64], "dec": [1, 8], "wout": [D, D], "wq": [D, 2048],
    "skt": [128, 16, 128], "ut": [D, 16384], "v": [16384, D],
}
CONST_NAMES = ["T_DFQ_C", "T_DFQ_S", "T_DFK_C", "T_DFK_S", "T_RTQ_C", "T_RTQ_S", "T_RTK_C", "T_RTK_S", "IDENT", "NA_MASK",
               "RT_RIJ", "RT_MIJ", "RT_RJI", "RT_MJI", "RT_IROW", "RT_IROWB", "RT_JCOL"]


class Ctx:
    pass


def build_program(nlayers=DEPTH, debug=None, stop_after=None):
    nc = bass.Bass("TRN2", target_bir_lowering=False)
    g = Ctx()
    g.nc = nc
    g.debug = debug or ()
    din = lambda name, shape, dt=F32: nc.dram_tensor(name, list(shape), dt, kind="ExternalInput").ap()
    dscr = lambda name, shape, dt: nc.dram_tensor(name, list(shape), dt, kind="Internal").ap()
    g.xin = din("xin", [NT, D])
    g.cvec = din("cvec", [128, 8, 2])
    g.final_g = din("final_g", [1, D])
    g.cst = {}
    cs = _consts()
    for n in CONST_NAMES:
        g.cst[n] = din(n, cs[n].shape)
    g.lay = []
    for l in range(nlayers):
        g.lay.append({k: din("L%d_%s" % (l, k), shp) for k, shp in LAYER_SHAPES.items()})
    g.out = nc.dram_tensor("out", [L, D], F32, kind="ExternalOutput").ap()
    g.MODS = dscr("MODS", [2, 6 * D], F32)
    g.X1 = dscr("X1", [NT, D], F32)
    g.X2 = dscr("X2", [NT, D], F32)
    g.FMS = dscr("FMS", [NFMC, 128, NT], BF16)
    g.VNA = dscr("VNA", [NT, 4, 65], BF16)
    g.VDF = dscr("VDF", [NT, 4, 65], BF16)
    g.VRT = dscr("VRT", [NT, 512], BF16)
    g.GRT = dscr("GRT", [NT, 512], BF16)
    g.KRTT = dscr("KRTT", [NT, 256], BF16)
    g.Y = dscr("Y", [NT, D], BF16)
    g.H2T = dscr("H2T", [128, 8, NT], BF16)
    g.QPT = dscr("QPT", [128, 16, NT], BF16)
    g.UTB = dscr("UTB", [128, 8, 16384], BF16)
    g.VB = dscr("VB", [128, 128, D], BF16)
    g.dbg = {}
    for name, shape, dt in g.debug:
        g.dbg[name] = nc.dram_tensor("dbg_" + name, list(shape), dt, kind="ExternalOutput").ap()
    p = Prog(nc)
    g.p = p
    g.scr = Buf("scratch")
    with p:
        g.ident_f = p.sb("ident_f", [128, 128], F32)
        g.ident = p.sb("ident", [128, 128], BF16)
        p.dma(g.ident_f, g.ident_f[:], g.cst["IDENT"])
        p.op('dve', lambda e: e.tensor_copy(out=g.ident[:], in_=g.ident_f[:]), r=[g.ident_f], w=[g.ident])
        stages = []
        for l in range(nlayers):
            last = (l == DEPTH - 1)
            xa = g.xin if l == 0 else g.X2
            stages += [("mod%d" % l, lambda l=l: phase_mod(g, l)),
                       ("p1_%d" % l, lambda l=l, xa=xa: phase_p1(g, l, xa)),
                       ("na_%d" % l, lambda l=l, last=last: phase_na(g, l, not last)),
                       ("df_%d" % l, lambda l=l, last=last: phase_diff(g, l, not last)),
                       ("rt_%d" % l, lambda l=l, last=last: (phase_ret(g, l, not last), dbg_dump(g, "y%d" % l, g.Y))),
                       ("p3_%d" % l, lambda l=l, last=last, xa=xa: (phase_p3(g, l, xa, last), dbg_dump(g, "x1_%d" % l, g.X1))),
                       ("peerq%d" % l, lambda l=l, last=last: phase_peerq(g, l, last)),
                       ("peer%d" % l, lambda l=l, last=last: (phase_peer(g, l, last), dbg_dump(g, "x2_%d" % l, g.X2)))]
        for name, fn in stages:
            fn()
            if stop_after == name:
                break
        p.finish()
    g.ninst = p.ninst
    return nc, g


def dbg_dump(g, name, src):
    if name in g.dbg:
        g.p.dma(None, g.dbg[name], src, q='sp')
        g.p.barrier()


def load_bcast(p, t, dram_row, n, q='sp'):
    p.dma(t, t[:, 0:n], dram_row.broadcast_to([128, n]), q=q)


def phase_mod(g, l):
    p = g.p
    Lw = g.lay[l]
    with p.phase():
        cv = p.sb("cv", [128, 8, 2], F32)
        sil = p.sb("sil", [128, 8, 2], F32)
        bad = p.sb("bad", [2, 6 * D], F32)
        modt = p.sb("modt", [2, 6 * D], F32)
        wa = [p.sb("wa%d" % i, [128, 8, 512], F32) for i in range(2)]
        ps = [p.ps("modps%d" % i, [128, 512], F32) for i in range(2)]
        p.dma(cv, cv[:], g.cvec)
        p.dma(bad, bad[:], Lw["b_ada"].broadcast_to([2, 6 * D]))
        p.op('act', lambda e: e.activation(out=sil[:], in_=cv[:], func=AF.Silu), r=[cv], w=[sil])
        wsrc = Lw["w_ada"].rearrange("(k p) n -> p k n", p=128)
        for n in range(12):
            w = wa[n % 2]
            pp = ps[n % 2]
            p.dma(w, w[:], wsrc[:, :, n * 512:(n + 1) * 512])
            for k in range(8):
                p.op('pe', lambda e, k=k, w=w, pp=pp: e.matmul(pp[0:2, :], lhsT=sil[:, k, :], rhs=w[:, k, :],
                                                             start=(k == 0), stop=(k == 7)), r=[sil, w], w=[pp])
            p.op('dve', lambda e, n=n, pp=pp: e.tensor_tensor(out=modt[:, n * 512:(n + 1) * 512], in0=pp[0:2, :],
                                                             in1=bad[:, n * 512:(n + 1) * 512], op=ALU.add),
                 r=[pp, bad], w=[modt])
        p.dma(None, g.MODS, modt[:], r=[modt], q='pool')
        if "mods" in g.dbg:
            p.dma(None, g.dbg["mods"], modt[:], r=[modt], q='pool')


def mod_row(g, which, idx):
    return g.MODS[which:which + 1, idx * D:(idx + 1) * D]


def make_norm_consts(g, tag, grow, idx_shift, idx_scale):
    p = g.p
    gb = p.sb(tag + "_gb", [128, D], F32)
    load_bcast(p, gb, grow, D)
    res = []
    for which in range(2):
        sc = p.sb(tag + "_sc%d" % which, [128, D], F32)
        sh = p.sb(tag + "_sh%d" % which, [128, D], F32)
        load_bcast(p, sc, mod_row(g, which, idx_scale), D)
        load_bcast(p, sh, mod_row(g, which, idx_shift), D)
        p.op('dve', lambda e, sc=sc: e.scalar_tensor_tensor(out=sc[:], in0=sc[:], scalar=1.0, in1=gb[:],
                                                            op0=ALU.add, op1=ALU.mult), r=[sc, gb], w=[sc])
        res.append((sc, sh))
    return res


def emit_norm(g, xt, gs, sh, hb, tmp, st):
    p = g.p
    p.op('act', lambda e: e.activation(out=tmp[:], in_=xt[:], func=AF.Square, accum_out=st[:, 0:1]), r=[xt], w=[tmp, st])
    p.op('dve', lambda e: e.tensor_scalar(out=st[:, 1:2], in0=st[:, 0:1], scalar1=1.0 / D, scalar2=EPS,
                                          op0=ALU.mult, op1=ALU.add), r=[st], w=[st])
    p.op('act', lambda e: e.activation(out=st[:, 2:3], in_=st[:, 1:2], func=AF.Sqrt), r=[st], w=[st])
    p.op('dve', lambda e: e.reciprocal(out=st[:, 3:4], in_=st[:, 2:3]), r=[st], w=[st])
    p.op('dve', lambda e: e.scalar_tensor_tensor(out=tmp[:], in0=xt[:], scalar=st[:, 3:4], in1=gs[:],
                                                 op0=ALU.mult, op1=ALU.mult), r=[xt, st, gs], w=[tmp])
    p.op('dve', lambda e: e.tensor_tensor(out=hb[:], in0=tmp[:], in1=sh[:], op=ALU.add), r=[tmp, sh], w=[hb])


def emit_transposes(g, src, pst, dst_ap, dst_buf, n=8, eng='act'):
    p = g.p
    for k in range(n):
        p.op('pe', lambda e, k=k: e.transpose(pst[:, k, :], src[:, k * 128:(k + 1) * 128], g.ident[:]),
             r=[src, g.ident], w=[pst])
    if eng == 'act':
        p.op('act', lambda e: e.copy(out=dst_ap, in_=pst[:, 0:n, :]), r=[pst], w=[dst_buf])
    else:
        p.op('dve', lambda e: e.tensor_copy(out=dst_ap, in_=pst[:, 0:n, :]), r=[pst], w=[dst_buf])


def tok_groups():
    gs = [(0, 256)]
    for i in range(8):
        gs.append((256 + i * 512, 512))
    return gs


def phase_p1(g, l, xa):
    p = g.p
    Lw = g.lay[l]
    with p.phase():
        HT = p.sb("HT", [128, 8, NT], BF16)
        with p.phase():
            nrm = make_norm_consts(g, "n1", Lw["g1"], 0, 1)
            xt = [p.sb("xt%d" % i, [128, D], F32) for i in range(2)]
            tmp = p.sb("ntmp", [128, D], F32)
            hb = [p.sb("hb%d" % i, [128, D], BF16) for i in range(2)]
            st = [p.sb("nst%d" % i, [128, 4], F32) for i in range(2)]
            pst = [p.ps("pst%d" % i, [128, 8, 128], BF16) for i in range(2)]
            for tt in range(NTILE):
                which = 1 if tt < 2 else 0
                x_ = xt[tt % 2]
                p.dma(x_, x_[:], xa[tt * 128:(tt + 1) * 128, :])
                emit_norm(g, x_, nrm[which][0], nrm[which][1], hb[tt % 2], tmp, st[tt % 2])
                emit_transposes(g, hb[tt % 2], pst[tt % 2], HT[:, :, tt * 128:(tt + 1) * 128], HT)
        if "ht" in g.dbg:
            p.dma(None, g.dbg["ht"], HT[:], r=[HT], q='pool')
        with p.phase():
            wf = [p.sb("wf%d" % i, [128, 8, 128], F32) for i in range(2)]
            wb = [p.sb("wb%d" % i, [128, 8, 128], BF16) for i in range(4)]
            tc_ = [p.sb("tc%d" % i, [128, 512], F32) for i in range(2)]
            ts_ = [p.sb("ts%d" % i, [128, 512], F32) for i in range(2)]
            t1 = [p.sb("rt1_%d" % i, [128, 512], F32) for i in range(2)]
            stg = [p.sb("fstg%d" % i, [128, 512], BF16) for i in range(2)]
            ktm = [p.sb("ktm%d" % i, [128, 4, 128], BF16) for i in range(2)]
            psA = [p.ps("psA%d" % i, [128, 512], F32) for i in range(2)]
            psB = [p.ps("psB%d" % i, [128, 512], F32) for i in range(2)]
            psT = [p.ps("psT%d" % i, [128, 8, 128], BF16) for i in range(2)]
            wsrc = Lw["wfm"].rearrange("(k p) n -> p k n", p=128)
            cnt = {"w": 0, "it": 0}

            def load_w(ci):
                i = cnt["w"]
                cnt["w"] += 1
                f, b = wf[i % 2], wb[i % 4]
                p.dma(f, f[:], wsrc[:, :, ci * 128:(ci + 1) * 128])
                p.op('pool', lambda e: e.tensor_copy(out=b[:], in_=f[:]), r=[f], w=[b])
                return b

            def mm(ps_, w_, t0, n):
                for k in range(8):
                    p.op('pe', lambda e, k=k: e.matmul(ps_[:, 0:n], lhsT=w_[:, k, :], rhs=HT[:, k, t0:t0 + n],
                                                       start=(k == 0), stop=(k == 7)), r=[w_, HT], w=[ps_])

            jobs = []
            wi = 0
            for j in range(2):
                jobs.append((C_QNA + j, wi, None, 0.125, None)); wi += 1
            for j in range(2):
                jobs.append((C_KNA + j, wi, None, 1.0, None)); wi += 1
            for dest, tn in ((C_QDF, "T_DFQ"), (C_KDA, "T_DFK"), (C_KDB, "T_DFK"), (C_QRT, "T_RTQ"), (C_KRT, "T_RTK")):
                for j in range(2):
                    jobs.append((dest + j, wi, wi + 1, 1.0, tn)); wi += 2
            for (dest, wm, ws, scale, tn) in jobs:
                wmb = load_w(wm)
                wsb = load_w(ws) if ws is not None else None
                for (t0, n) in tok_groups():
                    it = cnt["it"]
                    cnt["it"] += 1
                    a, b_ = psA[it % 2], psB[it % 2]
                    sg = stg[it % 2]
                    mm(a, wmb, t0, n)
                    if ws is None:
                        p.op('act', lambda e: e.activation(out=sg[:, 0:n], in_=a[:, 0:n], func=AF.Copy, scale=scale),
                             r=[a], w=[sg])
                    else:
                        mm(b_, wsb, t0, n)
                        tcc, tss, tt1 = tc_[it % 2], ts_[it % 2], t1[it % 2]
                        p.dma(tcc, tcc[:, 0:n], g.cst[tn + "_C"][:, t0:t0 + n])
                        p.dma(tss, tss[:, 0:n], g.cst[tn + "_S"][:, t0:t0 + n])
                        p.op('dve', lambda e: e.tensor_tensor(out=tcc[:, 0:n], in0=a[:, 0:n], in1=tcc[:, 0:n], op=ALU.mult),
                             r=[a, tcc], w=[tcc])
                        p.op('dve', lambda e: e.tensor_tensor(out=tss[:, 0:n], in0=b_[:, 0:n], in1=tss[:, 0:n], op=ALU.mult),
                             r=[b_, tss], w=[tss])
                        p.op('pool', lambda e: e.tensor_tensor(out=sg[:, 0:n], in0=tcc[:, 0:n], in1=tss[:, 0:n], op=ALU.add),
                             r=[tcc, tss], w=[sg])
                    p.dma(None, g.FMS[dest, :, t0:t0 + n], sg[:, 0:n], r=[sg], q='pool')
                    if dest in (C_KRT, C_KRT + 1):
                        nt_ = n // 128
                        pt, kt = psT[it % 2], ktm[it % 2]
                        emit_transposes(g, sg, pt, kt[:, 0:nt_, :], kt, n=nt_)
                        cj = dest - C_KRT
                        p.dma(None, g.KRTT[t0:t0 + n, cj * 128:(cj + 1) * 128].rearrange("(a p) c -> p a c", p=128),
                              kt[:, 0:nt_, :], r=[kt], q='pool')
        with p.phase():
            wtf = p.sb("wtf", [128, 8, 512], F32)
            wtb = p.sb("wtb", [128, 8, 1536], BF16)
            wsrc = Lw["wtm"].rearrange("(k p) n -> p k n", p=128)
            for c3 in range(3):
                p.dma(wtf, wtf[:], wsrc[:, :, c3 * 512:(c3 + 1) * 512])
                p.op('dve', lambda e, c3=c3: e.tensor_copy(out=wtb[:, :, c3 * 512:(c3 + 1) * 512], in_=wtf[:]), r=[wtf], w=[wtb])
            pv = [p.ps("pv%d" % i, [128, 512], F32) for i in range(4)]
            vst = [p.sb("vst%d" % i, [128, 2, 4, 65], BF16) for i in range(2)]
            rst = [p.sb("rst%d" % i, [128, 2, 512], BF16) for i in range(2)]
            for i in range(2):
                p.op('pool', lambda e, i=i: e.memset(vst[i][:], 1.0), w=[vst[i]])
            it = 0
            for tt in range(NTILE):
                vs, rs = vst[tt % 2], rst[tt % 2]
                for c3 in range(3):
                    ps_ = pv[it % 4]
                    it += 1
                    for k in range(8):
                        p.op('pe', lambda e, k=k, ps_=ps_, c3=c3: e.matmul(ps_[:], lhsT=HT[:, k, tt * 128:(tt + 1) * 128],
                                                                          rhs=wtb[:, k, c3 * 512:(c3 + 1) * 512],
                                                                          start=(k == 0), stop=(k == 7)), r=[HT, wtb], w=[ps_])
                    if c3 == 0:
                        p.op('act', lambda e, ps_=ps_: e.copy(out=vs[:, :, :, 0:64],
                                                             in_=ps_[:].rearrange("p (a h d) -> p a h d", a=2, h=4)),
                             r=[ps_], w=[vs])
                    elif c3 == 1:
                        p.op('dve', lambda e, ps_=ps_: e.tensor_copy(out=rs[:, 0, :], in_=ps_[:]), r=[ps_], w=[rs])
                    else:
                        p.op('act', lambda e, ps_=ps_: e.activation(out=rs[:, 1, :], in_=ps_[:], func=AF.Silu), r=[ps_], w=[rs])
                sl = slice(tt * 128, (tt + 1) * 128)
                p.dma(None, g.VNA[sl], vs[:, 0], r=[vs], q='pool')
                p.dma(None, g.VDF[sl], vs[:, 1], r=[vs], q='pool')
                p.dma(None, g.VRT[sl], rs[:, 0, :], r=[rs], q='pool')
                p.dma(None, g.GRT[sl], rs[:, 1, :], r=[rs], q='pool')
        for nm, src in (("fms", g.FMS), ("vna", g.VNA), ("vdf", g.VDF), ("vrt", g.VRT), ("grt", g.GRT), ("krtt", g.KRTT)):
            if nm in g.dbg:
                p.dma(None, g.dbg[nm], src, q='sp')


def phase_na(g, l, do_ctx):
    p = g.p
    Lw = g.lay[l]
    with p.phase():
        bias = p.sb("na_bias", [128, 4, 25, 128], BF16)
        with p.phase():
            stg = p.sb("na_bstg", [128, 25, 128], F32)
            msk = p.sb("na_msk", [128, 25, 128], F32)
            p.dma(msk, msk[:], g.cst["NA_MASK"].rearrange("p a b q -> p (a b) q"))
            for h in range(4):
                p.dma(stg, stg[:], Lw["nab"][:, h].rearrange("p a b q -> p (a b) q"))
                p.op('dve', lambda e, h=h: e.tensor_tensor(out=bias[:, h], in0=stg[:], in1=msk[:], op=ALU.add),
                     r=[stg, msk], w=[bias])
        V = p.sb("na_v", [128, NTILE, 260], BF16)
        p.dma(V, V[:], g.VNA.rearrange("(a p) h d -> p a (h d)", p=128))
        qT = [p.sb("na_q%d" % i, [128, NT], BF16) for i in range(2)]
        kT = [p.sb("na_k%d" % i, [128, NT], BF16) for i in range(2)]
        for i in range(2):
            p.dma(qT[i], qT[i][:], g.FMS[C_QNA + i])
            p.dma(kT[i], kT[i][:], g.FMS[C_KNA + i])
        psS = [[p.ps("na_s%d%d" % (i, j), [128, 512], F32) for j in range(2)] for i in range(2)]
        pso = [p.ps("na_o%d" % i, [128, 512], F32) for i in range(2)]
        PT = [p.sb("na_pt%d" % i, [128, 8, 128], BF16) for i in range(2)]
        ys = [p.sb("na_ys%d" % i, [128, 256], BF16) for i in range(2)]
        rc = [p.sb("na_rc%d" % i, [128, 1], F32) for i in range(2)]
        tiles = []
        if do_ctx:
            for q0 in (0, 128):
                tiles.append((q0, [(0, None), (128, None)], 0))
        for rp in range(32):
            r = 2 * rp
            kr0 = min(max(r - 4, 0), 54)
            pat = {0: 0, 2: 1, 60: 3, 62: 4}.get(r, 2)
            ch = [(LC + (kr0 + 2 * m) * 64, m) for m in range(5)] + [(0, None), (128, None)]
            tiles.append((LC + rp * 128, ch, pat))
        it = 0
        for qi, (q0, ch, pat) in enumerate(tiles):
            y_ = ys[qi % 2]
            for h in range(4):
                hp, base = h // 2, (h % 2) * 64
                sA, sB = psS[it % 2]
                po, pt, rc_ = pso[it % 2], PT[it % 2], rc[it % 2]
                it += 1
                n = len(ch)
                for i, (tok0, m) in enumerate(ch):
                    bank = sA if i < 4 else sB
                    col = (i % 4) * 128
                    p.op('pe', lambda e, bank=bank, col=col, tok0=tok0, m=m: e.matmul(
                        bank[:, col:col + 128], lhsT=kT[hp][base:base + 64, tok0:tok0 + 128],
                        rhs=qT[hp][base:base + 64, q0:q0 + 128], start=True, stop=(m is None)),
                        r=[kT[hp], qT[hp]], w=[bank])
                    if m is not None:
                        p.op('pe', lambda e, bank=bank, col=col, m=m: e.matmul(
                            bank[:, col:col + 128], lhsT=g.ident[:], rhs=bias[:, h, pat * 5 + m, :],
                            start=False, stop=True), r=[g.ident, bias], w=[bank])
                na_ = min(n, 4)
                p.op('act', lambda e: e.activation(out=pt[:, 0:na_, :], in_=sA[:, 0:na_ * 128].rearrange("p (a q) -> p a q", q=128),
                                                   func=AF.Exp), r=[sA], w=[pt])
                if n > 4:
                    p.op('act', lambda e: e.activation(out=pt[:, 4:n, :], in_=sB[:, 0:(n - 4) * 128].rearrange("p (a q) -> p a q", q=128),
                                                       func=AF.Exp), r=[sB], w=[pt])
                for i, (tok0, m) in enumerate(ch):
                    vt = tok0 // 128
                    p.op('pe', lambda e, i=i, vt=vt: e.matmul(po[:, 0:65], lhsT=pt[:, i, :], rhs=V[:, vt, h * 65:(h + 1) * 65],
                                                              start=(i == 0), stop=(i == n - 1)), r=[pt, V], w=[po])
                p.op('dve', lambda e: e.reciprocal(out=rc_[:], in_=po[:, 64:65]), r=[po], w=[rc_])
                p.op('dve', lambda e: e.tensor_scalar(out=y_[:, h * 64:(h + 1) * 64], in0=po[:, 0:64], scalar1=rc_[:, 0:1],
                                                      scalar2=None, op0=ALU.mult), r=[po, rc_], w=[y_])
            p.dma(None, g.Y[q0:q0 + 128, 0:256], y_[:], r=[y_], q='pool')


def phase_diff(g, l, do_ctx):
    p = g.p
    Lw = g.lay[l]
    import math
    lam_init = 0.8 - 0.6 * math.exp(-0.3 * l)
    with p.phase():
        lp = p.sb("df_lp", [128, 4, 32], F32)
        pr = p.sb("df_pr", [128, 2, 32], F32)
        sm = p.sb("df_sm", [128, 4], F32)
        neglam = p.sb("df_nl", [128, 1], F32)
        gsub = p.sb("df_gs", [128, 64], F32)
        p.dma(lp, lp[:].rearrange("p a b -> p (a b)"), Lw["lam"].broadcast_to([128, 128]))
        load_bcast(p, gsub, Lw["subg"], 64)
        p.op('dve', lambda e: e.tensor_scalar(out=gsub[:], in0=gsub[:], scalar1=1.0 - lam_init, scalar2=None, op0=ALU.mult),
             r=[gsub], w=[gsub])
        p.op('dve', lambda e: e.tensor_tensor(out=pr[:], in0=lp[:, 0:4:2, :], in1=lp[:, 1:4:2, :], op=ALU.mult), r=[lp], w=[pr])
        p.op('dve', lambda e: e.tensor_reduce(out=sm[:, 0:2], in_=pr[:], axis=AX.X, op=ALU.add), r=[pr], w=[sm])
        p.op('act', lambda e: e.activation(out=sm[:, 2:4], in_=sm[:, 0:2], func=AF.Exp), r=[sm], w=[sm])
        p.op('dve', lambda e: e.tensor_tensor(out=neglam[:], in0=sm[:, 3:4], in1=sm[:, 2:3], op=ALU.subtract), r=[sm], w=[neglam])
        p.op('dve', lambda e: e.tensor_scalar(out=neglam[:], in0=neglam[:], scalar1=-lam_init, scalar2=None, op0=ALU.add),
             r=[neglam], w=[neglam])
        V = p.sb("df_v", [128, NTILE, 260], BF16)
        p.dma(V, V[:], g.VDF.rearrange("(a p) h d -> p a (h d)", p=128))
        qT = [p.sb("df_q%d" % i, [128, NT], BF16) for i in range(2)]
        kA = [p.sb("df_ka%d" % i, [128, NT], BF16) for i in range(2)]
        kB = [p.sb("df_kb%d" % i, [128, NT], BF16) for i in range(2)]
        for i in range(2):
            p.dma(qT[i], qT[i][:], g.FMS[C_QDF + i])
            p.dma(kA[i], kA[i][:], g.FMS[C_KDA + i])
            p.dma(kB[i], kB[i][:], g.FMS[C_KDB + i])
        acc = [p.ps("df_acc%d" % i, [128, 512], F32) for i in range(4)]
        psS = [p.ps("df_s%d" % i, [128, 512], F32) for i in range(2)]
        PT = [p.sb("df_pt%d" % i, [128, 512], BF16) for i in range(2)]
        oc = [p.sb("df_oc%d" % i, [128, 4, 64], F32) for i in range(2)]
        o = p.sb("df_o", [128, 4, 64], F32)
        sq = p.sb("df_sq", [128, 4, 64], F32)
        ss = p.sb("df_ss", [128, 8], F32)
        rc = p.sb("df_rc", [128, 4], F32)
        yd = [p.sb("df_y%d" % i, [128, 4, 64], BF16) for i in range(2)]
        groups = []
        if do_ctx:
            groups.append((0, 256, [0, 1]))
        for gi in range(8):
            groups.append((LC + gi * 512, 512, list(range(NTILE))))
        it = 0
        gi_ = 0
        cgen = cast_gen(g, l)
        cstate = {"n": 0}

        def cast_tick():
            cstate["n"] += 1
            if cstate["n"] % 16 == 0:
                next(cgen, None)

        for h in range(4):
            hp, base = h // 2, (h % 2) * 64
            for (t0, n, kcs) in groups:
                ns = n // 128
                for c in range(2):
                    kX = (kA if c == 0 else kB)[hp]

                    def qk(ki):
                        kc = kcs[ki]
                        ps_ = psS[ki % 2]
                        p.op('pe', lambda e: e.matmul(
                            ps_[:, 0:n], lhsT=kX[base:base + 64, kc * 128:(kc + 1) * 128], rhs=qT[hp][base:base + 64, t0:t0 + n],
                            start=True, stop=True), r=[kX, qT[hp]], w=[ps_])

                    def ex(ki):
                        ps_, pt = psS[ki % 2], PT[ki % 2]
                        p.op('act', lambda e: e.activation(out=pt[:, 0:n], in_=ps_[:, 0:n], func=AF.Exp), r=[ps_], w=[pt])

                    def pv(ki):
                        kc = kcs[ki]
                        pt = PT[ki % 2]
                        for s in range(ns):
                            p.op('pe', lambda e, s=s: e.matmul(
                                acc[s][:, 0:65], lhsT=pt[:, s * 128:(s + 1) * 128], rhs=V[:, kc, h * 65:(h + 1) * 65],
                                start=(ki == 0), stop=(ki == len(kcs) - 1)), r=[pt, V], w=[acc[s]])

                    qk(0)
                    ex(0)
                    for ki in range(len(kcs)):
                        if ki + 1 < len(kcs):
                            qk(ki + 1)
                            ex(ki + 1)
                        pv(ki)
                        cast_tick()
                    for s in range(ns):
                        p.op('dve', lambda e, s=s: e.reciprocal(out=rc[:, s:s + 1], in_=acc[s][:, 64:65]), r=[acc[s]], w=[rc])
                        p.op('dve', lambda e, s=s, c=c: e.tensor_scalar(out=oc[c][:, s, :], in0=acc[s][:, 0:64], scalar1=rc[:, s:s + 1],
                                                                       scalar2=None, op0=ALU.mult), r=[acc[s], rc], w=[oc[c]])
                y_ = yd[gi_ % 2]
                gi_ += 1
                p.op('dve', lambda e: e.scalar_tensor_tensor(out=o[:, 0:ns, :], in0=oc[1][:, 0:ns, :], scalar=neglam[:, 0:1],
                                                             in1=oc[0][:, 0:ns, :], op0=ALU.mult, op1=ALU.add),
                     r=[oc[0], oc[1], neglam], w=[o])
                p.op('dve', lambda e: e.tensor_tensor(out=sq[:, 0:ns, :], in0=o[:, 0:ns, :], in1=o[:, 0:ns, :], op=ALU.mult), r=[o], w=[sq])
                p.op('dve', lambda e: e.tensor_reduce(out=ss[:, 0:ns], in_=sq[:, 0:ns, :], axis=AX.X, op=ALU.add), r=[sq], w=[ss])
                p.op('dve', lambda e: e.tensor_scalar(out=ss[:, 0:ns], in0=ss[:, 0:ns], scalar1=1.0 / 64, scalar2=EPS,
                                                      op0=ALU.mult, op1=ALU.add), r=[ss], w=[ss])
                p.op('act', lambda e: e.activation(out=ss[:, 4:4 + ns], in_=ss[:, 0:ns], func=AF.Sqrt), r=[ss], w=[ss])
                p.op('dve', lambda e: e.reciprocal(out=ss[:, 0:ns], in_=ss[:, 4:4 + ns]), r=[ss], w=[ss])
                p.op('dve', lambda e: e.tensor_tensor(out=sq[:, 0:ns, :], in0=o[:, 0:ns, :],
                                                      in1=ss[:, 0:ns].unsqueeze(2).broadcast_to([128, ns, 64]), op=ALU.mult),
                     r=[o, ss], w=[sq])
                p.op('dve', lambda e: e.tensor_tensor(out=y_[:, 0:ns, :], in0=sq[:, 0:ns, :],
                                                      in1=gsub[:].unsqueeze(1).broadcast_to([128, ns, 64]), op=ALU.mult),
                     r=[sq, gsub], w=[y_])
                p.dma(None, g.Y[t0:t0 + n, 256 + h * 64:256 + (h + 1) * 64].rearrange("(s p) d -> p s d", p=128),
                      y_[:, 0:ns, :], r=[y_], q='pool')
        for _ in cgen:
            pass


def phase_ret(g, l, do_ctx):
    p = g.p
    Lw = g.lay[l]
    with p.phase():
        dec = p.sb("rt_dec", [128, 8], F32)
        lg = p.sb("rt_lg", [128, 8], F32)
        cdec = p.sb("rt_cdec", [128, 8], F32)
        kdec = p.sb("rt_kdec", [128, 8], F32)
        jcol = p.sb("rt_jcol", [128, 2], F32)
        cm = {}
        for nm in ("RT_RIJ", "RT_MIJ", "RT_RJI", "RT_MJI", "RT_IROW", "RT_IROWB"):
            cm[nm] = p.sb("c_" + nm, [128, 128], F32)
            p.dma(cm[nm], cm[nm][:], g.cst[nm])
        p.dma(jcol, jcol[:], g.cst["RT_JCOL"])
        load_bcast(p, dec, Lw["dec"], 8)
        p.op('act', lambda e: e.activation(out=lg[:], in_=dec[:], func=AF.Sigmoid), r=[dec], w=[lg])
        p.op('act', lambda e: e.activation(out=lg[:], in_=lg[:], func=AF.Ln), r=[lg], w=[lg])
        p.op('act', lambda e: e.activation(out=cdec[:], in_=lg[:], func=AF.Exp, scale=128.0), r=[lg], w=[cdec])
        QDF = p.sb("rt_qdf", [128, 4, 128], F32)
        QDB = p.sb("rt_qdb", [128, 4, 128], F32)
        DB = p.sb("rt_db", [128, 4, 128], F32)
        t1 = p.sb("rt_t1", [128, 128], F32)
        t2 = p.sb("rt_t2", [128, 128], F32)
        for d_ in range(2):
            for h in range(4):
                c = d_ * 4 + h
                p.op('act', lambda e, c=c, d_=d_: e.activation(out=kdec[:, c:c + 1], in_=jcol[:, d_:d_ + 1], func=AF.Exp,
                                                               scale=lg[:, c:c + 1]), r=[jcol, lg], w=[kdec])
        for h in range(4):
            p.op('act', lambda e, h=h: e.activation(out=QDF[:, h, :], in_=cm["RT_IROW"][:], func=AF.Exp, scale=lg[:, h:h + 1]),
                 r=[cm["RT_IROW"], lg], w=[QDF])
            p.op('act', lambda e, h=h: e.activation(out=QDB[:, h, :], in_=cm["RT_IROWB"][:], func=AF.Exp, scale=lg[:, 4 + h:5 + h]),
                 r=[cm["RT_IROWB"], lg], w=[QDB])
            p.op('act', lambda e, h=h: e.activation(out=t1[:], in_=cm["RT_RIJ"][:], func=AF.Exp, scale=lg[:, h:h + 1]),
                 r=[cm["RT_RIJ"], lg], w=[t1])
            p.op('act', lambda e, h=h: e.activation(out=t2[:], in_=cm["RT_RJI"][:], func=AF.Exp, scale=lg[:, 4 + h:5 + h]),
                 r=[cm["RT_RJI"], lg], w=[t2])
            p.op('dve', lambda e: e.tensor_tensor(out=t1[:], in0=t1[:], in1=cm["RT_MIJ"][:], op=ALU.mult), r=[t1, cm["RT_MIJ"]], w=[t1])
            p.op('dve', lambda e: e.tensor_tensor(out=t2[:], in0=t2[:], in1=cm["RT_MJI"][:], op=ALU.mult), r=[t2, cm["RT_MJI"]], w=[t2])
            p.op('dve', lambda e, h=h: e.tensor_tensor(out=DB[:, h, :], in0=t1[:], in1=t2[:], op=ALU.add), r=[t1, t2], w=[DB])
        qT = p.sb("rt_q", [64, NT], BF16)
        kT = p.sb("rt_k", [64, NT], BF16)
        Ktm = p.sb("rt_ktm", [128, NTILE, 64], BF16)
        V = p.sb("rt_v", [128, NTILE, 128], BF16)
        G = p.sb("rt_g", [128, NTILE, 128], BF16)
        KF = p.sb("rt_kf", [128, NTILE, 64], BF16)
        KB = p.sb("rt_kb", [128, NTILE, 64], BF16)
        SFa = p.sb("rt_sfa", [64, NTILE, 128], BF16)
        SBa = p.sb("rt_sba", [64, NTILE, 128], BF16)
        S32 = [p.sb("rt_S32_%d" % i, [64, NTILE, 128], F32) for i in range(2)]
        psKV = [p.ps("rt_kv%d" % i, [128, 512], F32) for i in range(2)]
        psA = [p.ps("rt_att%d" % i, [128, 512], F32) for i in range(2)]
        psO = [p.ps("rt_o%d" % i, [128, 512], F32) for i in range(2)]
        attm = [p.sb("rt_attm%d" % i, [128, 128], BF16) for i in range(2)]
        qf = [p.sb("rt_qf%d" % i, [64, 128], BF16) for i in range(2)]
        qb = [p.sb("rt_qb%d" % i, [64, 128], BF16) for i in range(2)]
        junk = p.sb("rt_junk", [128, 128], F32)
        st = [p.sb("rt_st%d" % i, [128, 4], F32) for i in range(2)]
        ys = [p.sb("rt_ys%d" % i, [128, 128], BF16) for i in range(2)]
        it = 0
        for h in range(4):
            hp, base = h // 2, (h % 2) * 64
            p.dma(qT, qT[:], g.FMS[C_QRT + hp, base:base + 64, :])
            p.dma(kT, kT[:], g.FMS[C_KRT + hp, base:base + 64, :])
            p.dma(Ktm, Ktm[:], g.KRTT[:, h * 64:(h + 1) * 64].rearrange("(a p) d -> p a d", p=128))
            p.dma(V, V[:], g.VRT[:, h * 128:(h + 1) * 128].rearrange("(a p) d -> p a d", p=128))
            p.dma(G, G[:], g.GRT[:, h * 128:(h + 1) * 128].rearrange("(a p) d -> p a d", p=128))
            p.op('dve', lambda e: e.tensor_scalar(out=KF[:], in0=Ktm[:], scalar1=kdec[:, h:h + 1], scalar2=None, op0=ALU.mult),
                 r=[Ktm, kdec], w=[KF])
            p.op('dve', lambda e: e.tensor_scalar(out=KB[:], in0=Ktm[:], scalar1=kdec[:, 4 + h:5 + h], scalar2=None, op0=ALU.mult),
                 r=[Ktm, kdec], w=[KB])
            for d_, (Kd, Sa, order) in enumerate(((KF, SFa, list(range(NTILE))),
                                                  (KB, SBa, [1, 0] + list(range(NTILE - 1, 1, -1))))):
                S_ = [T("rt_Ss%d_%d_%d" % (h, d_, c), S32[d_][:, c, :]) for c in range(NTILE)]
                p.op('pool', lambda e: e.memset(S_[order[0]][:], 0.0), r=[S32[d_]], w=[S_[order[0]], S32[d_]])
                cd = cdec[0:64, d_ * 4 + h:d_ * 4 + h + 1]
                for oi, c in enumerate(order):
                    p.op('act', lambda e, c=c: e.copy(out=Sa[:, c, :], in_=S_[c][:]), r=[S_[c]], w=[Sa])
                    if oi == len(order) - 1:
                        p.op('act', lambda e: e.copy(out=Sa[:, c, 0:1], in_=S_[c][:, 0:1]), r=[S_[c]], w=[S32[d_]])
                        break
                    cn = order[oi + 1]
                    kv = psKV[it % 2]
                    it += 1
                    p.op('pe', lambda e, c=c, kv=kv: e.matmul(kv[0:64, 0:128], lhsT=Kd[:, c, :], rhs=V[:, c, :], start=True, stop=True),
                         r=[Kd, V], w=[kv])
                    p.op('dve', lambda e, kv=kv, c=c, cn=cn: e.scalar_tensor_tensor(out=S_[cn][:], in0=S_[c][:], scalar=cd,
                                                                                  in1=kv[0:64, 0:128], op0=ALU.mult, op1=ALU.add),
                         r=[S_[c], cdec, kv], w=[S_[cn]])
            for c in (range(NTILE) if do_ctx else range(2, NTILE)):
                tok = c * 128
                pa, po = psA[c % 2], psO[c % 2]
                am, qf_, qb_, st_, y_ = attm[c % 2], qf[c % 2], qb[c % 2], st[c % 2], ys[c % 2]
                p.op('pe', lambda e: e.matmul(pa[:, 0:128], lhsT=kT[:, tok:tok + 128], rhs=qT[:, tok:tok + 128], start=True, stop=True),
                     r=[kT, qT], w=[pa])
                p.op('dve', lambda e: e.tensor_tensor(out=am[:], in0=pa[:, 0:128], in1=DB[:, h, :], op=ALU.mult), r=[pa, DB], w=[am])
                p.op('pool', lambda e: e.tensor_tensor(out=qf_[:], in0=qT[:, tok:tok + 128], in1=QDF[0:64, h, :], op=ALU.mult),
                     r=[qT, QDF], w=[qf_])
                p.op('pool', lambda e: e.tensor_tensor(out=qb_[:], in0=qT[:, tok:tok + 128], in1=QDB[0:64, h, :], op=ALU.mult),
                     r=[qT, QDB], w=[qb_])
                p.op('pe', lambda e: e.matmul(po[:, 0:128], lhsT=am[:], rhs=V[:, c, :], start=True, stop=False), r=[am, V], w=[po])
                p.op('pe', lambda e: e.matmul(po[:, 0:128], lhsT=qf_[:], rhs=SFa[:, c, :], start=False, stop=False), r=[qf_, SFa], w=[po])
                p.op('pe', lambda e: e.matmul(po[:, 0:128], lhsT=qb_[:], rhs=SBa[:, c, :], start=False, stop=True), r=[qb_, SBa], w=[po])
                p.op('act', lambda e: e.activation(out=junk[:], in_=po[:, 0:128], func=AF.Square, accum_out=st_[:, 0:1]),
                     r=[po], w=[junk, st_])
                p.op('dve', lambda e: e.tensor_scalar(out=st_[:, 1:2], in0=st_[:, 0:1], scalar1=1.0 / 128, scalar2=EPS,
                                                      op0=ALU.mult, op1=ALU.add), r=[st_], w=[st_])
                p.op('act', lambda e: e.activation(out=st_[:, 2:3], in_=st_[:, 1:2], func=AF.Sqrt), r=[st_], w=[st_])
                p.op('dve', lambda e: e.reciprocal(out=st_[:, 3:4], in_=st_[:, 2:3]), r=[st_], w=[st_])
                p.op('dve', lambda e: e.scalar_tensor_tensor(out=y_[:], in0=po[:, 0:128], scalar=st_[:, 3:4], in1=G[:, c, :],
                                                             op0=ALU.mult, op1=ALU.mult), r=[po, st_, G], w=[y_])
                p.dma(None, g.Y[tok:tok + 128, 512 + h * 128:512 + (h + 1) * 128], y_[:], r=[y_], q='pool')


def phase_p3(g, l, xa, last):
    p = g.p
    Lw = g.lay[l]
    with p.phase():
        wob = p.sb("wob", [128, 8, D], BF16)
        with p.phase():
            wof = p.sb("wof", [128, 8, 512], F32)
            wsrc = Lw["wout"].rearrange("(k p) n -> p k n", p=128)
            for n in range(2):
                p.dma(wof, wof[:], wsrc[:, :, n * 512:(n + 1) * 512])
                p.op('dve', lambda e, n=n: e.tensor_copy(out=wob[:, :, n * 512:(n + 1) * 512], in_=wof[:]), r=[wof], w=[wob])
        gt = []
        for which in range(2):
            t = p.sb("p3_gt%d" % which, [128, D], F32)
            load_bcast(p, t, mod_row(g, which, 2), D)
            gt.append(t)
        nrm = make_norm_consts(g, "n2", Lw["g2"], 3, 4)
        yt = [p.sb("p3_y%d" % i, [128, D], BF16) for i in range(2)]
        xt = [p.sb("p3_x%d" % i, [128, D], F32) for i in range(2)]
        x1 = [p.sb("p3_x1%d" % i, [128, D], F32) for i in range(2)]
        yT = [p.sb("p3_yT%d" % i, [128, 8, 128], BF16) for i in range(2)]
        hT = [p.sb("p3_hT%d" % i, [128, 8, 128], BF16) for i in range(2)]
        hb = [p.sb("p3_hb%d" % i, [128, D], BF16) for i in range(2)]
        tmp = p.sb("p3_tmp", [128, D], F32)
        st = [p.sb("p3_st%d" % i, [128, 4], F32) for i in range(2)]
        pstY = [p.ps("p3_pty%d" % i, [128, 8, 128], BF16) for i in range(2)]
        pstH = [p.ps("p3_pth%d" % i, [128, 8, 128], BF16) for i in range(2)]
        psO = [[p.ps("p3_o%d%d" % (i, n), [128, 512], F32) for n in range(2)] for i in range(2)]
        for ti, tt in enumerate(range(2, NTILE) if last else range(NTILE)):
            which = 1 if tt < 2 else 0
            b = ti % 2
            sl = slice(tt * 128, (tt + 1) * 128)
            p.dma(yt[b], yt[b][:], g.Y[sl, :])
            p.dma(xt[b], xt[b][:], xa[sl, :])
            emit_transposes(g, yt[b], pstY[b], yT[b][:], yT[b])
            for n in range(2):
                po = psO[b][n]
                for k in range(8):
                    p.op('pe', lambda e, k=k, n=n, po=po: e.matmul(po[:], lhsT=yT[b][:, k, :], rhs=wob[:, k, n * 512:(n + 1) * 512],
                                                                  start=(k == 0), stop=(k == 7)), r=[yT[b], wob], w=[po])
                cs = slice(n * 512, (n + 1) * 512)
                p.op('dve', lambda e, po=po, cs=cs: e.tensor_tensor(out=tmp[:, cs], in0=po[:], in1=gt[which][:, cs], op=ALU.mult),
                     r=[po, gt[which]], w=[tmp])
                p.op('pool', lambda e, cs=cs: e.tensor_tensor(out=x1[b][:, cs], in0=tmp[:, cs], in1=xt[b][:, cs], op=ALU.add),
                     r=[tmp, xt[b]], w=[x1[b]])
            p.dma(None, g.X1[sl, :], x1[b][:], r=[x1[b]], q='pool')
            emit_norm(g, x1[b], nrm[which][0], nrm[which][1], hb[b], tmp, st[b])
            emit_transposes(g, hb[b], pstH[b], hT[b][:], hT[b])
            p.dma(None, g.H2T[:, :, sl], hT[b][:], r=[hT[b]], q='pool')


def cast_gen(g, l):
    p = g.p
    Lw = g.lay[l]
    f = [p.sb("cs_f%d" % i, [128, 2048], F32) for i in range(3)]
    b = [p.sb("cs_b%d" % i, [128, 2048], BF16) for i in range(3)]
    it = 0
    usrc = Lw["ut"].rearrange("(k p) e -> p k e", p=128)
    vsrc = Lw["v"].rearrange("(i p) d -> p i d", p=128)
    for k in range(8):
        for ec in range(8):
            i = it % 3
            it += 1
            p.dma(f[i], f[i][:], usrc[:, k, ec * 2048:(ec + 1) * 2048])
            p.op('dve', lambda e, i=i: e.tensor_copy(out=b[i][:], in_=f[i][:]), r=[f[i]], w=[b[i]])
            p.dma(None, g.UTB[:, k, ec * 2048:(ec + 1) * 2048], b[i][:], r=[b[i]], q='pool')
            yield
    for ic in range(64):
        i = it % 3
        it += 1
        p.dma(f[i], f[i][:].rearrange("p (a d) -> p a d", a=2), vsrc[:, ic * 2:(ic + 1) * 2, :])
        p.op('dve', lambda e, i=i: e.tensor_copy(out=b[i][:], in_=f[i][:]), r=[f[i]], w=[b[i]])
        p.dma(None, g.VB[:, ic * 2:(ic + 1) * 2, :], b[i][:].rearrange("p (a d) -> p a d", a=2), r=[b[i]], q='pool')
        yield


NI = 8


def phase_peerq(g, l, last):
    p = g.p
    Lw = g.lay[l]
    with p.phase():
        wq = p.sb("pq_wq", [128, 8, 2048], BF16)
        with p.phase():
            wqf = p.sb("pq_wqf", [128, 8, 512], F32)
            wsrc = Lw["wq"].rearrange("(k p) n -> p k n", p=128)
            for n in range(4):
                p.dma(wqf, wqf[:], wsrc[:, :, n * 512:(n + 1) * 512])
                p.op('dve', lambda e, n=n: e.tensor_copy(out=wq[:, :, n * 512:(n + 1) * 512], in_=wqf[:]), r=[wqf], w=[wq])
        ht = [p.sb("pq_ht%d" % i, [128, 8, 512], BF16) for i in range(2)]
        stg = [p.sb("pq_st%d" % i, [128, 4, 512], BF16) for i in range(2)]
        ps = [p.ps("pq_ps%d" % i, [128, 512], F32) for i in range(4)]
        it = 0
        for gi, (t0, n) in enumerate(tok_groups()):
            if last and t0 < LC:
                continue
            h_ = ht[gi % 2]
            p.dma(h_, h_[:, :, 0:n], g.H2T[:, :, t0:t0 + n])
            for jq in range(4):
                st_ = stg[(gi * 4 + jq) % 2]
                for jj in range(4):
                    j = jq * 4 + jj
                    ps_ = ps[it % 4]
                    it += 1
                    for k in range(8):
                        p.op('pe', lambda e, k=k, j=j, ps_=ps_: e.matmul(ps_[:, 0:n], lhsT=wq[:, k, j * 128:(j + 1) * 128], rhs=h_[:, k, 0:n],
                                                                        start=(k == 0), stop=(k == 7)), r=[wq, h_], w=[ps_])
                    if jj % 2 == 0:
                        p.op('act', lambda e, jj=jj, ps_=ps_: e.copy(out=st_[:, jj, 0:n], in_=ps_[:, 0:n]), r=[ps_], w=[st_])
                    else:
                        p.op('dve', lambda e, jj=jj, ps_=ps_: e.tensor_copy(out=st_[:, jj, 0:n], in_=ps_[:, 0:n]), r=[ps_], w=[st_])
                p.dma(None, g.QPT[:, jq * 4:(jq + 1) * 4, t0:t0 + n], st_[:, :, 0:n], r=[st_], q='pool')


def phase_peer(g, l, last):
    p = g.p
    Lw = g.lay[l]
    with p.phase():
        skt = p.sb("pe_skt", [128, 16, 128], BF16)
        with p.phase():
            sktf = p.sb("pe_sktf", [128, 16, 128], F32)
            p.dma(sktf, sktf[:], Lw["skt"])
            p.op('dve', lambda e: e.tensor_copy(out=skt[:], in_=sktf[:]), r=[sktf], w=[skt])
        gt = []
        for which in range(2):
            t = p.sb("pe_gt%d" % which, [128, D], F32)
            load_bcast(p, t, mod_row(g, which, 5), D)
            gt.append(t)
        if last:
            fg = p.sb("pe_fg", [128, D], F32)
            load_bcast(p, fg, g.final_g, D)
        h2t = [p.sb("pe_h2t%d" % i, [128, 8, 256], BF16) for i in range(2)]
        E = [p.sb("pe_E%d" % i, [128, 16, 128], F32) for i in range(2)]
        Tt = [p.sb("pe_T%d" % i, [128, 8], F32) for i in range(2)]
        kap = [p.sb("pe_kap%d" % i, [128, 8], F32) for i in range(2)]
        Dk = [[p.sb("pe_dk%d_%d" % (i, h), [128, 128], BF16) for h in range(8)] for i in range(2)]
        acc = [[p.ps("pe_acc%d%d" % (i, n), [128, 512], F32) for n in range(2)] for i in range(2)]
        psA = [p.ps("pe_A%d" % i, [128, 512], F32) for i in range(2)]
        psW = p.ps("pe_W", [128, 512], F32)
        psT_full = p.ps("pe_T", [128, 8, 128], BF16)
        psT = [T("pe_T%d" % i, psT_full[:, i * 4:(i + 1) * 4, :]) for i in range(2)]
        psM = psA[1]
        pairs = list(range(1, NTILE // 2)) if last else list(range(NTILE // 2))
        for pi, pr in enumerate(pairs):
            t0 = pr * 256
            which = 1 if pr == 0 else 0
            ht = h2t[pi % 2]
            p.dma(ht, ht[:], g.H2T[:, :, t0:t0 + 256])
            with p.phase():
                qTb = p.sb("pe_qT", [128, 16, 256], BF16)
                p.dma(qTb, qTb[:], g.QPT[:, :, t0:t0 + 256])
                S_all = [p.sb("pe_S%d" % i, [128, 16, 128], F32) for i in range(2)]
                m8 = p.sb("pe_m8", [128, 16, 16], F32)
                sr = p.sb("pe_sr", [128, 16, 128], F32)
                sd = p.sb("pe_sd", [128, 16, 128], F32)
                md = p.sb("pe_md", [128, 16, 16], F32)
                et = p.sb("pe_et", [128, 16, 16], F32)
                cand = p.sb("pe_cand", [128, 8, 256], F32)
                cr = p.sb("pe_cr", [128, 8, 256], F32)
                c16 = p.sb("pe_c16", [128, 8, 16], F32)
                zz = p.sb("pe_zz", [128, 8], F32)
                for sub in range(2):
                    S_ = S_all[sub]
                    for jq in range(4):
                        for jj in range(4):
                            j = jq * 4 + jj
                            p.op('pe', lambda e, j=j, jj=jj: e.matmul(psM[:, jj * 128:(jj + 1) * 128],
                                                                      lhsT=qTb[:, j, sub * 128:(sub + 1) * 128], rhs=skt[:, j, :],
                                                                      start=True, stop=True), r=[qTb, skt], w=[psM])
                        p.op('act', lambda e, jq=jq: e.copy(out=S_[:, jq * 4:(jq + 1) * 4, :],
                                                           in_=psM[:].rearrange("p (a n) -> p a n", a=4)), r=[psM], w=[S_])
                    for j in range(16):
                        p.op('dve', lambda e, j=j: e.max(out=m8[:, j, 0:8], in_=S_[:, j, :]), r=[S_], w=[m8])
                    for j in range(16):
                        p.op('dve', lambda e, j=j: e.match_replace(out=sr[:, j, :], in_to_replace=m8[:, j, 0:8], in_values=S_[:, j, :],
                                                                   imm_value=-1e30), r=[S_, m8], w=[sr])
                    for j in range(16):
                        p.op('dve', lambda e, j=j: e.max(out=m8[:, j, 8:16], in_=sr[:, j, :]), r=[sr], w=[m8])
                    p.op('dve', lambda e: e.tensor_tensor(out=sd[:], in0=S_[:], in1=m8[:, :, 0:1].broadcast_to([128, 16, 128]),
                                                          op=ALU.subtract), r=[S_, m8], w=[sd])
                    p.op('dve', lambda e: e.tensor_tensor(out=md[:], in0=m8[:], in1=m8[:, :, 0:1].broadcast_to([128, 16, 16]),
                                                          op=ALU.subtract), r=[m8], w=[md])
                    p.op('act', lambda e: e.activation(out=E[sub][:], in_=sd[:], func=AF.Exp), r=[sd], w=[E[sub]])
                    p.op('act', lambda e: e.activation(out=et[:], in_=md[:], func=AF.Exp), r=[md], w=[et])
                    et4 = et[:].rearrange("p (h c) a -> p h c a", c=2)
                    p.op('dve', lambda e: e.tensor_tensor(out=cand[:].rearrange("p h (a b) -> p h a b", a=16),
                                                          in0=et4[:, :, 0, :].unsqueeze(3).broadcast_to([128, 8, 16, 16]),
                                                          in1=et4[:, :, 1, :].unsqueeze(2).broadcast_to([128, 8, 16, 16]), op=ALU.mult),
                         r=[et], w=[cand])
                    for h in range(8):
                        p.op('dve', lambda e, h=h: e.max(out=c16[:, h, 0:8], in_=cand[:, h, :]), r=[cand], w=[c16])
                    for h in range(8):
                        p.op('dve', lambda e, h=h: e.match_replace(out=cr[:, h, :], in_to_replace=c16[:, h, 0:8], in_values=cand[:, h, :],
                                                                   imm_value=-1e30), r=[cand, c16], w=[cr])
                    for h in range(8):
                        p.op('dve', lambda e, h=h: e.max(out=c16[:, h, 8:16], in_=cr[:, h, :]), r=[cr], w=[c16])
                    p.op('dve', lambda e: e.tensor_scalar(out=Tt[sub][:], in0=c16[:, :, 15], scalar1=1.0 - 1e-6, scalar2=None, op0=ALU.mult),
                         r=[c16], w=[Tt[sub]])
                    p.op('dve', lambda e: e.tensor_reduce(out=zz[:], in_=c16[:], axis=AX.X, op=ALU.add), r=[c16], w=[zz])
                    p.op('dve', lambda e: e.reciprocal(out=kap[sub][:], in_=zz[:]), r=[zz], w=[kap[sub]])
                    for h in range(8):
                        p.op('dve', lambda e, h=h: e.tensor_scalar(out=Dk[sub][h][:], in0=g.ident_f[:], scalar1=kap[sub][:, h:h + 1],
                                                                   scalar2=None, op0=ALU.mult), r=[g.ident_f, kap[sub]], w=[Dk[sub][h]])
            with p.phase():
                NPT = 5
                Pt = [p.sb("pe_P%d" % i, [128, NI, 128], F32) for i in range(NPT)]
                G = [[p.sb("pe_G%d_%d" % (b_, i), [128, 8, NI * 128], BF16) for i in range(2)] for b_ in range(2)]
                ut = [p.sb("pe_ut%d" % i, [128, 8, 512], BF16) for i in range(2)]
                vb = [p.sb("pe_vb%d" % i, [128, 4, D], BF16) for i in range(2)]
                ga = [p.sb("pe_ga%d" % i, [128, 512], F32) for i in range(2)]
                wg = [p.sb("pe_wg%d" % i, [128, 512], BF16) for i in range(2)]
                wgT = [p.sb("pe_wgT%d" % i, [128, 4, 128], BF16) for i in range(2)]
                xs = [p.sb("pe_x%d" % i, [128, D], F32) for i in range(2)]
                xo = [p.sb("pe_xo%d" % i, [128, D], F32) for i in range(2)]
                if last:
                    hb = p.sb("pe_hb", [128, D], F32)
                    st = p.sb("pe_st", [128, 4], F32)
                nblk = 128 // NI
                nesb = NI * 128 // 512
                cnt = {"p": 0}

                def unit(ib, sub, h):
                    P_ = Pt[cnt["p"] % NPT]
                    cnt["p"] += 1
                    G_ = G[ib % 2][sub]
                    if h % 2 == 1:
                        for ii in range(NI):
                            i_ = ib * NI + ii
                            p.op('act', lambda e, ii=ii, i_=i_: e.activation(out=P_[:, ii, :], in_=E[sub][:, 2 * h + 1, :], func=AF.Copy,
                                                                            scale=E[sub][:, 2 * h, i_:i_ + 1]), r=[E[sub]], w=[P_])
                    else:
                        p.op('pool', lambda e: e.tensor_tensor(
                            out=P_[:], in0=E[sub][:, 2 * h, ib * NI:(ib + 1) * NI].unsqueeze(2).broadcast_to([128, NI, 128]),
                            in1=E[sub][:, 2 * h + 1, :].unsqueeze(1).broadcast_to([128, NI, 128]), op=ALU.mult),
                            r=[E[sub]], w=[P_])
                    p.op('dve', lambda e: e.scalar_tensor_tensor(
                        out=G_[:, h, :], in0=P_[:].rearrange("p a b -> p (a b)"), scalar=Tt[sub][:, h:h + 1],
                        in1=P_[:].rearrange("p a b -> p (a b)"), op0=ALU.is_ge, op1=ALU.mult),
                        r=[P_, Tt[sub]], w=[G_])

                iters = [(ib, esb, sub) for ib in range(nblk) for esb in range(nesb) for sub in range(2)]
                import os
                if os.environ.get("PEER_SKIP_MAIN"):
                    iters = iters[:4]
                    nblk = 1
                nit = len(iters)

                def s1(i):
                    ib, esb, sub = iters[i]
                    e0 = ib * NI * 128 + esb * 512
                    b_ = (i // 2) % 2
                    u_, v_ = ut[b_], vb[b_]
                    if sub == 0:
                        p.dma(u_, u_[:], g.UTB[:, :, e0:e0 + 512])
                        p.dma(v_, v_[:], g.VB[:, e0 // 128:e0 // 128 + 4, :])
                    pa, ga_ = psA[i % 2], ga[i % 2]
                    for k in range(8):
                        p.op('pe', lambda e, k=k: e.matmul(pa[:], lhsT=ht[:, k, sub * 128:(sub + 1) * 128], rhs=u_[:, k, :],
                                                           start=(k == 0), stop=(k == 7)), r=[ht, u_], w=[pa])
                    p.op('act', lambda e: e.activation(out=ga_[:], in_=pa[:], func=AF.Gelu), r=[pa], w=[ga_])

                def s2(i):
                    ib, esb, sub = iters[i]
                    G_ = G[ib % 2][sub]
                    ga_, wg_ = ga[i % 2], wg[i % 2]
                    for h in range(8):
                        p.op('pe', lambda e, h=h: e.matmul(psW[:], lhsT=Dk[sub][h][:], rhs=G_[:, h, esb * 512:(esb + 1) * 512],
                                                           start=(h == 0), stop=(h == 7)), r=[Dk[sub][h], G_], w=[psW])
                    p.op('dve', lambda e: e.tensor_tensor(out=wg_[:], in0=psW[:], in1=ga_[:], op=ALU.mult), r=[psW, ga_], w=[wg_])

                def s3(i):
                    emit_transposes(g, wg[i % 2], psT[i % 2], wgT[i % 2][:], wgT[i % 2], n=4)

                def s4(i):
                    ib, esb, sub = iters[i]
                    v_ = vb[(i // 2) % 2]
                    wgT_ = wgT[i % 2]
                    first = (ib == 0 and esb == 0)
                    lastm = (ib == nblk - 1 and esb == nesb - 1)
                    for c4 in range(4):
                        for n in range(2):
                            a_ = acc[sub][n]
                            p.op('pe', lambda e, c4=c4, n=n, a_=a_: e.matmul(
                                a_[:], lhsT=wgT_[:, c4, :], rhs=v_[:, c4, n * 512:(n + 1) * 512],
                                start=(first and c4 == 0), stop=(lastm and c4 == 3)), r=[wgT_, v_], w=[a_])

                pend = []
                for sub in range(2):
                    for h in range(8):
                        unit(0, sub, h)
                s1(0)
                s2(0)
                per_it = 16 // (nesb * 2)
                for i in range(nit):
                    ib = iters[i][0]
                    if i % (nesb * 2) == 0 and ib + 1 < nblk:
                        pend = [(ib + 1, sub, h) for sub in range(2) for h in range(8)]
                    if i + 1 < nit:
                        s1(i + 1)
                    s3(i)
                    if i + 1 < nit:
                        s2(i + 1)
                    for _ in range(per_it):
                        if pend:
                            unit(*pend.pop(0))
                    s4(i)
                for sub in range(2):
                    sl = slice(t0 + sub * 128, t0 + (sub + 1) * 128)
                    x_, o_ = xs[sub], xo[sub]
                    p.dma(x_, x_[:], g.X1[sl, :])
                    for n in range(2):
                        cs = slice(n * 512, (n + 1) * 512)
                        p.op('dve', lambda e, n=n, cs=cs: e.tensor_tensor(out=o_[:, cs], in0=acc[sub][n][:], in1=gt[which][:, cs], op=ALU.mult),
                             r=[acc[sub][n], gt[which]], w=[o_])
                    p.op('pool', lambda e: e.tensor_tensor(out=o_[:], in0=o_[:], in1=x_[:], op=ALU.add), r=[o_, x_], w=[o_])
                    if not last:
                        p.dma(None, g.X2[sl, :], o_[:], r=[o_], q='pool')
                    else:
                        p.op('act', lambda e: e.activation(out=hb[:], in_=o_[:], func=AF.Square, accum_out=st[:, 0:1]), r=[o_], w=[hb, st])
                        p.op('dve', lambda e: e.tensor_scalar(out=st[:, 1:2], in0=st[:, 0:1], scalar1=1.0 / D, scalar2=EPS,
                                                              op0=ALU.mult, op1=ALU.add), r=[st], w=[st])
                        p.op('act', lambda e: e.activation(out=st[:, 2:3], in_=st[:, 1:2], func=AF.Sqrt), r=[st], w=[st])
                        p.op('dve', lambda e: e.reciprocal(out=st[:, 3:4], in_=st[:, 2:3]), r=[st], w=[st])
                        p.op('dve', lambda e: e.scalar_tensor_tensor(out=hb[:], in0=o_[:], scalar=st[:, 3:4], in1=fg[:],
                                                                     op0=ALU.mult, op1=ALU.mult), r=[o_, st, fg], w=[hb])
                        p.dma(None, g.out[t0 - LC + sub * 128:t0 - LC + (sub + 1) * 128, :], hb[:], r=[hb], q='pool')


_PROG_CACHE = {}


def kernel(x, c, ctx, c_ctx, w_ada, b_ada, norm1_g, w_in, na_rpb, diff_lambda, diff_subln_g, ret_decay_logit,
           w_out, norm2_g, peer_wq, peer_subkeys, peer_u, peer_v, final_g):
    inp = dict(x=x, c=c, ctx=ctx, c_ctx=c_ctx, w_ada=w_ada, b_ada=b_ada, norm1_g=norm1_g, w_in=w_in, na_rpb=na_rpb,
               diff_lambda=diff_lambda, diff_subln_g=diff_subln_g, ret_decay_logit=ret_decay_logit, w_out=w_out,
               norm2_g=norm2_g, peer_wq=peer_wq, peer_subkeys=peer_subkeys, peer_u=peer_u, peer_v=peer_v, final_g=final_g)
    inp = {k: np.asarray(v) for k, v in inp.items()}
    B = inp["x"].shape[0]
    cs = _consts()
    shared = {"final_g": np.asarray(inp["final_g"], np.float32).reshape(1, D)}
    for n in CONST_NAMES:
        shared[n] = cs[n]
    for l in range(DEPTH):
        for k, v in _layer_inputs(inp, l).items():
            shared["L%d_%s" % (l, k)] = v
    ccv = np.asarray(inp["c_ctx"], np.float32).reshape(8, 128).T
    in_maps = []
    for b in range(B):
        m = dict(shared)
        m["xin"] = np.ascontiguousarray(np.concatenate([inp["ctx"][b], inp["x"][b]], axis=0).astype(np.float32))
        m["cvec"] = np.ascontiguousarray(np.stack([np.asarray(inp["c"][b], np.float32).reshape(8, 128).T, ccv], axis=-1))
        in_maps.append(m)
    if "nc" not in _PROG_CACHE:
        _PROG_CACHE["nc"] = build_program()[0]
    nc = _PROG_CACHE["nc"]
    res = run_bass_kernel_spmd(nc, in_maps, core_ids=list(range(B)))
    return np.stack([np.asarray(r["out"], dtype=np.float32) for r in res.results], axis=0)
```

```python
import numpy as np
from contextlib import ExitStack, contextmanager
import concourse.bass as bass
import concourse.mybir as mybir
from concourse.bass_utils import run_bass_kernel_spmd

F32 = mybir.dt.float32
BF16 = mybir.dt.bfloat16
AF = mybir.ActivationFunctionType
ALU = mybir.AluOpType
AX = mybir.AxisListType


class Buf:
    def __init__(self, name):
        self.name = name
        self.w = None
        self.r = {}


class T(Buf):
    def __init__(self, name, h):
        super().__init__(name)
        self.h = h

    def __getitem__(self, idx):
        return self.h[idx]


class Prog:
    ENG = ('pe', 'act', 'dve', 'pool', 'sp')

    def __init__(self, nc, ndma=48):
        self.nc = nc
        self.ndma = ndma
        self.es = ExitStack()
        self.scope = None
        self.drams = {}

    def __enter__(self):
        nc = self.nc
        self.es.__enter__()
        self.engs = {'pe': nc.tensor, 'act': nc.scalar, 'dve': nc.vector, 'pool': nc.gpsimd, 'sp': nc.sync}
        self.sems = {k: self.es.enter_context(nc.semaphore("s_" + k)) for k in ('pe', 'act', 'dve', 'pool')}
        self.cnt = {k: 0 for k in self.sems}
        self.dsem = [self.es.enter_context(nc.semaphore("d%d" % i)) for i in range(self.ndma)]
        self.dval = [0] * self.ndma
        self.dnext = 0
        self.dnext_sw = 0
        self.seen = {e: {} for e in self.ENG}
        self.scope = self.es
        self.ninst = 0
        return self

    def __exit__(self, *a):
        return self.es.__exit__(*a)

    def _uniq(self, name):
        self.nalloc = getattr(self, "nalloc", 0) + 1
        return "%s_%d" % (name, self.nalloc)

    def sb(self, name, shape, dtype):
        name = self._uniq(name)
        return T(name, self.scope.enter_context(self.nc.sbuf_tensor(name, list(shape), dtype)))

    def ps(self, name, shape, dtype):
        name = self._uniq(name)
        return T(name, self.scope.enter_context(self.nc.psum_tensor(name, list(shape), dtype)))

    def dram_buf(self, name):
        if name not in self.drams:
            self.drams[name] = Buf(name)
        return self.drams[name]

    @contextmanager
    def phase(self):
        old = self.scope
        with ExitStack() as st:
            self.scope = st
            yield
            self.barrier()
        self.scope = old

    def _semobj(self, key):
        return self.sems[key] if isinstance(key, str) else self.dsem[key[1]]

    def _wait(self, eng, tok, raw=False):
        key, val = tok
        if key == eng and not (raw and eng != 'pe'):
            return
        if self.seen[eng].get(key, 0) >= val:
            return
        self.engs[eng].wait_ge(self._semobj(key), val)
        self.seen[eng][key] = val
        self.ninst += 1

    def _deps(self, eng, r, w):
        for b in r:
            if b.w is not None:
                self._wait(eng, b.w, raw=True)
        for b in w:
            if b.w is not None:
                self._wait(eng, b.w)
            for k, v in b.r.items():
                self._wait(eng, (k, v))

    def _mark(self, tok, r, w):
        k, v = tok
        for b in r:
            if b.r.get(k, 0) < v:
                b.r[k] = v
        for b in w:
            b.w = tok
            b.r = {}

    def op(self, eng, fn, r=(), w=()):
        self._deps(eng, r, w)
        inst = fn(self.engs[eng])
        self.cnt[eng] += 1
        inst.then_inc(self.sems[eng], 1)
        self._mark((eng, self.cnt[eng]), r, w)
        self.ninst += 1

    def dma(self, wbuf, out_ap, in_ap, r=(), q='sp', **kw):
        half = self.ndma // 2
        if q == 'pool':
            i = half + self.dnext_sw
            self.dnext_sw = (self.dnext_sw + 1) % (self.ndma - half)
        else:
            i = self.dnext
            self.dnext = (self.dnext + 1) % half
        if self.dval[i] > 0:
            self._wait(q, (('d', i), self.dval[i]))
        w = [] if wbuf is None else ([wbuf] if isinstance(wbuf, Buf) else list(wbuf))
        self._deps(q, r, w)
        inst = self.engs[q].dma_start(out=out_ap, in_=in_ap, **kw)
        self.dval[i] += 16
        inst.then_inc(self.dsem[i], 16)
        self._mark((('d', i), self.dval[i]), r, w)
        self.ninst += 1

    def barrier(self):
        for e in self.ENG:
            for f in self.sems:
                if self.cnt[f] > 0:
                    self._wait(e, (f, self.cnt[f]))
            for i in range(self.ndma):
                if self.dval[i] > 0:
                    self._wait(e, (('d', i), self.dval[i]))

    def finish(self):
        self.barrier()


D = 1024
L = 4096
LC = 256
NT = L + LC
NTILE = NT // 128
DEPTH = 2
GW = 64
EPS = 1e-6
NEGB = -30000.0
O_NAQ, O_NAK, O_NAV, O_DFQ, O_DFK, O_DFV, O_RTQ, O_RTK, O_RTV, O_RTG = 0, 256, 512, 768, 1024, 1280, 1536, 1792, 2048, 2560
C_QNA, C_KNA, C_QDF, C_KDA, C_KDB, C_QRT, C_KRT = 0, 2, 4, 6, 8, 10, 12
NFMC = 14


def _rope_tables():
    t = np.arange(L)
    rows = (t // GW).astype(np.float32)
    cols = (t % GW).astype(np.float32)
    out = {}
    for name, dim in (("df", 32), ("rt", 64)):
        half = dim // 2
        inv = np.power(np.float32(10000.0), -np.arange(0, half, 2, dtype=np.float32) / np.float32(half)).astype(np.float32)
        ang = np.concatenate([rows[:, None] * inv, cols[:, None] * inv], axis=-1).astype(np.float32)
        cos = np.cos(ang).astype(np.float32)
        sin = np.sin(ang).astype(np.float32)
        C = np.ones((128, NT), np.float32)
        S = np.zeros((128, NT), np.float32)
        for r in range(128):
            d = r % dim
            pi = d // 2
            C[r, LC:] = cos[:, pi]
            S[r, LC:] = -sin[:, pi] if d % 2 == 0 else sin[:, pi]
        out[name] = (C, S)
    return out


def _na_patterns():
    pats = {}
    for pname, r in (("r0", 0), ("r2", 2), ("mid", 8), ("r60", 60), ("r62", 62)):
        kr0 = min(max(r - 4, 0), 54)
        chunks = []
        for m in range(5):
            dr = np.zeros((128, 128), np.int64)
            dc = np.zeros((128, 128), np.int64)
            va = np.zeros((128, 128), bool)
            for kk in range(128):
                krow = kr0 + 2 * m + kk // 64
                kc = kk % 64
                for qq in range(128):
                    qrow = r + qq // 64
                    qc = qq % 64
                    r0 = min(max(qrow - 4, 0), 56)
                    cs = min(max(qc - 8, 0), 48)
                    ok = (r0 <= krow < r0 + 8) and (cs <= kc < cs + 16) and krow < 64
                    va[kk, qq] = ok
                    if ok:
                        dr[kk, qq] = krow - qrow + 7
                        dc[kk, qq] = min(max(kc - qc, -15), 15) + 15
            chunks.append((dr, dc, va))
        pats[pname] = chunks
    return pats


_CONST_CACHE = {}


def _consts():
    if _CONST_CACHE:
        return _CONST_CACHE
    c = _CONST_CACHE
    rt = _rope_tables()
    s_df = np.float32(32 ** -0.5)
    c["T_DFQ_C"] = rt["df"][0] * s_df
    c["T_DFQ_S"] = rt["df"][1] * s_df
    c["T_DFK_C"] = rt["df"][0]
    c["T_DFK_S"] = rt["df"][1]
    c["T_RTQ_C"] = rt["rt"][0]
    c["T_RTQ_S"] = rt["rt"][1]
    c["T_RTK_C"] = rt["rt"][0] * np.float32(0.125)
    c["T_RTK_S"] = rt["rt"][1] * np.float32(0.125)
    c["IDENT"] = np.eye(128, dtype=np.float32)
    pats = _na_patterns()
    c["_pats"] = pats
    names = ["r0", "r2", "mid", "r60", "r62"]
    mask = np.zeros((128, 5, 5, 128), np.float32)
    for pi, pn in enumerate(names):
        for m in range(5):
            mask[:, pi, m, :] = np.where(pats[pn][m][2], 0.0, NEGB)
    c["NA_MASK"] = mask
    i = np.arange(128, dtype=np.float32)
    ij = i[None, :] - i[:, None]
    c["RT_RIJ"] = np.maximum(ij, 0).astype(np.float32)
    c["RT_MIJ"] = (ij >= 0).astype(np.float32)
    c["RT_RJI"] = np.maximum(-ij, 0).astype(np.float32)
    c["RT_MJI"] = (ij <= 0).astype(np.float32)
    c["RT_IROW"] = np.broadcast_to(i[None, :] + 1.0, (128, 128)).astype(np.float32).copy()
    c["RT_IROWB"] = np.broadcast_to(128.0 - i[None, :], (128, 128)).astype(np.float32).copy()
    c["RT_JCOL"] = np.stack([127.0 - i, i], axis=1).astype(np.float32)
    return c


def _layer_inputs(inp, l):
    c = _consts()
    w_in = np.asarray(inp["w_in"][l], np.float32)
    Z = np.zeros((D, 32), np.float32)

    def sw(cols):
        return cols ^ 1

    fm = []

    def add(cols):
        fm.append(w_in[:, cols])

    ar = np.arange
    for j in range(2):
        add(O_NAQ + j * 128 + ar(128))
    for j in range(2):
        add(O_NAK + j * 128 + ar(128))
    for j in range(2):
        cc = O_DFQ + j * 128 + ar(128)
        add(cc); add(sw(cc))
    for comp in range(2):
        for j in range(2):
            blocks_m, blocks_s = [], []
            for hh in range(2):
                h = 2 * j + hh
                cc = O_DFK + h * 64 + comp * 32 + ar(32)
                if comp == 0:
                    blocks_m += [w_in[:, cc], Z]; blocks_s += [w_in[:, sw(cc)], Z]
                else:
                    blocks_m += [Z, w_in[:, cc]]; blocks_s += [Z, w_in[:, sw(cc)]]
            fm.append(np.concatenate(blocks_m, axis=1)); fm.append(np.concatenate(blocks_s, axis=1))
    for j in range(2):
        cc = O_RTQ + j * 128 + ar(128)
        add(cc); add(sw(cc))
    for j in range(2):
        cc = O_RTK + j * 128 + ar(128)
        add(cc); add(sw(cc))
    WFM = np.ascontiguousarray(np.concatenate(fm, axis=1))
    WTM = np.ascontiguousarray(np.concatenate([w_in[:, O_NAV:O_NAV + 256], w_in[:, O_DFV:O_DFV + 256],
                                               w_in[:, O_RTV:O_RTV + 512], w_in[:, O_RTG:O_RTG + 512]], axis=1))
    rpb = np.asarray(inp["na_rpb"][l], np.float32)
    pats = c["_pats"]
    names = ["r0", "r2", "mid", "r60", "r62"]
    nab = np.zeros((128, 4, 5, 5, 128), np.float32)
    for pi, pn in enumerate(names):
        for m in range(5):
            dr, dc, va = pats[pn][m]
            for h in range(4):
                nab[:, h, pi, m, :] = rpb[h][dr, dc]
    sk = np.asarray(inp["peer_subkeys"][l], np.float32).reshape(16, 128, 128)
    skt = np.ascontiguousarray(sk.transpose(2, 0, 1))
    d = {
        "w_ada": np.asarray(inp["w_ada"][l], np.float32),
        "b_ada": np.asarray(inp["b_ada"][l], np.float32).reshape(1, 6 * D),
        "g1": np.asarray(inp["norm1_g"][l], np.float32).reshape(1, D),
        "g2": np.asarray(inp["norm2_g"][l], np.float32).reshape(1, D),
        "wfm": WFM, "wtm": WTM, "nab": nab,
        "lam": np.asarray(inp["diff_lambda"][l], np.float32).reshape(1, 128),
        "subg": np.asarray(inp["diff_subln_g"][l], np.float32).reshape(1, 64),
        "dec": np.asarray(inp["ret_decay_logit"][l], np.float32).reshape(1, 8),
        "wout": np.asarray(inp["w_out"][l], np.float32),
        "wq": np.asarray(inp["peer_wq"][l], np.float32),
        "skt": skt,
        "ut": np.ascontiguousarray(np.asarray(inp["peer_u"][l], np.float32).T),
        "v": np.asarray(inp["peer_v"][l], np.float32),
    }
    return d


LAYER_SHAPES = {
    "w_ada": [D, 6 * D], "b_ada": [1, 6 * D], "g1": [1, D], "g2": [1, D], "wfm": [D, 24 * 128], "wtm": [D, 1536],
    "nab": [128, 4, 5, 5, 128], "lam": [1, 128], "subg": [1, 64], "dec": [1, 8], "wout": [D, D], "wq": [D, 2048],
    "skt": [128, 16, 128], "ut": [D, 16384], "v": [16384, D],
}
CONST_NAMES = ["T_DFQ_C", "T_DFQ_S", "T_DFK_C", "T_DFK_S", "T_RTQ_C", "T_RTQ_S", "T_RTK_C", "T_RTK_S", "IDENT", "NA_MASK",
               "RT_RIJ", "RT_MIJ", "RT_RJI", "RT_MJI", "RT_IROW", "RT_IROWB", "RT_JCOL"]


class Ctx:
    pass


def build_program(nlayers=DEPTH, debug=None, stop_after=None):
    nc = bass.Bass("TRN2", target_bir_lowering=False)
    g = Ctx()
    g.nc = nc
    g.debug = debug or ()
    din = lambda name, shape, dt=F32: nc.dram_tensor(name, list(shape), dt, kind="ExternalInput").ap()
    dscr = lambda name, shape, dt: nc.dram_tensor(name, list(shape), dt, kind="Internal").ap()
    g.xin = din("xin", [NT, D])
    g.cvec = din("cvec", [128, 8, 2])
    g.final_g = din("final_g", [1, D])
    g.cst = {}
    cs = _consts()
    for n in CONST_NAMES:
        g.cst[n] = din(n, cs[n].shape)
    g.lay = []
    for l in range(nlayers):
        g.lay.append({k: din("L%d_%s" % (l, k), shp) for k, shp in LAYER_SHAPES.items()})
    g.out = nc.dram_tensor("out", [L, D], F32, kind="ExternalOutput").ap()
    g.MODS = dscr("MODS", [2, 6 * D], F32)
    g.X1 = dscr("X1", [NT, D], F32)
    g.X2 = dscr("X2", [NT, D], F32)
    g.FMS = dscr("FMS", [NFMC, 128, NT], BF16)
    g.VNA = dscr("VNA", [NT, 4, 65], BF16)
    g.VDF = dscr("VDF", [NT, 4, 65], BF16)
    g.VRT = dscr("VRT", [NT, 512], BF16)
    g.GRT = dscr("GRT", [NT, 512], BF16)
    g.KRTT = dscr("KRTT", [NT, 256], BF16)
    g.Y = dscr("Y", [NT, D], BF16)
    g.H2T = dscr("H2T", [128, 8, NT], BF16)
    g.QPT = dscr("QPT", [128, 16, NT], BF16)
    g.UTB = dscr("UTB", [128, 8, 16384], BF16)
    g.VB = dscr("VB", [128, 128, D], BF16)
    g.dbg = {}
    for name, shape, dt in g.debug:
        g.dbg[name] = nc.dram_tensor("dbg_" + name, list(shape), dt, kind="ExternalOutput").ap()
    p = Prog(nc)
    g.p = p
    g.scr = Buf("scratch")
    with p:
        g.ident_f = p.sb("ident_f", [128, 128], F32)
        g.ident = p.sb("ident", [128, 128], BF16)
        p.dma(g.ident_f, g.ident_f[:], g.cst["IDENT"])
        p.op('dve', lambda e: e.tensor_copy(out=g.ident[:], in_=g.ident_f[:]), r=[g.ident_f], w=[g.ident])
        stages = []
        for l in range(nlayers):
            last = (l == DEPTH - 1)
            xa = g.xin if l == 0 else g.X2
            stages += [("mod%d" % l, lambda l=l: phase_mod(g, l)),
                       ("p1_%d" % l, lambda l=l, xa=xa: phase_p1(g, l, xa)),
                       ("na_%d" % l, lambda l=l, last=last: phase_na(g, l, not last)),
                       ("df_%d" % l, lambda l=l, last=last: phase_diff(g, l, not last)),
                       ("rt_%d" % l, lambda l=l, last=last: (phase_ret(g, l, not last), dbg_dump(g, "y%d" % l, g.Y))),
                       ("p3_%d" % l, lambda l=l, last=last, xa=xa: (phase_p3(g, l, xa, last), dbg_dump(g, "x1_%d" % l, g.X1))),
                       ("peerq%d" % l, lambda l=l, last=last: phase_peerq(g, l, last)),
                       ("peer%d" % l, lambda l=l, last=last: (phase_peer(g, l, last), dbg_dump(g, "x2_%d" % l, g.X2)))]
        for name, fn in stages:
            fn()
            if stop_after == name:
                break
        p.finish()
    g.ninst = p.ninst
    return nc, g


def dbg_dump(g, name, src):
    if name in g.dbg:
        g.p.dma(None, g.dbg[name], src, q='sp')
        g.p.barrier()


def load_bcast(p, t, dram_row, n, q='sp'):
    p.dma(t, t[:, 0:n], dram_row.broadcast_to([128, n]), q=q)


def phase_mod(g, l):
    p = g.p
    Lw = g.lay[l]
    with p.phase():
        cv = p.sb("cv", [128, 8, 2], F32)
        sil = p.sb("sil", [128, 8, 2], F32)
        bad = p.sb("bad", [2, 6 * D], F32)
        modt = p.sb("modt", [2, 6 * D], F32)
        wa = [p.sb("wa%d" % i, [128, 8, 512], F32) for i in range(2)]
        ps = [p.ps("modps%d" % i, [128, 512], F32) for i in range(2)]
        p.dma(cv, cv[:], g.cvec)
        p.dma(bad, bad[:], Lw["b_ada"].broadcast_to([2, 6 * D]))
        p.op('act', lambda e: e.activation(out=sil[:], in_=cv[:], func=AF.Silu), r=[cv], w=[sil])
        wsrc = Lw["w_ada"].rearrange("(k p) n -> p k n", p=128)
        for n in range(12):
            w = wa[n % 2]
            pp = ps[n % 2]
            p.dma(w, w[:], wsrc[:, :, n * 512:(n + 1) * 512])
            for k in range(8):
                p.op('pe', lambda e, k=k, w=w, pp=pp: e.matmul(pp[0:2, :], lhsT=sil[:, k, :], rhs=w[:, k, :],
                                                             start=(k == 0), stop=(k == 7)), r=[sil, w], w=[pp])
            p.op('dve', lambda e, n=n, pp=pp: e.tensor_tensor(out=modt[:, n * 512:(n + 1) * 512], in0=pp[0:2, :],
                                                             in1=bad[:, n * 512:(n + 1) * 512], op=ALU.add),
                 r=[pp, bad], w=[modt])
        p.dma(None, g.MODS, modt[:], r=[modt], q='pool')
        if "mods" in g.dbg:
            p.dma(None, g.dbg["mods"], modt[:], r=[modt], q='pool')


def mod_row(g, which, idx):
    return g.MODS[which:which + 1, idx * D:(idx + 1) * D]


def make_norm_consts(g, tag, grow, idx_shift, idx_scale):
    p = g.p
    gb = p.sb(tag + "_gb", [128, D], F32)
    load_bcast(p, gb, grow, D)
    res = []
    for which in range(2):
        sc = p.sb(tag + "_sc%d" % which, [128, D], F32)
        sh = p.sb(tag + "_sh%d" % which, [128, D], F32)
        load_bcast(p, sc, mod_row(g, which, idx_scale), D)
        load_bcast(p, sh, mod_row(g, which, idx_shift), D)
        p.op('dve', lambda e, sc=sc: e.scalar_tensor_tensor(out=sc[:], in0=sc[:], scalar=1.0, in1=gb[:],
                                                            op0=ALU.add, op1=ALU.mult), r=[sc, gb], w=[sc])
        res.append((sc, sh))
    return res


def emit_norm(g, xt, gs, sh, hb, tmp, st):
    p = g.p
    p.op('act', lambda e: e.activation(out=tmp[:], in_=xt[:], func=AF.Square, accum_out=st[:, 0:1]), r=[xt], w=[tmp, st])
    p.op('dve', lambda e: e.tensor_scalar(out=st[:, 1:2], in0=st[:, 0:1], scalar1=1.0 / D, scalar2=EPS,
                                          op0=ALU.mult, op1=ALU.add), r=[st], w=[st])
    p.op('act', lambda e: e.activation(out=st[:, 2:3], in_=st[:, 1:2], func=AF.Sqrt), r=[st], w=[st])
    p.op('dve', lambda e: e.reciprocal(out=st[:, 3:4], in_=st[:, 2:3]), r=[st], w=[st])
    p.op('dve', lambda e: e.scalar_tensor_tensor(out=tmp[:], in0=xt[:], scalar=st[:, 3:4], in1=gs[:],
                                                 op0=ALU.mult, op1=ALU.mult), r=[xt, st, gs], w=[tmp])
    p.op('dve', lambda e: e.tensor_tensor(out=hb[:], in0=tmp[:], in1=sh[:], op=ALU.add), r=[tmp, sh], w=[hb])


def emit_transposes(g, src, pst, dst_ap, dst_buf, n=8, eng='act'):
    p = g.p
    for k in range(n):
        p.op('pe', lambda e, k=k: e.transpose(pst[:, k, :], src[:, k * 128:(k + 1) * 128], g.ident[:]),
             r=[src, g.ident], w=[pst])
    if eng == 'act':
        p.op('act', lambda e: e.copy(out=dst_ap, in_=pst[:, 0:n, :]), r=[pst], w=[dst_buf])
    else:
        p.op('dve', lambda e: e.tensor_copy(out=dst_ap, in_=pst[:, 0:n, :]), r=[pst], w=[dst_buf])


def tok_groups():
    gs = [(0, 256)]
    for i in range(8):
        gs.append((256 + i * 512, 512))
    return gs


def phase_p1(g, l, xa):
    p = g.p
    Lw = g.lay[l]
    with p.phase():
        HT = p.sb("HT", [128, 8, NT], BF16)
        with p.phase():
            nrm = make_norm_consts(g, "n1", Lw["g1"], 0, 1)
            xt = [p.sb("xt%d" % i, [128, D], F32) for i in range(2)]
            tmp = p.sb("ntmp", [128, D], F32)
            hb = [p.sb("hb%d" % i, [128, D], BF16) for i in range(2)]
            st = [p.sb("nst%d" % i, [128, 4], F32) for i in range(2)]
            pst = [p.ps("pst%d" % i, [128, 8, 128], BF16) for i in range(2)]
            for tt in range(NTILE):
                which = 1 if tt < 2 else 0
                x_ = xt[tt % 2]
                p.dma(x_, x_[:], xa[tt * 128:(tt + 1) * 128, :])
                emit_norm(g, x_, nrm[which][0], nrm[which][1], hb[tt % 2], tmp, st[tt % 2])
                emit_transposes(g, hb[tt % 2], pst[tt % 2], HT[:, :, tt * 128:(tt + 1) * 128], HT)
        if "ht" in g.dbg:
            p.dma(None, g.dbg["ht"], HT[:], r=[HT], q='pool')
        with p.phase():
            wf = [p.sb("wf%d" % i, [128, 8, 128], F32) for i in range(2)]
            wb = [p.sb("wb%d" % i, [128, 8, 128], BF16) for i in range(4)]
            tc_ = [p.sb("tc%d" % i, [128, 512], F32) for i in range(2)]
            ts_ = [p.sb("ts%d" % i, [128, 512], F32) for i in range(2)]
            t1 = [p.sb("rt1_%d" % i, [128, 512], F32) for i in range(2)]
            stg = [p.sb("fstg%d" % i, [128, 512], BF16) for i in range(2)]
            ktm = [p.sb("ktm%d" % i, [128, 4, 128], BF16) for i in range(2)]
            psA = [p.ps("psA%d" % i, [128, 512], F32) for i in range(2)]
            psB = [p.ps("psB%d" % i, [128, 512], F32) for i in range(2)]
            psT = [p.ps("psT%d" % i, [128, 8, 128], BF16) for i in range(2)]
            wsrc = Lw["wfm"].rearrange("(k p) n -> p k n", p=128)
            cnt = {"w": 0, "it": 0}

            def load_w(ci):
                i = cnt["w"]
                cnt["w"] += 1
                f, b = wf[i % 2], wb[i % 4]
                p.dma(f, f[:], wsrc[:, :, ci * 128:(ci + 1) * 128])
                p.op('pool', lambda e: e.tensor_copy(out=b[:], in_=f[:]), r=[f], w=[b])
                return b

            def mm(ps_, w_, t0, n):
                for k in range(8):
                    p.op('pe', lambda e, k=k: e.matmul(ps_[:, 0:n], lhsT=w_[:, k, :], rhs=HT[:, k, t0:t0 + n],
                                                       start=(k == 0), stop=(k == 7)), r=[w_, HT], w=[ps_])

            jobs = []
            wi = 0
            for j in range(2):
                jobs.append((C_QNA + j, wi, None, 0.125, None)); wi += 1
            for j in range(2):
                jobs.append((C_KNA + j, wi, None, 1.0, None)); wi += 1
            for dest, tn in ((C_QDF, "T_DFQ"), (C_KDA, "T_DFK"), (C_KDB, "T_DFK"), (C_QRT, "T_RTQ"), (C_KRT, "T_RTK")):
                for j in range(2):
                    jobs.append((dest + j, wi, wi + 1, 1.0, tn)); wi += 2
            for (dest, wm, ws, scale, tn) in jobs:
                wmb = load_w(wm)
                wsb = load_w(ws) if ws is not None else None
                for (t0, n) in tok_groups():
                    it = cnt["it"]
                    cnt["it"] += 1
                    a, b_ = psA[it % 2], psB[it % 2]
                    sg = stg[it % 2]
                    mm(a, wmb, t0, n)
                    if ws is None:
                        p.op('act', lambda e: e.activation(out=sg[:, 0:n], in_=a[:, 0:n], func=AF.Copy, scale=scale),
                             r=[a], w=[sg])
                    else:
                        mm(b_, wsb, t0, n)
                        tcc, tss, tt1 = tc_[it % 2], ts_[it % 2], t1[it % 2]
                        p.dma(tcc, tcc[:, 0:n], g.cst[tn + "_C"][:, t0:t0 + n])
                        p.dma(tss, tss[:, 0:n], g.cst[tn + "_S"][:, t0:t0 + n])
                        p.op('dve', lambda e: e.tensor_tensor(out=tcc[:, 0:n], in0=a[:, 0:n], in1=tcc[:, 0:n], op=ALU.mult),
                             r=[a, tcc], w=[tcc])
                        p.op('dve', lambda e: e.tensor_tensor(out=tss[:, 0:n], in0=b_[:, 0:n], in1=tss[:, 0:n], op=ALU.mult),
                             r=[b_, tss], w=[tss])
                        p.op('pool', lambda e: e.tensor_tensor(out=sg[:, 0:n], in0=tcc[:, 0:n], in1=tss[:, 0:n], op=ALU.add),
                             r=[tcc, tss], w=[sg])
                    p.dma(None, g.FMS[dest, :, t0:t0 + n], sg[:, 0:n], r=[sg], q='pool')
                    if dest in (C_KRT, C_KRT + 1):
                        nt_ = n // 128
                        pt, kt = psT[it % 2], ktm[it % 2]
                        emit_transposes(g, sg, pt, kt[:, 0:nt_, :], kt, n=nt_)
                        cj = dest - C_KRT
                        p.dma(None, g.KRTT[t0:t0 + n, cj * 128:(cj + 1) * 128].rearrange("(a p) c -> p a c", p=128),
                              kt[:, 0:nt_, :], r=[kt], q='pool')
        with p.phase():
            wtf = p.sb("wtf", [128, 8, 512], F32)
            wtb = p.sb("wtb", [128, 8, 1536], BF16)
            wsrc = Lw["wtm"].rearrange("(k p) n -> p k n", p=128)
            for c3 in range(3):
                p.dma(wtf, wtf[:], wsrc[:, :, c3 * 512:(c3 + 1) * 512])
                p.op('dve', lambda e, c3=c3: e.tensor_copy(out=wtb[:, :, c3 * 512:(c3 + 1) * 512], in_=wtf[:]), r=[wtf], w=[wtb])
            pv = [p.ps("pv%d" % i, [128, 512], F32) for i in range(4)]
            vst = [p.sb("vst%d" % i, [128, 2, 4, 65], BF16) for i in range(2)]
            rst = [p.sb("rst%d" % i, [128, 2, 512], BF16) for i in range(2)]
            for i in range(2):
                p.op('pool', lambda e, i=i: e.memset(vst[i][:], 1.0), w=[vst[i]])
            it = 0
            for tt in range(NTILE):
                vs, rs = vst[tt % 2], rst[tt % 2]
                for c3 in range(3):
                    ps_ = pv[it % 4]
                    it += 1
                    for k in range(8):
                        p.op('pe', lambda e, k=k, ps_=ps_, c3=c3: e.matmul(ps_[:], lhsT=HT[:, k, tt * 128:(tt + 1) * 128],
                                                                          rhs=wtb[:, k, c3 * 512:(c3 + 1) * 512],
                                                                          start=(k == 0), stop=(k == 7)), r=[HT, wtb], w=[ps_])
                    if c3 == 0:
                        p.op('act', lambda e, ps_=ps_: e.copy(out=vs[:, :, :, 0:64],
                                                             in_=ps_[:].rearrange("p (a h d) -> p a h d", a=2, h=4)),
                             r=[ps_], w=[vs])
                    elif c3 == 1:
                        p.op('dve', lambda e, ps_=ps_: e.tensor_copy(out=rs[:, 0, :], in_=ps_[:]), r=[ps_], w=[rs])
                    else:
                        p.op('act', lambda e, ps_=ps_: e.activation(out=rs[:, 1, :], in_=ps_[:], func=AF.Silu), r=[ps_], w=[rs])
                sl = slice(tt * 128, (tt + 1) * 128)
                p.dma(None, g.VNA[sl], vs[:, 0], r=[vs], q='pool')
                p.dma(None, g.VDF[sl], vs[:, 1], r=[vs], q='pool')
                p.dma(None, g.VRT[sl], rs[:, 0, :], r=[rs], q='pool')
                p.dma(None, g.GRT[sl], rs[:, 1, :], r=[rs], q='pool')
        for nm, src in (("fms", g.FMS), ("vna", g.VNA), ("vdf", g.VDF), ("vrt", g.VRT), ("grt", g.GRT), ("krtt", g.KRTT)):
            if nm in g.dbg:
                p.dma(None, g.dbg[nm], src, q='sp')


def phase_na(g, l, do_ctx):
    p = g.p
    Lw = g.lay[l]
    with p.phase():
        bias = p.sb("na_bias", [128, 4, 25, 128], BF16)
        with p.phase():
            stg = p.sb("na_bstg", [128, 25, 128], F32)
            msk = p.sb("na_msk", [128, 25, 128], F32)
            p.dma(msk, msk[:], g.cst["NA_MASK"].rearrange("p a b q -> p (a b) q"))
            for h in range(4):
                p.dma(stg, stg[:], Lw["nab"][:, h].rearrange("p a b q -> p (a b) q"))
                p.op('dve', lambda e, h=h: e.tensor_tensor(out=bias[:, h], in0=stg[:], in1=msk[:], op=ALU.add),
                     r=[stg, msk], w=[bias])
        V = p.sb("na_v", [128, NTILE, 260], BF16)
        p.dma(V, V[:], g.VNA.rearrange("(a p) h d -> p a (h d)", p=128))
        qT = [p.sb("na_q%d" % i, [128, NT], BF16) for i in range(2)]
        kT = [p.sb("na_k%d" % i, [128, NT], BF16) for i in range(2)]
        for i in range(2):
            p.dma(qT[i], qT[i][:], g.FMS[C_QNA + i])
            p.dma(kT[i], kT[i][:], g.FMS[C_KNA + i])
        psS = [[p.ps("na_s%d%d" % (i, j), [128, 512], F32) for j in range(2)] for i in range(2)]
        pso = [p.ps("na_o%d" % i, [128, 512], F32) for i in range(2)]
        PT = [p.sb("na_pt%d" % i, [128, 8, 128], BF16) for i in range(2)]
        ys = [p.sb("na_ys%d" % i, [128, 256], BF16) for i in range(2)]
        rc = [p.sb("na_rc%d" % i, [128, 1], F32) for i in range(2)]
        tiles = []
        if do_ctx:
            for q0 in (0, 128):
                tiles.append((q0, [(0, None), (128, None)], 0))
        for rp in range(32):
            r = 2 * rp
            kr0 = min(max(r - 4, 0), 54)
            pat = {0: 0, 2: 1, 60: 3, 62: 4}.get(r, 2)
            ch = [(LC + (kr0 + 2 * m) * 64, m) for m in range(5)] + [(0, None), (128, None)]
            tiles.append((LC + rp * 128, ch, pat))
        it = 0
        for qi, (q0, ch, pat) in enumerate(tiles):
            y_ = ys[qi % 2]
            for h in range(4):
                hp, base = h // 2, (h % 2) * 64
                sA, sB = psS[it % 2]
                po, pt, rc_ = pso[it % 2], PT[it % 2], rc[it % 2]
                it += 1
                n = len(ch)
                for i, (tok0, m) in enumerate(ch):
                    bank = sA if i < 4 else sB
                    col = (i % 4) * 128
                    p.op('pe', lambda e, bank=bank, col=col, tok0=tok0, m=m: e.matmul(
                        bank[:, col:col + 128], lhsT=kT[hp][base:base + 64, tok0:tok0 + 128],
                        rhs=qT[hp][base:base + 64, q0:q0 + 128], start=True, stop=(m is None)),
                        r=[kT[hp], qT[hp]], w=[bank])
                    if m is not None:
                        p.op('pe', lambda e, bank=bank, col=col, m=m: e.matmul(
                            bank[:, col:col + 128], lhsT=g.ident[:], rhs=bias[:, h, pat * 5 + m, :],
                            start=False, stop=True), r=[g.ident, bias], w=[bank])
                na_ = min(n, 4)
                p.op('act', lambda e: e.activation(out=pt[:, 0:na_, :], in_=sA[:, 0:na_ * 128].rearrange("p (a q) -> p a q", q=128),
                                                   func=AF.Exp), r=[sA], w=[pt])
                if n > 4:
                    p.op('act', lambda e: e.activation(out=pt[:, 4:n, :], in_=sB[:, 0:(n - 4) * 128].rearrange("p (a q) -> p a q", q=128),
                                                       func=AF.Exp), r=[sB], w=[pt])
                for i, (tok0, m) in enumerate(ch):
                    vt = tok0 // 128
                    p.op('pe', lambda e, i=i, vt=vt: e.matmul(po[:, 0:65], lhsT=pt[:, i, :], rhs=V[:, vt, h * 65:(h + 1) * 65],
                                                              start=(i == 0), stop=(i == n - 1)), r=[pt, V], w=[po])
                p.op('dve', lambda e: e.reciprocal(out=rc_[:], in_=po[:, 64:65]), r=[po], w=[rc_])
                p.op('dve', lambda e: e.tensor_scalar(out=y_[:, h * 64:(h + 1) * 64], in0=po[:, 0:64], scalar1=rc_[:, 0:1],
                                                      scalar2=None, op0=ALU.mult), r=[po, rc_], w=[y_])
            p.dma(None, g.Y[q0:q0 + 128, 0:256], y_[:], r=[y_], q='pool')


def phase_diff(g, l, do_ctx):
    p = g.p
    Lw = g.lay[l]
    import math
    lam_init = 0.8 - 0.6 * math.exp(-0.3 * l)
    with p.phase():
        lp = p.sb("df_lp", [128, 4, 32], F32)
        pr = p.sb("df_pr", [128, 2, 32], F32)
        sm = p.sb("df_sm", [128, 4], F32)
        neglam = p.sb("df_nl", [128, 1], F32)
        gsub = p.sb("df_gs", [128, 64], F32)
        p.dma(lp, lp[:].rearrange("p a b -> p (a b)"), Lw["lam"].broadcast_to([128, 128]))
        load_bcast(p, gsub, Lw["subg"], 64)
        p.op('dve', lambda e: e.tensor_scalar(out=gsub[:], in0=gsub[:], scalar1=1.0 - lam_init, scalar2=None, op0=ALU.mult),
             r=[gsub], w=[gsub])
        p.op('dve', lambda e: e.tensor_tensor(out=pr[:], in0=lp[:, 0:4:2, :], in1=lp[:, 1:4:2, :], op=ALU.mult), r=[lp], w=[pr])
        p.op('dve', lambda e: e.tensor_reduce(out=sm[:, 0:2], in_=pr[:], axis=AX.X, op=ALU.add), r=[pr], w=[sm])
        p.op('act', lambda e: e.activation(out=sm[:, 2:4], in_=sm[:, 0:2], func=AF.Exp), r=[sm], w=[sm])
        p.op('dve', lambda e: e.tensor_tensor(out=neglam[:], in0=sm[:, 3:4], in1=sm[:, 2:3], op=ALU.subtract), r=[sm], w=[neglam])
        p.op('dve', lambda e: e.tensor_scalar(out=neglam[:], in0=neglam[:], scalar1=-lam_init, scalar2=None, op0=ALU.add),
             r=[neglam], w=[neglam])
        V = p.sb("df_v", [128, NTILE, 260], BF16)
        p.dma(V, V[:], g.VDF.rearrange("(a p) h d -> p a (h d)", p=128))
        qT = [p.sb("df_q%d" % i, [128, NT], BF16) for i in range(2)]
        kA = [p.sb("df_ka%d" % i, [128, NT], BF16) for i in range(2)]
        kB = [p.sb("df_kb%d" % i, [128, NT], BF16) for i in range(2)]
        for i in range(2):
            p.dma(qT[i], qT[i][:], g.FMS[C_QDF + i])
            p.dma(kA[i], kA[i][:], g.FMS[C_KDA + i])
            p.dma(kB[i], kB[i][:], g.FMS[C_KDB + i])
        acc = [p.ps("df_acc%d" % i, [128, 512], F32) for i in range(4)]
        psS = [p.ps("df_s%d" % i, [128, 512], F32) for i in range(2)]
        PT = [p.sb("df_pt%d" % i, [128, 512], BF16) for i in range(2)]
        oc = [p.sb("df_oc%d" % i, [128, 4, 64], F32) for i in range(2)]
        o = p.sb("df_o", [128, 4, 64], F32)
        sq = p.sb("df_sq", [128, 4, 64], F32)
        ss = p.sb("df_ss", [128, 8], F32)
        rc = p.sb("df_rc", [128, 4], F32)
        yd = [p.sb("df_y%d" % i, [128, 4, 64], BF16) for i in range(2)]
        groups = []
        if do_ctx:
            groups.append((0, 256, [0, 1]))
        for gi in range(8):
            groups.append((LC + gi * 512, 512, list(range(NTILE))))
        it = 0
        gi_ = 0
        cgen = cast_gen(g, l)
        cstate = {"n": 0}

        def cast_tick():
            cstate["n"] += 1
            if cstate["n"] % 16 == 0:
                next(cgen, None)

        for h in range(4):
            hp, base = h // 2, (h % 2) * 64
            for (t0, n, kcs) in groups:
                ns = n // 128
                for c in range(2):
                    kX = (kA if c == 0 else kB)[hp]

                    def qk(ki):
                        kc = kcs[ki]
                        ps_ = psS[ki % 2]
                        p.op('pe', lambda e: e.matmul(
                            ps_[:, 0:n], lhsT=kX[base:base + 64, kc * 128:(kc + 1) * 128], rhs=qT[hp][base:base + 64, t0:t0 + n],
                            start=True, stop=True), r=[kX, qT[hp]], w=[ps_])

                    def ex(ki):
                        ps_, pt = psS[ki % 2], PT[ki % 2]
                        p.op('act', lambda e: e.activation(out=pt[:, 0:n], in_=ps_[:, 0:n], func=AF.Exp), r=[ps_], w=[pt])

                    def pv(ki):
                        kc = kcs[ki]
                        pt = PT[ki % 2]
                        for s in range(ns):
                            p.op('pe', lambda e, s=s: e.matmul(
                                acc[s][:, 0:65], lhsT=pt[:, s * 128:(s + 1) * 128], rhs=V[:, kc, h * 65:(h + 1) * 65],
                                start=(ki == 0), stop=(ki == len(kcs) - 1)), r=[pt, V], w=[acc[s]])

                    qk(0)
                    ex(0)
                    for ki in range(len(kcs)):
                        if ki + 1 < len(kcs):
                            qk(ki + 1)
                            ex(ki + 1)
                        pv(ki)
                        cast_tick()
                    for s in range(ns):
                        p.op('dve', lambda e, s=s: e.reciprocal(out=rc[:, s:s + 1], in_=acc[s][:, 64:65]), r=[acc[s]], w=[rc])
                        p.op('dve', lambda e, s=s, c=c: e.tensor_scalar(out=oc[c][:, s, :], in0=acc[s][:, 0:64], scalar1=rc[:, s:s + 1],
                                                                       scalar2=None, op0=ALU.mult), r=[acc[s], rc], w=[oc[c]])
                y_ = yd[gi_ % 2]
                gi_ += 1
                p.op('dve', lambda e: e.scalar_tensor_tensor(out=o[:, 0:ns, :], in0=oc[1][:, 0:ns, :], scalar=neglam[:, 0:1],
                                                             in1=oc[0][:, 0:ns, :], op0=ALU.mult, op1=ALU.add),
                     r=[oc[0], oc[1], neglam], w=[o])
                p.op('dve', lambda e: e.tensor_tensor(out=sq[:, 0:ns, :], in0=o[:, 0:ns, :], in1=o[:, 0:ns, :], op=ALU.mult), r=[o], w=[sq])
                p.op('dve', lambda e: e.tensor_reduce(out=ss[:, 0:ns], in_=sq[:, 0:ns, :], axis=AX.X, op=ALU.add), r=[sq], w=[ss])
                p.op('dve', lambda e: e.tensor_scalar(out=ss[:, 0:ns], in0=ss[:, 0:ns], scalar1=1.0 / 64, scalar2=EPS,
                                                      op0=ALU.mult, op1=ALU.add), r=[ss], w=[ss])
                p.op('act', lambda e: e.activation(out=ss[:, 4:4 + ns], in_=ss[:, 0:ns], func=AF.Sqrt), r=[ss], w=[ss])
                p.op('dve', lambda e: e.reciprocal(out=ss[:, 0:ns], in_=ss[:, 4:4 + ns]), r=[ss], w=[ss])
                p.op('dve', lambda e: e.tensor_tensor(out=sq[:, 0:ns, :], in0=o[:, 0:ns, :],
                                                      in1=ss[:, 0:ns].unsqueeze(2).broadcast_to([128, ns, 64]), op=ALU.mult),
                     r=[o, ss], w=[sq])
                p.op('dve', lambda e: e.tensor_tensor(out=y_[:, 0:ns, :], in0=sq[:, 0:ns, :],
                                                      in1=gsub[:].unsqueeze(1).broadcast_to([128, ns, 64]), op=ALU.mult),
                     r=[sq, gsub], w=[y_])
                p.dma(None, g.Y[t0:t0 + n, 256 + h * 64:256 + (h + 1) * 64].rearrange("(s p) d -> p s d", p=128),
                      y_[:, 0:ns, :], r=[y_], q='pool')
        for _ in cgen:
            pass


def phase_ret(g, l, do_ctx):
    p = g.p
    Lw = g.lay[l]
    with p.phase():
        dec = p.sb("rt_dec", [128, 8], F32)
        lg = p.sb("rt_lg", [128, 8], F32)
        cdec = p.sb("rt_cdec", [128, 8], F32)
        kdec = p.sb("rt_kdec", [128, 8], F32)
        jcol = p.sb("rt_jcol", [128, 2], F32)
        cm = {}
        for nm in ("RT_RIJ", "RT_MIJ", "RT_RJI", "RT_MJI", "RT_IROW", "RT_IROWB"):
            cm[nm] = p.sb("c_" + nm, [128, 128], F32)
            p.dma(cm[nm], cm[nm][:], g.cst[nm])
        p.dma(jcol, jcol[:], g.cst["RT_JCOL"])
        load_bcast(p, dec, Lw["dec"], 8)
        p.op('act', lambda e: e.activation(out=lg[:], in_=dec[:], func=AF.Sigmoid), r=[dec], w=[lg])
        p.op('act', lambda e: e.activation(out=lg[:], in_=lg[:], func=AF.Ln), r=[lg], w=[lg])
        p.op('act', lambda e: e.activation(out=cdec[:], in_=lg[:], func=AF.Exp, scale=128.0), r=[lg], w=[cdec])
        QDF = p.sb("rt_qdf", [128, 4, 128], F32)
        QDB = p.sb("rt_qdb", [128, 4, 128], F32)
        DB = p.sb("rt_db", [128, 4, 128], F32)
        t1 = p.sb("rt_t1", [128, 128], F32)
        t2 = p.sb("rt_t2", [128, 128], F32)
        for d_ in range(2):
            for h in range(4):
                c = d_ * 4 + h
                p.op('act', lambda e, c=c, d_=d_: e.activation(out=kdec[:, c:c + 1], in_=jcol[:, d_:d_ + 1], func=AF.Exp,
                                                               scale=lg[:, c:c + 1]), r=[jcol, lg], w=[kdec])
        for h in range(4):
            p.op('act', lambda e, h=h: e.activation(out=QDF[:, h, :], in_=cm["RT_IROW"][:], func=AF.Exp, scale=lg[:, h:h + 1]),
                 r=[cm["RT_IROW"], lg], w=[QDF])
            p.op('act', lambda e, h=h: e.activation(out=QDB[:, h, :], in_=cm["RT_IROWB"][:], func=AF.Exp, scale=lg[:, 4 + h:5 + h]),
                 r=[cm["RT_IROWB"], lg], w=[QDB])
            p.op('act', lambda e, h=h: e.activation(out=t1[:], in_=cm["RT_RIJ"][:], func=AF.Exp, scale=lg[:, h:h + 1]),
                 r=[cm["RT_RIJ"], lg], w=[t1])
            p.op('act', lambda e, h=h: e.activation(out=t2[:], in_=cm["RT_RJI"][:], func=AF.Exp, scale=lg[:, 4 + h:5 + h]),
                 r=[cm["RT_RJI"], lg], w=[t2])
            p.op('dve', lambda e: e.tensor_tensor(out=t1[:], in0=t1[:], in1=cm["RT_MIJ"][:], op=ALU.mult), r=[t1, cm["RT_MIJ"]], w=[t1])
            p.op('dve', lambda e: e.tensor_tensor(out=t2[:], in0=t2[:], in1=cm["RT_MJI"][:], op=ALU.mult), r=[t2, cm["RT_MJI"]], w=[t2])
            p.op('dve', lambda e, h=h: e.tensor_tensor(out=DB[:, h, :], in0=t1[:], in1=t2[:], op=ALU.add), r=[t1, t2], w=[DB])
        qT = p.sb("rt_q", [64, NT], BF16)
        kT = p.sb("rt_k", [64, NT], BF16)
        Ktm = p.sb("rt_ktm", [128, NTILE, 64], BF16)
        V = p.sb("rt_v", [128, NTILE, 128], BF16)
        G = p.sb("rt_g", [128, NTILE, 128], BF16)
        KF = p.sb("rt_kf", [128, NTILE, 64], BF16)
        KB = p.sb("rt_kb", [128, NTILE, 64], BF16)
        SFa = p.sb("rt_sfa", [64, NTILE, 128], BF16)
        SBa = p.sb("rt_sba", [64, NTILE, 128], BF16)
        S32 = [p.sb("rt_S32_%d" % i, [64, NTILE, 128], F32) for i in range(2)]
        psKV = [p.ps("rt_kv%d" % i, [128, 512], F32) for i in range(2)]
        psA = [p.ps("rt_att%d" % i, [128, 512], F32) for i in range(2)]
        psO = [p.ps("rt_o%d" % i, [128, 512], F32) for i in range(2)]
        attm = [p.sb("rt_attm%d" % i, [128, 128], BF16) for i in range(2)]
        qf = [p.sb("rt_qf%d" % i, [64, 128], BF16) for i in range(2)]
        qb = [p.sb("rt_qb%d" % i, [64, 128], BF16) for i in range(2)]
        junk = p.sb("rt_junk", [128, 128], F32)
        st = [p.sb("rt_st%d" % i, [128, 4], F32) for i in range(2)]
        ys = [p.sb("rt_ys%d" % i, [128, 128], BF16) for i in range(2)]
        it = 0
        for h in range(4):
            hp, base = h // 2, (h % 2) * 64
            p.dma(qT, qT[:], g.FMS[C_QRT + hp, base:base + 64, :])
            p.dma(kT, kT[:], g.FMS[C_KRT + hp, base:base + 64, :])
            p.dma(Ktm, Ktm[:], g.KRTT[:, h * 64:(h + 1) * 64].rearrange("(a p) d -> p a d", p=128))
            p.dma(V, V[:], g.VRT[:, h * 128:(h + 1) * 128].rearrange("(a p) d -> p a d", p=128))
            p.dma(G, G[:], g.GRT[:, h * 128:(h + 1) * 128].rearrange("(a p) d -> p a d", p=128))
            p.op('dve', lambda e: e.tensor_scalar(out=KF[:], in0=Ktm[:], scalar1=kdec[:, h:h + 1], scalar2=None, op0=ALU.mult),
                 r=[Ktm, kdec], w=[KF])
            p.op('dve', lambda e: e.tensor_scalar(out=KB[:], in0=Ktm[:], scalar1=kdec[:, 4 + h:5 + h], scalar2=None, op0=ALU.mult),
                 r=[Ktm, kdec], w=[KB])
            for d_, (Kd, Sa, order) in enumerate(((KF, SFa, list(range(NTILE))),
                                                  (KB, SBa, [1, 0] + list(range(NTILE - 1, 1, -1))))):
                S_ = [T("rt_Ss%d_%d_%d" % (h, d_, c), S32[d_][:, c, :]) for c in range(NTILE)]
                p.op('pool', lambda e: e.memset(S_[order[0]][:], 0.0), r=[S32[d_]], w=[S_[order[0]], S32[d_]])
                cd = cdec[0:64, d_ * 4 + h:d_ * 4 + h + 1]
                for oi, c in enumerate(order):
                    p.op('act', lambda e, c=c: e.copy(out=Sa[:, c, :], in_=S_[c][:]), r=[S_[c]], w=[Sa])
                    if oi == len(order) - 1:
                        p.op('act', lambda e: e.copy(out=Sa[:, c, 0:1], in_=S_[c][:, 0:1]), r=[S_[c]], w=[S32[d_]])
                        break
                    cn = order[oi + 1]
                    kv = psKV[it % 2]
                    it += 1
                    p.op('pe', lambda e, c=c, kv=kv: e.matmul(kv[0:64, 0:128], lhsT=Kd[:, c, :], rhs=V[:, c, :], start=True, stop=True),
                         r=[Kd, V], w=[kv])
                    p.op('dve', lambda e, kv=kv, c=c, cn=cn: e.scalar_tensor_tensor(out=S_[cn][:], in0=S_[c][:], scalar=cd,
                                                                                  in1=kv[0:64, 0:128], op0=ALU.mult, op1=ALU.add),
                         r=[S_[c], cdec, kv], w=[S_[cn]])
            for c in (range(NTILE) if do_ctx else range(2, NTILE)):
                tok = c * 128
                pa, po = psA[c % 2], psO[c % 2]
                am, qf_, qb_, st_, y_ = attm[c % 2], qf[c % 2], qb[c % 2], st[c % 2], ys[c % 2]
                p.op('pe', lambda e: e.matmul(pa[:, 0:128], lhsT=kT[:, tok:tok + 128], rhs=qT[:, tok:tok + 128], start=True, stop=True),
                     r=[kT, qT], w=[pa])
                p.op('dve', lambda e: e.tensor_tensor(out=am[:], in0=pa[:, 0:128], in1=DB[:, h, :], op=ALU.mult), r=[pa, DB], w=[am])
                p.op('pool', lambda e: e.tensor_tensor(out=qf_[:], in0=qT[:, tok:tok + 128], in1=QDF[0:64, h, :], op=ALU.mult),
                     r=[qT, QDF], w=[qf_])
                p.op('pool', lambda e: e.tensor_tensor(out=qb_[:], in0=qT[:, tok:tok + 128], in1=QDB[0:64, h, :], op=ALU.mult),
                     r=[qT, QDB], w=[qb_])
                p.op('pe', lambda e: e.matmul(po[:, 0:128], lhsT=am[:], rhs=V[:, c, :], start=True, stop=False), r=[am, V], w=[po])
                p.op('pe', lambda e: e.matmul(po[:, 0:128], lhsT=qf_[:], rhs=SFa[:, c, :], start=False, stop=False), r=[qf_, SFa], w=[po])
                p.op('pe', lambda e: e.matmul(po[:, 0:128], lhsT=qb_[:], rhs=SBa[:, c, :], start=False, stop=True), r=[qb_, SBa], w=[po])
                p.op('act', lambda e: e.activation(out=junk[:], in_=po[:, 0:128], func=AF.Square, accum_out=st_[:, 0:1]),
                     r=[po], w=[junk, st_])
                p.op('dve', lambda e: e.tensor_scalar(out=st_[:, 1:2], in0=st_[:, 0:1], scalar1=1.0 / 128, scalar2=EPS,
                                                      op0=ALU.mult, op1=ALU.add), r=[st_], w=[st_])
                p.op('act', lambda e: e.activation(out=st_[:, 2:3], in_=st_[:, 1:2], func=AF.Sqrt), r=[st_], w=[st_])
                p.op('dve', lambda e: e.reciprocal(out=st_[:, 3:4], in_=st_[:, 2:3]), r=[st_], w=[st_])
                p.op('dve', lambda e: e.scalar_tensor_tensor(out=y_[:], in0=po[:, 0:128], scalar=st_[:, 3:4], in1=G[:, c, :],
                                                             op0=ALU.mult, op1=ALU.mult), r=[po, st_, G], w=[y_])
                p.dma(None, g.Y[tok:tok + 128, 512 + h * 128:512 + (h + 1) * 128], y_[:], r=[y_], q='pool')


def phase_p3(g, l, xa, last):
    p = g.p
    Lw = g.lay[l]
    with p.phase():
        wob = p.sb("wob", [128, 8, D], BF16)
        with p.phase():
            wof = p.sb("wof", [128, 8, 512], F32)
            wsrc = Lw["wout"].rearrange("(k p) n -> p k n", p=128)
            for n in range(2):
                p.dma(wof, wof[:], wsrc[:, :, n * 512:(n + 1) * 512])
                p.op('dve', lambda e, n=n: e.tensor_copy(out=wob[:, :, n * 512:(n + 1) * 512], in_=wof[:]), r=[wof], w=[wob])
        gt = []
        for which in range(2):
            t = p.sb("p3_gt%d" % which, [128, D], F32)
            load_bcast(p, t, mod_row(g, which, 2), D)
            gt.append(t)
        nrm = make_norm_consts(g, "n2", Lw["g2"], 3, 4)
        yt = [p.sb("p3_y%d" % i, [128, D], BF16) for i in range(2)]
        xt = [p.sb("p3_x%d" % i, [128, D], F32) for i in range(2)]
        x1 = [p.sb("p3_x1%d" % i, [128, D], F32) for i in range(2)]
        yT = [p.sb("p3_yT%d" % i, [128, 8, 128], BF16) for i in range(2)]
        hT = [p.sb("p3_hT%d" % i, [128, 8, 128], BF16) for i in range(2)]
        hb = [p.sb("p3_hb%d" % i, [128, D], BF16) for i in range(2)]
        tmp = p.sb("p3_tmp", [128, D], F32)
        st = [p.sb("p3_st%d" % i, [128, 4], F32) for i in range(2)]
        pstY = [p.ps("p3_pty%d" % i, [128, 8, 128], BF16) for i in range(2)]
        pstH = [p.ps("p3_pth%d" % i, [128, 8, 128], BF16) for i in range(2)]
        psO = [[p.ps("p3_o%d%d" % (i, n), [128, 512], F32) for n in range(2)] for i in range(2)]
        for ti, tt in enumerate(range(2, NTILE) if last else range(NTILE)):
            which = 1 if tt < 2 else 0
            b = ti % 2
            sl = slice(tt * 128, (tt + 1) * 128)
            p.dma(yt[b], yt[b][:], g.Y[sl, :])
            p.dma(xt[b], xt[b][:], xa[sl, :])
            emit_transposes(g, yt[b], pstY[b], yT[b][:], yT[b])
            for n in range(2):
                po = psO[b][n]
                for k in range(8):
                    p.op('pe', lambda e, k=k, n=n, po=po: e.matmul(po[:], lhsT=yT[b][:, k, :], rhs=wob[:, k, n * 512:(n + 1) * 512],
                                                                  start=(k == 0), stop=(k == 7)), r=[yT[b], wob], w=[po])
                cs = slice(n * 512, (n + 1) * 512)
                p.op('dve', lambda e, po=po, cs=cs: e.tensor_tensor(out=tmp[:, cs], in0=po[:], in1=gt[which][:, cs], op=ALU.mult),
                     r=[po, gt[which]], w=[tmp])
                p.op('pool', lambda e, cs=cs: e.tensor_tensor(out=x1[b][:, cs], in0=tmp[:, cs], in1=xt[b][:, cs], op=ALU.add),
                     r=[tmp, xt[b]], w=[x1[b]])
            p.dma(None, g.X1[sl, :], x1[b][:], r=[x1[b]], q='pool')
            emit_norm(g, x1[b], nrm[which][0], nrm[which][1], hb[b], tmp, st[b])
            emit_transposes(g, hb[b], pstH[b], hT[b][:], hT[b])
            p.dma(None, g.H2T[:, :, sl], hT[b][:], r=[hT[b]], q='pool')


def cast_gen(g, l):
    p = g.p
    Lw = g.lay[l]
    f = [p.sb("cs_f%d" % i, [128, 2048], F32) for i in range(3)]
    b = [p.sb("cs_b%d" % i, [128, 2048], BF16) for i in range(3)]
    it = 0
    usrc = Lw["ut"].rearrange("(k p) e -> p k e", p=128)
    vsrc = Lw["v"].rearrange("(i p) d -> p i d", p=128)
    for k in range(8):
        for ec in range(8):
            i = it % 3
            it += 1
            p.dma(f[i], f[i][:], usrc[:, k, ec * 2048:(ec + 1) * 2048])
            p.op('dve', lambda e, i=i: e.tensor_copy(out=b[i][:], in_=f[i][:]), r=[f[i]], w=[b[i]])
            p.dma(None, g.UTB[:, k, ec * 2048:(ec + 1) * 2048], b[i][:], r=[b[i]], q='pool')
            yield
    for ic in range(64):
        i = it % 3
        it += 1
        p.dma(f[i], f[i][:].rearrange("p (a d) -> p a d", a=2), vsrc[:, ic * 2:(ic + 1) * 2, :])
        p.op('dve', lambda e, i=i: e.tensor_copy(out=b[i][:], in_=f[i][:]), r=[f[i]], w=[b[i]])
        p.dma(None, g.VB[:, ic * 2:(ic + 1) * 2, :], b[i][:].rearrange("p (a d) -> p a d", a=2), r=[b[i]], q='pool')
        yield


NI = 8


def phase_peerq(g, l, last):
    p = g.p
    Lw = g.lay[l]
    with p.phase():
        wq = p.sb("pq_wq", [128, 8, 2048], BF16)
        with p.phase():
            wqf = p.sb("pq_wqf", [128, 8, 512], F32)
            wsrc = Lw["wq"].rearrange("(k p) n -> p k n", p=128)
            for n in range(4):
                p.dma(wqf, wqf[:], wsrc[:, :, n * 512:(n + 1) * 512])
                p.op('dve', lambda e, n=n: e.tensor_copy(out=wq[:, :, n * 512:(n + 1) * 512], in_=wqf[:]), r=[wqf], w=[wq])
        ht = [p.sb("pq_ht%d" % i, [128, 8, 512], BF16) for i in range(2)]
        stg = [p.sb("pq_st%d" % i, [128, 4, 512], BF16) for i in range(2)]
        ps = [p.ps("pq_ps%d" % i, [128, 512], F32) for i in range(4)]
        it = 0
        for gi, (t0, n) in enumerate(tok_groups()):
            if last and t0 < LC:
                continue
            h_ = ht[gi % 2]
            p.dma(h_, h_[:, :, 0:n], g.H2T[:, :, t0:t0 + n])
            for jq in range(4):
                st_ = stg[(gi * 4 + jq) % 2]
                for jj in range(4):
                    j = jq * 4 + jj
                    ps_ = ps[it % 4]
                    it += 1
                    for k in range(8):
                        p.op('pe', lambda e, k=k, j=j, ps_=ps_: e.matmul(ps_[:, 0:n], lhsT=wq[:, k, j * 128:(j + 1) * 128], rhs=h_[:, k, 0:n],
                                                                        start=(k == 0), stop=(k == 7)), r=[wq, h_], w=[ps_])
                    if jj % 2 == 0:
                        p.op('act', lambda e, jj=jj, ps_=ps_: e.copy(out=st_[:, jj, 0:n], in_=ps_[:, 0:n]), r=[ps_], w=[st_])
                    else:
                        p.op('dve', lambda e, jj=jj, ps_=ps_: e.tensor_copy(out=st_[:, jj, 0:n], in_=ps_[:, 0:n]), r=[ps_], w=[st_])
                p.dma(None, g.QPT[:, jq * 4:(jq + 1) * 4, t0:t0 + n], st_[:, :, 0:n], r=[st_], q='pool')


def phase_peer(g, l, last):
    p = g.p
    Lw = g.lay[l]
    with p.phase():
        skt = p.sb("pe_skt", [128, 16, 128], BF16)
        with p.phase():
            sktf = p.sb("pe_sktf", [128, 16, 128], F32)
            p.dma(sktf, sktf[:], Lw["skt"])
            p.op('dve', lambda e: e.tensor_copy(out=skt[:], in_=sktf[:]), r=[sktf], w=[skt])
        gt = []
        for which in range(2):
            t = p.sb("pe_gt%d" % which, [128, D], F32)
            load_bcast(p, t, mod_row(g, which, 5), D)
            gt.append(t)
        if last:
            fg = p.sb("pe_fg", [128, D], F32)
            load_bcast(p, fg, g.final_g, D)
        h2t = [p.sb("pe_h2t%d" % i, [128, 8, 256], BF16) for i in range(2)]
        E = [p.sb("pe_E%d" % i, [128, 16, 128], F32) for i in range(2)]
        Tt = [p.sb("pe_T%d" % i, [128, 8], F32) for i in range(2)]
        kap = [p.sb("pe_kap%d" % i, [128, 8], F32) for i in range(2)]
        Dk = [[p.sb("pe_dk%d_%d" % (i, h), [128, 128], BF16) for h in range(8)] for i in range(2)]
        acc = [[p.ps("pe_acc%d%d" % (i, n), [128, 512], F32) for n in range(2)] for i in range(2)]
        psA = [p.ps("pe_A%d" % i, [128, 512], F32) for i in range(2)]
        psW = p.ps("pe_W", [128, 512], F32)
        psT_full = p.ps("pe_T", [128, 8, 128], BF16)
        psT = [T("pe_T%d" % i, psT_full[:, i * 4:(i + 1) * 4, :]) for i in range(2)]
        psM = psA[1]
        pairs = list(range(1, NTILE // 2)) if last else list(range(NTILE // 2))
        for pi, pr in enumerate(pairs):
            t0 = pr * 256
            which = 1 if pr == 0 else 0
            ht = h2t[pi % 2]
            p.dma(ht, ht[:], g.H2T[:, :, t0:t0 + 256])
            with p.phase():
                qTb = p.sb("pe_qT", [128, 16, 256], BF16)
                p.dma(qTb, qTb[:], g.QPT[:, :, t0:t0 + 256])
                S_all = [p.sb("pe_S%d" % i, [128, 16, 128], F32) for i in range(2)]
                m8 = p.sb("pe_m8", [128, 16, 16], F32)
                sr = p.sb("pe_sr", [128, 16, 128], F32)
                sd = p.sb("pe_sd", [128, 16, 128], F32)
                md = p.sb("pe_md", [128, 16, 16], F32)
                et = p.sb("pe_et", [128, 16, 16], F32)
                cand = p.sb("pe_cand", [128, 8, 256], F32)
                cr = p.sb("pe_cr", [128, 8, 256], F32)
                c16 = p.sb("pe_c16", [128, 8, 16], F32)
                zz = p.sb("pe_zz", [128, 8], F32)
                for sub in range(2):
                    S_ = S_all[sub]
                    for jq in range(4):
                        for jj in range(4):
                            j = jq * 4 + jj
                            p.op('pe', lambda e, j=j, jj=jj: e.matmul(psM[:, jj * 128:(jj + 1) * 128],
                                                                      lhsT=qTb[:, j, sub * 128:(sub + 1) * 128], rhs=skt[:, j, :],
                                                                      start=True, stop=True), r=[qTb, skt], w=[psM])
                        p.op('act', lambda e, jq=jq: e.copy(out=S_[:, jq * 4:(jq + 1) * 4, :],
                                                           in_=psM[:].rearrange("p (a n) -> p a n", a=4)), r=[psM], w=[S_])
                    for j in range(16):
                        p.op('dve', lambda e, j=j: e.max(out=m8[:, j, 0:8], in_=S_[:, j, :]), r=[S_], w=[m8])
                    for j in range(16):
                        p.op('dve', lambda e, j=j: e.match_replace(out=sr[:, j, :], in_to_replace=m8[:, j, 0:8], in_values=S_[:, j, :],
                                                                   imm_value=-1e30), r=[S_, m8], w=[sr])
                    for j in range(16):
                        p.op('dve', lambda e, j=j: e.max(out=m8[:, j, 8:16], in_=sr[:, j, :]), r=[sr], w=[m8])
                    p.op('dve', lambda e: e.tensor_tensor(out=sd[:], in0=S_[:], in1=m8[:, :, 0:1].broadcast_to([128, 16, 128]),
                                                          op=ALU.subtract), r=[S_, m8], w=[sd])
                    p.op('dve', lambda e: e.tensor_tensor(out=md[:], in0=m8[:], in1=m8[:, :, 0:1].broadcast_to([128, 16, 16]),
                                                          op=ALU.subtract), r=[m8], w=[md])
                    p.op('act', lambda e: e.activation(out=E[sub][:], in_=sd[:], func=AF.Exp), r=[sd], w=[E[sub]])
                    p.op('act', lambda e: e.activation(out=et[:], in_=md[:], func=AF.Exp), r=[md], w=[et])
                    et4 = et[:].rearrange("p (h c) a -> p h c a", c=2)
                    p.op('dve', lambda e: e.tensor_tensor(out=cand[:].rearrange("p h (a b) -> p h a b", a=16),
                                                          in0=et4[:, :, 0, :].unsqueeze(3).broadcast_to([128, 8, 16, 16]),
                                                          in1=et4[:, :, 1, :].unsqueeze(2).broadcast_to([128, 8, 16, 16]), op=ALU.mult),
                         r=[et], w=[cand])
                    for h in range(8):
                        p.op('dve', lambda e, h=h: e.max(out=c16[:, h, 0:8], in_=cand[:, h, :]), r=[cand], w=[c16])
                    for h in range(8):
                        p.op('dve', lambda e, h=h: e.match_replace(out=cr[:, h, :], in_to_replace=c16[:, h, 0:8], in_values=cand[:, h, :],
                                                                   imm_value=-1e30), r=[cand, c16], w=[cr])
                    for h in range(8):
                        p.op('dve', lambda e, h=h: e.max(out=c16[:, h, 8:16], in_=cr[:, h, :]), r=[cr], w=[c16])
                    p.op('dve', lambda e: e.tensor_scalar(out=Tt[sub][:], in0=c16[:, :, 15], scalar1=1.0 - 1e-6, scalar2=None, op0=ALU.mult),
                         r=[c16], w=[Tt[sub]])
                    p.op('dve', lambda e: e.tensor_reduce(out=zz[:], in_=c16[:], axis=AX.X, op=ALU.add), r=[c16], w=[zz])
                    p.op('dve', lambda e: e.reciprocal(out=kap[sub][:], in_=zz[:]), r=[zz], w=[kap[sub]])
                    for h in range(8):
                        p.op('dve', lambda e, h=h: e.tensor_scalar(out=Dk[sub][h][:], in0=g.ident_f[:], scalar1=kap[sub][:, h:h + 1],
                                                                   scalar2=None, op0=ALU.mult), r=[g.ident_f, kap[sub]], w=[Dk[sub][h]])
            with p.phase():
                NPT = 5
                Pt = [p.sb("pe_P%d" % i, [128, NI, 128], F32) for i in range(NPT)]
                G = [[p.sb("pe_G%d_%d" % (b_, i), [128, 8, NI * 128], BF16) for i in range(2)] for b_ in range(2)]
                ut = [p.sb("pe_ut%d" % i, [128, 8, 512], BF16) for i in range(2)]
                vb = [p.sb("pe_vb%d" % i, [128, 4, D], BF16) for i in range(2)]
                ga = [p.sb("pe_ga%d" % i, [128, 512], F32) for i in range(2)]
                wg = [p.sb("pe_wg%d" % i, [128, 512], BF16) for i in range(2)]
                wgT = [p.sb("pe_wgT%d" % i, [128, 4, 128], BF16) for i in range(2)]
                xs = [p.sb("pe_x%d" % i, [128, D], F32) for i in range(2)]
                xo = [p.sb("pe_xo%d" % i, [128, D], F32) for i in range(2)]
                if last:
                    hb = p.sb("pe_hb", [128, D], F32)
                    st = p.sb("pe_st", [128, 4], F32)
                nblk = 128 // NI
                nesb = NI * 128 // 512
                cnt = {"p": 0}

                def unit(ib, sub, h):
                    P_ = Pt[cnt["p"] % NPT]
                    cnt["p"] += 1
                    G_ = G[ib % 2][sub]
                    if h % 2 == 1:
                        for ii in range(NI):
                            i_ = ib * NI + ii
                            p.op('act', lambda e, ii=ii, i_=i_: e.activation(out=P_[:, ii, :], in_=E[sub][:, 2 * h + 1, :], func=AF.Copy,
                                                                            scale=E[sub][:, 2 * h, i_:i_ + 1]), r=[E[sub]], w=[P_])
                    else:
                        p.op('pool', lambda e: e.tensor_tensor(
                            out=P_[:], in0=E[sub][:, 2 * h, ib * NI:(ib + 1) * NI].unsqueeze(2).broadcast_to([128, NI, 128]),
                            in1=E[sub][:, 2 * h + 1, :].unsqueeze(1).broadcast_to([128, NI, 128]), op=ALU.mult),
                            r=[E[sub]], w=[P_])
                    p.op('dve', lambda e: e.scalar_tensor_tensor(
                        out=G_[:, h, :], in0=P_[:].rearrange("p a b -> p (a b)"), scalar=Tt[sub][:, h:h + 1],
                        in1=P_[:].rearrange("p a b -> p (a b)"), op0=ALU.is_ge, op1=ALU.mult),
                        r=[P_, Tt[sub]], w=[G_])

                iters = [(ib, esb, sub) for ib in range(nblk) for esb in range(nesb) for sub in range(2)]
                nit = len(iters)

                def s1(i):
                    ib, esb, sub = iters[i]
                    e0 = ib * NI * 128 + esb * 512
                    b_ = (i // 2) % 2
                    u_, v_ = ut[b_], vb[b_]
                    if sub == 0:
                        p.dma(u_, u_[:], g.UTB[:, :, e0:e0 + 512])
                        p.dma(v_, v_[:], g.VB[:, e0 // 128:e0 // 128 + 4, :])
                    pa, ga_ = psA[i % 2], ga[i % 2]
                    for k in range(8):
                        p.op('pe', lambda e, k=k: e.matmul(pa[:], lhsT=ht[:, k, sub * 128:(sub + 1) * 128], rhs=u_[:, k, :],
                                                           start=(k == 0), stop=(k == 7)), r=[ht, u_], w=[pa])
                    p.op('act', lambda e: e.activation(out=ga_[:], in_=pa[:], func=AF.Gelu), r=[pa], w=[ga_])

                def s2(i):
                    ib, esb, sub = iters[i]
                    G_ = G[ib % 2][sub]
                    ga_, wg_ = ga[i % 2], wg[i % 2]
                    for h in range(8):
                        p.op('pe', lambda e, h=h: e.matmul(psW[:], lhsT=Dk[sub][h][:], rhs=G_[:, h, esb * 512:(esb + 1) * 512],
                                                           start=(h == 0), stop=(h == 7)), r=[Dk[sub][h], G_], w=[psW])
                    p.op('dve', lambda e: e.tensor_tensor(out=wg_[:], in0=psW[:], in1=ga_[:], op=ALU.mult), r=[psW, ga_], w=[wg_])

                def s3(i):
                    emit_transposes(g, wg[i % 2], psT[i % 2], wgT[i % 2][:], wgT[i % 2], n=4)

                def s4(i):
                    ib, esb, sub = iters[i]
                    v_ = vb[(i // 2) % 2]
                    wgT_ = wgT[i % 2]
                    first = (ib == 0 and esb == 0)
                    lastm = (ib == nblk - 1 and esb == nesb - 1)
                    for c4 in range(4):
                        for n in range(2):
                            a_ = acc[sub][n]
                            p.op('pe', lambda e, c4=c4, n=n, a_=a_: e.matmul(
                                a_[:], lhsT=wgT_[:, c4, :], rhs=v_[:, c4, n * 512:(n + 1) * 512],
                                start=(first and c4 == 0), stop=(lastm and c4 == 3)), r=[wgT_, v_], w=[a_])

                pend = []
                for sub in range(2):
                    for h in range(8):
                        unit(0, sub, h)
                s1(0)
                s2(0)
                per_it = 16 // (nesb * 2)
                for i in range(nit):
                    ib = iters[i][0]
                    if i % (nesb * 2) == 0 and ib + 1 < nblk:
                        pend = [(ib + 1, sub, h) for sub in range(2) for h in range(8)]
                    if i + 1 < nit:
                        s1(i + 1)
                    s3(i)
                    if i + 1 < nit:
                        s2(i + 1)
                    for _ in range(per_it):
                        if pend:
                            unit(*pend.pop(0))
                    s4(i)
                for sub in range(2):
                    sl = slice(t0 + sub * 128, t0 + (sub + 1) * 128)
                    x_, o_ = xs[sub], xo[sub]
                    p.dma(x_, x_[:], g.X1[sl, :])
                    for n in range(2):
                        cs = slice(n * 512, (n + 1) * 512)
                        p.op('dve', lambda e, n=n, cs=cs: e.tensor_tensor(out=o_[:, cs], in0=acc[sub][n][:], in1=gt[which][:, cs], op=ALU.mult),
                             r=[acc[sub][n], gt[which]], w=[o_])
                    p.op('pool', lambda e: e.tensor_tensor(out=o_[:], in0=o_[:], in1=x_[:], op=ALU.add), r=[o_, x_], w=[o_])
                    if not last:
                        p.dma(None, g.X2[sl, :], o_[:], r=[o_], q='pool')
                    else:
                        p.op('act', lambda e: e.activation(out=hb[:], in_=o_[:], func=AF.Square, accum_out=st[:, 0:1]), r=[o_], w=[hb, st])
                        p.op('dve', lambda e: e.tensor_scalar(out=st[:, 1:2], in0=st[:, 0:1], scalar1=1.0 / D, scalar2=EPS,
                                                              op0=ALU.mult, op1=ALU.add), r=[st], w=[st])
                        p.op('act', lambda e: e.activation(out=st[:, 2:3], in_=st[:, 1:2], func=AF.Sqrt), r=[st], w=[st])
                        p.op('dve', lambda e: e.reciprocal(out=st[:, 3:4], in_=st[:, 2:3]), r=[st], w=[st])
                        p.op('dve', lambda e: e.scalar_tensor_tensor(out=hb[:], in0=o_[:], scalar=st[:, 3:4], in1=fg[:],
                                                                     op0=ALU.mult, op1=ALU.mult), r=[o_, st, fg], w=[hb])
                        p.dma(None, g.out[t0 - LC + sub * 128:t0 - LC + (sub + 1) * 128, :], hb[:], r=[hb], q='pool')


_PROG_CACHE = {}


def kernel(x, c, ctx, c_ctx, w_ada, b_ada, norm1_g, w_in, na_rpb, diff_lambda, diff_subln_g, ret_decay_logit,
           w_out, norm2_g, peer_wq, peer_subkeys, peer_u, peer_v, final_g):
    inp = dict(x=x, c=c, ctx=ctx, c_ctx=c_ctx, w_ada=w_ada, b_ada=b_ada, norm1_g=norm1_g, w_in=w_in, na_rpb=na_rpb,
               diff_lambda=diff_lambda, diff_subln_g=diff_subln_g, ret_decay_logit=ret_decay_logit, w_out=w_out,
               norm2_g=norm2_g, peer_wq=peer_wq, peer_subkeys=peer_subkeys, peer_u=peer_u, peer_v=peer_v, final_g=final_g)
    inp = {k: np.asarray(v) for k, v in inp.items()}
    B = inp["x"].shape[0]
    cs = _consts()
    shared = {"final_g": np.asarray(inp["final_g"], np.float32).reshape(1, D)}
    for n in CONST_NAMES:
        shared[n] = cs[n]
    for l in range(DEPTH):
        for k, v in _layer_inputs(inp, l).items():
            shared["L%d_%s" % (l, k)] = v
    ccv = np.asarray(inp["c_ctx"], np.float32).reshape(8, 128).T
    in_maps = []
    for b in range(B):
        m = dict(shared)
        m["xin"] = np.ascontiguousarray(np.concatenate([inp["ctx"][b], inp["x"][b]], axis=0).astype(np.float32))
        m["cvec"] = np.ascontiguousarray(np.stack([np.asarray(inp["c"][b], np.float32).reshape(8, 128).T, ccv], axis=-1))
        in_maps.append(m)
    if "nc" not in _PROG_CACHE:
        _PROG_CACHE["nc"] = build_program()[0]
    nc = _PROG_CACHE["nc"]
    res = run_bass_kernel_spmd(nc, in_maps, core_ids=list(range(B)))
    return np.stack([np.asarray(r["out"], dtype=np.float32) for r in res.results], axis=0)
```

```python
import numpy as np
from contextlib import ExitStack, contextmanager
import concourse.bass as bass
import concourse.mybir as mybir
from concourse.bass_utils import run_bass_kernel_spmd

F32 = mybir.dt.float32
BF16 = mybir.dt.bfloat16
AF = mybir.ActivationFunctionType
ALU = mybir.AluOpType
AX = mybir.AxisListType


class Buf:
    def __init__(self, name):
        self.name = name
        self.w = None
        self.r = {}


class T(Buf):
    def __init__(self, name, h):
        super().__init__(name)
        self.h = h

    def __getitem__(self, idx):
        return self.h[idx]


class Prog:
    ENG = ('pe', 'act', 'dve', 'pool', 'sp')

    def __init__(self, nc, ndma=48):
        self.nc = nc
        self.ndma = ndma
        self.es = ExitStack()
        self.scope = None
        self.drams = {}

    def __enter__(self):
        nc = self.nc
        self.es.__enter__()
        self.engs = {'pe': nc.tensor, 'act': nc.scalar, 'dve': nc.vector, 'pool': nc.gpsimd, 'sp': nc.sync}
        self.sems = {k: self.es.enter_context(nc.semaphore("s_" + k)) for k in ('pe', 'act', 'dve', 'pool')}
        self.cnt = {k: 0 for k in self.sems}
        self.dsem = [self.es.enter_context(nc.semaphore("d%d" % i)) for i in range(self.ndma)]
        self.dval = [0] * self.ndma
        self.dnext = 0
        self.dnext_sw = 0
        self.seen = {e: {} for e in self.ENG}
        self.scope = self.es
        self.ninst = 0
        return self

    def __exit__(self, *a):
        return self.es.__exit__(*a)

    def _uniq(self, name):
        self.nalloc = getattr(self, "nalloc", 0) + 1
        return "%s_%d" % (name, self.nalloc)

    def sb(self, name, shape, dtype):
        name = self._uniq(name)
        return T(name, self.scope.enter_context(self.nc.sbuf_tensor(name, list(shape), dtype)))

    def ps(self, name, shape, dtype):
        name = self._uniq(name)
        return T(name, self.scope.enter_context(self.nc.psum_tensor(name, list(shape), dtype)))

    def dram_buf(self, name):
        if name not in self.drams:
            self.drams[name] = Buf(name)
        return self.drams[name]

    @contextmanager
    def phase(self):
        old = self.scope
        with ExitStack() as st:
            self.scope = st
            yield
            self.barrier()
        self.scope = old

    def _semobj(self, key):
        return self.sems[key] if isinstance(key, str) else self.dsem[key[1]]

    def _wait(self, eng, tok, raw=False):
        key, val = tok
        if key == eng and not (raw and eng != 'pe'):
            return
        if self.seen[eng].get(key, 0) >= val:
            return
        self.engs[eng].wait_ge(self._semobj(key), val)
        self.seen[eng][key] = val
        self.ninst += 1

    def _deps(self, eng, r, w):
        for b in r:
            if b.w is not None:
                self._wait(eng, b.w, raw=True)
        for b in w:
            if b.w is not None:
                self._wait(eng, b.w)
            for k, v in b.r.items():
                self._wait(eng, (k, v))

    def _mark(self, tok, r, w):
        k, v = tok
        for b in r:
            if b.r.get(k, 0) < v:
                b.r[k] = v
        for b in w:
            b.w = tok
            b.r = {}

    def op(self, eng, fn, r=(), w=()):
        self._deps(eng, r, w)
        inst = fn(self.engs[eng])
        self.cnt[eng] += 1
        inst.then_inc(self.sems[eng], 1)
        self._mark((eng, self.cnt[eng]), r, w)
        self.ninst += 1

    def dma(self, wbuf, out_ap, in_ap, r=(), q='sp', **kw):
        half = self.ndma // 2
        if q == 'pool':
            i = half + self.dnext_sw
            self.dnext_sw = (self.dnext_sw + 1) % (self.ndma - half)
        else:
            i = self.dnext
            self.dnext = (self.dnext + 1) % half
        if self.dval[i] > 0:
            self._wait(q, (('d', i), self.dval[i]))
        w = [] if wbuf is None else ([wbuf] if isinstance(wbuf, Buf) else list(wbuf))
        self._deps(q, r, w)
        inst = self.engs[q].dma_start(out=out_ap, in_=in_ap, **kw)
        self.dval[i] += 16
        inst.then_inc(self.dsem[i], 16)
        self._mark((('d', i), self.dval[i]), r, w)
        self.ninst += 1

    def barrier(self):
        for e in self.ENG:
            for f in self.sems:
                if self.cnt[f] > 0:
                    self._wait(e, (f, self.cnt[f]))
            for i in range(self.ndma):
                if self.dval[i] > 0:
                    self._wait(e, (('d', i), self.dval[i]))

    def finish(self):
        self.barrier()


D = 1024
L = 4096
LC = 256
NT = L + LC
NTILE = NT // 128
DEPTH = 2
GW = 64
EPS = 1e-6
NEGB = -30000.0
O_NAQ, O_NAK, O_NAV, O_DFQ, O_DFK, O_DFV, O_RTQ, O_RTK, O_RTV, O_RTG = 0, 256, 512, 768, 1024, 1280, 1536, 1792, 2048, 2560
C_QNA, C_KNA, C_QDF, C_KDA, C_KDB, C_QRT, C_KRT = 0, 2, 4, 6, 8, 10, 12
NFMC = 14


def _rope_tables():
    t = np.arange(L)
    rows = (t // GW).astype(np.float32)
    cols = (t % GW).astype(np.float32)
    out = {}
    for name, dim in (("df", 32), ("rt", 64)):
        half = dim // 2
        inv = np.power(np.float32(10000.0), -np.arange(0, half, 2, dtype=np.float32) / np.float32(half)).astype(np.float32)
        ang = np.concatenate([rows[:, None] * inv, cols[:, None] * inv], axis=-1).astype(np.float32)
        cos = np.cos(ang).astype(np.float32)
        sin = np.sin(ang).astype(np.float32)
        C = np.ones((128, NT), np.float32)
        S = np.zeros((128, NT), np.float32)
        for r in range(128):
            d = r % dim
            pi = d // 2
            C[r, LC:] = cos[:, pi]
            S[r, LC:] = -sin[:, pi] if d % 2 == 0 else sin[:, pi]
        out[name] = (C, S)
    return out


def _na_patterns():
    pats = {}
    for pname, r in (("r0", 0), ("r2", 2), ("mid", 8), ("r60", 60), ("r62", 62)):
        kr0 = min(max(r - 4, 0), 54)
        chunks = []
        for m in range(5):
            dr = np.zeros((128, 128), np.int64)
            dc = np.zeros((128, 128), np.int64)
            va = np.zeros((128, 128), bool)
            for kk in range(128):
                krow = kr0 + 2 * m + kk // 64
                kc = kk % 64
                for qq in range(128):
                    qrow = r + qq // 64
                    qc = qq % 64
                    r0 = min(max(qrow - 4, 0), 56)
                    cs = min(max(qc - 8, 0), 48)
                    ok = (r0 <= krow < r0 + 8) and (cs <= kc < cs + 16) and krow < 64
                    va[kk, qq] = ok
                    if ok:
                        dr[kk, qq] = krow - qrow + 7
                        dc[kk, qq] = min(max(kc - qc, -15), 15) + 15
            chunks.append((dr, dc, va))
        pats[pname] = chunks
    return pats


_CONST_CACHE = {}


def _consts():
    if _CONST_CACHE:
        return _CONST_CACHE
    c = _CONST_CACHE
    rt = _rope_tables()
    s_df = np.float32(32 ** -0.5)
    c["T_DFQ_C"] = rt["df"][0] * s_df
    c["T_DFQ_S"] = rt["df"][1] * s_df
    c["T_DFK_C"] = rt["df"][0]
    c["T_DFK_S"] = rt["df"][1]
    c["T_RTQ_C"] = rt["rt"][0]
    c["T_RTQ_S"] = rt["rt"][1]
    c["T_RTK_C"] = rt["rt"][0] * np.float32(0.125)
    c["T_RTK_S"] = rt["rt"][1] * np.float32(0.125)
    c["IDENT"] = np.eye(128, dtype=np.float32)
    pats = _na_patterns()
    c["_pats"] = pats
    names = ["r0", "r2", "mid", "r60", "r62"]
    mask = np.zeros((128, 5, 5, 128), np.float32)
    for pi, pn in enumerate(names):
        for m in range(5):
            mask[:, pi, m, :] = np.where(pats[pn][m][2], 0.0, NEGB)
    c["NA_MASK"] = mask
    i = np.arange(128, dtype=np.float32)
    ij = i[None, :] - i[:, None]
    c["RT_RIJ"] = np.maximum(ij, 0).astype(np.float32)
    c["RT_MIJ"] = (ij >= 0).astype(np.float32)
    c["RT_RJI"] = np.maximum(-ij, 0).astype(np.float32)
    c["RT_MJI"] = (ij <= 0).astype(np.float32)
    c["RT_IROW"] = np.broadcast_to(i[None, :] + 1.0, (128, 128)).astype(np.float32).copy()
    c["RT_IROWB"] = np.broadcast_to(128.0 - i[None, :], (128, 128)).astype(np.float32).copy()
    c["RT_JCOL"] = np.stack([127.0 - i, i], axis=1).astype(np.float32)
    return c


def _layer_inputs(inp, l):
    c = _consts()
    w_in = np.asarray(inp["w_in"][l], np.float32)
    Z = np.zeros((D, 32), np.float32)

    def sw(cols):
        return cols ^ 1

    fm = []

    def add(cols):
        fm.append(w_in[:, cols])

    ar = np.arange
    for j in range(2):
        add(O_NAQ + j * 128 + ar(128))
    for j in range(2):
        add(O_NAK + j * 128 + ar(128))
    for j in range(2):
        cc = O_DFQ + j * 128 + ar(128)
        add(cc); add(sw(cc))
    for comp in range(2):
        for j in range(2):
            blocks_m, blocks_s = [], []
            for hh in range(2):
                h = 2 * j + hh
                cc = O_DFK + h * 64 + comp * 32 + ar(32)
                if comp == 0:
                    blocks_m += [w_in[:, cc], Z]; blocks_s += [w_in[:, sw(cc)], Z]
                else:
                    blocks_m += [Z, w_in[:, cc]]; blocks_s += [Z, w_in[:, sw(cc)]]
            fm.append(np.concatenate(blocks_m, axis=1)); fm.append(np.concatenate(blocks_s, axis=1))
    for j in range(2):
        cc = O_RTQ + j * 128 + ar(128)
        add(cc); add(sw(cc))
    for j in range(2):
        cc = O_RTK + j * 128 + ar(128)
        add(cc); add(sw(cc))
    WFM = np.ascontiguousarray(np.concatenate(fm, axis=1))
    WTM = np.ascontiguousarray(np.concatenate([w_in[:, O_NAV:O_NAV + 256], w_in[:, O_DFV:O_DFV + 256],
                                               w_in[:, O_RTV:O_RTV + 512], w_in[:, O_RTG:O_RTG + 512]], axis=1))
    rpb = np.asarray(inp["na_rpb"][l], np.float32)
    pats = c["_pats"]
    names = ["r0", "r2", "mid", "r60", "r62"]
    nab = np.zeros((128, 4, 5, 5, 128), np.float32)
    for pi, pn in enumerate(names):
        for m in range(5):
            dr, dc, va = pats[pn][m]
            for h in range(4):
                nab[:, h, pi, m, :] = rpb[h][dr, dc]
    sk = np.asarray(inp["peer_subkeys"][l], np.float32).reshape(16, 128, 128)
    skt = np.ascontiguousarray(sk.transpose(2, 0, 1))
    d = {
        "w_ada": np.asarray(inp["w_ada"][l], np.float32),
        "b_ada": np.asarray(inp["b_ada"][l], np.float32).reshape(1, 6 * D),
        "g1": np.asarray(inp["norm1_g"][l], np.float32).reshape(1, D),
        "g2": np.asarray(inp["norm2_g"][l], np.float32).reshape(1, D),
        "wfm": WFM, "wtm": WTM, "nab": nab,
        "lam": np.asarray(inp["diff_lambda"][l], np.float32).reshape(1, 128),
        "subg": np.asarray(inp["diff_subln_g"][l], np.float32).reshape(1, 64),
        "dec": np.asarray(inp["ret_decay_logit"][l], np.float32).reshape(1, 8),
        "wout": np.asarray(inp["w_out"][l], np.float32),
        "wq": np.asarray(inp["peer_wq"][l], np.float32),
        "skt": skt,
        "ut": np.ascontiguousarray(np.asarray(inp["peer_u"][l], np.float32).T),
        "v": np.asarray(inp["peer_v"][l], np.float32),
    }
    return d


LAYER_SHAPES = {
    "w_ada": [D, 6 * D], "b_ada": [1, 6 * D], "g1": [1, D], "g2": [1, D], "wfm": [D, 24 * 128], "wtm": [D, 1536],
    "nab": [128, 4, 5, 5, 128], "lam": [1, 128], "subg": [1, 64], "dec": [1, 8], "wout": [D, D], "wq": [D, 2048],
    "skt": [128, 16, 128], "ut": [D, 16384], "v": [16384, D],
}
CONST_NAMES = ["T_DFQ_C", "T_DFQ_S", "T_DFK_C", "T_DFK_S", "T_RTQ_C", "T_RTQ_S", "T_RTK_C", "T_RTK_S", "IDENT", "NA_MASK",
               "RT_RIJ", "RT_MIJ", "RT_RJI", "RT_MJI", "RT_IROW", "RT_IROWB", "RT_JCOL"]


class Ctx:
    pass


def build_program(nlayers=DEPTH, debug=None, stop_after=None):
    nc = bass.Bass("TRN2", target_bir_lowering=False)
    g = Ctx()
    g.nc = nc
    g.debug = debug or ()
    din = lambda name, shape, dt=F32: nc.dram_tensor(name, list(shape), dt, kind="ExternalInput").ap()
    dscr = lambda name, shape, dt: nc.dram_tensor(name, list(shape), dt, kind="Internal").ap()
    g.xin = din("xin", [NT, D])
    g.cvec = din("cvec", [128, 8, 2])
    g.final_g = din("final_g", [1, D])
    g.cst = {}
    cs = _consts()
    for n in CONST_NAMES:
        g.cst[n] = din(n, cs[n].shape)
    g.lay = []
    for l in range(nlayers):
        g.lay.append({k: din("L%d_%s" % (l, k), shp) for k, shp in LAYER_SHAPES.items()})
    g.out = nc.dram_tensor("out", [L, D], F32, kind="ExternalOutput").ap()
    g.MODS = dscr("MODS", [2, 6 * D], F32)
    g.X1 = dscr("X1", [NT, D], F32)
    g.X2 = dscr("X2", [NT, D], F32)
    g.FMS = dscr("FMS", [NFMC, 128, NT], BF16)
    g.VNA = dscr("VNA", [NT, 4, 65], BF16)
    g.VDF = dscr("VDF", [NT, 4, 65], BF16)
    g.VRT = dscr("VRT", [NT, 512], BF16)
    g.GRT = dscr("GRT", [NT, 512], BF16)
    g.KRTT = dscr("KRTT", [NT, 256], BF16)
    g.Y = dscr("Y", [NT, D], BF16)
    g.H2T = dscr("H2T", [128, 8, NT], BF16)
    g.QPT = dscr("QPT", [128, 16, NT], BF16)
    g.UTB = dscr("UTB", [128, 8, 16384], BF16)
    g.VB = dscr("VB", [128, 128, D], BF16)
    g.dbg = {}
    for name, shape, dt in g.debug:
        g.dbg[name] = nc.dram_tensor("dbg_" + name, list(shape), dt, kind="ExternalOutput").ap()
    p = Prog(nc)
    g.p = p
    g.scr = Buf("scratch")
    with p:
        g.ident_f = p.sb("ident_f", [128, 128], F32)
        g.ident = p.sb("ident", [128, 128], BF16)
        p.dma(g.ident_f, g.ident_f[:], g.cst["IDENT"])
        p.op('dve', lambda e: e.tensor_copy(out=g.ident[:], in_=g.ident_f[:]), r=[g.ident_f], w=[g.ident])
        stages = []
        for l in range(nlayers):
            last = (l == DEPTH - 1)
            xa = g.xin if l == 0 else g.X2
            stages += [("mod%d" % l, lambda l=l: phase_mod(g, l)),
                       ("p1_%d" % l, lambda l=l, xa=xa: phase_p1(g, l, xa)),
                       ("na_%d" % l, lambda l=l, last=last: phase_na(g, l, not last)),
                       ("df_%d" % l, lambda l=l, last=last: phase_diff(g, l, not last)),
                       ("rt_%d" % l, lambda l=l, last=last: (phase_ret(g, l, not last), dbg_dump(g, "y%d" % l, g.Y))),
                       ("p3_%d" % l, lambda l=l, last=last, xa=xa: (phase_p3(g, l, xa, last), dbg_dump(g, "x1_%d" % l, g.X1))),
                       ("peerq%d" % l, lambda l=l, last=last: phase_peerq(g, l, last)),
                       ("peer%d" % l, lambda l=l, last=last: (phase_peer(g, l, last), dbg_dump(g, "x2_%d" % l, g.X2)))]
        for name, fn in stages:
            fn()
            if stop_after == name:
                break
        p.finish()
    g.ninst = p.ninst
    return nc, g


def dbg_dump(g, name, src):
    if name in g.dbg:
        g.p.dma(None, g.dbg[name], src, q='sp')
        g.p.barrier()


def load_bcast(p, t, dram_row, n, q='sp'):
    p.dma(t, t[:, 0:n], dram_row.broadcast_to([128, n]), q=q)


def phase_mod(g, l):
    p = g.p
    Lw = g.lay[l]
    with p.phase():
        cv = p.sb("cv", [128, 8, 2], F32)
        sil = p.sb("sil", [128, 8, 2], F32)
        bad = p.sb("bad", [2, 6 * D], F32)
        modt = p.sb("modt", [2, 6 * D], F32)
        wa = [p.sb("wa%d" % i, [128, 8, 512], F32) for i in range(2)]
        ps = [p.ps("modps%d" % i, [128, 512], F32) for i in range(2)]
        p.dma(cv, cv[:], g.cvec)
        p.dma(bad, bad[:], Lw["b_ada"].broadcast_to([2, 6 * D]))
        p.op('act', lambda e: e.activation(out=sil[:], in_=cv[:], func=AF.Silu), r=[cv], w=[sil])
        wsrc = Lw["w_ada"].rearrange("(k p) n -> p k n", p=128)
        for n in range(12):
            w = wa[n % 2]
            pp = ps[n % 2]
            p.dma(w, w[:], wsrc[:, :, n * 512:(n + 1) * 512])
            for k in range(8):
                p.op('pe', lambda e, k=k, w=w, pp=pp: e.matmul(pp[0:2, :], lhsT=sil[:, k, :], rhs=w[:, k, :],
                                                             start=(k == 0), stop=(k == 7)), r=[sil, w], w=[pp])
            p.op('dve', lambda e, n=n, pp=pp: e.tensor_tensor(out=modt[:, n * 512:(n + 1) * 512], in0=pp[0:2, :],
                                                             in1=bad[:, n * 512:(n + 1) * 512], op=ALU.add),
                 r=[pp, bad], w=[modt])
        p.dma(None, g.MODS, modt[:], r=[modt], q='pool')
        if "mods" in g.dbg:
            p.dma(None, g.dbg["mods"], modt[:], r=[modt], q='pool')


def mod_row(g, which, idx):
    return g.MODS[which:which + 1, idx * D:(idx + 1) * D]


def make_norm_consts(g, tag, grow, idx_shift, idx_scale):
    p = g.p
    gb = p.sb(tag + "_gb", [128, D], F32)
    load_bcast(p, gb, grow, D)
    res = []
    for which in range(2):
        sc = p.sb(tag + "_sc%d" % which, [128, D], F32)
        sh = p.sb(tag + "_sh%d" % which, [128, D], F32)
        load_bcast(p, sc, mod_row(g, which, idx_scale), D)
        load_bcast(p, sh, mod_row(g, which, idx_shift), D)
        p.op('dve', lambda e, sc=sc: e.scalar_tensor_tensor(out=sc[:], in0=sc[:], scalar=1.0, in1=gb[:],
                                                            op0=ALU.add, op1=ALU.mult), r=[sc, gb], w=[sc])
        res.append((sc, sh))
    return res


def emit_norm(g, xt, gs, sh, hb, tmp, st):
    p = g.p
    p.op('act', lambda e: e.activation(out=tmp[:], in_=xt[:], func=AF.Square, accum_out=st[:, 0:1]), r=[xt], w=[tmp, st])
    p.op('dve', lambda e: e.tensor_scalar(out=st[:, 1:2], in0=st[:, 0:1], scalar1=1.0 / D, scalar2=EPS,
                                          op0=ALU.mult, op1=ALU.add), r=[st], w=[st])
    p.op('act', lambda e: e.activation(out=st[:, 2:3], in_=st[:, 1:2], func=AF.Sqrt), r=[st], w=[st])
    p.op('dve', lambda e: e.reciprocal(out=st[:, 3:4], in_=st[:, 2:3]), r=[st], w=[st])
    p.op('dve', lambda e: e.scalar_tensor_tensor(out=tmp[:], in0=xt[:], scalar=st[:, 3:4], in1=gs[:],
                                                 op0=ALU.mult, op1=ALU.mult), r=[xt, st, gs], w=[tmp])
    p.op('dve', lambda e: e.tensor_tensor(out=hb[:], in0=tmp[:], in1=sh[:], op=ALU.add), r=[tmp, sh], w=[hb])


def emit_transposes(g, src, pst, dst_ap, dst_buf, n=8, eng='act'):
    p = g.p
    for k in range(n):
        p.op('pe', lambda e, k=k: e.transpose(pst[:, k, :], src[:, k * 128:(k + 1) * 128], g.ident[:]),
             r=[src, g.ident], w=[pst])
    if eng == 'act':
        p.op('act', lambda e: e.copy(out=dst_ap, in_=pst[:, 0:n, :]), r=[pst], w=[dst_buf])
    else:
        p.op('dve', lambda e: e.tensor_copy(out=dst_ap, in_=pst[:, 0:n, :]), r=[pst], w=[dst_buf])


def tok_groups():
    gs = [(0, 256)]
    for i in range(8):
        gs.append((256 + i * 512, 512))
    return gs


def phase_p1(g, l, xa):
    p = g.p
    Lw = g.lay[l]
    with p.phase():
        HT = p.sb("HT", [128, 8, NT], BF16)
        with p.phase():
            nrm = make_norm_consts(g, "n1", Lw["g1"], 0, 1)
            xt = [p.sb("xt%d" % i, [128, D], F32) for i in range(2)]
            tmp = p.sb("ntmp", [128, D], F32)
            hb = [p.sb("hb%d" % i, [128, D], BF16) for i in range(2)]
            st = [p.sb("nst%d" % i, [128, 4], F32) for i in range(2)]
            pst = [p.ps("pst%d" % i, [128, 8, 128], BF16) for i in range(2)]
            for tt in range(NTILE):
                which = 1 if tt < 2 else 0
                x_ = xt[tt % 2]
                p.dma(x_, x_[:], xa[tt * 128:(tt + 1) * 128, :])
                emit_norm(g, x_, nrm[which][0], nrm[which][1], hb[tt % 2], tmp, st[tt % 2])
                emit_transposes(g, hb[tt % 2], pst[tt % 2], HT[:, :, tt * 128:(tt + 1) * 128], HT)
        if "ht" in g.dbg:
            p.dma(None, g.dbg["ht"], HT[:], r=[HT], q='pool')
        with p.phase():
            wf = [p.sb("wf%d" % i, [128, 8, 128], F32) for i in range(2)]
            wb = [p.sb("wb%d" % i, [128, 8, 128], BF16) for i in range(4)]
            tc_ = [p.sb("tc%d" % i, [128, 512], F32) for i in range(2)]
            ts_ = [p.sb("ts%d" % i, [128, 512], F32) for i in range(2)]
            t1 = [p.sb("rt1_%d" % i, [128, 512], F32) for i in range(2)]
            stg = [p.sb("fstg%d" % i, [128, 512], BF16) for i in range(2)]
            ktm = [p.sb("ktm%d" % i, [128, 4, 128], BF16) for i in range(2)]
            psA = [p.ps("psA%d" % i, [128, 512], F32) for i in range(2)]
            psB = [p.ps("psB%d" % i, [128, 512], F32) for i in range(2)]
            psT = [p.ps("psT%d" % i, [128, 8, 128], BF16) for i in range(2)]
            wsrc = Lw["wfm"].rearrange("(k p) n -> p k n", p=128)
            cnt = {"w": 0, "it": 0}

            def load_w(ci):
                i = cnt["w"]
                cnt["w"] += 1
                f, b = wf[i % 2], wb[i % 4]
                p.dma(f, f[:], wsrc[:, :, ci * 128:(ci + 1) * 128])
                p.op('pool', lambda e: e.tensor_copy(out=b[:], in_=f[:]), r=[f], w=[b])
                return b

            def mm(ps_, w_, t0, n):
                for k in range(8):
                    p.op('pe', lambda e, k=k: e.matmul(ps_[:, 0:n], lhsT=w_[:, k, :], rhs=HT[:, k, t0:t0 + n],
                                                       start=(k == 0), stop=(k == 7)), r=[w_, HT], w=[ps_])

            jobs = []
            wi = 0
            for j in range(2):
                jobs.append((C_QNA + j, wi, None, 0.125, None)); wi += 1
            for j in range(2):
                jobs.append((C_KNA + j, wi, None, 1.0, None)); wi += 1
            for dest, tn in ((C_QDF, "T_DFQ"), (C_KDA, "T_DFK"), (C_KDB, "T_DFK"), (C_QRT, "T_RTQ"), (C_KRT, "T_RTK")):
                for j in range(2):
                    jobs.append((dest + j, wi, wi + 1, 1.0, tn)); wi += 2
            for (dest, wm, ws, scale, tn) in jobs:
                wmb = load_w(wm)
                wsb = load_w(ws) if ws is not None else None
                for (t0, n) in tok_groups():
                    it = cnt["it"]
                    cnt["it"] += 1
                    a, b_ = psA[it % 2], psB[it % 2]
                    sg = stg[it % 2]
                    mm(a, wmb, t0, n)
                    if ws is None:
                        p.op('act', lambda e: e.activation(out=sg[:, 0:n], in_=a[:, 0:n], func=AF.Copy, scale=scale),
                             r=[a], w=[sg])
                    else:
                        mm(b_, wsb, t0, n)
                        tcc, tss, tt1 = tc_[it % 2], ts_[it % 2], t1[it % 2]
                        p.dma(tcc, tcc[:, 0:n], g.cst[tn + "_C"][:, t0:t0 + n])
                        p.dma(tss, tss[:, 0:n], g.cst[tn + "_S"][:, t0:t0 + n])
                        p.op('dve', lambda e: e.tensor_tensor(out=tcc[:, 0:n], in0=a[:, 0:n], in1=tcc[:, 0:n], op=ALU.mult),
                             r=[a, tcc], w=[tcc])
                        p.op('dve', lambda e: e.tensor_tensor(out=tss[:, 0:n], in0=b_[:, 0:n], in1=tss[:, 0:n], op=ALU.mult),
                             r=[b_, tss], w=[tss])
                        p.op('pool', lambda e: e.tensor_tensor(out=sg[:, 0:n], in0=tcc[:, 0:n], in1=tss[:, 0:n], op=ALU.add),
                             r=[tcc, tss], w=[sg])
                    p.dma(None, g.FMS[dest, :, t0:t0 + n], sg[:, 0:n], r=[sg], q='pool')
                    if dest in (C_KRT, C_KRT + 1):
                        nt_ = n // 128
                        pt, kt = psT[it % 2], ktm[it % 2]
                        emit_transposes(g, sg, pt, kt[:, 0:nt_, :], kt, n=nt_)
                        cj = dest - C_KRT
                        p.dma(None, g.KRTT[t0:t0 + n, cj * 128:(cj + 1) * 128].rearrange("(a p) c -> p a c", p=128),
                              kt[:, 0:nt_, :], r=[kt], q='pool')
        with p.phase():
            wtf = p.sb("wtf", [128, 8, 512], F32)
            wtb = p.sb("wtb", [128, 8, 1536], BF16)
            wsrc = Lw["wtm"].rearrange("(k p) n -> p k n", p=128)
            for c3 in range(3):
                p.dma(wtf, wtf[:], wsrc[:, :, c3 * 512:(c3 + 1) * 512])
                p.op('dve', lambda e, c3=c3: e.tensor_copy(out=wtb[:, :, c3 * 512:(c3 + 1) * 512], in_=wtf[:]), r=[wtf], w=[wtb])
            pv = [p.ps("pv%d" % i, [128, 512], F32) for i in range(4)]
            vst = [p.sb("vst%d" % i, [128, 2, 4, 65], BF16) for i in range(2)]
            rst = [p.sb("rst%d" % i, [128, 2, 512], BF16) for i in range(2)]
            for i in range(2):
                p.op('pool', lambda e, i=i: e.memset(vst[i][:], 1.0), w=[vst[i]])
            it = 0
            for tt in range(NTILE):
                vs, rs = vst[tt % 2], rst[tt % 2]
                for c3 in range(3):
                    ps_ = pv[it % 4]
                    it += 1
                    for k in range(8):
                        p.op('pe', lambda e, k=k, ps_=ps_, c3=c3: e.matmul(ps_[:], lhsT=HT[:, k, tt * 128:(tt + 1) * 128],
                                                                          rhs=wtb[:, k, c3 * 512:(c3 + 1) * 512],
                                                                          start=(k == 0), stop=(k == 7)), r=[HT, wtb], w=[ps_])
                    if c3 == 0:
                        p.op('act', lambda e, ps_=ps_: e.copy(out=vs[:, :, :, 0:64],
                                                             in_=ps_[:].rearrange("p (a h d) -> p a h d", a=2, h=4)),
                             r=[ps_], w=[vs])
                    elif c3 == 1:
                        p.op('dve', lambda e, ps_=ps_: e.tensor_copy(out=rs[:, 0, :], in_=ps_[:]), r=[ps_], w=[rs])
                    else:
                        p.op('act', lambda e, ps_=ps_: e.activation(out=rs[:, 1, :], in_=ps_[:], func=AF.Silu), r=[ps_], w=[rs])
                sl = slice(tt * 128, (tt + 1) * 128)
                p.dma(None, g.VNA[sl], vs[:, 0], r=[vs], q='pool')
                p.dma(None, g.VDF[sl], vs[:, 1], r=[vs], q='pool')
                p.dma(None, g.VRT[sl], rs[:, 0, :], r=[rs], q='pool')
                p.dma(None, g.GRT[sl], rs[:, 1, :], r=[rs], q='pool')
        for nm, src in (("fms", g.FMS), ("vna", g.VNA), ("vdf", g.VDF), ("vrt", g.VRT), ("grt", g.GRT), ("krtt", g.KRTT)):
            if nm in g.dbg:
                p.dma(None, g.dbg[nm], src, q='sp')


def phase_na(g, l, do_ctx):
    p = g.p
    Lw = g.lay[l]
    with p.phase():
        bias = p.sb("na_bias", [128, 4, 25, 128], BF16)
        with p.phase():
            stg = p.sb("na_bstg", [128, 25, 128], F32)
            msk = p.sb("na_msk", [128, 25, 128], F32)
            p.dma(msk, msk[:], g.cst["NA_MASK"].rearrange("p a b q -> p (a b) q"))
            for h in range(4):
                p.dma(stg, stg[:], Lw["nab"][:, h].rearrange("p a b q -> p (a b) q"))
                p.op('dve', lambda e, h=h: e.tensor_tensor(out=bias[:, h], in0=stg[:], in1=msk[:], op=ALU.add),
                     r=[stg, msk], w=[bias])
        V = p.sb("na_v", [128, NTILE, 260], BF16)
        p.dma(V, V[:], g.VNA.rearrange("(a p) h d -> p a (h d)", p=128))
        qT = [p.sb("na_q%d" % i, [128, NT], BF16) for i in range(2)]
        kT = [p.sb("na_k%d" % i, [128, NT], BF16) for i in range(2)]
        for i in range(2):
            p.dma(qT[i], qT[i][:], g.FMS[C_QNA + i])
            p.dma(kT[i], kT[i][:], g.FMS[C_KNA + i])
        psS = [[p.ps("na_s%d%d" % (i, j), [128, 512], F32) for j in range(2)] for i in range(2)]
        pso = [p.ps("na_o%d" % i, [128, 512], F32) for i in range(2)]
        PT = [p.sb("na_pt%d" % i, [128, 8, 128], BF16) for i in range(2)]
        ys = [p.sb("na_ys%d" % i, [128, 256], BF16) for i in range(2)]
        rc = [p.sb("na_rc%d" % i, [128, 1], F32) for i in range(2)]
        tiles = []
        if do_ctx:
            for q0 in (0, 128):
                tiles.append((q0, [(0, None), (128, None)], 0))
        for rp in range(32):
            r = 2 * rp
            kr0 = min(max(r - 4, 0), 54)
            pat = {0: 0, 2: 1, 60: 3, 62: 4}.get(r, 2)
            ch = [(LC + (kr0 + 2 * m) * 64, m) for m in range(5)] + [(0, None), (128, None)]
            tiles.append((LC + rp * 128, ch, pat))
        it = 0
        for qi, (q0, ch, pat) in enumerate(tiles):
            y_ = ys[qi % 2]
            for h in range(4):
                hp, base = h // 2, (h % 2) * 64
                sA, sB = psS[it % 2]
                po, pt, rc_ = pso[it % 2], PT[it % 2], rc[it % 2]
                it += 1
                n = len(ch)
                for i, (tok0, m) in enumerate(ch):
                    bank = sA if i < 4 else sB
                    col = (i % 4) * 128
                    p.op('pe', lambda e, bank=bank, col=col, tok0=tok0, m=m: e.matmul(
                        bank[:, col:col + 128], lhsT=kT[hp][base:base + 64, tok0:tok0 + 128],
                        rhs=qT[hp][base:base + 64, q0:q0 + 128], start=True, stop=(m is None)),
                        r=[kT[hp], qT[hp]], w=[bank])
                    if m is not None:
                        p.op('pe', lambda e, bank=bank, col=col, m=m: e.matmul(
                            bank[:, col:col + 128], lhsT=g.ident[:], rhs=bias[:, h, pat * 5 + m, :],
                            start=False, stop=True), r=[g.ident, bias], w=[bank])
                na_ = min(n, 4)
                p.op('act', lambda e: e.activation(out=pt[:, 0:na_, :], in_=sA[:, 0:na_ * 128].rearrange("p (a q) -> p a q", q=128),
                                                   func=AF.Exp), r=[sA], w=[pt])
                if n > 4:
                    p.op('act', lambda e: e.activation(out=pt[:, 4:n, :], in_=sB[:, 0:(n - 4) * 128].rearrange("p (a q) -> p a q", q=128),
                                                       func=AF.Exp), r=[sB], w=[pt])
                for i, (tok0, m) in enumerate(ch):
                    vt = tok0 // 128
                    p.op('pe', lambda e, i=i, vt=vt: e.matmul(po[:, 0:65], lhsT=pt[:, i, :], rhs=V[:, vt, h * 65:(h + 1) * 65],
                                                              start=(i == 0), stop=(i == n - 1)), r=[pt, V], w=[po])
                p.op('dve', lambda e: e.reciprocal(out=rc_[:], in_=po[:, 64:65]), r=[po], w=[rc_])
                p.op('dve', lambda e: e.tensor_scalar(out=y_[:, h * 64:(h + 1) * 64], in0=po[:, 0:64], scalar1=rc_[:, 0:1],
                                                      scalar2=None, op0=ALU.mult), r=[po, rc_], w=[y_])
            p.dma(None, g.Y[q0:q0 + 128, 0:256], y_[:], r=[y_], q='pool')


def phase_diff(g, l, do_ctx):
    p = g.p
    Lw = g.lay[l]
    import math
    lam_init = 0.8 - 0.6 * math.exp(-0.3 * l)
    with p.phase():
        lp = p.sb("df_lp", [128, 4, 32], F32)
        pr = p.sb("df_pr", [128, 2, 32], F32)
        sm = p.sb("df_sm", [128, 4], F32)
        neglam = p.sb("df_nl", [128, 1], F32)
        gsub = p.sb("df_gs", [128, 64], F32)
        p.dma(lp, lp[:].rearrange("p a b -> p (a b)"), Lw["lam"].broadcast_to([128, 128]))
        load_bcast(p, gsub, Lw["subg"], 64)
        p.op('dve', lambda e: e.tensor_scalar(out=gsub[:], in0=gsub[:], scalar1=1.0 - lam_init, scalar2=None, op0=ALU.mult),
             r=[gsub], w=[gsub])
        p.op('dve', lambda e: e.tensor_tensor(out=pr[:], in0=lp[:, 0:4:2, :], in1=lp[:, 1:4:2, :], op=ALU.mult), r=[lp], w=[pr])
        p.op('dve', lambda e: e.tensor_reduce(out=sm[:, 0:2], in_=pr[:], axis=AX.X, op=ALU.add), r=[pr], w=[sm])
        p.op('act', lambda e: e.activation(out=sm[:, 2:4], in_=sm[:, 0:2], func=AF.Exp), r=[sm], w=[sm])
        p.op('dve', lambda e: e.tensor_tensor(out=neglam[:], in0=sm[:, 3:4], in1=sm[:, 2:3], op=ALU.subtract), r=[sm], w=[neglam])
        p.op('dve', lambda e: e.tensor_scalar(out=neglam[:], in0=neglam[:], scalar1=-lam_init, scalar2=None, op0=ALU.add),
             r=[neglam], w=[neglam])
        V = p.sb("df_v", [128, NTILE, 260], BF16)
        p.dma(V, V[:], g.VDF.rearrange("(a p) h d -> p a (h d)", p=128))
        qT = [p.sb("df_q%d" % i, [128, NT], BF16) for i in range(2)]
        kA = [p.sb("df_ka%d" % i, [128, NT], BF16) for i in range(2)]
        kB = [p.sb("df_kb%d" % i, [128, NT], BF16) for i in range(2)]
        for i in range(2):
            p.dma(qT[i], qT[i][:], g.FMS[C_QDF + i])
            p.dma(kA[i], kA[i][:], g.FMS[C_KDA + i])
            p.dma(kB[i], kB[i][:], g.FMS[C_KDB + i])
        acc = [p.ps("df_acc%d" % i, [128, 512], F32) for i in range(4)]
        psS = [p.ps("df_s%d" % i, [128, 512], F32) for i in range(2)]
        PT = [p.sb("df_pt%d" % i, [128, 512], BF16) for i in range(2)]
        oc = [p.sb("df_oc%d" % i, [128, 4, 64], F32) for i in range(2)]
        o = p.sb("df_o", [128, 4, 64], F32)
        sq = p.sb("df_sq", [128, 4, 64], F32)
        ss = p.sb("df_ss", [128, 8], F32)
        rc = p.sb("df_rc", [128, 4], F32)
        yd = [p.sb("df_y%d" % i, [128, 4, 64], BF16) for i in range(2)]
        groups = []
        if do_ctx:
            groups.append((0, 256, [0, 1]))
        for gi in range(8):
            groups.append((LC + gi * 512, 512, list(range(NTILE))))
        it = 0
        gi_ = 0
        cgen = cast_gen(g, l)
        cstate = {"n": 0}

        def cast_tick():
            cstate["n"] += 1
            if cstate["n"] % 16 == 0:
                next(cgen, None)

        for h in range(4):
            hp, base = h // 2, (h % 2) * 64
            for (t0, n, kcs) in groups:
                ns = n // 128
                for c in range(2):
                    kX = (kA if c == 0 else kB)[hp]

                    def qk(ki):
                        kc = kcs[ki]
                        ps_ = psS[ki % 2]
                        p.op('pe', lambda e: e.matmul(
                            ps_[:, 0:n], lhsT=kX[base:base + 64, kc * 128:(kc + 1) * 128], rhs=qT[hp][base:base + 64, t0:t0 + n],
                            start=True, stop=True), r=[kX, qT[hp]], w=[ps_])

                    def ex(ki):
                        ps_, pt = psS[ki % 2], PT[ki % 2]
                        p.op('act', lambda e: e.activation(out=pt[:, 0:n], in_=ps_[:, 0:n], func=AF.Exp), r=[ps_], w=[pt])

                    def pv(ki):
                        kc = kcs[ki]
                        pt = PT[ki % 2]
                        for s in range(ns):
                            p.op('pe', lambda e, s=s: e.matmul(
                                acc[s][:, 0:65], lhsT=pt[:, s * 128:(s + 1) * 128], rhs=V[:, kc, h * 65:(h + 1) * 65],
                                start=(ki == 0), stop=(ki == len(kcs) - 1)), r=[pt, V], w=[acc[s]])

                    qk(0)
                    ex(0)
                    for ki in range(len(kcs)):
                        if ki + 1 < len(kcs):
                            qk(ki + 1)
                            ex(ki + 1)
                        pv(ki)
                        cast_tick()
                    for s in range(ns):
                        p.op('dve', lambda e, s=s: e.reciprocal(out=rc[:, s:s + 1], in_=acc[s][:, 64:65]), r=[acc[s]], w=[rc])
                        p.op('dve', lambda e, s=s, c=c: e.tensor_scalar(out=oc[c][:, s, :], in0=acc[s][:, 0:64], scalar1=rc[:, s:s + 1],
                                                                       scalar2=None, op0=ALU.mult), r=[acc[s], rc], w=[oc[c]])
                y_ = yd[gi_ % 2]
                gi_ += 1
                p.op('dve', lambda e: e.scalar_tensor_tensor(out=o[:, 0:ns, :], in0=oc[1][:, 0:ns, :], scalar=neglam[:, 0:1],
                                                             in1=oc[0][:, 0:ns, :], op0=ALU.mult, op1=ALU.add),
                     r=[oc[0], oc[1], neglam], w=[o])
                p.op('dve', lambda e: e.tensor_tensor(out=sq[:, 0:ns, :], in0=o[:, 0:ns, :], in1=o[:, 0:ns, :], op=ALU.mult), r=[o], w=[sq])
                p.op('dve', lambda e: e.tensor_reduce(out=ss[:, 0:ns], in_=sq[:, 0:ns, :], axis=AX.X, op=ALU.add), r=[sq], w=[ss])
                p.op('dve', lambda e: e.tensor_scalar(out=ss[:, 0:ns], in0=ss[:, 0:ns], scalar1=1.0 / 64, scalar2=EPS,
                                                      op0=ALU.mult, op1=ALU.add), r=[ss], w=[ss])
                p.op('act', lambda e: e.activation(out=ss[:, 4:4 + ns], in_=ss[:, 0:ns], func=AF.Sqrt), r=[ss], w=[ss])
                p.op('dve', lambda e: e.reciprocal(out=ss[:, 0:ns], in_=ss[:, 4:4 + ns]), r=[ss], w=[ss])
                p.op('dve', lambda e: e.tensor_tensor(out=sq[:, 0:ns, :], in0=o[:, 0:ns, :],
                                                      in1=ss[:, 0:ns].unsqueeze(2).broadcast_to([128, ns, 64]), op=ALU.mult),
                     r=[o, ss], w=[sq])
                p.op('dve', lambda e: e.tensor_tensor(out=y_[:, 0:ns, :], in0=sq[:, 0:ns, :],
                                                      in1=gsub[:].unsqueeze(1).broadcast_to([128, ns, 64]), op=ALU.mult),
                     r=[sq, gsub], w=[y_])
                p.dma(None, g.Y[t0:t0 + n, 256 + h * 64:256 + (h + 1) * 64].rearrange("(s p) d -> p s d", p=128),
                      y_[:, 0:ns, :], r=[y_], q='pool')
        for _ in cgen:
            pass


def phase_ret(g, l, do_ctx):
    p = g.p
    Lw = g.lay[l]
    with p.phase():
        dec = p.sb("rt_dec", [128, 8], F32)
        lg = p.sb("rt_lg", [128, 8], F32)
        cdec = p.sb("rt_cdec", [128, 8], F32)
        kdec = p.sb("rt_kdec", [128, 8], F32)
        jcol = p.sb("rt_jcol", [128, 2], F32)
        cm = {}
        for nm in ("RT_RIJ", "RT_MIJ", "RT_RJI", "RT_MJI", "RT_IROW", "RT_IROWB"):
            cm[nm] = p.sb("c_" + nm, [128, 128], F32)
            p.dma(cm[nm], cm[nm][:], g.cst[nm])
        p.dma(jcol, jcol[:], g.cst["RT_JCOL"])
        load_bcast(p, dec, Lw["dec"], 8)
        p.op('act', lambda e: e.activation(out=lg[:], in_=dec[:], func=AF.Sigmoid), r=[dec], w=[lg])
        p.op('act', lambda e: e.activation(out=lg[:], in_=lg[:], func=AF.Ln), r=[lg], w=[lg])
        p.op('act', lambda e: e.activation(out=cdec[:], in_=lg[:], func=AF.Exp, scale=128.0), r=[lg], w=[cdec])
        QDF = p.sb("rt_qdf", [128, 4, 128], F32)
        QDB = p.sb("rt_qdb", [128, 4, 128], F32)
        DB = p.sb("rt_db", [128, 4, 128], F32)
        t1 = p.sb("rt_t1", [128, 128], F32)
        t2 = p.sb("rt_t2", [128, 128], F32)
        for d_ in range(2):
            for h in range(4):
                c = d_ * 4 + h
                p.op('act', lambda e, c=c, d_=d_: e.activation(out=kdec[:, c:c + 1], in_=jcol[:, d_:d_ + 1], func=AF.Exp,
                                                               scale=lg[:, c:c + 1]), r=[jcol, lg], w=[kdec])
        for h in range(4):
            p.op('act', lambda e, h=h: e.activation(out=QDF[:, h, :], in_=cm["RT_IROW"][:], func=AF.Exp, scale=lg[:, h:h + 1]),
                 r=[cm["RT_IROW"], lg], w=[QDF])
            p.op('act', lambda e, h=h: e.activation(out=QDB[:, h, :], in_=cm["RT_IROWB"][:], func=AF.Exp, scale=lg[:, 4 + h:5 + h]),
                 r=[cm["RT_IROWB"], lg], w=[QDB])
            p.op('act', lambda e, h=h: e.activation(out=t1[:], in_=cm["RT_RIJ"][:], func=AF.Exp, scale=lg[:, h:h + 1]),
                 r=[cm["RT_RIJ"], lg], w=[t1])
            p.op('act', lambda e, h=h: e.activation(out=t2[:], in_=cm["RT_RJI"][:], func=AF.Exp, scale=lg[:, 4 + h:5 + h]),
                 r=[cm["RT_RJI"], lg], w=[t2])
            p.op('dve', lambda e: e.tensor_tensor(out=t1[:], in0=t1[:], in1=cm["RT_MIJ"][:], op=ALU.mult), r=[t1, cm["RT_MIJ"]], w=[t1])
            p.op('dve', lambda e: e.tensor_tensor(out=t2[:], in0=t2[:], in1=cm["RT_MJI"][:], op=ALU.mult), r=[t2, cm["RT_MJI"]], w=[t2])
            p.op('dve', lambda e, h=h: e.tensor_tensor(out=DB[:, h, :], in0=t1[:], in1=t2[:], op=ALU.add), r=[t1, t2], w=[DB])
        qT = p.sb("rt_q", [64, NT], BF16)
        kT = p.sb("rt_k", [64, NT], BF16)
        Ktm = p.sb("rt_ktm", [128, NTILE, 64], BF16)
        V = p.sb("rt_v", [128, NTILE, 128], BF16)
        G = p.sb("rt_g", [128, NTILE, 128], BF16)
        KF = p.sb("rt_kf", [128, NTILE, 64], BF16)
        KB = p.sb("rt_kb", [128, NTILE, 64], BF16)
        SFa = p.sb("rt_sfa", [64, NTILE, 128], BF16)
        SBa = p.sb("rt_sba", [64, NTILE, 128], BF16)
        S32 = [p.sb("rt_S32_%d" % i, [64, NTILE, 128], F32) for i in range(2)]
        psKV = [p.ps("rt_kv%d" % i, [128, 512], F32) for i in range(2)]
        psA = [p.ps("rt_att%d" % i, [128, 512], F32) for i in range(2)]
        psO = [p.ps("rt_o%d" % i, [128, 512], F32) for i in range(2)]
        attm = [p.sb("rt_attm%d" % i, [128, 128], BF16) for i in range(2)]
        qf = [p.sb("rt_qf%d" % i, [64, 128], BF16) for i in range(2)]
        qb = [p.sb("rt_qb%d" % i, [64, 128], BF16) for i in range(2)]
        junk = p.sb("rt_junk", [128, 128], F32)
        st = [p.sb("rt_st%d" % i, [128, 4], F32) for i in range(2)]
        ys = [p.sb("rt_ys%d" % i, [128, 128], BF16) for i in range(2)]
        it = 0
        for h in range(4):
            hp, base = h // 2, (h % 2) * 64
            p.dma(qT, qT[:], g.FMS[C_QRT + hp, base:base + 64, :])
            p.dma(kT, kT[:], g.FMS[C_KRT + hp, base:base + 64, :])
            p.dma(Ktm, Ktm[:], g.KRTT[:, h * 64:(h + 1) * 64].rearrange("(a p) d -> p a d", p=128))
            p.dma(V, V[:], g.VRT[:, h * 128:(h + 1) * 128].rearrange("(a p) d -> p a d", p=128))
            p.dma(G, G[:], g.GRT[:, h * 128:(h + 1) * 128].rearrange("(a p) d -> p a d", p=128))
            p.op('dve', lambda e: e.tensor_scalar(out=KF[:], in0=Ktm[:], scalar1=kdec[:, h:h + 1], scalar2=None, op0=ALU.mult),
                 r=[Ktm, kdec], w=[KF])
            p.op('dve', lambda e: e.tensor_scalar(out=KB[:], in0=Ktm[:], scalar1=kdec[:, 4 + h:5 + h], scalar2=None, op0=ALU.mult),
                 r=[Ktm, kdec], w=[KB])
            for d_, (Kd, Sa, order) in enumerate(((KF, SFa, list(range(NTILE))),
                                                  (KB, SBa, [1, 0] + list(range(NTILE - 1, 1, -1))))):
                S_ = [T("rt_Ss%d_%d_%d" % (h, d_, c), S32[d_][:, c, :]) for c in range(NTILE)]
                p.op('pool', lambda e: e.memset(S_[order[0]][:], 0.0), r=[S32[d_]], w=[S_[order[0]], S32[d_]])
                cd = cdec[0:64, d_ * 4 + h:d_ * 4 + h + 1]
                for oi, c in enumerate(order):
                    p.op('act', lambda e, c=c: e.copy(out=Sa[:, c, :], in_=S_[c][:]), r=[S_[c]], w=[Sa])
                    if oi == len(order) - 1:
                        p.op('act', lambda e: e.copy(out=Sa[:, c, 0:1], in_=S_[c][:, 0:1]), r=[S_[c]], w=[S32[d_]])
                        break
                    cn = order[oi + 1]
                    kv = psKV[it % 2]
                    it += 1
                    p.op('pe', lambda e, c=c, kv=kv: e.matmul(kv[0:64, 0:128], lhsT=Kd[:, c, :], rhs=V[:, c, :], start=True, stop=True),
                         r=[Kd, V], w=[kv])
                    p.op('dve', lambda e, kv=kv, c=c, cn=cn: e.scalar_tensor_tensor(out=S_[cn][:], in0=S_[c][:], scalar=cd,
                                                                                  in1=kv[0:64, 0:128], op0=ALU.mult, op1=ALU.add),
                         r=[S_[c], cdec, kv], w=[S_[cn]])
            for c in (range(NTILE) if do_ctx else range(2, NTILE)):
                tok = c * 128
                pa, po = psA[c % 2], psO[c % 2]
                am, qf_, qb_, st_, y_ = attm[c % 2], qf[c % 2], qb[c % 2], st[c % 2], ys[c % 2]
                p.op('pe', lambda e: e.matmul(pa[:, 0:128], lhsT=kT[:, tok:tok + 128], rhs=qT[:, tok:tok + 128], start=True, stop=True),
                     r=[kT, qT], w=[pa])
                p.op('dve', lambda e: e.tensor_tensor(out=am[:], in0=pa[:, 0:128], in1=DB[:, h, :], op=ALU.mult), r=[pa, DB], w=[am])
                p.op('pool', lambda e: e.tensor_tensor(out=qf_[:], in0=qT[:, tok:tok + 128], in1=QDF[0:64, h, :], op=ALU.mult),
                     r=[qT, QDF], w=[qf_])
                p.op('pool', lambda e: e.tensor_tensor(out=qb_[:], in0=qT[:, tok:tok + 128], in1=QDB[0:64, h, :], op=ALU.mult),
                     r=[qT, QDB], w=[qb_])
                p.op('pe', lambda e: e.matmul(po[:, 0:128], lhsT=am[:], rhs=V[:, c, :], start=True, stop=False), r=[am, V], w=[po])
                p.op('pe', lambda e: e.matmul(po[:, 0:128], lhsT=qf_[:], rhs=SFa[:, c, :], start=False, stop=False), r=[qf_, SFa], w=[po])
                p.op('pe', lambda e: e.matmul(po[:, 0:128], lhsT=qb_[:], rhs=SBa[:, c, :], start=False, stop=True), r=[qb_, SBa], w=[po])
                p.op('act', lambda e: e.activation(out=junk[:], in_=po[:, 0:128], func=AF.Square, accum_out=st_[:, 0:1]),
                     r=[po], w=[junk, st_])
                p.op('dve', lambda e: e.tensor_scalar(out=st_[:, 1:2], in0=st_[:, 0:1], scalar1=1.0 / 128, scalar2=EPS,
                                                      op0=ALU.mult, op1=ALU.add), r=[st_], w=[st_])
                p.op('act', lambda e: e.activation(out=st_[:, 2:3], in_=st_[:, 1:2], func=AF.Sqrt), r=[st_], w=[st_])
                p.op('dve', lambda e: e.reciprocal(out=st_[:, 3:4], in_=st_[:, 2:3]), r=[st_], w=[st_])
                p.op('dve', lambda e: e.scalar_tensor_tensor(out=y_[:], in0=po[:, 0:128], scalar=st_[:, 3:4], in1=G[:, c, :],
                                                             op0=ALU.mult, op1=ALU.mult), r=[po, st_, G], w=[y_])
                p.dma(None, g.Y[tok:tok + 128, 512 + h * 128:512 + (h + 1) * 128], y_[:], r=[y_], q='pool')


def phase_p3(g, l, xa, last):
    p = g.p
    Lw = g.lay[l]
    with p.phase():
        wob = p.sb("wob", [128, 8, D], BF16)
        with p.phase():
            wof = p.sb("wof", [128, 8, 512], F32)
            wsrc = Lw["wout"].rearrange("(k p) n -> p k n", p=128)
            for n in range(2):
                p.dma(wof, wof[:], wsrc[:, :, n * 512:(n + 1) * 512])
                p.op('dve', lambda e, n=n: e.tensor_copy(out=wob[:, :, n * 512:(n + 1) * 512], in_=wof[:]), r=[wof], w=[wob])
        gt = []
        for which in range(2):
            t = p.sb("p3_gt%d" % which, [128, D], F32)
            load_bcast(p, t, mod_row(g, which, 2), D)
            gt.append(t)
        nrm = make_norm_consts(g, "n2", Lw["g2"], 3, 4)
        yt = [p.sb("p3_y%d" % i, [128, D], BF16) for i in range(2)]
        xt = [p.sb("p3_x%d" % i, [128, D], F32) for i in range(2)]
        x1 = [p.sb("p3_x1%d" % i, [128, D], F32) for i in range(2)]
        yT = [p.sb("p3_yT%d" % i, [128, 8, 128], BF16) for i in range(2)]
        hT = [p.sb("p3_hT%d" % i, [128, 8, 128], BF16) for i in range(2)]
        hb = [p.sb("p3_hb%d" % i, [128, D], BF16) for i in range(2)]
        tmp = p.sb("p3_tmp", [128, D], F32)
        st = [p.sb("p3_st%d" % i, [128, 4], F32) for i in range(2)]
        pstY = [p.ps("p3_pty%d" % i, [128, 8, 128], BF16) for i in range(2)]
        pstH = [p.ps("p3_pth%d" % i, [128, 8, 128], BF16) for i in range(2)]
        psO = [[p.ps("p3_o%d%d" % (i, n), [128, 512], F32) for n in range(2)] for i in range(2)]
        for ti, tt in enumerate(range(2, NTILE) if last else range(NTILE)):
            which = 1 if tt < 2 else 0
            b = ti % 2
            sl = slice(tt * 128, (tt + 1) * 128)
            p.dma(yt[b], yt[b][:], g.Y[sl, :])
            p.dma(xt[b], xt[b][:], xa[sl, :])
            emit_transposes(g, yt[b], pstY[b], yT[b][:], yT[b])
            for n in range(2):
                po = psO[b][n]
                for k in range(8):
                    p.op('pe', lambda e, k=k, n=n, po=po: e.matmul(po[:], lhsT=yT[b][:, k, :], rhs=wob[:, k, n * 512:(n + 1) * 512],
                                                                  start=(k == 0), stop=(k == 7)), r=[yT[b], wob], w=[po])
                cs = slice(n * 512, (n + 1) * 512)
                p.op('dve', lambda e, po=po, cs=cs: e.tensor_tensor(out=tmp[:, cs], in0=po[:], in1=gt[which][:, cs], op=ALU.mult),
                     r=[po, gt[which]], w=[tmp])
                p.op('pool', lambda e, cs=cs: e.tensor_tensor(out=x1[b][:, cs], in0=tmp[:, cs], in1=xt[b][:, cs], op=ALU.add),
                     r=[tmp, xt[b]], w=[x1[b]])
            p.dma(None, g.X1[sl, :], x1[b][:], r=[x1[b]], q='pool')
            emit_norm(g, x1[b], nrm[which][0], nrm[which][1], hb[b], tmp, st[b])
            emit_transposes(g, hb[b], pstH[b], hT[b][:], hT[b])
            p.dma(None, g.H2T[:, :, sl], hT[b][:], r=[hT[b]], q='pool')


def cast_gen(g, l):
    p = g.p
    Lw = g.lay[l]
    f = [p.sb("cs_f%d" % i, [128, 2048], F32) for i in range(3)]
    b = [p.sb("cs_b%d" % i, [128, 2048], BF16) for i in range(3)]
    it = 0
    usrc = Lw["ut"].rearrange("(k p) e -> p k e", p=128)
    vsrc = Lw["v"].rearrange("(i p) d -> p i d", p=128)
    for k in range(8):
        for ec in range(8):
            i = it % 3
            it += 1
            p.dma(f[i], f[i][:], usrc[:, k, ec * 2048:(ec + 1) * 2048])
            p.op('dve', lambda e, i=i: e.tensor_copy(out=b[i][:], in_=f[i][:]), r=[f[i]], w=[b[i]])
            p.dma(None, g.UTB[:, k, ec * 2048:(ec + 1) * 2048], b[i][:], r=[b[i]], q='pool')
            yield
    for ic in range(64):
        i = it % 3
        it += 1
        p.dma(f[i], f[i][:].rearrange("p (a d) -> p a d", a=2), vsrc[:, ic * 2:(ic + 1) * 2, :])
        p.op('dve', lambda e, i=i: e.tensor_copy(out=b[i][:], in_=f[i][:]), r=[f[i]], w=[b[i]])
        p.dma(None, g.VB[:, ic * 2:(ic + 1) * 2, :], b[i][:].rearrange("p (a d) -> p a d", a=2), r=[b[i]], q='pool')
        yield


NI = 8


def phase_peerq(g, l, last):
    p = g.p
    Lw = g.lay[l]
    with p.phase():
        wq = p.sb("pq_wq", [128, 8, 2048], BF16)
        with p.phase():
            wqf = p.sb("pq_wqf", [128, 8, 512], F32)
            wsrc = Lw["wq"].rearrange("(k p) n -> p k n", p=128)
            for n in range(4):
                p.dma(wqf, wqf[:], wsrc[:, :, n * 512:(n + 1) * 512])
                p.op('dve', lambda e, n=n: e.tensor_copy(out=wq[:, :, n * 512:(n + 1) * 512], in_=wqf[:]), r=[wqf], w=[wq])
        ht = [p.sb("pq_ht%d" % i, [128, 8, 512], BF16) for i in range(2)]
        stg = [p.sb("pq_st%d" % i, [128, 4, 512], BF16) for i in range(2)]
        ps = [p.ps("pq_ps%d" % i, [128, 512], F32) for i in range(4)]
        it = 0
        for gi, (t0, n) in enumerate(tok_groups()):
            if last and t0 < LC:
                continue
            h_ = ht[gi % 2]
            p.dma(h_, h_[:, :, 0:n], g.H2T[:, :, t0:t0 + n])
            for jq in range(4):
                st_ = stg[(gi * 4 + jq) % 2]
                for jj in range(4):
                    j = jq * 4 + jj
                    ps_ = ps[it % 4]
                    it += 1
                    for k in range(8):
                        p.op('pe', lambda e, k=k, j=j, ps_=ps_: e.matmul(ps_[:, 0:n], lhsT=wq[:, k, j * 128:(j + 1) * 128], rhs=h_[:, k, 0:n],
                                                                        start=(k == 0), stop=(k == 7)), r=[wq, h_], w=[ps_])
                    if jj % 2 == 0:
                        p.op('act', lambda e, jj=jj, ps_=ps_: e.copy(out=st_[:, jj, 0:n], in_=ps_[:, 0:n]), r=[ps_], w=[st_])
                    else:
                        p.op('dve', lambda e, jj=jj, ps_=ps_: e.tensor_copy(out=st_[:, jj, 0:n], in_=ps_[:, 0:n]), r=[ps_], w=[st_])
                p.dma(None, g.QPT[:, jq * 4:(jq + 1) * 4, t0:t0 + n], st_[:, :, 0:n], r=[st_], q='pool')


def phase_peer(g, l, last):
    p = g.p
    Lw = g.lay[l]
    with p.phase():
        skt = p.sb("pe_skt", [128, 16, 128], BF16)
        with p.phase():
            sktf = p.sb("pe_sktf", [128, 16, 128], F32)
            p.dma(sktf, sktf[:], Lw["skt"])
            p.op('dve', lambda e: e.tensor_copy(out=skt[:], in_=sktf[:]), r=[sktf], w=[skt])
        gt = []
        for which in range(2):
            t = p.sb("pe_gt%d" % which, [128, D], F32)
            load_bcast(p, t, mod_row(g, which, 5), D)
            gt.append(t)
        if last:
            fg = p.sb("pe_fg", [128, D], F32)
            load_bcast(p, fg, g.final_g, D)
        h2t = [p.sb("pe_h2t%d" % i, [128, 8, 256], BF16) for i in range(2)]
        E = [p.sb("pe_E%d" % i, [128, 16, 128], F32) for i in range(2)]
        Tt = [p.sb("pe_T%d" % i, [128, 8], F32) for i in range(2)]
        kap = [p.sb("pe_kap%d" % i, [128, 8], F32) for i in range(2)]
        Dk = [[p.sb("pe_dk%d_%d" % (i, h), [128, 128], BF16) for h in range(8)] for i in range(2)]
        acc = [[p.ps("pe_acc%d%d" % (i, n), [128, 512], F32) for n in range(2)] for i in range(2)]
        psA = [p.ps("pe_A%d" % i, [128, 512], F32) for i in range(2)]
        psW = p.ps("pe_W", [128, 512], F32)
        psT_full = p.ps("pe_T", [128, 8, 128], BF16)
        psT = [T("pe_T%d" % i, psT_full[:, i * 4:(i + 1) * 4, :]) for i in range(2)]
        psM = psA[1]
        pairs = list(range(1, NTILE // 2)) if last else list(range(NTILE // 2))
        for pi, pr in enumerate(pairs):
            t0 = pr * 256
            which = 1 if pr == 0 else 0
            ht = h2t[pi % 2]
            if pi == 0:
                p.dma(ht, ht[:], g.H2T[:, :, t0:t0 + 256])
            with p.phase():
                qTb = p.sb("pe_qT", [128, 16, 256], BF16)
                p.dma(qTb, qTb[:], g.QPT[:, :, t0:t0 + 256])
                S_all = [p.sb("pe_S%d" % i, [128, 16, 128], F32) for i in range(2)]
                m8 = p.sb("pe_m8", [128, 16, 16], F32)
                sr = p.sb("pe_sr", [128, 16, 128], F32)
                sd = p.sb("pe_sd", [128, 16, 128], F32)
                md = p.sb("pe_md", [128, 16, 16], F32)
                et = p.sb("pe_et", [128, 16, 16], F32)
                cand = p.sb("pe_cand", [128, 8, 256], F32)
                cr = p.sb("pe_cr", [128, 8, 256], F32)
                c16 = p.sb("pe_c16", [128, 8, 16], F32)
                zz = p.sb("pe_zz", [128, 8], F32)
                for sub in range(2):
                    S_ = S_all[sub]
                    for jq in range(4):
                        for jj in range(4):
                            j = jq * 4 + jj
                            p.op('pe', lambda e, j=j, jj=jj: e.matmul(psM[:, jj * 128:(jj + 1) * 128],
                                                                      lhsT=qTb[:, j, sub * 128:(sub + 1) * 128], rhs=skt[:, j, :],
                                                                      start=True, stop=True), r=[qTb, skt], w=[psM])
                        p.op('act', lambda e, jq=jq: e.copy(out=S_[:, jq * 4:(jq + 1) * 4, :],
                                                           in_=psM[:].rearrange("p (a n) -> p a n", a=4)), r=[psM], w=[S_])
                    for j in range(16):
                        p.op('dve', lambda e, j=j: e.max(out=m8[:, j, 0:8], in_=S_[:, j, :]), r=[S_], w=[m8])
                    for j in range(16):
                        p.op('dve', lambda e, j=j: e.match_replace(out=sr[:, j, :], in_to_replace=m8[:, j, 0:8], in_values=S_[:, j, :],
                                                                   imm_value=-1e30), r=[S_, m8], w=[sr])
                    for j in range(16):
                        p.op('dve', lambda e, j=j: e.max(out=m8[:, j, 8:16], in_=sr[:, j, :]), r=[sr], w=[m8])
                    p.op('dve', lambda e: e.tensor_tensor(out=sd[:], in0=S_[:], in1=m8[:, :, 0:1].broadcast_to([128, 16, 128]),
                                                          op=ALU.subtract), r=[S_, m8], w=[sd])
                    p.op('dve', lambda e: e.tensor_tensor(out=md[:], in0=m8[:], in1=m8[:, :, 0:1].broadcast_to([128, 16, 16]),
                                                          op=ALU.subtract), r=[m8], w=[md])
                    p.op('act', lambda e: e.activation(out=E[sub][:], in_=sd[:], func=AF.Exp), r=[sd], w=[E[sub]])
                    p.op('act', lambda e: e.activation(out=et[:], in_=md[:], func=AF.Exp), r=[md], w=[et])
                    et4 = et[:].rearrange("p (h c) a -> p h c a", c=2)
                    p.op('dve', lambda e: e.tensor_tensor(out=cand[:].rearrange("p h (a b) -> p h a b", a=16),
                                                          in0=et4[:, :, 0, :].unsqueeze(3).broadcast_to([128, 8, 16, 16]),
                                                          in1=et4[:, :, 1, :].unsqueeze(2).broadcast_to([128, 8, 16, 16]), op=ALU.mult),
                         r=[et], w=[cand])
                    for h in range(8):
                        p.op('dve', lambda e, h=h: e.max(out=c16[:, h, 0:8], in_=cand[:, h, :]), r=[cand], w=[c16])
                    for h in range(8):
                        p.op('dve', lambda e, h=h: e.match_replace(out=cr[:, h, :], in_to_replace=c16[:, h, 0:8], in_values=cand[:, h, :],
                                                                   imm_value=-1e30), r=[cand, c16], w=[cr])
                    for h in range(8):
                        p.op('dve', lambda e, h=h: e.max(out=c16[:, h, 8:16], in_=cr[:, h, :]), r=[cr], w=[c16])
                    p.op('dve', lambda e: e.tensor_scalar(out=Tt[sub][:], in0=c16[:, :, 15], scalar1=1.0 - 1e-6, scalar2=None, op0=ALU.mult),
                         r=[c16], w=[Tt[sub]])
                    p.op('dve', lambda e: e.tensor_reduce(out=zz[:], in_=c16[:], axis=AX.X, op=ALU.add), r=[c16], w=[zz])
                    p.op('dve', lambda e: e.reciprocal(out=kap[sub][:], in_=zz[:]), r=[zz], w=[kap[sub]])
                    for h in range(8):
                        p.op('dve', lambda e, h=h: e.tensor_scalar(out=Dk[sub][h][:], in0=g.ident_f[:], scalar1=kap[sub][:, h:h + 1],
                                                                   scalar2=None, op0=ALU.mult), r=[g.ident_f, kap[sub]], w=[Dk[sub][h]])
            with p.phase():
                NPT = 5
                Pt = [p.sb("pe_P%d" % i, [128, NI, 128], F32) for i in range(NPT)]
                G = [[p.sb("pe_G%d_%d" % (b_, i), [128, 8, NI * 128], BF16) for i in range(2)] for b_ in range(2)]
                ut = [p.sb("pe_ut%d" % i, [128, 8, 512], BF16) for i in range(2)]
                vb = [p.sb("pe_vb%d" % i, [128, 4, D], BF16) for i in range(2)]
                ga = [p.sb("pe_ga%d" % i, [128, 512], F32) for i in range(2)]
                wg = [p.sb("pe_wg%d" % i, [128, 512], BF16) for i in range(2)]
                wgT = [p.sb("pe_wgT%d" % i, [128, 4, 128], BF16) for i in range(2)]
                xs = [p.sb("pe_x%d" % i, [128, D], F32) for i in range(2)]
                xo = [p.sb("pe_xo%d" % i, [128, D], F32) for i in range(2)]
                if last:
                    hb = p.sb("pe_hb", [128, D], F32)
                    st = p.sb("pe_st", [128, 4], F32)
                nblk = 128 // NI
                nesb = NI * 128 // 512
                cnt = {"p": 0}
                for sub in range(2):
                    p.dma(xs[sub], xs[sub][:], g.X1[t0 + sub * 128:t0 + (sub + 1) * 128, :])
                if pi + 1 < len(pairs):
                    nt0 = pairs[pi + 1] * 256
                    p.dma(h2t[(pi + 1) % 2], h2t[(pi + 1) % 2][:], g.H2T[:, :, nt0:nt0 + 256])

                def unit(ib, sub, h):
                    P_ = Pt[cnt["p"] % NPT]
                    cnt["p"] += 1
                    G_ = G[ib % 2][sub]
                    if h % 2 == 1:
                        for ii in range(NI):
                            i_ = ib * NI + ii
                            p.op('act', lambda e, ii=ii, i_=i_: e.activation(out=P_[:, ii, :], in_=E[sub][:, 2 * h + 1, :], func=AF.Copy,
                                                                            scale=E[sub][:, 2 * h, i_:i_ + 1]), r=[E[sub]], w=[P_])
                    else:
                        p.op('pool', lambda e: e.tensor_tensor(
                            out=P_[:], in0=E[sub][:, 2 * h, ib * NI:(ib + 1) * NI].unsqueeze(2).broadcast_to([128, NI, 128]),
                            in1=E[sub][:, 2 * h + 1, :].unsqueeze(1).broadcast_to([128, NI, 128]), op=ALU.mult),
                            r=[E[sub]], w=[P_])
                    p.op('dve', lambda e: e.scalar_tensor_tensor(
                        out=G_[:, h, :], in0=P_[:].rearrange("p a b -> p (a b)"), scalar=Tt[sub][:, h:h + 1],
                        in1=P_[:].rearrange("p a b -> p (a b)"), op0=ALU.is_ge, op1=ALU.mult),
                        r=[P_, Tt[sub]], w=[G_])

                iters = [(ib, esb, sub) for ib in range(nblk) for esb in range(nesb) for sub in range(2)]
                nit = len(iters)

                def s1(i):
                    ib, esb, sub = iters[i]
                    e0 = ib * NI * 128 + esb * 512
                    b_ = (i // 2) % 2
                    u_, v_ = ut[b_], vb[b_]
                    if sub == 0:
                        p.dma(u_, u_[:], g.UTB[:, :, e0:e0 + 512])
                        p.dma(v_, v_[:], g.VB[:, e0 // 128:e0 // 128 + 4, :])
                    pa, ga_ = psA[i % 2], ga[i % 2]
                    for k in range(8):
                        p.op('pe', lambda e, k=k: e.matmul(pa[:], lhsT=ht[:, k, sub * 128:(sub + 1) * 128], rhs=u_[:, k, :],
                                                           start=(k == 0), stop=(k == 7)), r=[ht, u_], w=[pa])
                    p.op('act', lambda e: e.activation(out=ga_[:], in_=pa[:], func=AF.Gelu), r=[pa], w=[ga_])

                def s2(i):
                    ib, esb, sub = iters[i]
                    G_ = G[ib % 2][sub]
                    ga_, wg_ = ga[i % 2], wg[i % 2]
                    for h in range(8):
                        p.op('pe', lambda e, h=h: e.matmul(psW[:], lhsT=Dk[sub][h][:], rhs=G_[:, h, esb * 512:(esb + 1) * 512],
                                                           start=(h == 0), stop=(h == 7)), r=[Dk[sub][h], G_], w=[psW])
                    p.op('dve', lambda e: e.tensor_tensor(out=wg_[:], in0=psW[:], in1=ga_[:], op=ALU.mult), r=[psW, ga_], w=[wg_])

                def s3(i):
                    emit_transposes(g, wg[i % 2], psT[i % 2], wgT[i % 2][:], wgT[i % 2], n=4)

                def s4(i):
                    ib, esb, sub = iters[i]
                    v_ = vb[(i // 2) % 2]
                    wgT_ = wgT[i % 2]
                    first = (ib == 0 and esb == 0)
                    lastm = (ib == nblk - 1 and esb == nesb - 1)
                    for c4 in range(4):
                        for n in range(2):
                            a_ = acc[sub][n]
                            p.op('pe', lambda e, c4=c4, n=n, a_=a_: e.matmul(
                                a_[:], lhsT=wgT_[:, c4, :], rhs=v_[:, c4, n * 512:(n + 1) * 512],
                                start=(first and c4 == 0), stop=(lastm and c4 == 3)), r=[wgT_, v_], w=[a_])

                pend = []
                for sub in range(2):
                    for h in range(8):
                        unit(0, sub, h)
                s1(0)
                s2(0)
                per_it = 16 // (nesb * 2)
                for i in range(nit):
                    ib = iters[i][0]
                    if i % (nesb * 2) == 0 and ib + 1 < nblk:
                        pend = [(ib + 1, sub, h) for sub in range(2) for h in range(8)]
                    if i + 1 < nit:
                        s1(i + 1)
                    s3(i)
                    if i + 1 < nit:
                        s2(i + 1)
                    for _ in range(per_it):
                        if pend:
                            unit(*pend.pop(0))
                    s4(i)
                for sub in range(2):
                    sl = slice(t0 + sub * 128, t0 + (sub + 1) * 128)
                    x_, o_ = xs[sub], xo[sub]
                    for n in range(2):
                        cs = slice(n * 512, (n + 1) * 512)
                        p.op('dve', lambda e, n=n, cs=cs: e.tensor_tensor(out=o_[:, cs], in0=acc[sub][n][:], in1=gt[which][:, cs], op=ALU.mult),
                             r=[acc[sub][n], gt[which]], w=[o_])
                    p.op('pool', lambda e: e.tensor_tensor(out=o_[:], in0=o_[:], in1=x_[:], op=ALU.add), r=[o_, x_], w=[o_])
                    if not last:
                        p.dma(None, g.X2[sl, :], o_[:], r=[o_], q='pool')
                    else:
                        p.op('act', lambda e: e.activation(out=hb[:], in_=o_[:], func=AF.Square, accum_out=st[:, 0:1]), r=[o_], w=[hb, st])
                        p.op('dve', lambda e: e.tensor_scalar(out=st[:, 1:2], in0=st[:, 0:1], scalar1=1.0 / D, scalar2=EPS,
                                                              op0=ALU.mult, op1=ALU.add), r=[st], w=[st])
                        p.op('act', lambda e: e.activation(out=st[:, 2:3], in_=st[:, 1:2], func=AF.Sqrt), r=[st], w=[st])
                        p.op('dve', lambda e: e.reciprocal(out=st[:, 3:4], in_=st[:, 2:3]), r=[st], w=[st])
                        p.op('dve', lambda e: e.scalar_tensor_tensor(out=hb[:], in0=o_[:], scalar=st[:, 3:4], in1=fg[:],
                                                                     op0=ALU.mult, op1=ALU.mult), r=[o_, st, fg], w=[hb])
                        p.dma(None, g.out[t0 - LC + sub * 128:t0 - LC + (sub + 1) * 128, :], hb[:], r=[hb], q='pool')


_PROG_CACHE = {}


def kernel(x, c, ctx, c_ctx, w_ada, b_ada, norm1_g, w_in, na_rpb, diff_lambda, diff_subln_g, ret_decay_logit,
           w_out, norm2_g, peer_wq, peer_subkeys, peer_u, peer_v, final_g):
    inp = dict(x=x, c=c, ctx=ctx, c_ctx=c_ctx, w_ada=w_ada, b_ada=b_ada, norm1_g=norm1_g, w_in=w_in, na_rpb=na_rpb,
               diff_lambda=diff_lambda, diff_subln_g=diff_subln_g, ret_decay_logit=ret_decay_logit, w_out=w_out,
               norm2_g=norm2_g, peer_wq=peer_wq, peer_subkeys=peer_subkeys, peer_u=peer_u, peer_v=peer_v, final_g=final_g)
    inp = {k: np.asarray(v) for k, v in inp.items()}
    B = inp["x"].shape[0]
    cs = _consts()
    shared = {"final_g": np.asarray(inp["final_g"], np.float32).reshape(1, D)}
    for n in CONST_NAMES:
        shared[n] = cs[n]
    for l in range(DEPTH):
        for k, v in _layer_inputs(inp, l).items():
            shared["L%d_%s" % (l, k)] = v
    ccv = np.asarray(inp["c_ctx"], np.float32).reshape(8, 128).T
    in_maps = []
    for b in range(B):
        m = dict(shared)
        m["xin"] = np.ascontiguousarray(np.concatenate([inp["ctx"][b], inp["x"][b]], axis=0).astype(np.float32))
        m["cvec"] = np.ascontiguousarray(np.stack([np.asarray(inp["c"][b], np.float32).reshape(8, 128).T, ccv], axis=-1))
        in_maps.append(m)
    if "nc" not in _PROG_CACHE:
        _PROG_CACHE["nc"] = build_program()[0]
    nc = _PROG_CACHE["nc"]
    res = run_bass_kernel_spmd(nc, in_maps, core_ids=list(range(B)))
    return np.stack([np.asarray(r["out"], dtype=np.float32) for r in res.results], axis=0)
```
